# Optimizing a Trainium2 kernel written in Bass

```python
import jax, jax.numpy as jnp
from jax import lax
import numpy as np

D_MODEL = 1024
BATCH = 1
SEQ = 16384
DEPTH = 1

ATTN_HEADS = 8
ATTN_HEAD_DIM = 64
ATTN_WIDTH = ATTN_HEADS * ATTN_HEAD_DIM
MOBA_BLOCK = 256
MOBA_TOPK = 3
Q_BLOCK = 128
MLSTM_HEADS = 4
MLSTM_HEAD_DIM = 128
MLSTM_WIDTH = MLSTM_HEADS * MLSTM_HEAD_DIM
MLSTM_CHUNK = 128
CONV_K = 4
SPLIT_SIZES = (ATTN_WIDTH, ATTN_WIDTH, ATTN_WIDTH,
               MLSTM_WIDTH, MLSTM_WIDTH, MLSTM_WIDTH, MLSTM_WIDTH,
               MLSTM_HEADS, MLSTM_HEADS,
               D_MODEL, D_MODEL)
IN_WIDTH = sum(SPLIT_SIZES)
N_GROUPS = 4
EXPERTS_PER_GROUP = 8
N_EXPERTS = N_GROUPS * EXPERTS_PER_GROUP
TOPK_IN_GROUP = 2
EXPERT_FF = 512
ROW_BLOCK = 256
PLE_DIM = 256
EPS = 1e-6

kernel_name = "hybrid_moba_mlstm_hmoe_block"


def rmsnorm(x, g):
    xf = x.astype(jnp.float32)
    y = xf * lax.rsqrt(jnp.mean(xf * xf, axis=-1, keepdims=True) + EPS)
    return (y * g.astype(jnp.float32)).astype(x.dtype)


def split_cols(t, sizes):
    outs, start = [], 0
    for s in sizes:
        outs.append(t[..., start:start + s])
        start += s
    return outs


def to_heads(t, n_heads):
    b, s, _ = t.shape
    return t.reshape(b, s, n_heads, -1).transpose(0, 2, 1, 3)


def from_heads(t):
    b, h, s, d = t.shape
    return t.transpose(0, 2, 1, 3).reshape(b, s, h * d)


def causal_dwconv(x, w, b):
    s = x.shape[1]
    xp = jnp.pad(x, ((0, 0), (CONV_K - 1, 0), (0, 0)))
    y = b
    for k in range(CONV_K):
        y = y + xp[:, k:k + s, :] * w[k]
    return y


def moba_attention(q, k, v):
    b, h, s, d = q.shape
    nb = -(-s // MOBA_BLOCK)
    pad = nb * MOBA_BLOCK - s
    kp = jnp.pad(k, ((0, 0), (0, 0), (0, pad), (0, 0)))
    vp = jnp.pad(v, ((0, 0), (0, 0), (0, pad), (0, 0)))
    kb = kp.reshape(b, h, nb, MOBA_BLOCK, d)
    vb = vp.reshape(b, h, nb, MOBA_BLOCK, d)
    kmean = jnp.mean(kb.astype(jnp.float32), axis=3)
    topk = min(MOBA_TOPK, nb)
    scale = ATTN_HEAD_DIM ** -0.5
    b_idx = jnp.arange(b)[:, None, None, None]
    h_idx = jnp.arange(h)[None, :, None, None]
    n_qblocks = s // Q_BLOCK

    def block_fn(qi):
        q0 = qi * Q_BLOCK
        qb = lax.dynamic_slice_in_dim(q, q0, Q_BLOCK, axis=2)
        qpos = q0 + jnp.arange(Q_BLOCK)
        cur = q0 // MOBA_BLOCK
        gate = jnp.einsum('bhqd,bhnd->bhqn', qb.astype(jnp.float32), kmean)
        gate = jnp.where(jnp.arange(nb) < cur, gate, -jnp.inf)
        _, sel = lax.top_k(gate, topk)
        sel_valid = jnp.arange(topk) < cur
        ks = kb[b_idx, h_idx, sel]
        vs = vb[b_idx, h_idx, sel]
        s_sel = jnp.einsum('bhqd,bhqjkd->bhqjk', qb, ks).astype(jnp.float32) * scale
        s_sel = jnp.where(sel_valid[:, None], s_sel, -jnp.inf)
        s_sel = s_sel.reshape(b, h, Q_BLOCK, topk * MOBA_BLOCK)
        ko = lax.dynamic_slice_in_dim(kp, cur * MOBA_BLOCK, MOBA_BLOCK, axis=2)
        vo = lax.dynamic_slice_in_dim(vp, cur * MOBA_BLOCK, MOBA_BLOCK, axis=2)
        kpos = cur * MOBA_BLOCK + jnp.arange(MOBA_BLOCK)
        s_own = jnp.einsum('bhqd,bhkd->bhqk', qb, ko).astype(jnp.float32) * scale
        s_own = jnp.where(kpos[None, :] <= qpos[:, None], s_own, -jnp.inf)
        probs = jax.nn.softmax(jnp.concatenate([s_sel, s_own], axis=-1), axis=-1)
        p_sel = probs[..., :topk * MOBA_BLOCK].reshape(b, h, Q_BLOCK, topk, MOBA_BLOCK).astype(v.dtype)
        p_own = probs[..., topk * MOBA_BLOCK:].astype(v.dtype)
        return (jnp.einsum('bhqjk,bhqjkd->bhqd', p_sel, vs)
                + jnp.einsum('bhqk,bhkd->bhqd', p_own, vo))

    out = lax.map(block_fn, jnp.arange(n_qblocks))
    return jnp.moveaxis(out, 0, 2).reshape(b, h, s, d)


def mlstm_chunkwise(q, k, v, i_pre, f_pre):
    b, h, s, dk = q.shape
    dv = v.shape[-1]
    L = MLSTM_CHUNK
    nc = s // L
    f32 = jnp.float32
    log_f = jax.nn.log_sigmoid(f_pre.astype(f32))
    log_i = i_pre.astype(f32)

    def chunks(t):
        return jnp.moveaxis(t.reshape(b, h, nc, L, *t.shape[3:]), 2, 0)

    xs = (chunks(q.astype(f32)), chunks(k.astype(f32)), chunks(v.astype(f32)), chunks(log_i), chunks(log_f))
    causal = jnp.tril(jnp.ones((L, L), dtype=bool))

    def step(carry, inp):
        C, n, m = carry
        qc, kc, vc, lic, lfc = inp
        bcum = jnp.cumsum(lfc, axis=-1)
        dmat = bcum[..., :, None] - bcum[..., None, :] + lic[..., None, :]
        dmat = jnp.where(causal, dmat, -jnp.inf)
        inter = bcum + m[..., None]
        m_t = jnp.maximum(inter, jnp.max(dmat, axis=-1))
        w_ts = jnp.exp(dmat - m_t[..., None])
        sc_inter = jnp.exp(inter - m_t)
        sqk = jnp.einsum('bhtd,bhsd->bhts', qc, kc) * w_ts
        num = (jnp.einsum('bhts,bhsv->bhtv', sqk, vc)
               + sc_inter[..., None] * jnp.einsum('bhtd,bhdv->bhtv', qc, C))
        den = jnp.sum(sqk, axis=-1) + sc_inter * jnp.einsum('bhtd,bhd->bht', qc, n)
        h_t = num / jnp.maximum(jnp.abs(den), jnp.exp(-m_t))[..., None]
        b_last = bcum[..., -1]
        lw = b_last[..., None] - bcum + lic
        m_new = jnp.maximum(b_last + m, jnp.max(lw, axis=-1))
        w_s = jnp.exp(lw - m_new[..., None])
        decay = jnp.exp(b_last + m - m_new)
        C_new = decay[..., None, None] * C + jnp.einsum('bhsd,bhsv->bhdv', kc * w_s[..., None], vc)
        n_new = decay[..., None] * n + jnp.einsum('bhs,bhsd->bhd', w_s, kc)
        return (C_new, n_new, m_new), h_t

    init = (jnp.zeros((b, h, dk, dv), f32), jnp.zeros((b, h, dk), f32), jnp.zeros((b, h), f32))
    _, hs = lax.scan(step, init, xs)
    return jnp.moveaxis(hs, 0, 2).reshape(b, h, s, dv)


def hierarchical_moe(xn, rg_w, rg_b, re_w, re_b, w_gate, w_up, w_down):
    b, s, d = xn.shape
    T = b * s
    xt = xn.reshape(T, d)
    g_logits = (xt @ rg_w + rg_b).astype(jnp.float32)
    g_prob = jax.nn.softmax(g_logits, axis=-1)
    grp = jnp.argmax(g_logits, axis=-1)
    p_grp = jnp.take_along_axis(g_prob, grp[:, None], axis=-1)[:, 0]
    e_logits = (xt @ re_w + re_b).astype(jnp.float32).reshape(T, N_GROUPS, EXPERTS_PER_GROUP)
    e_logits = jnp.take_along_axis(e_logits, grp[:, None, None], axis=1)[:, 0]
    e_prob = jax.nn.softmax(e_logits, axis=-1)
    w_top, i_top = lax.top_k(e_prob, TOPK_IN_GROUP)
    w_top = w_top / jnp.sum(w_top, axis=-1, keepdims=True) * p_grp[:, None]
    eid = grp[:, None] * EXPERTS_PER_GROUP + i_top

    A = T * TOPK_IN_GROUP
    flat_e = eid.reshape(A)
    flat_tok = jnp.repeat(jnp.arange(T, dtype=jnp.int32), TOPK_IN_GROUP)
    flat_w = w_top.reshape(A)
    order = jnp.argsort(flat_e)
    se, stok, sw = flat_e[order], flat_tok[order], flat_w[order]
    counts = jax.ops.segment_sum(jnp.ones((A,), jnp.int32), flat_e, num_segments=N_EXPERTS)
    offsets = jnp.cumsum(counts) - counts
    pcounts = (counts + ROW_BLOCK - 1) // ROW_BLOCK * ROW_BLOCK
    pends = jnp.cumsum(pcounts)
    poffs = pends - pcounts
    dest = poffs[se] + (jnp.arange(A, dtype=jnp.int32) - offsets[se])
    n_pad = (A + N_EXPERTS * (ROW_BLOCK - 1) + ROW_BLOCK - 1) // ROW_BLOCK * ROW_BLOCK
    n_blk = n_pad // ROW_BLOCK
    row_tok = jnp.zeros((n_pad,), jnp.int32).at[dest].set(stok)
    row_w = jnp.zeros((n_pad,), jnp.float32).at[dest].set(sw)
    blk_e = jnp.minimum(jnp.searchsorted(pends, jnp.arange(n_blk) * ROW_BLOCK, side='right'),
                        N_EXPERTS - 1)

    def blk(args):
        bi, e = args
        toks = lax.dynamic_slice_in_dim(row_tok, bi * ROW_BLOCK, ROW_BLOCK)
        wr = lax.dynamic_slice_in_dim(row_w, bi * ROW_BLOCK, ROW_BLOCK)
        xb = xt[toks]
        hid = jax.nn.silu(xb @ w_gate[e]) * (xb @ w_up[e])
        return ((hid @ w_down[e]) * wr[:, None]).astype(xt.dtype)

    yb = lax.map(blk, (jnp.arange(n_blk), blk_e))
    y = jax.ops.segment_sum(yb.reshape(n_pad, d), row_tok, num_segments=T)
    return y.reshape(b, s, d)


def setup_inputs(seed: int = 0) -> dict:
    key = jax.random.key(seed)
    ks = jax.random.split(key, 24)
    nrm = jax.random.normal
    L = DEPTH
    f32 = jnp.float32

    def gain(k, n):
        return 1.0 + 0.01 * nrm(k, (L, n), f32)

    if_b = jnp.concatenate([0.1 * nrm(ks[6], (L, MLSTM_HEADS), f32),
                            3.0 + 3.0 * jax.random.uniform(ks[7], (L, MLSTM_HEADS), f32)], axis=-1)
    return {
        "x": nrm(ks[0], (BATCH, SEQ, D_MODEL), f32),
        "p": nrm(ks[1], (DEPTH, BATCH, SEQ, PLE_DIM), f32),
        "mix_norm_g": gain(ks[2], D_MODEL),
        "w_in": nrm(ks[3], (L, D_MODEL, IN_WIDTH), f32) * D_MODEL ** -0.5,
        "b_gate": 0.01 * nrm(ks[4], (L, 2 * D_MODEL), f32),
        "conv_w": nrm(ks[5], (L, CONV_K, 2 * MLSTM_WIDTH), f32) * CONV_K ** -0.5,
        "conv_b": 0.01 * nrm(ks[8], (L, 2 * MLSTM_WIDTH), f32),
        "mlstm_if_b": if_b,
        "mlstm_norm_g": gain(ks[9], MLSTM_WIDTH),
        "w_up_attn": nrm(ks[10], (L, ATTN_WIDTH, D_MODEL), f32) * ATTN_WIDTH ** -0.5,
        "w_up_mlstm": nrm(ks[11], (L, MLSTM_WIDTH, D_MODEL), f32) * MLSTM_WIDTH ** -0.5,
        "w_out": nrm(ks[12], (L, D_MODEL, D_MODEL), f32) * D_MODEL ** -0.5,
        "ffn_norm_g": gain(ks[13], D_MODEL),
        "router_group_w": nrm(ks[14], (L, D_MODEL, N_GROUPS), f32) * D_MODEL ** -0.5,
        "router_group_b": 0.01 * nrm(ks[15], (L, N_GROUPS), f32),
        "router_expert_w": nrm(ks[16], (L, D_MODEL, N_EXPERTS), f32) * D_MODEL ** -0.5,
        "router_expert_b": 0.01 * nrm(ks[17], (L, N_EXPERTS), f32),
        "expert_w_gate": nrm(ks[18], (L, N_EXPERTS, D_MODEL, EXPERT_FF), f32) * D_MODEL ** -0.5,
        "expert_w_up": nrm(ks[19], (L, N_EXPERTS, D_MODEL, EXPERT_FF), f32) * D_MODEL ** -0.5,
        "expert_w_down": nrm(ks[20], (L, N_EXPERTS, EXPERT_FF, D_MODEL), f32) * EXPERT_FF ** -0.5,
        "ple_norm_g": gain(ks[21], D_MODEL),
        "w_ple_gate": nrm(ks[22], (L, D_MODEL, D_MODEL), f32) * D_MODEL ** -0.5,
        "w_ple_proj": nrm(ks[23], (L, PLE_DIM, D_MODEL), f32) * PLE_DIM ** -0.5,
        "final_norm_g": 1.0 + 0.01 * nrm(jax.random.fold_in(key, 99), (D_MODEL,), f32),
    }


def reference(x, p, mix_norm_g, w_in, b_gate, conv_w, conv_b, mlstm_if_b, mlstm_norm_g,
              w_up_attn, w_up_mlstm, w_out, ffn_norm_g, router_group_w, router_group_b,
              router_expert_w, router_expert_b, expert_w_gate, expert_w_up, expert_w_down,
              ple_norm_g, w_ple_gate, w_ple_proj, final_norm_g):
    h = x
    for i in range(DEPTH):
        xn = rmsnorm(h, mix_norm_g[i])
        proj = xn @ w_in[i]
        aq, ak, av, mq, mk, mv, mo, mi, mf, ga, gm = split_cols(proj, SPLIT_SIZES)
        ya = from_heads(moba_attention(to_heads(aq, ATTN_HEADS), to_heads(ak, ATTN_HEADS),
                                       to_heads(av, ATTN_HEADS)))
        qk = jax.nn.silu(causal_dwconv(jnp.concatenate([mq, mk], axis=-1), conv_w[i], conv_b[i]))
        mq_c, mk_c = qk[..., :MLSTM_WIDTH], qk[..., MLSTM_WIDTH:]
        i_pre = (mi + mlstm_if_b[i, :MLSTM_HEADS]).transpose(0, 2, 1)
        f_pre = (mf + mlstm_if_b[i, MLSTM_HEADS:]).transpose(0, 2, 1)
        h_tilde = mlstm_chunkwise(to_heads(mq_c, MLSTM_HEADS),
                                  to_heads(mk_c, MLSTM_HEADS) * MLSTM_HEAD_DIM ** -0.5,
                                  to_heads(mv, MLSTM_HEADS), i_pre, f_pre)
        h_cell = jax.nn.sigmoid(to_heads(mo, MLSTM_HEADS).astype(jnp.float32)) * h_tilde
        h_cell = rmsnorm(h_cell, mlstm_norm_g[i].reshape(MLSTM_HEADS, 1, MLSTM_HEAD_DIM))
        ym = from_heads(h_cell).astype(h.dtype)
        gates = jax.nn.sigmoid(jnp.concatenate([ga, gm], axis=-1) + b_gate[i])
        merged = (gates[..., :D_MODEL] * (ya @ w_up_attn[i])
                  + gates[..., D_MODEL:] * (ym @ w_up_mlstm[i]))
        h = h + merged @ w_out[i]
        h = h + hierarchical_moe(rmsnorm(h, ffn_norm_g[i]), router_group_w[i], router_group_b[i],
                                 router_expert_w[i], router_expert_b[i], expert_w_gate[i],
                                 expert_w_up[i], expert_w_down[i])
        ple_gate = jax.nn.sigmoid(rmsnorm(h, ple_norm_g[i]) @ w_ple_gate[i])
        h = h + (p[i] @ w_ple_proj[i]) * ple_gate
    return rmsnorm(h, final_norm_g)
```

```python
import numpy as np
import ml_dtypes
from contextlib import ExitStack
import concourse.bass as bass
import concourse.mybir as mybir
from concourse.bass_utils import run_bass_kernel_spmd

F32 = mybir.dt.float32
BF16 = mybir.dt.bfloat16
ALU = mybir.AluOpType
AF = mybir.ActivationFunctionType
AX = mybir.AxisListType
ENGS = ("pe", "act", "dve", "pool", "sp")
NCORES = 8
EPS = 1e-6
NEGB = 30000.0


class Buf:
    __slots__ = ("name", "w", "r", "dsem")

    def __init__(self, name):
        self.name = name
        self.w = None
        self.r = []
        self.dsem = None


class Prog:
    def __init__(self, nc):
        self.nc = nc
        self.ops = {e: [] for e in ENGS}
        self.cnt = {e: 0 for e in ENGS}
        self.seen = {e: {} for e in ENGS}
        self.dma_cnt = {}
        self.semkeys = []
        self.semset = set()
        self.n_inst = 0

    LIM = 4000

    def _tok(self, eng, val):
        ep = (val - 1) // self.LIM
        key = "%s#%d" % (eng, ep)
        if key not in self.semset:
            self.semset.add(key)
            self.semkeys.append(key)
        return (key, val - ep * self.LIM)

    def _need(self, eng, deps):
        best = {}
        for k, v in deps:
            if eng == "pe" and k.startswith("pe#"):
                continue
            if v <= self.seen[eng].get(k, 0):
                continue
            if v > best.get(k, 0):
                best[k] = v
        for k, v in best.items():
            self.seen[eng][k] = v
        return list(best.items())

    def _deps(self, reads, writes):
        deps = []
        for b in reads:
            if b.w is not None:
                deps.append(b.w)
        for b in writes:
            if b.w is not None:
                deps.append(b.w)
            deps.extend(b.r)
        return deps

    def op(self, eng, fn, reads=(), writes=(), inc=True):
        waits = self._need(eng, self._deps(reads, writes))
        val = self.cnt[eng] + 1
        if inc:
            self.cnt[eng] = val
        tok = self._tok(eng, val)
        self.ops[eng].append((waits, fn, (tok[0], 1) if inc else None))
        for b in reads:
            b.r.append(tok)
            if len(b.r) > 24:
                b.r = self._compact(b.r)
        for b in writes:
            b.w = tok
            b.r = []
        self.n_inst += 1

    @staticmethod
    def _compact(r):
        best = {}
        for k, v in r:
            if v > best.get(k, 0):
                best[k] = v
        return list(best.items())

    def dma(self, eng, fn, anchor, reads=(), writes=(), amt=16):
        if anchor.dsem is None:
            anchor.dsem = "d%d" % len(self.semkeys)
            self.semkeys.append(anchor.dsem)
            self.dma_cnt[anchor.dsem] = 0
        k = anchor.dsem
        waits = self._need(eng, self._deps(reads, writes))
        self.dma_cnt[k] += amt
        tok = (k, self.dma_cnt[k])
        self.ops[eng].append((waits, fn, (k, amt)))
        for b in reads:
            b.r.append(tok)
        for b in writes:
            b.w = tok
            b.r = []
        self.n_inst += 1

    def barrier(self):
        deps = [self._tok(e, self.cnt[e]) for e in ENGS if self.cnt[e] > 0]
        deps += [(k, v) for k, v in self.dma_cnt.items() if v > 0]
        for e in ENGS:
            waits = self._need(e, deps)
            if waits:
                self.ops[e].append((waits, None, None))

    def emit(self):
        nc = self.nc
        with ExitStack() as st:
            sems = {}
            for k in self.semkeys:
                sems[k] = st.enter_context(nc.semaphore("s_" + k.replace("#", "_")))
            block = st.enter_context(nc.Block())

            def run(eobj, name):
                for waits, fn, inc in self.ops[name]:
                    for k, v in waits:
                        eobj.wait_ge(sems[k], v)
                    if fn is None:
                        continue
                    ins = fn(eobj)
                    if inc is not None:
                        ins.then_inc(sems[inc[0]], inc[1])

            @block.tensor
            def _(e):
                run(e, "pe")

            @block.scalar
            def _(e):
                run(e, "act")

            @block.vector
            def _(e):
                run(e, "dve")

            @block.gpsimd
            def _(e):
                run(e, "pool")

            @block.sync
            def _(e):
                run(e, "sp")


class Arena:
    def __init__(self, nc, words):
        self.t = nc.alloc_sbuf_tensor("arena", [128, words], F32)
        self.words = words
        self.top = 0
        self.peak = 0

    def mark(self):
        return self.top

    def release(self, m):
        self.top = m

    def alloc(self, cols, dtype=F32):
        w = cols if dtype == F32 else (cols + 1) // 2
        w = (w + 7) // 8 * 8
        a = self.top
        self.top += w
        self.peak = max(self.peak, self.top)
        assert self.top <= self.words, "SBUF arena overflow %d > %d" % (self.top, self.words)
        ap = self.t[:, a:a + w]
        if dtype != F32:
            ap = ap.bitcast(dtype)
        return ap[:, 0:cols]


def v3(ap, a):
    return ap.rearrange("p (a b) -> p a b", a=a)


class K:
    def __init__(self, nc, dbg=False):
        self.nc = nc
        self.P = Prog(nc)
        self.A = Arena(nc, 52600)
        self.dbg = dbg
        self.psb = []
        for i in range(8):
            t = nc.alloc_psum_tensor("psb%d" % i, [128, 512], F32)
            self.psb.append((t, Buf("ps%d" % i)))
        self.rr = {}

    def ps(self, group, banks):
        i = self.rr.get(group, 0)
        self.rr[group] = i + 1
        t, b = self.psb[banks[i % len(banks)]]
        return t, b

    def mm(self, out, lhsT, rhs, start, stop, reads, writes, inc=True):
        self.P.op("pe", lambda e: e.matmul(out, lhsT=lhsT, rhs=rhs, start=start, stop=stop), reads, writes, inc)

    def tr(self, out, in_, ident, reads, writes, inc=True):
        self.P.op("pe", lambda e: e.transpose(out=out, in_=in_, identity=ident), reads, writes, inc)

    def act(self, out, in_, func, reads, writes, bias=None, scale=None, accum_out=None, eng="act"):
        kw = {}
        if bias is not None:
            kw["bias"] = bias
        if scale is not None:
            kw["scale"] = scale
        if accum_out is not None:
            kw["accum_out"] = accum_out
        self.P.op(eng, lambda e: e.activation(out=out, in_=in_, func=func, **kw), reads, writes)

    def cp(self, eng, out, in_, reads, writes):
        if eng == "act":
            self.P.op("act", lambda e: e.copy(out=out, in_=in_), reads, writes)
        else:
            self.P.op(eng, lambda e: e.tensor_copy(out=out, in_=in_), reads, writes)

    def ts(self, out, in0, s1, s2, op0, op1, reads, writes, eng="dve"):
        if op1 is None:
            self.P.op(eng, lambda e: e.tensor_scalar(out=out, in0=in0, scalar1=s1, scalar2=None, op0=op0), reads, writes)
        else:
            self.P.op(eng, lambda e: e.tensor_scalar(out=out, in0=in0, scalar1=s1, scalar2=s2, op0=op0, op1=op1), reads, writes)

    def stt(self, out, in0, scalar, in1, op0, op1, reads, writes, eng="dve"):
        self.P.op(eng, lambda e: e.scalar_tensor_tensor(out=out, in0=in0, scalar=scalar, in1=in1, op0=op0, op1=op1), reads, writes)

    def tt(self, out, in0, in1, op, reads, writes, eng="dve"):
        self.P.op(eng, lambda e: e.tensor_tensor(out=out, in0=in0, in1=in1, op=op), reads, writes)

    def recip(self, out, in_, reads, writes):
        self.P.op("dve", lambda e: e.reciprocal(out=out, in_=in_), reads, writes)

    def memset(self, ap, val, writes, eng="dve"):
        self.P.op(eng, lambda e: e.memset(ap, val), (), writes)

    def ld(self, out, in_, buf, eng="sp", reads=()):
        self.P.dma(eng, lambda e: e.dma_start(out=out, in_=in_), buf, reads=reads, writes=[buf])

    def st(self, out, in_, buf, dbuf, eng="sp"):
        self.P.dma(eng, lambda e: e.dma_start(out=out, in_=in_), buf, reads=[buf], writes=[dbuf])

    def ldw(self, dst3, src, kc, buf, eng="pool"):
        s3 = src.rearrange("(kc p) n -> p kc n", p=128)
        for k in range(kc):
            o = dst3[:, k, :]
            i = s3[:, k, :]
            self.P.dma(eng, lambda e, o=o, i=i: e.dma_start(out=o, in_=i), buf, writes=[buf])

    def rstd(self, ss, inv_n, tmp, out, rb, wb):
        self.act(tmp, ss, AF.Ln, list(rb) + [self.b_eps], wb, bias=self.eps_ap, scale=inv_n)
        self.act(out, tmp, AF.Exp, wb, wb, scale=-0.5)


def build_program(dbg=False, stop=None, NT=128, dummy=None, NE=32):
    nc = bass.Bass("TRN2", target_bir_lowering=False)
    k = K(nc, dbg)
    P, A = k.P, k.A

    def din(name, shape, dt=F32):
        return nc.dram_tensor(name, list(shape), dt, kind="ExternalInput").ap()

    x_shift = din("x_shift", [NT * 128, 1024])
    x_own = din("x_own", [16 * 128, 1024])
    p_own = din("p_own", [16 * 128, 256])
    g_mix = din("g_mix", [1, 1024]); g_ffn = din("g_ffn", [1, 1024]); g_ple = din("g_ple", [1, 1024]); g_fin = din("g_fin", [1, 1024])
    g_ml = din("g_ml", [1, 512])
    w_kv = [din("w_kv%d" % h, [1024, 512]) for h in range(2)]
    w_q = [din("w_q%d" % h, [1024, 256]) for h in range(2)]
    w_ms = din("w_ms", [1024, 1032])
    w_mo = din("w_mo", [1024, 1024])
    w_g = din("w_g", [1024, 2048])
    b_gate = din("b_gate", [1, 2048])
    conv_wb = din("conv_wb", [128, 40])
    ifb = din("ifb", [1, 8])
    w_ua = din("w_ua", [512, 1024]); w_um = din("w_um", [512, 1024]); w_out = din("w_out", [1024, 1024])
    w_r = din("w_r", [1024, 36]); b_r = din("b_r", [1, 36])
    c_f32 = din("c_f32", [128, 3 * 128])
    c_bf = din("c_bf", [128, 4 * 128], BF16)
    c_e = din("c_e", [64, 64 * 128], BF16)
    c_core = din("c_core", [128, 130])
    c_gmask = din("c_gmask", [1, 16 * 64])
    out_own = nc.dram_tensor("out_own", [16 * 128, 1024], F32, kind="ExternalOutput").ap()
    okind = "ExternalOutput" if dbg else "Internal"
    s_xnT = nc.dram_tensor("s_xnT", [(NT + 16) * 128, 1024], BF16).ap()
    s_ya = nc.dram_tensor("s_ya", [16 * 128, 512], BF16, kind=okind).ap()
    s_ym = nc.dram_tensor("s_ym", [16 * 128, 512], BF16, kind=okind).ap()
    d_xnT = [Buf("dxnT%d" % i) for i in range(NT + 16)]
    d_ya = [Buf("dya%d" % i) for i in range(16)]
    d_ym = [Buf("dym%d" % i) for i in range(16)]
    d_out = Buf("dout")

    cf = A.alloc(384); b_cf = Buf("cf")
    cb = A.alloc(512, BF16); b_cb = Buf("cb")
    cc = A.alloc(130); b_cc = Buf("cc")
    epsb = A.alloc(1); b_eps = Buf("eps")
    k.ld(cf, c_f32, b_cf); k.ld(cb, c_bf, b_cb); k.ld(cc, c_core, b_cc)
    k.memset(epsb, EPS, [b_eps])
    k.eps_ap = epsb[:, 0:1]; k.b_eps = b_eps
    NUT, NONES, ID32 = cf[:, 0:128], cf[:, 128:256], cf[:, 256:384]
    IDB, UTB, CM = cb[:, 0:128], cb[:, 128:256], [cb[:, 256:384], cb[:, 384:512]]
    VALID, PAR, NPAR = cc[:, 0:128], cc[:, 128:129], cc[:, 129:130]
    junk = A.alloc(1024, BF16); b_junk = Buf("junk")
    st4 = [A.alloc(4) for _ in range(2)]; b_st4 = [Buf("st%d" % i) for i in range(2)]
    base_mark = A.mark()

    def bc_load(dst, src_row, buf):
        k.ld(dst, src_row.partition_broadcast(128), buf)

    gb = A.alloc(1024); b_gb = Buf("gb")
    bc_load(gb, g_mix, b_gb)
    xt = [A.alloc(1024) for _ in range(2)]; b_xt = [Buf("xt%d" % i) for i in range(2)]
    xn = [A.alloc(1024, BF16) for _ in range(2)]; b_xn = [Buf("xn%d" % i) for i in range(2)]
    xT_s = [A.alloc(1024, BF16) for _ in range(2)]; b_xTs = [Buf("xTs%d" % i) for i in range(2)]

    def norm_tile(src_ap, sl, gbt, b_gbt, dst_bf, b_dst):
        s = st4[sl]; bs = b_st4[sl]
        k.act(junk, src_ap[0], AF.Square, [src_ap[1]], [b_junk, bs], accum_out=s[:, 0:1])
        k.rstd(s[:, 0:1], 1.0 / 1024, s[:, 1:2], s[:, 2:3], [bs], [bs])
        k.stt(dst_bf, src_ap[0], s[:, 2:3], gbt, ALU.mult, ALU.mult, [src_ap[1], bs, b_gbt], [b_dst])

    def transpose_to(dst_bf3, b_dst, src_bf, b_src, nchunk, grp="misc", banks=(5, 6, 7), eng="act"):
        pt, bp = k.ps(grp, banks)
        ptb = pt[:, :].bitcast(BF16)
        for c in range(nchunk):
            k.tr(ptb[:, c * 128:(c + 1) * 128], src_bf[:, c * 128:(c + 1) * 128], IDB, [b_src, b_cb], [bp], inc=(c == nchunk - 1))
        k.cp(eng, dst_bf3, ptb[:, 0:nchunk * 128], [bp], [b_dst])

    for T in range(NT + 16):
        sl = T % 2
        src = x_shift[T * 128:(T + 1) * 128, :] if T < NT else x_own[(T - NT) * 128:(T - NT + 1) * 128, :]
        k.ld(xt[sl], src, b_xt[sl])
        norm_tile((xt[sl], b_xt[sl]), sl, gb, b_gb, xn[sl], b_xn[sl])
        transpose_to(xT_s[sl], b_xTs[sl], xn[sl], b_xn[sl], 8)
        k.st(s_xnT[T * 128:(T + 1) * 128, :], xT_s[sl], b_xTs[sl], d_xnT[T], eng="act")
    P.barrier()
    A.release(base_mark)

    wms = A.alloc(8 * 1032, BF16); b_wms = Buf("wms"); wms3 = v3(wms, 8)
    wmo = A.alloc(8 * 1024, BF16); b_wmo = Buf("wmo"); wmo3 = v3(wmo, 8)
    k.ldw(wms3, w_ms, 8, b_wms); k.ldw(wmo3, w_mo, 8, b_wmo)
    cwb = A.alloc(40); b_cwb = Buf("cwb"); k.ld(cwb, conv_wb, b_cwb)
    ifbb = A.alloc(8); b_ifbb = Buf("ifbb"); bc_load(ifbb, ifb, b_ifbb)
    gml = A.alloc(512); b_gml = Buf("gml"); bc_load(gml, g_ml, b_gml)
    xT = [A.alloc(1024, BF16) for _ in range(2)]; b_xT = [Buf("xT%d" % i) for i in range(2)]
    C32 = A.alloc(4 * 129); b_C32 = [Buf("C32_%d" % h) for h in range(4)]; C32v = v3(C32, 4)
    Cb = A.alloc(4 * 130, BF16); b_Cb = [Buf("Cb_%d" % h) for h in range(4)]; Cbv = v3(Cb, 4)
    k.memset(C32, 0.0, b_C32); k.memset(Cb, 0.0, b_Cb)
    kpre = [A.alloc(132) for _ in range(4)]; b_kpre = [Buf("kpre%d" % h) for h in range(4)]
    qpre = [A.alloc(132) for _ in range(4)]; b_qpre = [Buf("qpre%d" % h) for h in range(4)]
    for h in range(4):
        k.memset(kpre[h], 0.0, [b_kpre[h]]); k.memset(qpre[h], 0.0, [b_qpre[h]])
    NSL = 2
    def mk(n, cols, dt=F32, pre=""):
        return [A.alloc(cols, dt) for _ in range(n)], [Buf(pre + str(i)) for i in range(n)]
    lif, b_lif = mk(NSL, 8, pre="lif"); gt, b_gt = mk(NSL, 24, pre="gt")
    ck, b_ck = mk(NSL, 128, pre="ck"); ex, b_ex = mk(NSL, 128, pre="ex")
    kTb, b_kTb = mk(NSL, 128, BF16, "kTb"); qTb, b_qTb = mk(NSL, 128, BF16, "qTb")
    ktok, b_ktok = mk(NSL, 128, BF16, "ktok"); vp, b_vp = mk(NSL, 130, BF16, "vp")
    tmpc, b_tmpc = mk(NSL, 129, pre="tmpc"); Sm, b_Sm = mk(NSL, 128, BF16, "Sm")
    hc, b_hc = mk(NSL, 128, pre="hc"); sm8, b_sm8 = mk(NSL, 8, pre="sm8")
    sig = A.alloc(512); b_sig = Buf("sig")
    ympos = [A.alloc(512, BF16) for _ in range(2)]; b_ympos = [Buf("ympos%d" % i) for i in range(2)]
    ymd = A.alloc(512); b_ymd = Buf("ymd")
    ymo = [A.alloc(512, BF16) for _ in range(2)]; b_ymo = [Buf("ymo%d" % i) for i in range(2)]
    hcnt = [0]

    def conv_silu(pre, b_pre, ps_src, b_ps, hq, out_bf, b_out, scale, sl):
        k.cp("act", pre[:, 3:131], ps_src, [b_ps], [b_pre])
        c = ck[sl]; bcx = b_ck[sl]; e_ = ex[sl]; be = b_ex[sl]
        w0 = cwb[:, hq * 4:hq * 4 + 1]
        k.ts(c, pre[:, 0:128], w0, cwb[:, 32 + hq:33 + hq], ALU.mult, ALU.add, [b_pre, b_cwb], [bcx])
        for tap in range(1, 4):
            k.stt(c, pre[:, tap:tap + 128], cwb[:, hq * 4 + tap:hq * 4 + tap + 1], c, ALU.mult, ALU.add, [b_pre, b_cwb, bcx], [bcx])
        k.cp("pool", pre[:, 0:3], pre[:, 128:131], [b_pre], [b_pre])
        k.act(e_, c, AF.Exp, [bcx], [be], scale=-1.0)
        k.ts(e_, e_, 1.0, None, ALU.add, None, [be], [be])
        k.recip(e_, e_, [be], [be])
        k.stt(out_bf, c, scale, e_, ALU.mult, ALU.mult, [bcx, be], [b_out])

    k.ld(xT[0], s_xnT[0:128, :], b_xT[0], reads=[d_xnT[0]])
    for T in range(NT):
        sl = T % 2
        x3 = v3(xT[sl], 8); bx = b_xT[sl]
        if T + 1 < NT:
            k.ld(xT[(T + 1) % 2], s_xnT[(T + 1) * 128:(T + 2) * 128, :], b_xT[(T + 1) % 2], reads=[d_xnT[T + 1]])
        pos = T % 8
        outp = pos >= 6
        pv, bpv = k.ps("pmv", (0, 1))
        for kc in range(8):
            k.mm(pv[:, 0:512], x3[:, kc, :], wms3[:, kc, 512:1024], kc == 0, kc == 7, [bx, b_wms], [bpv], inc=(kc == 7))
        pif, bpif = k.ps("pmisc", (5, 6, 7))
        for kc in range(8):
            k.mm(pif[:, 0:8], x3[:, kc, :], wms3[:, kc, 1024:1032], kc == 0, kc == 7, [bx, b_wms], [bpif], inc=(kc == 7))
        L = lif[sl]; bL = b_lif[sl]; G = gt[sl]; bG = b_gt[sl]
        k.tt(L, pif[:, 0:8], ifbb, ALU.add, [bpif, b_ifbb], [bL])
        k.act(G[:, 0:4], L[:, 4:8], AF.Exp, [bL], [bG], scale=-1.0)
        k.act(G[:, 0:4], G[:, 0:4], AF.Ln, [bG], [bG], bias=1.0)
        pb, bpb = k.ps("pmisc", (5, 6, 7))
        k.mm(pb[:, 0:4], NUT, G[:, 0:4], True, True, [b_cf, bG], [bpb], inc=False)
        k.mm(pb[:, 4:8], NONES, G[:, 0:4], True, True, [b_cf, bG], [bpb])
        k.act(G[:, 8:16], pb[:, 0:8], AF.Exp, [bpb], [bG])
        k.tt(G[:, 16:20], L[:, 0:4], pb[:, 0:4], ALU.subtract, [bL, bpb], [bG])
        k.act(G[:, 4:8], G[:, 16:20], AF.Exp, [bG], [bG])
        k.ts(G[:, 4:8], G[:, 4:8], VALID[:, T:T + 1], None, ALU.mult, None, [bG, b_cc], [bG])
        if outp:
            po, bpo = k.ps("pmv", (0, 1))
            for kc in range(8):
                k.mm(po[:, 0:512], x3[:, kc, :], wmo3[:, kc, 512:1024], kc == 0, kc == 7, [bx, b_wmo], [bpo], inc=(kc == 7))
            k.act(sig, po[:, 0:512], AF.Exp, [bpo], [b_sig], scale=-1.0)
            k.ts(sig, sig, 1.0, None, ALU.add, None, [b_sig], [b_sig])
            k.recip(sig, sig, [b_sig], [b_sig])
        for h in range(4):
            hs = hcnt[0] % NSL; hcnt[0] += 1
            pk, bpk = k.ps("pmk", (2, 3, 4))
            for kc in range(8):
                k.mm(pk[:, 0:128], wms3[:, kc, 128 * h:128 * h + 128], x3[:, kc, :], kc == 0, kc == 7, [bx, b_wms], [bpk], inc=(kc == 7))
            conv_silu(kpre[h], b_kpre[h], pk[:, 0:128], bpk, 4 + h, kTb[hs], b_kTb[hs], 128 ** -0.5, hs)
            if pos >= 5:
                pq, bpq = k.ps("pmk", (2, 3, 4))
                for kc in range(8):
                    k.mm(pq[:, 0:128], wmo3[:, kc, 128 * h:128 * h + 128], x3[:, kc, :], kc == 0, kc == 7, [bx, b_wmo], [bpq], inc=(kc == 7))
                if outp:
                    conv_silu(qpre[h], b_qpre[h], pq[:, 0:128], bpq, h, qTb[hs], b_qTb[hs], 1.0, hs)
                else:
                    k.cp("act", qpre[h][:, 3:131], pq[:, 0:128], [bpq], [b_qpre[h]])
                    k.cp("pool", qpre[h][:, 0:3], qpre[h][:, 128:131], [b_qpre[h]], [b_qpre[h]])
            ptk, bptk = k.ps("pmisc", (5, 6, 7))
            ptkb = ptk[:, :].bitcast(BF16)
            k.tr(ptkb[:, 0:128], kTb[hs], IDB, [b_kTb[hs], b_cb], [bptk])
            k.cp("act", ktok[hs], ptkb[:, 0:128], [bptk], [b_ktok[hs]])
            V = vp[hs]; bV = b_vp[hs]
            k.ts(V[:, 0:128], pv[:, 128 * h:128 * h + 128], G[:, 4 + h:5 + h], None, ALU.mult, None, [bpv, bG], [bV])
            k.cp("pool", V[:, 128:129], G[:, 4 + h:5 + h], [bG], [bV])
            if outp:
                pss, bpss = k.ps("pmk", (2, 3, 4))
                k.mm(pss[:, 0:128], kTb[hs], qTb[hs], True, True, [b_kTb[hs], b_qTb[hs]], [bpss])
                k.tt(Sm[hs], pss[:, 0:128], UTB, ALU.mult, [bpss, b_cb], [b_Sm[hs]])
                pn, bpn = k.ps("pmk", (2, 3, 4))
                k.mm(pn[:, 0:129], Sm[hs], V[:, 0:129], True, False, [b_Sm[hs], bV], [bpn], inc=False)
                k.mm(pn[:, 0:129], qTb[hs], Cbv[:, h, 0:129], False, True, [b_qTb[hs], b_Cb[h]], [bpn])
                s8 = sm8[hs]; bs8 = b_sm8[hs]
                et = G[:, 8 + h:9 + h]
                k.tt(s8[:, 0:1], pn[:, 128:129], et, ALU.mult, [bpn, bG], [bs8])
                k.stt(s8[:, 1:2], s8[:, 0:1], -1.0, s8[:, 0:1], ALU.mult, ALU.max, [bs8], [bs8])
                k.ts(s8[:, 1:2], s8[:, 1:2], 1.0, None, ALU.max, None, [bs8], [bs8])
                k.recip(s8[:, 2:3], s8[:, 1:2], [bs8], [bs8])
                k.tt(s8[:, 3:4], s8[:, 2:3], et, ALU.mult, [bs8, bG], [bs8])
                H = hc[hs]; bH = b_hc[hs]
                k.stt(H, pn[:, 0:128], s8[:, 3:4], sig[:, 128 * h:128 * h + 128], ALU.mult, ALU.mult, [bpn, bs8, b_sig], [bH])
                k.act(ex[hs], H, AF.Square, [bH], [b_ex[hs], bs8], accum_out=s8[:, 4:5])
                k.rstd(s8[:, 4:5], 1.0 / 128, s8[:, 5:6], s8[:, 6:7], [bs8], [bs8])
                k.stt(ympos[pos - 6][:, 128 * h:128 * h + 128], H, s8[:, 6:7], gml[:, 128 * h:128 * h + 128], ALU.mult, ALU.mult,
                      [bH, bs8, b_gml], [b_ympos[pos - 6]])
            pc, bpc = k.ps("pmk", (2, 3, 4))
            k.mm(pc[:, 0:129], ktok[hs], V[:, 0:129], True, True, [b_ktok[hs], bV], [bpc])
            tc_, btc = tmpc[hs], b_tmpc[hs]
            k.tt(tc_, pc[:, 0:129], C32v[:, h, :], ALU.add, [bpc, b_C32[h]], [btc])
            k.ts(C32v[:, h, :], tc_, G[:, 12 + h:13 + h], None, ALU.mult, None, [btc, bG], [b_C32[h]])
            k.ts(Cbv[:, h, 0:129], tc_, G[:, 12 + h:13 + h], None, ALU.mult, None, [btc, bG], [b_Cb[h]])
        if pos == 7:
            j = T // 8
            k.tt(ymd, ympos[1], ympos[0], ALU.subtract, b_ympos, [b_ymd])
            yo = ymo[j % 2]; byo = b_ymo[j % 2]
            k.stt(yo, ymd, PAR, ympos[0], ALU.mult, ALU.add, [b_ymd, b_cc, b_ympos[0]], [byo])
            k.st(s_ym[j * 128:(j + 1) * 128, :], yo, byo, d_ym[j])
    P.barrier()
    A.release(base_mark)
    if stop == "PM":
        P.emit(); print("instructions:", P.n_inst, {e: P.cnt[e] for e in ENGS}); return nc

    KT = A.alloc(2 * 16384, BF16); KT3 = v3(KT, 2)
    b_KT = [Buf("KT%d" % t) for t in range(NT)]
    V1 = A.alloc(NT * 260, BF16); V14 = V1.rearrange("p (t h d) -> p t h d", t=NT, h=4)
    b_V1 = [Buf("V1_%d" % t) for t in range(NT)]
    Eb = A.alloc(64 * 128, BF16); b_Eb = Buf("Eb"); Eb3 = v3(Eb, 64)
    k.ld(Eb[0:64, :], c_e, b_Eb)
    gmask = A.alloc(1024); b_gmask = Buf("gmask"); bc_load(gmask, c_gmask, b_gmask); gmask3 = v3(gmask, 16)
    wkv = A.alloc(8 * 512, BF16); b_wkv = Buf("wkv"); wkv3 = v3(wkv, 8)
    wq = A.alloc(8 * 256, BF16); b_wq = Buf("wq"); wq3 = v3(wq, 8)
    xT = [A.alloc(1024, BF16) for _ in range(2)]; b_xT = [Buf("axT%d" % i) for i in range(2)]
    xTo = A.alloc(1024, BF16); b_xTo = Buf("xTo")
    ksum = A.alloc(2 * NT); b_ksum = Buf("ksum"); ksum3 = v3(ksum, 2)
    kmT = A.alloc(2 * 64); b_kmT = Buf("kmT"); kmT3 = v3(kmT, 2)
    QTs = A.alloc(512, BF16); b_QTs = Buf("QTs"); QTs4 = QTs.rearrange("p (a s q) -> p a s q", a=2, s=2)
    QT32 = A.alloc(512); b_QT32 = Buf("QT32"); QT324 = QT32.rearrange("p (a s q) -> p a s q", a=2, s=2)
    k.memset(QTs, 0.0, [b_QTs]); k.memset(QT32, 0.0, [b_QT32])
    gm_, b_gm = mk(4, 64, pre="gm"); t8, b_t8 = mk(4, 16, pre="t8")
    b01, b_b01 = mk(4, 64, BF16, "b01"); BT, b_BT = mk(4, 128, BF16, "BT")
    PT, b_PT = mk(3, 512, BF16, "PT")
    yat = [A.alloc(512, BF16) for _ in range(2)]; b_yat = [Buf("yat%d" % i) for i in range(2)]
    r1, b_r1 = mk(2, 2, pre="r1")
    hcount = [0]; gcount = [0]

    for hp in range(2):
        if hp == 1:
            P.barrier()
        k.ldw(wkv3, w_kv[hp], 8, b_wkv); k.ldw(wq3, w_q[hp], 8, b_wq)
        k.memset(kmT, 0.0, [b_kmT]); k.memset(ksum, 0.0, [b_ksum])
        if hp == 0:
            for t0 in range(0, NT, 16):
                k.memset(V1[:, t0 * 260:(t0 + 16) * 260], 1.0, b_V1[t0:t0 + 16])
        k.ld(xT[0], s_xnT[0:128, :], b_xT[0], reads=[d_xnT[0]])
        for T in range(NT):
            sl = T % 2
            x3 = v3(xT[sl], 8); bx = b_xT[sl]
            if T + 1 < NT:
                k.ld(xT[(T + 1) % 2], s_xnT[(T + 1) * 128:(T + 2) * 128, :], b_xT[(T + 1) % 2], reads=[d_xnT[T + 1]])
            for p in range(2):
                pk, bpk = k.ps("paproj", (5, 6, 7))
                for kc in range(8):
                    k.mm(pk[:, 0:128], wkv3[:, kc, 128 * p:128 * p + 128], x3[:, kc, :], kc == 0, kc == 7, [bx, b_wkv], [bpk], inc=(kc == 7))
                k.act(KT3[:, p, T * 128:(T + 1) * 128], pk[:, 0:128], AF.Identity, [bpk], [b_KT[T], b_ksum], accum_out=ksum3[:, p, T:T + 1])
            pv, bpv = k.ps("paproj", (5, 6, 7))
            for kc in range(8):
                k.mm(pv[:, 0:256], x3[:, kc, :], wkv3[:, kc, 256:512], kc == 0, kc == 7, [bx, b_wkv], [bpv], inc=(kc == 7))
            k.cp("dve", V14[:, T, :, 0:64], pv[:, 0:256].rearrange("p (h d) -> p h d", h=4), [bpv], [b_V1[T]])
            if T % 2 == 1:
                n = T // 2
                k.tt(kmT3[:, :, n:n + 1], ksum3[:, :, T - 1:T], ksum3[:, :, T:T + 1], ALU.add, [b_ksum], [b_kmT])
            if T % 8 != 7:
                continue
            j = T // 8
            k.ld(xTo, s_xnT[(NT + j) * 128:(NT + j + 1) * 128, :], b_xTo, reads=[d_xnT[NT + j]])
            xo3 = v3(xTo, 8)
            for p in range(2):
                pq, bpq = k.ps("paproj", (5, 6, 7))
                for kc in range(8):
                    k.mm(pq[:, 0:128], wq3[:, kc, 128 * p:128 * p + 128], xo3[:, kc, :], kc == 0, kc == 7, [b_xTo, b_wq], [bpq], inc=(kc == 7))
                for s_ in range(2):
                    k.ts(QTs4[64 * s_:64 * s_ + 64, p, s_, :], pq[64 * s_:64 * s_ + 64, 0:128], 0.125, None, ALU.mult, None, [bpq], [b_QTs])
                    k.cp("dve", QT324[64 * s_:64 * s_ + 64, p, s_, :], pq[64 * s_:64 * s_ + 64, 0:128], [bpq], [b_QT32])
            ya_t = yat[j % 2]; b_ya_t = b_yat[j % 2]
            nkt = 8 * j + 8
            def prologue_a(hl):
                p, s = hl // 2, hl % 2
                pg, bpg = k.ps("paproj", (5, 6, 7))
                k.mm(pg[:, 0:64], QT324[:, p, s, :], kmT3[:, p, :], True, True, [b_QT32, b_kmT], [bpg])
                g_ = gm_[hl]; bg_ = b_gm[hl]; t_ = t8[hl]; bt_ = b_t8[hl]
                k.tt(g_, pg[:, 0:64], gmask3[:, j, :], ALU.add, [bpg, b_gmask], [bg_])
                P.op("dve", lambda e, o=t_[:, 0:8], i=g_: e.max(out=o, in_=i), [bg_], [bt_])
                k.ts(t_[:, 8:9], t_[:, 2:3], -1e29, None, ALU.max, None, [bt_], [bt_])
                k.ts(b01[hl], g_, t_[:, 8:9], 1.0, ALU.is_ge, ALU.subtract, [bg_, bt_], [b_b01[hl]])

            def prologue_b(hl):
                pbt, bpbt = k.ps("paproj", (5, 6, 7))
                pbtb = pbt[:, :].bitcast(BF16)
                k.tr(pbtb[0:64, 0:128], b01[hl], IDB, [b_b01[hl], b_cb], [bpbt])
                k.cp("dve", BT[hl][0:64, :], pbtb[0:64, 0:128], [bpbt], [b_BT[hl]])

            def qk_group(hl, g0):
                p, s = hl // 2, hl % 2
                pS, bpS = k.ps("pas", (2, 3, 4))
                for i in range(4):
                    kt = g0 + i
                    col = pS[:, i * 128:(i + 1) * 128]
                    k.mm(col, KT3[:, p, kt * 128:(kt + 1) * 128], QTs4[:, p, s, :], True, False, [b_KT[kt], b_QTs], [bpS], inc=False)
                    if kt < nkt - 2:
                        k.mm(col, Eb3[0:64, kt // 2, :], BT[hl][0:64, :], False, True, [b_Eb, b_BT[hl]], [bpS], inc=(i == 3))
                    else:
                        k.mm(col, IDB, CM[kt - (nkt - 2)], False, True, [b_cb], [bpS], inc=(i == 3))
                return pS, bpS

            def pv_group(hl, g0, pS, bpS, po, bpo):
                pt_ = PT[gcount[0] % 3]; bpt_ = b_PT[gcount[0] % 3]; gcount[0] += 1
                k.act(pt_, pS[:, 0:512], AF.Exp, [bpS], [bpt_])
                for i in range(4):
                    kt = g0 + i
                    k.mm(po[:, 0:65], pt_[:, i * 128:(i + 1) * 128], V14[:, kt, hl, :], kt == 0, kt == nkt - 1, [bpt_, b_V1[kt]], [bpo],
                         inc=(i == 3))

            groups = list(range(0, nkt, 4))
            prologue_a(0); prologue_b(0)
            for hl in range(4):
                po, bpo = k.ps("pao", (0, 1))
                cur = qk_group(hl, groups[0])
                for gi, g0 in enumerate(groups):
                    nxt = qk_group(hl, groups[gi + 1]) if gi + 1 < len(groups) else None
                    if hl < 3 and gi == 0:
                        prologue_a(hl + 1)
                    if hl < 3 and gi == min(1, len(groups) - 1):
                        prologue_b(hl + 1)
                    pv_group(hl, g0, cur[0], cur[1], po, bpo)
                    cur = nxt
                r_ = r1[hl % 2]; br_ = b_r1[hl % 2]
                k.recip(r_[:, 0:1], po[:, 64:65], [bpo], [br_])
                k.ts(ya_t[:, 64 * hl:64 * hl + 64], po[:, 0:64], r_[:, 0:1], None, ALU.mult, None, [bpo, br_], [b_ya_t])
            k.st(s_ya[j * 128:(j + 1) * 128, 256 * hp:256 * hp + 256], ya_t[:, 0:256], b_ya_t, d_ya[j])
    P.barrier()
    A.release(base_mark)
    if stop == "PA":
        P.emit(); print("instructions:", P.n_inst, {e: P.cnt[e] for e in ENGS}); return nc

    e_wg = din("e_wg", [NE * 1024, 512]); e_wu = din("e_wu", [NE * 1024, 512]); e_wd = din("e_wd", [NE * 512, 1024])
    w_pg = din("w_pg", [1024, 1024]); w_pp = din("w_pp", [256, 1024])
    h1 = A.alloc(16 * 1024); h13 = v3(h1, 16); b_h1 = [Buf("h1_%d" % j) for j in range(16)]
    hnT = A.alloc(8 * 2048, BF16); hnT3 = v3(hnT, 8); b_hnT = [Buf("hnT%d" % j) for j in range(16)]
    cw = A.alloc(16 * 32); cw3 = v3(cw, 16); b_cw = [Buf("cw%d" % j) for j in range(16)]
    NJ = NT // 8
    if NJ < 16:
        k.memset(hnT, 0.0, b_hnT); k.memset(cw, 0.0, b_cw); k.memset(h1, 0.0, b_h1)
    pb_mark = A.mark()
    wg_ = A.alloc(8 * 2048, BF16); b_wg = Buf("wg"); wg3 = v3(wg_, 8)
    wua = A.alloc(4 * 1024, BF16); b_wua = Buf("wua"); wua3 = v3(wua, 4)
    wum = A.alloc(4 * 1024, BF16); b_wum = Buf("wum"); wum3 = v3(wum, 4)
    wo = A.alloc(8 * 1024, BF16); b_wo = Buf("wo"); wo3 = v3(wo, 8)
    wr = A.alloc(8 * 36); b_wr = Buf("wr"); wr3 = v3(wr, 8)
    k.ldw(wg3, w_g, 8, b_wg); k.ldw(wua3, w_ua, 4, b_wua); k.ldw(wum3, w_um, 4, b_wum); k.ldw(wo3, w_out, 8, b_wo)
    for kc in range(8):
        k.ld(wr3[:, kc, :], w_r[kc * 128:(kc + 1) * 128, :], b_wr)
    bgb = A.alloc(2048, BF16); b_bgb = Buf("bgb")
    br32 = A.alloc(36); b_br32 = Buf("br32"); bc_load(br32, b_r, b_br32)
    ones = A.alloc(128); b_ones = Buf("ones"); k.memset(ones, 1.0, [b_ones])
    onesb = A.alloc(128, BF16); b_onesb = Buf("onesb"); k.memset(onesb, 1.0, [b_onesb])
    gfb = A.alloc(1024); b_gfb = Buf("gfb"); bc_load(gfb, g_ffn, b_gfb)
    xTo = A.alloc(1024, BF16); b_xTo = Buf("bxTo")
    yab = A.alloc(512, BF16); b_yab = Buf("yab"); ymb = A.alloc(512, BF16); b_ymb = Buf("ymb")
    yaT = A.alloc(512, BF16); b_yaT = Buf("yaT"); ymT = A.alloc(512, BF16); b_ymT = Buf("ymT")
    yaT3 = v3(yaT, 4); ymT3 = v3(ymT, 4)
    sga = A.alloc(512); b_sga = Buf("sga"); sgm = A.alloc(512); b_sgm = Buf("sgm")
    mrg = A.alloc(1024, BF16); b_mrg = Buf("mrg"); mrgT = A.alloc(1024, BF16); b_mrgT = Buf("mrgT"); mrgT3 = v3(mrgT, 8)
    hn32 = A.alloc(1024); b_hn32 = Buf("hn32")
    hnhi = A.alloc(1024, BF16); b_hnhi = Buf("hnhi"); hnlo = A.alloc(1024, BF16); b_hnlo = Buf("hnlo")
    hiT = A.alloc(1024, BF16); b_hiT = Buf("hiT"); hiT3 = v3(hiT, 8)
    loT = A.alloc(1024, BF16); b_loT = Buf("loT"); loT3 = v3(loT, 8)
    wrh = A.alloc(8 * 36, BF16); b_wrh = Buf("wrh"); wrh3 = v3(wrh, 8)
    wrl = A.alloc(8 * 36, BF16); b_wrl = Buf("wrl"); wrl3 = v3(wrl, 8)
    k.cp("dve", wrh, wr, [b_wr], [b_wrh])
    k.tt(wrl, wr, wrh, ALU.subtract, [b_wr, b_wrh], [b_wrl])
    Lr = A.alloc(40); b_Lr = Buf("Lr"); rs = A.alloc(16); b_rs = Buf("rs"); elm = A.alloc(32); b_elm = Buf("elm")
    c1 = A.alloc(32); b_c1 = Buf("c1")
    for q4 in range(4):
        k.ld(sga, b_gate[:, 512 * q4:512 * q4 + 512].partition_broadcast(128), b_sga)
        k.cp("dve", bgb[:, 512 * q4:512 * q4 + 512], sga, [b_sga], [b_bgb])

    def sigmoid_from(dst, b_dst, ps_ap, b_ps):
        k.act(dst, ps_ap, AF.Exp, [b_ps], [b_dst], scale=-1.0)
        k.ts(dst, dst, 1.0, None, ALU.add, None, [b_dst], [b_dst])
        k.recip(dst, dst, [b_dst], [b_dst])

    for j in range(NJ):
        k.ld(h13[:, j, :], x_own[j * 128:(j + 1) * 128, :], b_h1[j])
        k.ld(xTo, s_xnT[(NT + j) * 128:(NT + j + 1) * 128, :], b_xTo, reads=[d_xnT[NT + j]])
        k.ld(yab, s_ya[j * 128:(j + 1) * 128, :], b_yab, reads=[d_ya[j]])
        k.ld(ymb, s_ym[j * 128:(j + 1) * 128, :], b_ymb, reads=[d_ym[j]])
        xo3 = v3(xTo, 8)
        transpose_to(yaT, b_yaT, yab, b_yab, 4, grp="pbm", banks=(6, 7), eng="dve")
        transpose_to(ymT, b_ymT, ymb, b_ymb, 4, grp="pbm", banks=(6, 7), eng="dve")
        for b in range(2):
            cs = slice(512 * b, 512 * b + 512)
            pga, bpga = k.ps("pbg", (0, 1, 2))
            for kc in range(8):
                k.mm(pga[:, 0:512], xo3[:, kc, :], wg3[:, kc, 512 * b:512 * b + 512], kc == 0, kc == 7, [b_xTo, b_wg], [bpga], inc=(kc == 7))
            k.tt(sga, pga[:, 0:512], bgb[:, 512 * b:512 * b + 512], ALU.add, [bpga, b_bgb], [b_sga])
            sigmoid_from(sga, b_sga, sga, b_sga)
            pua, bpua = k.ps("pbu", (3, 4, 5))
            for fc in range(4):
                k.mm(pua[:, 0:512], yaT3[:, fc, :], wua3[:, fc, cs], fc == 0, fc == 3, [b_yaT, b_wua], [bpua], inc=(fc == 3))
            k.tt(sga, sga, pua[:, 0:512], ALU.mult, [b_sga, bpua], [b_sga])
            pgm, bpgm = k.ps("pbg", (0, 1, 2))
            for kc in range(8):
                k.mm(pgm[:, 0:512], xo3[:, kc, :], wg3[:, kc, 1024 + 512 * b:1024 + 512 * b + 512], kc == 0, kc == 7, [b_xTo, b_wg], [bpgm], inc=(kc == 7))
            k.tt(sgm, pgm[:, 0:512], bgb[:, 1024 + 512 * b:1024 + 512 * b + 512], ALU.add, [bpgm, b_bgb], [b_sgm])
            sigmoid_from(sgm, b_sgm, sgm, b_sgm)
            pum, bpum = k.ps("pbu", (3, 4, 5))
            for fc in range(4):
                k.mm(pum[:, 0:512], ymT3[:, fc, :], wum3[:, fc, cs], fc == 0, fc == 3, [b_ymT, b_wum], [bpum], inc=(fc == 3))
            k.tt(sgm, sgm, pum[:, 0:512], ALU.mult, [b_sgm, bpum], [b_sgm])
            k.tt(mrg[:, cs], sga, sgm, ALU.add, [b_sga, b_sgm], [b_mrg])
        transpose_to(mrgT, b_mrgT, mrg, b_mrg, 8, grp="pbm", banks=(6, 7), eng="act")
        for b in range(2):
            ph, bph = k.ps("pbg", (0, 1, 2))
            for kc in range(8):
                k.mm(ph[:, 0:512], mrgT3[:, kc, :], wo3[:, kc, 512 * b:512 * b + 512], kc == 0, kc == 7, [b_mrgT, b_wo], [bph], inc=(kc == 7))
            k.tt(h13[:, j, 512 * b:512 * b + 512], h13[:, j, 512 * b:512 * b + 512], ph[:, 0:512], ALU.add, [b_h1[j], bph], [b_h1[j]])
        if stop == "PB1":
            continue
        s = st4[0]; bs = b_st4[0]
        k.act(junk, h13[:, j, :], AF.Square, [b_h1[j]], [b_junk, bs], accum_out=s[:, 0:1])
        k.rstd(s[:, 0:1], 1.0 / 1024, s[:, 1:2], s[:, 2:3], [bs], [bs])
        k.stt(hn32, h13[:, j, :], s[:, 2:3], gfb, ALU.mult, ALU.mult, [b_h1[j], bs, b_gfb], [b_hn32])
        k.cp("dve", hnhi, hn32, [b_hn32], [b_hnhi])
        k.tt(hnlo, hn32, hnhi, ALU.subtract, [b_hn32, b_hnhi], [b_hnlo])
        transpose_to(hiT, b_hiT, hnhi, b_hnhi, 8, grp="pbm", banks=(6, 7), eng="act")
        transpose_to(loT, b_loT, hnlo, b_hnlo, 8, grp="pbm", banks=(6, 7), eng="act")
        k.cp("dve", hnT3[:, :, j * 128:(j + 1) * 128], hiT3, [b_hiT], [b_hnT[j]])
        pr, bpr = k.ps("pbu", (3, 4, 5))
        n_ = 0
        for (aT, baT, w_, bw_) in ((hiT3, b_hiT, wrh3, b_wrh), (hiT3, b_hiT, wrl3, b_wrl), (loT3, b_loT, wrh3, b_wrh)):
            for kc in range(8):
                k.mm(pr[:, 0:36], aT[:, kc, :], w_[:, kc, :], n_ == 0, n_ == 23, [baT, bw_], [bpr], inc=(n_ == 23))
                n_ += 1
        if stop == "PB2":
            continue
        k.tt(Lr[:, 0:36], pr[:, 0:36], br32, ALU.add, [bpr, b_br32], [b_Lr])
        P.op("dve", lambda e, o=rs[:, 0:1], i=Lr[:, 0:4]: e.tensor_reduce(out=o, in_=i, axis=AX.X, op=ALU.max), [b_Lr], [b_rs])
        k.ts(rs[:, 1:2], rs[:, 0:1], -1.0, None, ALU.mult, None, [b_rs], [b_rs])
        k.act(Lr[:, 36:40], Lr[:, 0:4], AF.Exp, [b_Lr, b_rs], [b_Lr, b_rs], bias=rs[:, 1:2], accum_out=rs[:, 2:3])
        k.recip(rs[:, 3:4], rs[:, 2:3], [b_rs], [b_rs])
        k.ts(Lr[:, 36:40], Lr[:, 0:4], rs[:, 0:1], 1.0, ALU.is_equal, ALU.subtract, [b_Lr, b_rs], [b_Lr])
        k.ts(Lr[:, 36:40], Lr[:, 36:40], 1e30, None, ALU.mult, None, [b_Lr], [b_Lr])
        for g in range(4):
            k.ts(elm[:, 8 * g:8 * g + 8], Lr[:, 4 + 8 * g:12 + 8 * g], Lr[:, 36 + g:37 + g], None, ALU.add, None, [b_Lr], [b_elm])
        P.op("dve", lambda e, o=rs[:, 8:16], i=elm: e.max(out=o, in_=i), [b_elm], [b_rs])
        k.tt(rs[:, 4:5], rs[:, 9:10], rs[:, 8:9], ALU.subtract, [b_rs], [b_rs])
        k.act(rs[:, 4:5], rs[:, 4:5], AF.Exp, [b_rs], [b_rs])
        k.ts(rs[:, 4:5], rs[:, 4:5], 1.0, None, ALU.add, None, [b_rs], [b_rs])
        k.recip(rs[:, 5:6], rs[:, 4:5], [b_rs], [b_rs])
        k.tt(rs[:, 6:7], rs[:, 5:6], rs[:, 3:4], ALU.mult, [b_rs], [b_rs])
        k.tt(rs[:, 7:8], rs[:, 3:4], rs[:, 6:7], ALU.subtract, [b_rs], [b_rs])
        k.ts(c1, elm, rs[:, 8:9], rs[:, 6:7], ALU.is_equal, ALU.mult, [b_elm, b_rs], [b_c1])
        k.ts(cw3[:, j, :], elm, rs[:, 9:10], rs[:, 7:8], ALU.is_equal, ALU.mult, [b_elm, b_rs], [b_cw[j]])
        k.tt(cw3[:, j, :], cw3[:, j, :], c1, ALU.add, [b_cw[j], b_c1], [b_cw[j]])
    P.barrier()
    A.release(pb_mark)
    if stop in ("PB", "PB1", "PB2"):
        P.emit(); return nc

    ewg = [A.alloc(8 * 512, BF16) for _ in range(2)]; ewu = [A.alloc(8 * 512, BF16) for _ in range(2)]
    ewd = [A.alloc(4 * 1024, BF16) for _ in range(2)]
    b_ew = [[Buf("ewg%d" % i), Buf("ewu%d" % i), Buf("ewd%d" % i)] for i in range(2)]
    hidT = A.alloc(4 * 2048, BF16); hidT3 = v3(hidT, 4); b_hid = [Buf("hid%d" % t) for t in range(4)]
    sgt = [A.alloc(512, BF16) for _ in range(2)]; b_sgt = [Buf("sgt%d" % i) for i in range(2)]

    def load_expert(e):
        sl = e % 2
        k.ldw(v3(ewg[sl], 8), e_wg[e * 1024:(e + 1) * 1024, :], 8, b_ew[sl][0])
        k.ldw(v3(ewu[sl], 8), e_wu[e * 1024:(e + 1) * 1024, :], 8, b_ew[sl][1])
        k.ldw(v3(ewd[sl], 4), e_wd[e * 512:(e + 1) * 512, :], 4, b_ew[sl][2])

    load_expert(0)
    scnt = [0]
    for e in range(NE):
        sl = e % 2
        if e + 1 < NE:
            load_expert(e + 1)
        g3, u3, d3 = v3(ewg[sl], 8), v3(ewu[sl], 8), v3(ewd[sl], 4)
        bwg, bwu, bwd = b_ew[sl]
        for tg in range((NJ + 3) // 4):
            rb = [b_hnT[4 * tg + i] for i in range(4)]
            for fc in range(4):
                pG, bpG = k.ps("peg", (0, 1, 2, 3))
                for kc in range(8):
                    k.mm(pG[:, 0:512], g3[:, kc, fc * 128:(fc + 1) * 128], hnT3[:, kc, tg * 512:(tg + 1) * 512], kc == 0, kc == 7, rb + [bwg], [bpG],
                         inc=(kc == 7))
                pU, bpU = k.ps("peg", (0, 1, 2, 3))
                for kc in range(8):
                    k.mm(pU[:, 0:512], u3[:, kc, fc * 128:(fc + 1) * 128], hnT3[:, kc, tg * 512:(tg + 1) * 512], kc == 0, kc == 7, rb + [bwu], [bpU],
                         inc=(kc == 7))
                sg = sgt[scnt[0] % 2]; bsg = b_sgt[scnt[0] % 2]; scnt[0] += 1
                k.act(sg, pG[:, 0:512], AF.Silu, [bpG], [bsg])
                k.tt(hidT3[:, fc, tg * 512:(tg + 1) * 512], sg, pU[:, 0:512], ALU.mult, [bsg, bpU], [b_hid[tg]])
        for jt in range(NJ):
            for b in range(2):
                py, bpy = k.ps("pey", (4, 5, 6, 7))
                for fc in range(4):
                    k.mm(py[:, 0:512], hidT3[:, fc, jt * 128:(jt + 1) * 128], d3[:, fc, 512 * b:512 * b + 512], fc == 0, fc == 3, [b_hid[jt // 4], bwd], [bpy],
                         inc=(fc == 3))
                hsl = h13[:, jt, 512 * b:512 * b + 512]
                k.stt(hsl, py[:, 0:512], cw3[:, jt, e:e + 1], hsl, ALU.mult, ALU.add, [bpy, b_cw[jt], b_h1[jt]], [b_h1[jt]])
    P.barrier()
    A.release(pb_mark)
    if stop == "PE":
        P.emit(); return nc

    wpg = A.alloc(8 * 1024, BF16); b_wpg = Buf("wpg"); wpg3 = v3(wpg, 8)
    wpp = A.alloc(2 * 1024, BF16); b_wpp = Buf("wpp"); wpp3 = v3(wpp, 2)
    k.ldw(wpg3, w_pg, 8, b_wpg); k.ldw(wpp3, w_pp, 2, b_wpp)
    gpb = A.alloc(1024); b_gpb = Buf("gpb"); bc_load(gpb, g_ple, b_gpb)
    gfin = A.alloc(1024); b_gfin = Buf("gfin"); bc_load(gfin, g_fin, b_gfin)
    pn = [A.alloc(1024, BF16) for _ in range(2)]; b_pn = [Buf("pn%d" % i) for i in range(2)]
    pnT = [A.alloc(1024, BF16) for _ in range(2)]; b_pnT = [Buf("pnT%d" % i) for i in range(2)]
    p32 = [A.alloc(256) for _ in range(2)]; b_p32 = [Buf("p32%d" % i) for i in range(2)]
    pbf = [A.alloc(256, BF16) for _ in range(2)]; b_pbf = [Buf("pbf%d" % i) for i in range(2)]
    pT = [A.alloc(256, BF16) for _ in range(2)]; b_pT = [Buf("pT%d" % i) for i in range(2)]
    sgp = [A.alloc(512) for _ in range(2)]; b_sgp = [Buf("sgp%d" % i) for i in range(2)]
    ot = [A.alloc(1024) for _ in range(2)]; b_ot = [Buf("ot%d" % i) for i in range(2)]
    cnt = [0]
    for j in range(NJ):
        sl = j % 2
        k.ld(p32[sl], p_own[j * 128:(j + 1) * 128, :], b_p32[sl])
        k.cp("dve", pbf[sl], p32[sl], [b_p32[sl]], [b_pbf[sl]])
        transpose_to(pT[sl], b_pT[sl], pbf[sl], b_pbf[sl], 2, grp="pfm", banks=(6, 7), eng="act")
        norm_tile((h13[:, j, :], b_h1[j]), sl, gpb, b_gpb, pn[sl], b_pn[sl])
        transpose_to(pnT[sl], b_pnT[sl], pn[sl], b_pn[sl], 8, grp="pfm", banks=(6, 7), eng="act")
        n3 = v3(pnT[sl], 8); t3 = v3(pT[sl], 2)
        for b in range(2):
            pgt, bpgt = k.ps("pfg", (0, 1, 2))
            for kc in range(8):
                k.mm(pgt[:, 0:512], n3[:, kc, :], wpg3[:, kc, 512 * b:512 * b + 512], kc == 0, kc == 7, [b_pnT[sl], b_wpg], [bpgt], inc=(kc == 7))
            sg = sgp[cnt[0] % 2]; bsg = b_sgp[cnt[0] % 2]; cnt[0] += 1
            sigmoid_from(sg, bsg, pgt[:, 0:512], bpgt)
            ppp, bppp = k.ps("pfp", (3, 4, 5))
            for c in range(2):
                k.mm(ppp[:, 0:512], t3[:, c, :], wpp3[:, c, 512 * b:512 * b + 512], c == 0, c == 1, [b_pT[sl], b_wpp], [bppp], inc=(c == 1))
            k.tt(sg, sg, ppp[:, 0:512], ALU.mult, [bsg, bppp], [bsg])
            hsl = h13[:, j, 512 * b:512 * b + 512]
            k.tt(hsl, hsl, sg, ALU.add, [b_h1[j], bsg], [b_h1[j]])
        s = st4[sl]; bs = b_st4[sl]
        k.act(junk, h13[:, j, :], AF.Square, [b_h1[j]], [b_junk, bs], accum_out=s[:, 0:1])
        k.rstd(s[:, 0:1], 1.0 / 1024, s[:, 1:2], s[:, 2:3], [bs], [bs])
        k.stt(ot[sl], h13[:, j, :], s[:, 2:3], gfin, ALU.mult, ALU.mult, [b_h1[j], bs, b_gfin], [b_ot[sl]])
        k.st(out_own[j * 128:(j + 1) * 128, :], ot[sl], b_ot[sl], d_out)
    if dummy is not None:
        dz = A.alloc(8); b_dz = Buf("dz")
        for i in range(dummy[1]):
            if dummy[0] == "pe":
                k.mm(k.psb[0][0][:, 0:2], IDB, IDB[:, 0:2], True, True, [b_cb], [k.psb[0][1]], inc=(i % 64 == 63))
            else:
                k.memset(dz, float(i % 7), [b_dz], eng=dummy[0])
    P.barrier()
    P.emit()
    print("per-engine stream lengths (instr + waits):", {e: len(P.ops[e]) + sum(len(w) for w, _, _ in P.ops[e]) for e in ENGS})
    print("instructions:", P.n_inst, "sems:", len(P.semkeys), "sbuf peak words:", A.peak,
          "counts:", {e: P.cnt[e] for e in ENGS})
    return nc


def _consts():
    bf = ml_dtypes.bfloat16
    s = np.arange(128)[:, None]; t = np.arange(128)[None, :]
    ut = (s <= t).astype(np.float32)
    c_f32 = np.concatenate([-ut, -np.ones((128, 128), np.float32), np.eye(128, dtype=np.float32)], axis=1)
    c_e = np.zeros((64, 64, 128), np.float32)
    for n in range(64):
        c_e[n, n, :] = NEGB
    return c_f32, ut, c_e.reshape(64, 64 * 128).astype(bf)


def make_in_maps(inp):
    bf = ml_dtypes.bfloat16
    f = lambda a: np.ascontiguousarray(np.asarray(a, dtype=np.float32))
    x = f(inp["x"])[0]; p = f(inp["p"])[0, 0]
    w_in = f(inp["w_in"])[0]
    o = np.cumsum([0, 512, 512, 512, 512, 512, 512, 512, 4, 4, 1024, 1024])
    aq, ak, av, mq, mk_, mv, mo, mi, mf, ga, gm = [w_in[:, o[i]:o[i + 1]] for i in range(11)]
    shared = {
        "g_mix": f(inp["mix_norm_g"]), "g_ffn": f(inp["ffn_norm_g"]), "g_ple": f(inp["ple_norm_g"]),
        "g_fin": f(inp["final_norm_g"]).reshape(1, 1024), "g_ml": f(inp["mlstm_norm_g"]),
        "w_kv0": f(np.concatenate([ak[:, 0:256], av[:, 0:256]], 1)), "w_kv1": f(np.concatenate([ak[:, 256:512], av[:, 256:512]], 1)),
        "w_q0": f(aq[:, 0:256]), "w_q1": f(aq[:, 256:512]),
        "w_ms": f(np.concatenate([mk_, mv, mi, mf], 1)), "w_mo": f(np.concatenate([mq, mo], 1)),
        "w_g": f(np.concatenate([ga, gm], 1)), "b_gate": f(inp["b_gate"]),
        "ifb": f(inp["mlstm_if_b"]),
        "w_ua": f(inp["w_up_attn"])[0], "w_um": f(inp["w_up_mlstm"])[0], "w_out": f(inp["w_out"])[0],
        "w_r": f(np.concatenate([f(inp["router_group_w"])[0], f(inp["router_expert_w"])[0]], 1)),
        "b_r": f(np.concatenate([f(inp["router_group_b"]), f(inp["router_expert_b"])], 1)),
        "e_wg": f(inp["expert_w_gate"])[0].reshape(32 * 1024, 512), "e_wu": f(inp["expert_w_up"])[0].reshape(32 * 1024, 512),
        "e_wd": f(inp["expert_w_down"])[0].reshape(32 * 512, 1024),
        "w_pg": f(inp["w_ple_gate"])[0], "w_pp": f(inp["w_ple_proj"])[0],
    }
    cw_ = f(inp["conv_w"])[0]; cb_ = f(inp["conv_b"])[0]
    conv_wb = np.zeros((128, 40), np.float32)
    for hq in range(8):
        ch = np.arange(128) + 128 * hq
        conv_wb[:, hq * 4:hq * 4 + 4] = cw_[:, ch].T
        conv_wb[:, 32 + hq] = cb_[ch]
    shared["conv_wb"] = conv_wb
    c_f32, ut, c_e = _consts()
    shared["c_f32"] = c_f32; shared["c_e"] = c_e
    x_t = x.reshape(128, 128, 1024); p_t = p.reshape(128, 128, 256)
    kk = np.arange(128)[:, None]; qq = np.arange(128)[None, :]
    tri = np.where(kk <= qq, 0.0, -NEGB).astype(np.float32)
    maps = []
    for c in range(NCORES):
        par = c % 2; pad = 2 * ((7 - c) // 2)
        xs = np.zeros((128, 128, 1024), np.float32)
        xs[pad:] = x_t[:128 - pad]
        own = [8 * j + c for j in range(16)]
        cm0 = tri if par == 0 else np.zeros((128, 128), np.float32)
        cm1 = np.full((128, 128), -NEGB, np.float32) if par == 0 else tri
        c_bf = np.concatenate([np.eye(128, dtype=np.float32), ut, cm0, cm1], 1).astype(bf)
        c_core = np.zeros((128, 130), np.float32)
        c_core[:, 0:128] = (np.arange(128)[None, :] >= pad).astype(np.float32)
        c_core[:, 128] = par; c_core[:, 129] = 1 - par
        gmask = np.full((16, 64), -1e30, np.float32)
        for j in range(16):
            gmask[j, pad // 2:4 * j + 3] = 0.0
        m = dict(shared)
        m.update({"x_shift": xs.reshape(128 * 128, 1024), "x_own": np.ascontiguousarray(x_t[own]).reshape(2048, 1024),
                  "p_own": np.ascontiguousarray(p_t[own]).reshape(2048, 256), "c_bf": c_bf, "c_core": c_core,
                  "c_gmask": gmask.reshape(1, 1024)})
        maps.append(m)
    return maps


_NC_CACHE = {}


def kernel(**inputs):
    if "nc" not in _NC_CACHE:
        _NC_CACHE["nc"] = build_program(False)
    nc = _NC_CACHE["nc"]
    maps = make_in_maps(inputs)
    res = run_bass_kernel_spmd(nc, maps, core_ids=list(range(NCORES)))
    out = np.zeros((128, 128, 1024), np.float32)
    for c in range(NCORES):
        o = np.asarray(res.results[c]["out_own"], dtype=np.float32).reshape(16, 128, 1024)
        for j in range(16):
            out[8 * j + c] = o[j]
    return out.reshape(1, 16384, 1024)
```

```python
import numpy as np
import ml_dtypes
from contextlib import ExitStack
import concourse.bass as bass
import concourse.mybir as mybir
from concourse.bass_utils import run_bass_kernel_spmd

F32 = mybir.dt.float32
BF16 = mybir.dt.bfloat16
ALU = mybir.AluOpType
AF = mybir.ActivationFunctionType
AX = mybir.AxisListType
ENGS = ("pe", "act", "dve", "pool", "sp")
NCORES = 8
EPS = 1e-6
NEGB = 30000.0


class Buf:
    __slots__ = ("name", "w", "r", "dsem")

    def __init__(self, name):
        self.name = name
        self.w = None
        self.r = []
        self.dsem = None


class Prog:
    def __init__(self, nc):
        self.nc = nc
        self.ops = {e: [] for e in ENGS}
        self.cnt = {e: 0 for e in ENGS}
        self.seen = {e: {} for e in ENGS}
        self.dma_cnt = {}
        self.semkeys = []
        self.semset = set()
        self.n_inst = 0

    LIM = 4000

    def _tok(self, eng, val):
        ep = (val - 1) // self.LIM
        key = "%s#%d" % (eng, ep)
        if key not in self.semset:
            self.semset.add(key)
            self.semkeys.append(key)
        return (key, val - ep * self.LIM)

    def _need(self, eng, deps):
        best = {}
        for k, v in deps:
            if eng == "pe" and k.startswith("pe#"):
                continue
            if v <= self.seen[eng].get(k, 0):
                continue
            if v > best.get(k, 0):
                best[k] = v
        for k, v in best.items():
            self.seen[eng][k] = v
        return list(best.items())

    def _deps(self, reads, writes):
        deps = []
        for b in reads:
            if b.w is not None:
                deps.append(b.w)
        for b in writes:
            if b.w is not None:
                deps.append(b.w)
            deps.extend(b.r)
        return deps

    def op(self, eng, fn, reads=(), writes=(), inc=True):
        waits = self._need(eng, self._deps(reads, writes))
        val = self.cnt[eng] + 1
        if inc:
            self.cnt[eng] = val
        tok = self._tok(eng, val)
        self.ops[eng].append((waits, fn, (tok[0], 1) if inc else None))
        for b in reads:
            b.r.append(tok)
            if len(b.r) > 24:
                b.r = self._compact(b.r)
        for b in writes:
            b.w = tok
            b.r = []
        self.n_inst += 1

    @staticmethod
    def _compact(r):
        best = {}
        for k, v in r:
            if v > best.get(k, 0):
                best[k] = v
        return list(best.items())

    def dma(self, eng, fn, anchor, reads=(), writes=(), amt=16):
        if anchor.dsem is None:
            anchor.dsem = "d%d" % len(self.semkeys)
            self.semkeys.append(anchor.dsem)
            self.dma_cnt[anchor.dsem] = 0
        k = anchor.dsem
        waits = self._need(eng, self._deps(reads, writes))
        self.dma_cnt[k] += amt
        tok = (k, self.dma_cnt[k])
        self.ops[eng].append((waits, fn, (k, amt)))
        for b in reads:
            b.r.append(tok)
        for b in writes:
            b.w = tok
            b.r = []
        self.n_inst += 1

    def barrier(self):
        deps = [self._tok(e, self.cnt[e]) for e in ENGS if self.cnt[e] > 0]
        deps += [(k, v) for k, v in self.dma_cnt.items() if v > 0]
        for e in ENGS:
            waits = self._need(e, deps)
            if waits:
                self.ops[e].append((waits, None, None))

    def emit(self):
        nc = self.nc
        with ExitStack() as st:
            sems = {}
            for k in self.semkeys:
                sems[k] = st.enter_context(nc.semaphore("s_" + k.replace("#", "_")))
            block = st.enter_context(nc.Block())

            def run(eobj, name):
                for waits, fn, inc in self.ops[name]:
                    for k, v in waits:
                        eobj.wait_ge(sems[k], v)
                    if fn is None:
                        continue
                    ins = fn(eobj)
                    if inc is not None:
                        ins.then_inc(sems[inc[0]], inc[1])

            @block.tensor
            def _(e):
                run(e, "pe")

            @block.scalar
            def _(e):
                run(e, "act")

            @block.vector
            def _(e):
                run(e, "dve")

            @block.gpsimd
            def _(e):
                run(e, "pool")

            @block.sync
            def _(e):
                run(e, "sp")


class Arena:
    def __init__(self, nc, words):
        self.t = nc.alloc_sbuf_tensor("arena", [128, words], F32)
        self.words = words
        self.top = 0
        self.peak = 0

    def mark(self):
        return self.top

    def release(self, m):
        self.top = m

    def alloc(self, cols, dtype=F32):
        w = cols if dtype == F32 else (cols + 1) // 2
        w = (w + 7) // 8 * 8
        a = self.top
        self.top += w
        self.peak = max(self.peak, self.top)
        assert self.top <= self.words, "SBUF arena overflow %d > %d" % (self.top, self.words)
        ap = self.t[:, a:a + w]
        if dtype != F32:
            ap = ap.bitcast(dtype)
        return ap[:, 0:cols]


def v3(ap, a):
    return ap.rearrange("p (a b) -> p a b", a=a)


class K:
    def __init__(self, nc, dbg=False):
        self.nc = nc
        self.P = Prog(nc)
        self.A = Arena(nc, 52600)
        self.dbg = dbg
        self.psb = []
        for i in range(8):
            t = nc.alloc_psum_tensor("psb%d" % i, [128, 512], F32)
            self.psb.append((t, Buf("ps%d" % i)))
        self.rr = {}

    def ps(self, group, banks):
        i = self.rr.get(group, 0)
        self.rr[group] = i + 1
        t, b = self.psb[banks[i % len(banks)]]
        return t, b

    def mm(self, out, lhsT, rhs, start, stop, reads, writes, inc=True):
        self.P.op("pe", lambda e: e.matmul(out, lhsT=lhsT, rhs=rhs, start=start, stop=stop), reads, writes, inc)

    def tr(self, out, in_, ident, reads, writes, inc=True):
        self.P.op("pe", lambda e: e.transpose(out=out, in_=in_, identity=ident), reads, writes, inc)

    def act(self, out, in_, func, reads, writes, bias=None, scale=None, accum_out=None, eng="act"):
        kw = {}
        if bias is not None:
            kw["bias"] = bias
        if scale is not None:
            kw["scale"] = scale
        if accum_out is not None:
            kw["accum_out"] = accum_out
        self.P.op(eng, lambda e: e.activation(out=out, in_=in_, func=func, **kw), reads, writes)

    def cp(self, eng, out, in_, reads, writes):
        if eng == "act":
            self.P.op("act", lambda e: e.copy(out=out, in_=in_), reads, writes)
        else:
            self.P.op(eng, lambda e: e.tensor_copy(out=out, in_=in_), reads, writes)

    def ts(self, out, in0, s1, s2, op0, op1, reads, writes, eng="dve"):
        if op1 is None:
            self.P.op(eng, lambda e: e.tensor_scalar(out=out, in0=in0, scalar1=s1, scalar2=None, op0=op0), reads, writes)
        else:
            self.P.op(eng, lambda e: e.tensor_scalar(out=out, in0=in0, scalar1=s1, scalar2=s2, op0=op0, op1=op1), reads, writes)

    def stt(self, out, in0, scalar, in1, op0, op1, reads, writes, eng="dve"):
        self.P.op(eng, lambda e: e.scalar_tensor_tensor(out=out, in0=in0, scalar=scalar, in1=in1, op0=op0, op1=op1), reads, writes)

    def tt(self, out, in0, in1, op, reads, writes, eng="dve"):
        self.P.op(eng, lambda e: e.tensor_tensor(out=out, in0=in0, in1=in1, op=op), reads, writes)

    def recip(self, out, in_, reads, writes):
        self.P.op("dve", lambda e: e.reciprocal(out=out, in_=in_), reads, writes)

    def memset(self, ap, val, writes, eng="dve"):
        self.P.op(eng, lambda e: e.memset(ap, val), (), writes)

    def ld(self, out, in_, buf, eng="sp", reads=()):
        self.P.dma(eng, lambda e: e.dma_start(out=out, in_=in_), buf, reads=reads, writes=[buf])

    def st(self, out, in_, buf, dbuf, eng="sp"):
        self.P.dma(eng, lambda e: e.dma_start(out=out, in_=in_), buf, reads=[buf], writes=[dbuf])

    def ldw(self, dst3, src, kc, buf, eng="pool"):
        s3 = src.rearrange("(kc p) n -> p kc n", p=128)
        for k in range(kc):
            o = dst3[:, k, :]
            i = s3[:, k, :]
            self.P.dma(eng, lambda e, o=o, i=i: e.dma_start(out=o, in_=i), buf, writes=[buf])

    def rstd(self, ss, inv_n, tmp, out, rb, wb):
        self.act(tmp, ss, AF.Ln, list(rb) + [self.b_eps], wb, bias=self.eps_ap, scale=inv_n)
        self.act(out, tmp, AF.Exp, wb, wb, scale=-0.5)


def build_program(dbg=False, stop=None, NT=128, dummy=None, NE=32):
    nc = bass.Bass("TRN2", target_bir_lowering=False)
    k = K(nc, dbg)
    P, A = k.P, k.A

    def din(name, shape, dt=F32):
        return nc.dram_tensor(name, list(shape), dt, kind="ExternalInput").ap()

    x_shift = din("x_shift", [NT * 128, 1024])
    x_own = din("x_own", [16 * 128, 1024])
    p_own = din("p_own", [16 * 128, 256])
    g_mix = din("g_mix", [1, 1024]); g_ffn = din("g_ffn", [1, 1024]); g_ple = din("g_ple", [1, 1024]); g_fin = din("g_fin", [1, 1024])
    g_ml = din("g_ml", [1, 512])
    w_kv = [din("w_kv%d" % h, [1024, 512]) for h in range(2)]
    w_q = [din("w_q%d" % h, [1024, 256]) for h in range(2)]
    w_ms = din("w_ms", [1024, 1032])
    w_mo = din("w_mo", [1024, 1024])
    w_g = din("w_g", [1024, 2048])
    b_gate = din("b_gate", [1, 2048])
    conv_wb = din("conv_wb", [128, 40])
    ifb = din("ifb", [1, 8])
    w_ua = din("w_ua", [512, 1024]); w_um = din("w_um", [512, 1024]); w_out = din("w_out", [1024, 1024])
    w_r = din("w_r", [1024, 36]); b_r = din("b_r", [1, 36])
    c_f32 = din("c_f32", [128, 3 * 128])
    c_bf = din("c_bf", [128, 4 * 128], BF16)
    c_e = din("c_e", [64, 64 * 128], BF16)
    c_core = din("c_core", [128, 130])
    c_gmask = din("c_gmask", [1, 16 * 64])
    out_own = nc.dram_tensor("out_own", [16 * 128, 1024], F32, kind="ExternalOutput").ap()
    okind = "ExternalOutput" if dbg else "Internal"
    s_xnT = nc.dram_tensor("s_xnT", [(NT + 16) * 128, 1024], BF16).ap()
    s_ya = nc.dram_tensor("s_ya", [16 * 128, 512], BF16, kind=okind).ap()
    s_ym = nc.dram_tensor("s_ym", [16 * 128, 512], BF16, kind=okind).ap()
    d_xnT = [Buf("dxnT%d" % i) for i in range(NT + 16)]
    d_ya = [Buf("dya%d" % i) for i in range(16)]
    d_ym = [Buf("dym%d" % i) for i in range(16)]
    d_out = Buf("dout")

    cf = A.alloc(384); b_cf = Buf("cf")
    cb = A.alloc(512, BF16); b_cb = Buf("cb")
    cc = A.alloc(130); b_cc = Buf("cc")
    epsb = A.alloc(1); b_eps = Buf("eps")
    k.ld(cf, c_f32, b_cf); k.ld(cb, c_bf, b_cb); k.ld(cc, c_core, b_cc)
    k.memset(epsb, EPS, [b_eps])
    k.eps_ap = epsb[:, 0:1]; k.b_eps = b_eps
    NUT, NONES, ID32 = cf[:, 0:128], cf[:, 128:256], cf[:, 256:384]
    IDB, UTB, CM = cb[:, 0:128], cb[:, 128:256], [cb[:, 256:384], cb[:, 384:512]]
    VALID, PAR, NPAR = cc[:, 0:128], cc[:, 128:129], cc[:, 129:130]
    junk = A.alloc(1024, BF16); b_junk = Buf("junk")
    st4 = [A.alloc(4) for _ in range(2)]; b_st4 = [Buf("st%d" % i) for i in range(2)]
    base_mark = A.mark()

    def bc_load(dst, src_row, buf):
        k.ld(dst, src_row.partition_broadcast(128), buf)

    gb = A.alloc(1024); b_gb = Buf("gb")
    bc_load(gb, g_mix, b_gb)
    xt = [A.alloc(1024) for _ in range(2)]; b_xt = [Buf("xt%d" % i) for i in range(2)]
    xn = [A.alloc(1024, BF16) for _ in range(2)]; b_xn = [Buf("xn%d" % i) for i in range(2)]
    xT_s = [A.alloc(1024, BF16) for _ in range(2)]; b_xTs = [Buf("xTs%d" % i) for i in range(2)]

    def norm_tile(src_ap, sl, gbt, b_gbt, dst_bf, b_dst):
        s = st4[sl]; bs = b_st4[sl]
        k.act(junk, src_ap[0], AF.Square, [src_ap[1]], [b_junk, bs], accum_out=s[:, 0:1])
        k.rstd(s[:, 0:1], 1.0 / 1024, s[:, 1:2], s[:, 2:3], [bs], [bs])
        k.stt(dst_bf, src_ap[0], s[:, 2:3], gbt, ALU.mult, ALU.mult, [src_ap[1], bs, b_gbt], [b_dst])

    def transpose_to(dst_bf3, b_dst, src_bf, b_src, nchunk, grp="misc", banks=(5, 6, 7), eng="act"):
        pt, bp = k.ps(grp, banks)
        ptb = pt[:, :].bitcast(BF16)
        for c in range(nchunk):
            k.tr(ptb[:, c * 128:(c + 1) * 128], src_bf[:, c * 128:(c + 1) * 128], IDB, [b_src, b_cb], [bp], inc=(c == nchunk - 1))
        k.cp(eng, dst_bf3, ptb[:, 0:nchunk * 128], [bp], [b_dst])

    for T in range(NT + 16):
        sl = T % 2
        src = x_shift[T * 128:(T + 1) * 128, :] if T < NT else x_own[(T - NT) * 128:(T - NT + 1) * 128, :]
        k.ld(xt[sl], src, b_xt[sl])
        norm_tile((xt[sl], b_xt[sl]), sl, gb, b_gb, xn[sl], b_xn[sl])
        transpose_to(xT_s[sl], b_xTs[sl], xn[sl], b_xn[sl], 8)
        k.st(s_xnT[T * 128:(T + 1) * 128, :], xT_s[sl], b_xTs[sl], d_xnT[T], eng="act")
    P.barrier()
    A.release(base_mark)

    wms = A.alloc(8 * 1032, BF16); b_wms = Buf("wms"); wms3 = v3(wms, 8)
    wmo = A.alloc(8 * 1024, BF16); b_wmo = Buf("wmo"); wmo3 = v3(wmo, 8)
    k.ldw(wms3, w_ms, 8, b_wms); k.ldw(wmo3, w_mo, 8, b_wmo)
    cwb = A.alloc(40); b_cwb = Buf("cwb"); k.ld(cwb, conv_wb, b_cwb)
    ifbb = A.alloc(8); b_ifbb = Buf("ifbb"); bc_load(ifbb, ifb, b_ifbb)
    gml = A.alloc(512); b_gml = Buf("gml"); bc_load(gml, g_ml, b_gml)
    xT = [A.alloc(1024, BF16) for _ in range(2)]; b_xT = [Buf("xT%d" % i) for i in range(2)]
    C32 = A.alloc(4 * 129); b_C32 = [Buf("C32_%d" % h) for h in range(4)]; C32v = v3(C32, 4)
    Cb = A.alloc(4 * 130, BF16); b_Cb = [Buf("Cb_%d" % h) for h in range(4)]; Cbv = v3(Cb, 4)
    k.memset(C32, 0.0, b_C32); k.memset(Cb, 0.0, b_Cb)
    kpre = [A.alloc(132) for _ in range(4)]; b_kpre = [Buf("kpre%d" % h) for h in range(4)]
    qpre = [A.alloc(132) for _ in range(4)]; b_qpre = [Buf("qpre%d" % h) for h in range(4)]
    for h in range(4):
        k.memset(kpre[h], 0.0, [b_kpre[h]]); k.memset(qpre[h], 0.0, [b_qpre[h]])
    NSL = 4
    def mk(n, cols, dt=F32, pre=""):
        return [A.alloc(cols, dt) for _ in range(n)], [Buf(pre + str(i)) for i in range(n)]
    lif, b_lif = mk(2, 8, pre="lif"); gt, b_gt = mk(2, 24, pre="gt")
    ck, b_ck = mk(NSL, 128, pre="ck"); ex, b_ex = mk(NSL, 128, pre="ex")
    kTb, b_kTb = mk(NSL, 128, BF16, "kTb"); qTb, b_qTb = mk(NSL, 128, BF16, "qTb")
    ktok, b_ktok = mk(NSL, 128, BF16, "ktok"); vp, b_vp = mk(NSL, 130, BF16, "vp")
    tmpc, b_tmpc = mk(NSL, 129, pre="tmpc"); Sm, b_Sm = mk(NSL, 128, BF16, "Sm")
    hc, b_hc = mk(NSL, 128, pre="hc"); sm8, b_sm8 = mk(NSL, 8, pre="sm8")
    sig2, b_sig2 = mk(2, 512, pre="sig")
    ympos = [A.alloc(512, BF16) for _ in range(2)]; b_ympos = [Buf("ympos%d" % i) for i in range(2)]
    ymd = A.alloc(512); b_ymd = Buf("ymd")
    ymo = [A.alloc(512, BF16) for _ in range(2)]; b_ymo = [Buf("ymo%d" % i) for i in range(2)]

    def conv_silu(pre, b_pre, ps_src, b_ps, hq, out_bf, b_out, scale, sl):
        k.cp("act", pre[:, 3:131], ps_src, [b_ps], [b_pre])
        c = ck[sl]; bcx = b_ck[sl]; e_ = ex[sl]; be = b_ex[sl]
        w0 = cwb[:, hq * 4:hq * 4 + 1]
        k.ts(c, pre[:, 0:128], w0, cwb[:, 32 + hq:33 + hq], ALU.mult, ALU.add, [b_pre, b_cwb], [bcx])
        for tap in range(1, 4):
            k.stt(c, pre[:, tap:tap + 128], cwb[:, hq * 4 + tap:hq * 4 + tap + 1], c, ALU.mult, ALU.add, [b_pre, b_cwb, bcx], [bcx])
        k.cp("pool", pre[:, 0:3], pre[:, 128:131], [b_pre], [b_pre])
        k.act(e_, c, AF.Exp, [bcx], [be], scale=-1.0)
        k.ts(e_, e_, 1.0, None, ALU.add, None, [be], [be])
        k.recip(e_, e_, [be], [be])
        k.stt(out_bf, c, scale, e_, ALU.mult, ALU.mult, [bcx, be], [b_out])

    def pm_prologue(T):
        sl = T % 2
        x3 = v3(xT[sl], 8); bx = b_xT[sl]
        if T + 1 < NT:
            k.ld(xT[(T + 1) % 2], s_xnT[(T + 1) * 128:(T + 2) * 128, :], b_xT[(T + 1) % 2], reads=[d_xnT[T + 1]])
        pos = T % 8
        outp = pos >= 6
        pv, bpv = k.ps("pmv", (0, 1))
        for kc in range(8):
            k.mm(pv[:, 0:512], x3[:, kc, :], wms3[:, kc, 512:1024], kc == 0, kc == 7, [bx, b_wms], [bpv], inc=(kc == 7))
        pif, bpif = k.ps("pmisc", (5, 6, 7))
        for kc in range(8):
            k.mm(pif[:, 0:8], x3[:, kc, :], wms3[:, kc, 1024:1032], kc == 0, kc == 7, [bx, b_wms], [bpif], inc=(kc == 7))
        L = lif[sl]; bL = b_lif[sl]; G = gt[sl]; bG = b_gt[sl]
        k.tt(L, pif[:, 0:8], ifbb, ALU.add, [bpif, b_ifbb], [bL])
        k.act(G[:, 0:4], L[:, 4:8], AF.Exp, [bL], [bG], scale=-1.0)
        k.act(G[:, 0:4], G[:, 0:4], AF.Ln, [bG], [bG], bias=1.0)
        pb, bpb = k.ps("pmisc", (5, 6, 7))
        k.mm(pb[:, 0:4], NUT, G[:, 0:4], True, True, [b_cf, bG], [bpb], inc=False)
        k.mm(pb[:, 4:8], NONES, G[:, 0:4], True, True, [b_cf, bG], [bpb])
        k.act(G[:, 8:16], pb[:, 0:8], AF.Exp, [bpb], [bG])
        k.tt(G[:, 16:20], L[:, 0:4], pb[:, 0:4], ALU.subtract, [bL, bpb], [bG])
        k.act(G[:, 4:8], G[:, 16:20], AF.Exp, [bG], [bG])
        k.ts(G[:, 4:8], G[:, 4:8], VALID[:, T:T + 1], None, ALU.mult, None, [bG, b_cc], [bG])
        sig = b_sig = None
        if outp:
            sig = sig2[pos - 6]; b_sig = b_sig2[pos - 6]
            po, bpo = k.ps("pmk", (2, 3, 4))
            for kc in range(8):
                k.mm(po[:, 0:512], x3[:, kc, :], wmo3[:, kc, 512:1024], kc == 0, kc == 7, [bx, b_wmo], [bpo], inc=(kc == 7))
            k.act(sig, po[:, 0:512], AF.Exp, [bpo], [b_sig], scale=-1.0)
            k.ts(sig, sig, 1.0, None, ALU.add, None, [b_sig], [b_sig])
            k.recip(sig, sig, [b_sig], [b_sig])
        return (T, x3, bx, pos, outp, pv, bpv, G, bG, sig, b_sig)

    def pm_stage_a(ctx, h, hs):
        T, x3, bx, pos, outp, pv, bpv, G, bG, sig, b_sig = ctx
        pk, bpk = k.ps("pmk", (2, 3, 4))
        for kc in range(8):
            k.mm(pk[:, 0:128], wms3[:, kc, 128 * h:128 * h + 128], x3[:, kc, :], kc == 0, kc == 7, [bx, b_wms], [bpk], inc=(kc == 7))
        conv_silu(kpre[h], b_kpre[h], pk[:, 0:128], bpk, 4 + h, kTb[hs], b_kTb[hs], 128 ** -0.5, hs)
        if pos >= 5:
            pq, bpq = k.ps("pmk", (2, 3, 4))
            for kc in range(8):
                k.mm(pq[:, 0:128], wmo3[:, kc, 128 * h:128 * h + 128], x3[:, kc, :], kc == 0, kc == 7, [bx, b_wmo], [bpq], inc=(kc == 7))
            if outp:
                conv_silu(qpre[h], b_qpre[h], pq[:, 0:128], bpq, h, qTb[hs], b_qTb[hs], 1.0, hs)
            else:
                k.cp("act", qpre[h][:, 3:131], pq[:, 0:128], [bpq], [b_qpre[h]])
                k.cp("pool", qpre[h][:, 0:3], qpre[h][:, 128:131], [b_qpre[h]], [b_qpre[h]])
        V = vp[hs]; bV = b_vp[hs]
        k.ts(V[:, 0:128], pv[:, 128 * h:128 * h + 128], G[:, 4 + h:5 + h], None, ALU.mult, None, [bpv, bG], [bV])
        k.cp("pool", V[:, 128:129], G[:, 4 + h:5 + h], [bG], [bV])

    def pm_stage_b(ctx, h, hs):
        T, x3, bx, pos, outp, pv, bpv, G, bG, sig, b_sig = ctx
        V = vp[hs]; bV = b_vp[hs]
        ptk, bptk = k.ps("pmisc", (5, 6, 7))
        ptkb = ptk[:, :].bitcast(BF16)
        k.tr(ptkb[:, 0:128], kTb[hs], IDB, [b_kTb[hs], b_cb], [bptk])
        k.cp("act", ktok[hs], ptkb[:, 0:128], [bptk], [b_ktok[hs]])
        if outp:
            pss, bpss = k.ps("pmk", (2, 3, 4))
            k.mm(pss[:, 0:128], kTb[hs], qTb[hs], True, True, [b_kTb[hs], b_qTb[hs]], [bpss])
            k.tt(Sm[hs], pss[:, 0:128], UTB, ALU.mult, [bpss, b_cb], [b_Sm[hs]])
            pn, bpn = k.ps("pmk", (2, 3, 4))
            k.mm(pn[:, 0:129], Sm[hs], V[:, 0:129], True, False, [b_Sm[hs], bV], [bpn], inc=False)
            k.mm(pn[:, 0:129], qTb[hs], Cbv[:, h, 0:129], False, True, [b_qTb[hs], b_Cb[h]], [bpn])
            s8 = sm8[hs]; bs8 = b_sm8[hs]
            et = G[:, 8 + h:9 + h]
            k.tt(s8[:, 0:1], pn[:, 128:129], et, ALU.mult, [bpn, bG], [bs8])
            k.stt(s8[:, 1:2], s8[:, 0:1], -1.0, s8[:, 0:1], ALU.mult, ALU.max, [bs8], [bs8])
            k.ts(s8[:, 1:2], s8[:, 1:2], 1.0, None, ALU.max, None, [bs8], [bs8])
            k.recip(s8[:, 2:3], s8[:, 1:2], [bs8], [bs8])
            k.tt(s8[:, 3:4], s8[:, 2:3], et, ALU.mult, [bs8, bG], [bs8])
            H = hc[hs]; bH = b_hc[hs]
            k.stt(H, pn[:, 0:128], s8[:, 3:4], sig[:, 128 * h:128 * h + 128], ALU.mult, ALU.mult, [bpn, bs8, b_sig], [bH])
            k.act(ex[hs], H, AF.Square, [bH], [b_ex[hs], bs8], accum_out=s8[:, 4:5])
            k.rstd(s8[:, 4:5], 1.0 / 128, s8[:, 5:6], s8[:, 6:7], [bs8], [bs8])
            k.stt(ympos[pos - 6][:, 128 * h:128 * h + 128], H, s8[:, 6:7], gml[:, 128 * h:128 * h + 128], ALU.mult, ALU.mult,
                  [bH, bs8, b_gml], [b_ympos[pos - 6]])
        pc, bpc = k.ps("pmk", (2, 3, 4))
        k.mm(pc[:, 0:129], ktok[hs], V[:, 0:129], True, True, [b_ktok[hs], bV], [bpc])
        tc_, btc = tmpc[hs], b_tmpc[hs]
        k.tt(tc_, pc[:, 0:129], C32v[:, h, :], ALU.add, [bpc, b_C32[h]], [btc])
        k.ts(C32v[:, h, :], tc_, G[:, 12 + h:13 + h], None, ALU.mult, None, [btc, bG], [b_C32[h]])
        k.ts(Cbv[:, h, 0:129], tc_, G[:, 12 + h:13 + h], None, ALU.mult, None, [btc, bG], [b_Cb[h]])
        if h == 3 and pos == 7:
            j = T // 8
            k.tt(ymd, ympos[1], ympos[0], ALU.subtract, b_ympos, [b_ymd])
            yo = ymo[j % 2]; byo = b_ymo[j % 2]
            k.stt(yo, ymd, PAR, ympos[0], ALU.mult, ALU.add, [b_ymd, b_cc, b_ympos[0]], [byo])
            k.st(s_ym[j * 128:(j + 1) * 128, :], yo, byo, d_ym[j])

    k.ld(xT[0], s_xnT[0:128, :], b_xT[0], reads=[d_xnT[0]])
    items = [(T, h) for T in range(NT) for h in range(4)]
    ctxs = {}

    def pm_emit_a(i):
        T, h = items[i]
        if h == 0:
            ctxs[T] = pm_prologue(T)
            ctxs.pop(T - 2, None)
        pm_stage_a(ctxs[T], h, i % NSL)

    pm_emit_a(0)
    for i in range(len(items)):
        if i + 1 < len(items):
            pm_emit_a(i + 1)
        T, h = items[i]
        pm_stage_b(ctxs[T], h, i % NSL)
    P.barrier()
    A.release(base_mark)
    if stop == "PM":
        P.emit(); print("instructions:", P.n_inst, {e: P.cnt[e] for e in ENGS}); return nc

    KT = A.alloc(2 * 16384, BF16); KT3 = v3(KT, 2)
    b_KT = [Buf("KT%d" % t) for t in range(NT)]
    V1 = A.alloc(NT * 260, BF16); V14 = V1.rearrange("p (t h d) -> p t h d", t=NT, h=4)
    b_V1 = [Buf("V1_%d" % t) for t in range(NT)]
    Eb = A.alloc(64 * 128, BF16); b_Eb = Buf("Eb"); Eb3 = v3(Eb, 64)
    k.ld(Eb[0:64, :], c_e, b_Eb)
    gmask = A.alloc(1024); b_gmask = Buf("gmask"); bc_load(gmask, c_gmask, b_gmask); gmask3 = v3(gmask, 16)
    wkv = A.alloc(8 * 512, BF16); b_wkv = Buf("wkv"); wkv3 = v3(wkv, 8)
    wq = A.alloc(8 * 256, BF16); b_wq = Buf("wq"); wq3 = v3(wq, 8)
    xT = [A.alloc(1024, BF16) for _ in range(2)]; b_xT = [Buf("axT%d" % i) for i in range(2)]
    xTo = A.alloc(1024, BF16); b_xTo = Buf("xTo")
    ksum = A.alloc(2 * NT); b_ksum = Buf("ksum"); ksum3 = v3(ksum, 2)
    kmT = A.alloc(2 * 64); b_kmT = Buf("kmT"); kmT3 = v3(kmT, 2)
    QTs = A.alloc(512, BF16); b_QTs = Buf("QTs"); QTs4 = QTs.rearrange("p (a s q) -> p a s q", a=2, s=2)
    QT32 = A.alloc(512); b_QT32 = Buf("QT32"); QT324 = QT32.rearrange("p (a s q) -> p a s q", a=2, s=2)
    k.memset(QTs, 0.0, [b_QTs]); k.memset(QT32, 0.0, [b_QT32])
    gm_, b_gm = mk(4, 64, pre="gm"); t8, b_t8 = mk(4, 16, pre="t8")
    b01, b_b01 = mk(4, 64, BF16, "b01"); BT, b_BT = mk(4, 128, BF16, "BT")
    PT, b_PT = mk(3, 512, BF16, "PT")
    yat = [A.alloc(512, BF16) for _ in range(2)]; b_yat = [Buf("yat%d" % i) for i in range(2)]
    r1, b_r1 = mk(2, 2, pre="r1")
    hcount = [0]; gcount = [0]

    for hp in range(2):
        if hp == 1:
            P.barrier()
        k.ldw(wkv3, w_kv[hp], 8, b_wkv); k.ldw(wq3, w_q[hp], 8, b_wq)
        k.memset(kmT, 0.0, [b_kmT]); k.memset(ksum, 0.0, [b_ksum])
        if hp == 0:
            for t0 in range(0, NT, 16):
                k.memset(V1[:, t0 * 260:(t0 + 16) * 260], 1.0, b_V1[t0:t0 + 16])
        k.ld(xT[0], s_xnT[0:128, :], b_xT[0], reads=[d_xnT[0]])
        for T in range(NT):
            sl = T % 2
            x3 = v3(xT[sl], 8); bx = b_xT[sl]
            if T + 1 < NT:
                k.ld(xT[(T + 1) % 2], s_xnT[(T + 1) * 128:(T + 2) * 128, :], b_xT[(T + 1) % 2], reads=[d_xnT[T + 1]])
            for p in range(2):
                pk, bpk = k.ps("paproj", (5, 6, 7))
                for kc in range(8):
                    k.mm(pk[:, 0:128], wkv3[:, kc, 128 * p:128 * p + 128], x3[:, kc, :], kc == 0, kc == 7, [bx, b_wkv], [bpk], inc=(kc == 7))
                k.act(KT3[:, p, T * 128:(T + 1) * 128], pk[:, 0:128], AF.Identity, [bpk], [b_KT[T], b_ksum], accum_out=ksum3[:, p, T:T + 1])
            pv, bpv = k.ps("paproj", (5, 6, 7))
            for kc in range(8):
                k.mm(pv[:, 0:256], x3[:, kc, :], wkv3[:, kc, 256:512], kc == 0, kc == 7, [bx, b_wkv], [bpv], inc=(kc == 7))
            k.cp("dve", V14[:, T, :, 0:64], pv[:, 0:256].rearrange("p (h d) -> p h d", h=4), [bpv], [b_V1[T]])
            if T % 2 == 1:
                n = T // 2
                k.tt(kmT3[:, :, n:n + 1], ksum3[:, :, T - 1:T], ksum3[:, :, T:T + 1], ALU.add, [b_ksum], [b_kmT])
            if T % 8 != 7:
                continue
            j = T // 8
            k.ld(xTo, s_xnT[(NT + j) * 128:(NT + j + 1) * 128, :], b_xTo, reads=[d_xnT[NT + j]])
            xo3 = v3(xTo, 8)
            for p in range(2):
                pq, bpq = k.ps("paproj", (5, 6, 7))
                for kc in range(8):
                    k.mm(pq[:, 0:128], wq3[:, kc, 128 * p:128 * p + 128], xo3[:, kc, :], kc == 0, kc == 7, [b_xTo, b_wq], [bpq], inc=(kc == 7))
                for s_ in range(2):
                    k.ts(QTs4[64 * s_:64 * s_ + 64, p, s_, :], pq[64 * s_:64 * s_ + 64, 0:128], 0.125, None, ALU.mult, None, [bpq], [b_QTs])
                    k.cp("dve", QT324[64 * s_:64 * s_ + 64, p, s_, :], pq[64 * s_:64 * s_ + 64, 0:128], [bpq], [b_QT32])
            ya_t = yat[j % 2]; b_ya_t = b_yat[j % 2]
            nkt = 8 * j + 8
            def prologue_a(hl):
                p, s = hl // 2, hl % 2
                pg, bpg = k.ps("paproj", (5, 6, 7))
                k.mm(pg[:, 0:64], QT324[:, p, s, :], kmT3[:, p, :], True, True, [b_QT32, b_kmT], [bpg])
                g_ = gm_[hl]; bg_ = b_gm[hl]; t_ = t8[hl]; bt_ = b_t8[hl]
                k.tt(g_, pg[:, 0:64], gmask3[:, j, :], ALU.add, [bpg, b_gmask], [bg_])
                P.op("dve", lambda e, o=t_[:, 0:8], i=g_: e.max(out=o, in_=i), [bg_], [bt_])
                k.ts(t_[:, 8:9], t_[:, 2:3], -1e29, None, ALU.max, None, [bt_], [bt_])
                k.ts(b01[hl], g_, t_[:, 8:9], 1.0, ALU.is_ge, ALU.subtract, [bg_, bt_], [b_b01[hl]])

            def prologue_b(hl):
                pbt, bpbt = k.ps("paproj", (5, 6, 7))
                pbtb = pbt[:, :].bitcast(BF16)
                k.tr(pbtb[0:64, 0:128], b01[hl], IDB, [b_b01[hl], b_cb], [bpbt])
                k.cp("dve", BT[hl][0:64, :], pbtb[0:64, 0:128], [bpbt], [b_BT[hl]])

            def qk_group(hl, g0):
                p, s = hl // 2, hl % 2
                pS, bpS = k.ps("pas", (2, 3, 4))
                for i in range(4):
                    kt = g0 + i
                    col = pS[:, i * 128:(i + 1) * 128]
                    k.mm(col, KT3[:, p, kt * 128:(kt + 1) * 128], QTs4[:, p, s, :], True, False, [b_KT[kt], b_QTs], [bpS], inc=False)
                    if kt < nkt - 2:
                        k.mm(col, Eb3[0:64, kt // 2, :], BT[hl][0:64, :], False, True, [b_Eb, b_BT[hl]], [bpS], inc=(i == 3))
                    else:
                        k.mm(col, IDB, CM[kt - (nkt - 2)], False, True, [b_cb], [bpS], inc=(i == 3))
                return pS, bpS

            def pv_group(hl, g0, pS, bpS, po, bpo):
                pt_ = PT[gcount[0] % 3]; bpt_ = b_PT[gcount[0] % 3]; gcount[0] += 1
                k.act(pt_, pS[:, 0:512], AF.Exp, [bpS], [bpt_])
                for i in range(4):
                    kt = g0 + i
                    k.mm(po[:, 0:65], pt_[:, i * 128:(i + 1) * 128], V14[:, kt, hl, :], kt == 0, kt == nkt - 1, [bpt_, b_V1[kt]], [bpo],
                         inc=(i == 3))

            groups = list(range(0, nkt, 4))
            prologue_a(0); prologue_b(0)
            for hl in range(4):
                po, bpo = k.ps("pao", (0, 1))
                cur = qk_group(hl, groups[0])
                for gi, g0 in enumerate(groups):
                    nxt = qk_group(hl, groups[gi + 1]) if gi + 1 < len(groups) else None
                    if hl < 3 and gi == 0:
                        prologue_a(hl + 1)
                    if hl < 3 and gi == min(1, len(groups) - 1):
                        prologue_b(hl + 1)
                    pv_group(hl, g0, cur[0], cur[1], po, bpo)
                    cur = nxt
                r_ = r1[hl % 2]; br_ = b_r1[hl % 2]
                k.recip(r_[:, 0:1], po[:, 64:65], [bpo], [br_])
                k.ts(ya_t[:, 64 * hl:64 * hl + 64], po[:, 0:64], r_[:, 0:1], None, ALU.mult, None, [bpo, br_], [b_ya_t])
            k.st(s_ya[j * 128:(j + 1) * 128, 256 * hp:256 * hp + 256], ya_t[:, 0:256], b_ya_t, d_ya[j])
    P.barrier()
    A.release(base_mark)
    if stop == "PA":
        P.emit(); print("instructions:", P.n_inst, {e: P.cnt[e] for e in ENGS}); return nc

    e_wg = din("e_wg", [NE * 1024, 512]); e_wu = din("e_wu", [NE * 1024, 512]); e_wd = din("e_wd", [NE * 512, 1024])
    w_pg = din("w_pg", [1024, 1024]); w_pp = din("w_pp", [256, 1024])
    h1 = A.alloc(16 * 1024); h13 = v3(h1, 16); b_h1 = [Buf("h1_%d" % j) for j in range(16)]
    hnT = A.alloc(8 * 2048, BF16); hnT3 = v3(hnT, 8); b_hnT = [Buf("hnT%d" % j) for j in range(16)]
    cw = A.alloc(16 * 32); cw3 = v3(cw, 16); b_cw = [Buf("cw%d" % j) for j in range(16)]
    NJ = NT // 8
    if NJ < 16:
        k.memset(hnT, 0.0, b_hnT); k.memset(cw, 0.0, b_cw); k.memset(h1, 0.0, b_h1)
    pb_mark = A.mark()
    wg_ = A.alloc(8 * 2048, BF16); b_wg = Buf("wg"); wg3 = v3(wg_, 8)
    wua = A.alloc(4 * 1024, BF16); b_wua = Buf("wua"); wua3 = v3(wua, 4)
    wum = A.alloc(4 * 1024, BF16); b_wum = Buf("wum"); wum3 = v3(wum, 4)
    wo = A.alloc(8 * 1024, BF16); b_wo = Buf("wo"); wo3 = v3(wo, 8)
    wr = A.alloc(8 * 36); b_wr = Buf("wr"); wr3 = v3(wr, 8)
    k.ldw(wg3, w_g, 8, b_wg); k.ldw(wua3, w_ua, 4, b_wua); k.ldw(wum3, w_um, 4, b_wum); k.ldw(wo3, w_out, 8, b_wo)
    for kc in range(8):
        k.ld(wr3[:, kc, :], w_r[kc * 128:(kc + 1) * 128, :], b_wr)
    bgb = A.alloc(2048, BF16); b_bgb = Buf("bgb")
    br32 = A.alloc(36); b_br32 = Buf("br32"); bc_load(br32, b_r, b_br32)
    ones = A.alloc(128); b_ones = Buf("ones"); k.memset(ones, 1.0, [b_ones])
    onesb = A.alloc(128, BF16); b_onesb = Buf("onesb"); k.memset(onesb, 1.0, [b_onesb])
    gfb = A.alloc(1024); b_gfb = Buf("gfb"); bc_load(gfb, g_ffn, b_gfb)
    xTo = A.alloc(1024, BF16); b_xTo = Buf("bxTo")
    yab = A.alloc(512, BF16); b_yab = Buf("yab"); ymb = A.alloc(512, BF16); b_ymb = Buf("ymb")
    yaT = A.alloc(512, BF16); b_yaT = Buf("yaT"); ymT = A.alloc(512, BF16); b_ymT = Buf("ymT")
    yaT3 = v3(yaT, 4); ymT3 = v3(ymT, 4)
    sga = A.alloc(512); b_sga = Buf("sga"); sgm = A.alloc(512); b_sgm = Buf("sgm")
    mrg = A.alloc(1024, BF16); b_mrg = Buf("mrg"); mrgT = A.alloc(1024, BF16); b_mrgT = Buf("mrgT"); mrgT3 = v3(mrgT, 8)
    hn32 = A.alloc(1024); b_hn32 = Buf("hn32")
    hnhi = A.alloc(1024, BF16); b_hnhi = Buf("hnhi"); hnlo = A.alloc(1024, BF16); b_hnlo = Buf("hnlo")
    hiT = A.alloc(1024, BF16); b_hiT = Buf("hiT"); hiT3 = v3(hiT, 8)
    loT = A.alloc(1024, BF16); b_loT = Buf("loT"); loT3 = v3(loT, 8)
    wrh = A.alloc(8 * 36, BF16); b_wrh = Buf("wrh"); wrh3 = v3(wrh, 8)
    wrl = A.alloc(8 * 36, BF16); b_wrl = Buf("wrl"); wrl3 = v3(wrl, 8)
    k.cp("dve", wrh, wr, [b_wr], [b_wrh])
    k.tt(wrl, wr, wrh, ALU.subtract, [b_wr, b_wrh], [b_wrl])
    Lr = A.alloc(40); b_Lr = Buf("Lr"); rs = A.alloc(16); b_rs = Buf("rs"); elm = A.alloc(32); b_elm = Buf("elm")
    c1 = A.alloc(32); b_c1 = Buf("c1")
    for q4 in range(4):
        k.ld(sga, b_gate[:, 512 * q4:512 * q4 + 512].partition_broadcast(128), b_sga)
        k.cp("dve", bgb[:, 512 * q4:512 * q4 + 512], sga, [b_sga], [b_bgb])

    def sigmoid_from(dst, b_dst, ps_ap, b_ps):
        k.act(dst, ps_ap, AF.Exp, [b_ps], [b_dst], scale=-1.0)
        k.ts(dst, dst, 1.0, None, ALU.add, None, [b_dst], [b_dst])
        k.recip(dst, dst, [b_dst], [b_dst])

    for j in range(NJ):
        k.ld(h13[:, j, :], x_own[j * 128:(j + 1) * 128, :], b_h1[j])
        k.ld(xTo, s_xnT[(NT + j) * 128:(NT + j + 1) * 128, :], b_xTo, reads=[d_xnT[NT + j]])
        k.ld(yab, s_ya[j * 128:(j + 1) * 128, :], b_yab, reads=[d_ya[j]])
        k.ld(ymb, s_ym[j * 128:(j + 1) * 128, :], b_ymb, reads=[d_ym[j]])
        xo3 = v3(xTo, 8)
        transpose_to(yaT, b_yaT, yab, b_yab, 4, grp="pbm", banks=(6, 7), eng="dve")
        transpose_to(ymT, b_ymT, ymb, b_ymb, 4, grp="pbm", banks=(6, 7), eng="dve")
        for b in range(2):
            cs = slice(512 * b, 512 * b + 512)
            pga, bpga = k.ps("pbg", (0, 1, 2))
            for kc in range(8):
                k.mm(pga[:, 0:512], xo3[:, kc, :], wg3[:, kc, 512 * b:512 * b + 512], kc == 0, kc == 7, [b_xTo, b_wg], [bpga], inc=(kc == 7))
            k.tt(sga, pga[:, 0:512], bgb[:, 512 * b:512 * b + 512], ALU.add, [bpga, b_bgb], [b_sga])
            sigmoid_from(sga, b_sga, sga, b_sga)
            pua, bpua = k.ps("pbu", (3, 4, 5))
            for fc in range(4):
                k.mm(pua[:, 0:512], yaT3[:, fc, :], wua3[:, fc, cs], fc == 0, fc == 3, [b_yaT, b_wua], [bpua], inc=(fc == 3))
            k.tt(sga, sga, pua[:, 0:512], ALU.mult, [b_sga, bpua], [b_sga])
            pgm, bpgm = k.ps("pbg", (0, 1, 2))
            for kc in range(8):
                k.mm(pgm[:, 0:512], xo3[:, kc, :], wg3[:, kc, 1024 + 512 * b:1024 + 512 * b + 512], kc == 0, kc == 7, [b_xTo, b_wg], [bpgm], inc=(kc == 7))
            k.tt(sgm, pgm[:, 0:512], bgb[:, 1024 + 512 * b:1024 + 512 * b + 512], ALU.add, [bpgm, b_bgb], [b_sgm])
            sigmoid_from(sgm, b_sgm, sgm, b_sgm)
            pum, bpum = k.ps("pbu", (3, 4, 5))
            for fc in range(4):
                k.mm(pum[:, 0:512], ymT3[:, fc, :], wum3[:, fc, cs], fc == 0, fc == 3, [b_ymT, b_wum], [bpum], inc=(fc == 3))
            k.tt(sgm, sgm, pum[:, 0:512], ALU.mult, [b_sgm, bpum], [b_sgm])
            k.tt(mrg[:, cs], sga, sgm, ALU.add, [b_sga, b_sgm], [b_mrg])
        transpose_to(mrgT, b_mrgT, mrg, b_mrg, 8, grp="pbm", banks=(6, 7), eng="act")
        for b in range(2):
            ph, bph = k.ps("pbg", (0, 1, 2))
            for kc in range(8):
                k.mm(ph[:, 0:512], mrgT3[:, kc, :], wo3[:, kc, 512 * b:512 * b + 512], kc == 0, kc == 7, [b_mrgT, b_wo], [bph], inc=(kc == 7))
            k.tt(h13[:, j, 512 * b:512 * b + 512], h13[:, j, 512 * b:512 * b + 512], ph[:, 0:512], ALU.add, [b_h1[j], bph], [b_h1[j]])
        if stop == "PB1":
            continue
        s = st4[0]; bs = b_st4[0]
        k.act(junk, h13[:, j, :], AF.Square, [b_h1[j]], [b_junk, bs], accum_out=s[:, 0:1])
        k.rstd(s[:, 0:1], 1.0 / 1024, s[:, 1:2], s[:, 2:3], [bs], [bs])
        k.stt(hn32, h13[:, j, :], s[:, 2:3], gfb, ALU.mult, ALU.mult, [b_h1[j], bs, b_gfb], [b_hn32])
        k.cp("dve", hnhi, hn32, [b_hn32], [b_hnhi])
        k.tt(hnlo, hn32, hnhi, ALU.subtract, [b_hn32, b_hnhi], [b_hnlo])
        transpose_to(hiT, b_hiT, hnhi, b_hnhi, 8, grp="pbm", banks=(6, 7), eng="act")
        transpose_to(loT, b_loT, hnlo, b_hnlo, 8, grp="pbm", banks=(6, 7), eng="act")
        k.cp("dve", hnT3[:, :, j * 128:(j + 1) * 128], hiT3, [b_hiT], [b_hnT[j]])
        pr, bpr = k.ps("pbu", (3, 4, 5))
        n_ = 0
        for (aT, baT, w_, bw_) in ((hiT3, b_hiT, wrh3, b_wrh), (hiT3, b_hiT, wrl3, b_wrl), (loT3, b_loT, wrh3, b_wrh)):
            for kc in range(8):
                k.mm(pr[:, 0:36], aT[:, kc, :], w_[:, kc, :], n_ == 0, n_ == 23, [baT, bw_], [bpr], inc=(n_ == 23))
                n_ += 1
        if stop == "PB2":
            continue
        k.tt(Lr[:, 0:36], pr[:, 0:36], br32, ALU.add, [bpr, b_br32], [b_Lr])
        P.op("dve", lambda e, o=rs[:, 0:1], i=Lr[:, 0:4]: e.tensor_reduce(out=o, in_=i, axis=AX.X, op=ALU.max), [b_Lr], [b_rs])
        k.ts(rs[:, 1:2], rs[:, 0:1], -1.0, None, ALU.mult, None, [b_rs], [b_rs])
        k.act(Lr[:, 36:40], Lr[:, 0:4], AF.Exp, [b_Lr, b_rs], [b_Lr, b_rs], bias=rs[:, 1:2], accum_out=rs[:, 2:3])
        k.recip(rs[:, 3:4], rs[:, 2:3], [b_rs], [b_rs])
        k.ts(Lr[:, 36:40], Lr[:, 0:4], rs[:, 0:1], 1.0, ALU.is_equal, ALU.subtract, [b_Lr, b_rs], [b_Lr])
        k.ts(Lr[:, 36:40], Lr[:, 36:40], 1e30, None, ALU.mult, None, [b_Lr], [b_Lr])
        for g in range(4):
            k.ts(elm[:, 8 * g:8 * g + 8], Lr[:, 4 + 8 * g:12 + 8 * g], Lr[:, 36 + g:37 + g], None, ALU.add, None, [b_Lr], [b_elm])
        P.op("dve", lambda e, o=rs[:, 8:16], i=elm: e.max(out=o, in_=i), [b_elm], [b_rs])
        k.tt(rs[:, 4:5], rs[:, 9:10], rs[:, 8:9], ALU.subtract, [b_rs], [b_rs])
        k.act(rs[:, 4:5], rs[:, 4:5], AF.Exp, [b_rs], [b_rs])
        k.ts(rs[:, 4:5], rs[:, 4:5], 1.0, None, ALU.add, None, [b_rs], [b_rs])
        k.recip(rs[:, 5:6], rs[:, 4:5], [b_rs], [b_rs])
        k.tt(rs[:, 6:7], rs[:, 5:6], rs[:, 3:4], ALU.mult, [b_rs], [b_rs])
        k.tt(rs[:, 7:8], rs[:, 3:4], rs[:, 6:7], ALU.subtract, [b_rs], [b_rs])
        k.ts(c1, elm, rs[:, 8:9], rs[:, 6:7], ALU.is_equal, ALU.mult, [b_elm, b_rs], [b_c1])
        k.ts(cw3[:, j, :], elm, rs[:, 9:10], rs[:, 7:8], ALU.is_equal, ALU.mult, [b_elm, b_rs], [b_cw[j]])
        k.tt(cw3[:, j, :], cw3[:, j, :], c1, ALU.add, [b_cw[j], b_c1], [b_cw[j]])
    P.barrier()
    A.release(pb_mark)
    if stop in ("PB", "PB1", "PB2"):
        P.emit(); return nc

    ewg = [A.alloc(8 * 512, BF16) for _ in range(2)]; ewu = [A.alloc(8 * 512, BF16) for _ in range(2)]
    ewd = [A.alloc(4 * 1024, BF16) for _ in range(2)]
    b_ew = [[Buf("ewg%d" % i), Buf("ewu%d" % i), Buf("ewd%d" % i)] for i in range(2)]
    hidT = A.alloc(4 * 2048, BF16); hidT3 = v3(hidT, 4); b_hid = [Buf("hid%d" % t) for t in range(4)]
    sgt = [A.alloc(512, BF16) for _ in range(2)]; b_sgt = [Buf("sgt%d" % i) for i in range(2)]

    def load_expert(e):
        sl = e % 2
        k.ldw(v3(ewg[sl], 8), e_wg[e * 1024:(e + 1) * 1024, :], 8, b_ew[sl][0])
        k.ldw(v3(ewu[sl], 8), e_wu[e * 1024:(e + 1) * 1024, :], 8, b_ew[sl][1])
        k.ldw(v3(ewd[sl], 4), e_wd[e * 512:(e + 1) * 512, :], 4, b_ew[sl][2])

    load_expert(0)
    scnt = [0]
    for e in range(NE):
        sl = e % 2
        if e + 1 < NE:
            load_expert(e + 1)
        g3, u3, d3 = v3(ewg[sl], 8), v3(ewu[sl], 8), v3(ewd[sl], 4)
        bwg, bwu, bwd = b_ew[sl]
        for tg in range((NJ + 3) // 4):
            rb = [b_hnT[4 * tg + i] for i in range(4)]
            for fc in range(4):
                pG, bpG = k.ps("peg", (0, 1, 2, 3))
                for kc in range(8):
                    k.mm(pG[:, 0:512], g3[:, kc, fc * 128:(fc + 1) * 128], hnT3[:, kc, tg * 512:(tg + 1) * 512], kc == 0, kc == 7, rb + [bwg], [bpG],
                         inc=(kc == 7))
                pU, bpU = k.ps("peg", (0, 1, 2, 3))
                for kc in range(8):
                    k.mm(pU[:, 0:512], u3[:, kc, fc * 128:(fc + 1) * 128], hnT3[:, kc, tg * 512:(tg + 1) * 512], kc == 0, kc == 7, rb + [bwu], [bpU],
                         inc=(kc == 7))
                sg = sgt[scnt[0] % 2]; bsg = b_sgt[scnt[0] % 2]; scnt[0] += 1
                k.act(sg, pG[:, 0:512], AF.Silu, [bpG], [bsg])
                k.tt(hidT3[:, fc, tg * 512:(tg + 1) * 512], sg, pU[:, 0:512], ALU.mult, [bsg, bpU], [b_hid[tg]])
        for jt in range(NJ):
            for b in range(2):
                py, bpy = k.ps("pey", (4, 5, 6, 7))
                for fc in range(4):
                    k.mm(py[:, 0:512], hidT3[:, fc, jt * 128:(jt + 1) * 128], d3[:, fc, 512 * b:512 * b + 512], fc == 0, fc == 3, [b_hid[jt // 4], bwd], [bpy],
                         inc=(fc == 3))
                hsl = h13[:, jt, 512 * b:512 * b + 512]
                k.stt(hsl, py[:, 0:512], cw3[:, jt, e:e + 1], hsl, ALU.mult, ALU.add, [bpy, b_cw[jt], b_h1[jt]], [b_h1[jt]])
    P.barrier()
    A.release(pb_mark)
    if stop == "PE":
        P.emit(); return nc

    wpg = A.alloc(8 * 1024, BF16); b_wpg = Buf("wpg"); wpg3 = v3(wpg, 8)
    wpp = A.alloc(2 * 1024, BF16); b_wpp = Buf("wpp"); wpp3 = v3(wpp, 2)
    k.ldw(wpg3, w_pg, 8, b_wpg); k.ldw(wpp3, w_pp, 2, b_wpp)
    gpb = A.alloc(1024); b_gpb = Buf("gpb"); bc_load(gpb, g_ple, b_gpb)
    gfin = A.alloc(1024); b_gfin = Buf("gfin"); bc_load(gfin, g_fin, b_gfin)
    pn = [A.alloc(1024, BF16) for _ in range(2)]; b_pn = [Buf("pn%d" % i) for i in range(2)]
    pnT = [A.alloc(1024, BF16) for _ in range(2)]; b_pnT = [Buf("pnT%d" % i) for i in range(2)]
    p32 = [A.alloc(256) for _ in range(2)]; b_p32 = [Buf("p32%d" % i) for i in range(2)]
    pbf = [A.alloc(256, BF16) for _ in range(2)]; b_pbf = [Buf("pbf%d" % i) for i in range(2)]
    pT = [A.alloc(256, BF16) for _ in range(2)]; b_pT = [Buf("pT%d" % i) for i in range(2)]
    sgp = [A.alloc(512) for _ in range(2)]; b_sgp = [Buf("sgp%d" % i) for i in range(2)]
    ot = [A.alloc(1024) for _ in range(2)]; b_ot = [Buf("ot%d" % i) for i in range(2)]
    cnt = [0]
    for j in range(NJ):
        sl = j % 2
        k.ld(p32[sl], p_own[j * 128:(j + 1) * 128, :], b_p32[sl])
        k.cp("dve", pbf[sl], p32[sl], [b_p32[sl]], [b_pbf[sl]])
        transpose_to(pT[sl], b_pT[sl], pbf[sl], b_pbf[sl], 2, grp="pfm", banks=(6, 7), eng="act")
        norm_tile((h13[:, j, :], b_h1[j]), sl, gpb, b_gpb, pn[sl], b_pn[sl])
        transpose_to(pnT[sl], b_pnT[sl], pn[sl], b_pn[sl], 8, grp="pfm", banks=(6, 7), eng="act")
        n3 = v3(pnT[sl], 8); t3 = v3(pT[sl], 2)
        for b in range(2):
            pgt, bpgt = k.ps("pfg", (0, 1, 2))
            for kc in range(8):
                k.mm(pgt[:, 0:512], n3[:, kc, :], wpg3[:, kc, 512 * b:512 * b + 512], kc == 0, kc == 7, [b_pnT[sl], b_wpg], [bpgt], inc=(kc == 7))
            sg = sgp[cnt[0] % 2]; bsg = b_sgp[cnt[0] % 2]; cnt[0] += 1
            sigmoid_from(sg, bsg, pgt[:, 0:512], bpgt)
            ppp, bppp = k.ps("pfp", (3, 4, 5))
            for c in range(2):
                k.mm(ppp[:, 0:512], t3[:, c, :], wpp3[:, c, 512 * b:512 * b + 512], c == 0, c == 1, [b_pT[sl], b_wpp], [bppp], inc=(c == 1))
            k.tt(sg, sg, ppp[:, 0:512], ALU.mult, [bsg, bppp], [bsg])
            hsl = h13[:, j, 512 * b:512 * b + 512]
            k.tt(hsl, hsl, sg, ALU.add, [b_h1[j], bsg], [b_h1[j]])
        s = st4[sl]; bs = b_st4[sl]
        k.act(junk, h13[:, j, :], AF.Square, [b_h1[j]], [b_junk, bs], accum_out=s[:, 0:1])
        k.rstd(s[:, 0:1], 1.0 / 1024, s[:, 1:2], s[:, 2:3], [bs], [bs])
        k.stt(ot[sl], h13[:, j, :], s[:, 2:3], gfin, ALU.mult, ALU.mult, [b_h1[j], bs, b_gfin], [b_ot[sl]])
        k.st(out_own[j * 128:(j + 1) * 128, :], ot[sl], b_ot[sl], d_out)
    if dummy is not None:
        dz = A.alloc(8); b_dz = Buf("dz")
        for i in range(dummy[1]):
            if dummy[0] == "pe":
                k.mm(k.psb[0][0][:, 0:2], IDB, IDB[:, 0:2], True, True, [b_cb], [k.psb[0][1]], inc=(i % 64 == 63))
            else:
                k.memset(dz, float(i % 7), [b_dz], eng=dummy[0])
    P.barrier()
    P.emit()
    print("per-engine stream lengths (instr + waits):", {e: len(P.ops[e]) + sum(len(w) for w, _, _ in P.ops[e]) for e in ENGS})
    print("instructions:", P.n_inst, "sems:", len(P.semkeys), "sbuf peak words:", A.peak,
          "counts:", {e: P.cnt[e] for e in ENGS})
    return nc


def _consts():
    bf = ml_dtypes.bfloat16
    s = np.arange(128)[:, None]; t = np.arange(128)[None, :]
    ut = (s <= t).astype(np.float32)
    c_f32 = np.concatenate([-ut, -np.ones((128, 128), np.float32), np.eye(128, dtype=np.float32)], axis=1)
    c_e = np.zeros((64, 64, 128), np.float32)
    for n in range(64):
        c_e[n, n, :] = NEGB
    return c_f32, ut, c_e.reshape(64, 64 * 128).astype(bf)


def make_in_maps(inp):
    bf = ml_dtypes.bfloat16
    f = lambda a: np.ascontiguousarray(np.asarray(a, dtype=np.float32))
    x = f(inp["x"])[0]; p = f(inp["p"])[0, 0]
    w_in = f(inp["w_in"])[0]
    o = np.cumsum([0, 512, 512, 512, 512, 512, 512, 512, 4, 4, 1024, 1024])
    aq, ak, av, mq, mk_, mv, mo, mi, mf, ga, gm = [w_in[:, o[i]:o[i + 1]] for i in range(11)]
    shared = {
        "g_mix": f(inp["mix_norm_g"]), "g_ffn": f(inp["ffn_norm_g"]), "g_ple": f(inp["ple_norm_g"]),
        "g_fin": f(inp["final_norm_g"]).reshape(1, 1024), "g_ml": f(inp["mlstm_norm_g"]),
        "w_kv0": f(np.concatenate([ak[:, 0:256], av[:, 0:256]], 1)), "w_kv1": f(np.concatenate([ak[:, 256:512], av[:, 256:512]], 1)),
        "w_q0": f(aq[:, 0:256]), "w_q1": f(aq[:, 256:512]),
        "w_ms": f(np.concatenate([mk_, mv, mi, mf], 1)), "w_mo": f(np.concatenate([mq, mo], 1)),
        "w_g": f(np.concatenate([ga, gm], 1)), "b_gate": f(inp["b_gate"]),
        "ifb": f(inp["mlstm_if_b"]),
        "w_ua": f(inp["w_up_attn"])[0], "w_um": f(inp["w_up_mlstm"])[0], "w_out": f(inp["w_out"])[0],
        "w_r": f(np.concatenate([f(inp["router_group_w"])[0], f(inp["router_expert_w"])[0]], 1)),
        "b_r": f(np.concatenate([f(inp["router_group_b"]), f(inp["router_expert_b"])], 1)),
        "e_wg": f(inp["expert_w_gate"])[0].reshape(32 * 1024, 512), "e_wu": f(inp["expert_w_up"])[0].reshape(32 * 1024, 512),
        "e_wd": f(inp["expert_w_down"])[0].reshape(32 * 512, 1024),
        "w_pg": f(inp["w_ple_gate"])[0], "w_pp": f(inp["w_ple_proj"])[0],
    }
    cw_ = f(inp["conv_w"])[0]; cb_ = f(inp["conv_b"])[0]
    conv_wb = np.zeros((128, 40), np.float32)
    for hq in range(8):
        ch = np.arange(128) + 128 * hq
        conv_wb[:, hq * 4:hq * 4 + 4] = cw_[:, ch].T
        conv_wb[:, 32 + hq] = cb_[ch]
    shared["conv_wb"] = conv_wb
    c_f32, ut, c_e = _consts()
    shared["c_f32"] = c_f32; shared["c_e"] = c_e
    x_t = x.reshape(128, 128, 1024); p_t = p.reshape(128, 128, 256)
    kk = np.arange(128)[:, None]; qq = np.arange(128)[None, :]
    tri = np.where(kk <= qq, 0.0, -NEGB).astype(np.float32)
    maps = []
    for c in range(NCORES):
        par = c % 2; pad = 2 * ((7 - c) // 2)
        xs = np.zeros((128, 128, 1024), np.float32)
        xs[pad:] = x_t[:128 - pad]
        own = [8 * j + c for j in range(16)]
        cm0 = tri if par == 0 else np.zeros((128, 128), np.float32)
        cm1 = np.full((128, 128), -NEGB, np.float32) if par == 0 else tri
        c_bf = np.concatenate([np.eye(128, dtype=np.float32), ut, cm0, cm1], 1).astype(bf)
        c_core = np.zeros((128, 130), np.float32)
        c_core[:, 0:128] = (np.arange(128)[None, :] >= pad).astype(np.float32)
        c_core[:, 128] = par; c_core[:, 129] = 1 - par
        gmask = np.full((16, 64), -1e30, np.float32)
        for j in range(16):
            gmask[j, pad // 2:4 * j + 3] = 0.0
        m = dict(shared)
        m.update({"x_shift": xs.reshape(128 * 128, 1024), "x_own": np.ascontiguousarray(x_t[own]).reshape(2048, 1024),
                  "p_own": np.ascontiguousarray(p_t[own]).reshape(2048, 256), "c_bf": c_bf, "c_core": c_core,
                  "c_gmask": gmask.reshape(1, 1024)})
        maps.append(m)
    return maps


_NC_CACHE = {}


def kernel(**inputs):
    if "nc" not in _NC_CACHE:
        _NC_CACHE["nc"] = build_program(False)
    nc = _NC_CACHE["nc"]
    maps = make_in_maps(inputs)
    res = run_bass_kernel_spmd(nc, maps, core_ids=list(range(NCORES)))
    out = np.zeros((128, 128, 1024), np.float32)
    for c in range(NCORES):
        o = np.asarray(res.results[c]["out_own"], dtype=np.float32).reshape(16, 128, 1024)
        for j in range(16):
            out[8 * j + c] = o[j]
    return out.reshape(1, 16384, 1024)
```

```python
import numpy as np
import ml_dtypes
from contextlib import ExitStack
import concourse.bass as bass
import concourse.mybir as mybir
from concourse.bass_utils import run_bass_kernel_spmd

F32 = mybir.dt.float32
BF16 = mybir.dt.bfloat16
ALU = mybir.AluOpType
AF = mybir.ActivationFunctionType
AX = mybir.AxisListType
ENGS = ("pe", "act", "dve", "pool", "sp")
NCORES = 8
EPS = 1e-6
NEGB = 30000.0


class Buf:
    __slots__ = ("name", "w", "r", "dsem")

    def __init__(self, name):
        self.name = name
        self.w = None
        self.r = []
        self.dsem = None


class Prog:
    def __init__(self, nc):
        self.nc = nc
        self.ops = {e: [] for e in ENGS}
        self.cnt = {e: 0 for e in ENGS}
        self.seen = {e: {} for e in ENGS}
        self.dma_cnt = {}
        self.semkeys = []
        self.semset = set()
        self.n_inst = 0

    LIM = 4000

    def _tok(self, eng, val):
        ep = (val - 1) // self.LIM
        key = "%s#%d" % (eng, ep)
        if key not in self.semset:
            self.semset.add(key)
            self.semkeys.append(key)
        return (key, val - ep * self.LIM)

    def _need(self, eng, deps):
        best = {}
        for k, v in deps:
            if eng == "pe" and k.startswith("pe#"):
                continue
            if v <= self.seen[eng].get(k, 0):
                continue
            if v > best.get(k, 0):
                best[k] = v
        for k, v in best.items():
            self.seen[eng][k] = v
        return list(best.items())

    def _deps(self, reads, writes):
        deps = []
        for b in reads:
            if b.w is not None:
                deps.append(b.w)
        for b in writes:
            if b.w is not None:
                deps.append(b.w)
            deps.extend(b.r)
        return deps

    def op(self, eng, fn, reads=(), writes=(), inc=True):
        waits = self._need(eng, self._deps(reads, writes))
        val = self.cnt[eng] + 1
        if inc:
            self.cnt[eng] = val
        tok = self._tok(eng, val)
        self.ops[eng].append((waits, fn, (tok[0], 1) if inc else None))
        for b in reads:
            b.r.append(tok)
            if len(b.r) > 24:
                b.r = self._compact(b.r)
        for b in writes:
            b.w = tok
            b.r = []
        self.n_inst += 1

    @staticmethod
    def _compact(r):
        best = {}
        for k, v in r:
            if v > best.get(k, 0):
                best[k] = v
        return list(best.items())

    def dma(self, eng, fn, anchor, reads=(), writes=(), amt=16):
        if anchor.dsem is None:
            anchor.dsem = "d%d" % len(self.semkeys)
            self.semkeys.append(anchor.dsem)
            self.dma_cnt[anchor.dsem] = 0
        k = anchor.dsem
        waits = self._need(eng, self._deps(reads, writes))
        self.dma_cnt[k] += amt
        tok = (k, self.dma_cnt[k])
        self.ops[eng].append((waits, fn, (k, amt)))
        for b in reads:
            b.r.append(tok)
        for b in writes:
            b.w = tok
            b.r = []
        self.n_inst += 1

    def barrier(self):
        deps = [self._tok(e, self.cnt[e]) for e in ENGS if self.cnt[e] > 0]
        deps += [(k, v) for k, v in self.dma_cnt.items() if v > 0]
        for e in ENGS:
            waits = self._need(e, deps)
            if waits:
                self.ops[e].append((waits, None, None))

    def emit(self):
        nc = self.nc
        with ExitStack() as st:
            sems = {}
            for k in self.semkeys:
                sems[k] = st.enter_context(nc.semaphore("s_" + k.replace("#", "_")))
            block = st.enter_context(nc.Block())

            def run(eobj, name):
                for waits, fn, inc in self.ops[name]:
                    for k, v in waits:
                        eobj.wait_ge(sems[k], v)
                    if fn is None:
                        continue
                    ins = fn(eobj)
                    if inc is not None:
                        ins.then_inc(sems[inc[0]], inc[1])

            @block.tensor
            def _(e):
                run(e, "pe")

            @block.scalar
            def _(e):
                run(e, "act")

            @block.vector
            def _(e):
                run(e, "dve")

            @block.gpsimd
            def _(e):
                run(e, "pool")

            @block.sync
            def _(e):
                run(e, "sp")


class Arena:
    def __init__(self, nc, words):
        self.t = nc.alloc_sbuf_tensor("arena", [128, words], F32)
        self.words = words
        self.top = 0
        self.peak = 0

    def mark(self):
        return self.top

    def release(self, m):
        self.top = m

    def alloc(self, cols, dtype=F32):
        w = cols if dtype == F32 else (cols + 1) // 2
        w = (w + 7) // 8 * 8
        a = self.top
        self.top += w
        self.peak = max(self.peak, self.top)
        assert self.top <= self.words, "SBUF arena overflow %d > %d" % (self.top, self.words)
        ap = self.t[:, a:a + w]
        if dtype != F32:
            ap = ap.bitcast(dtype)
        return ap[:, 0:cols]


def v3(ap, a):
    return ap.rearrange("p (a b) -> p a b", a=a)


class K:
    def __init__(self, nc, dbg=False):
        self.nc = nc
        self.P = Prog(nc)
        self.A = Arena(nc, 52600)
        self.dbg = dbg
        self.psb = []
        for i in range(8):
            t = nc.alloc_psum_tensor("psb%d" % i, [128, 512], F32)
            self.psb.append((t, Buf("ps%d" % i)))
        self.rr = {}

    def ps(self, group, banks):
        i = self.rr.get(group, 0)
        self.rr[group] = i + 1
        t, b = self.psb[banks[i % len(banks)]]
        return t, b

    def mm(self, out, lhsT, rhs, start, stop, reads, writes, inc=True):
        self.P.op("pe", lambda e: e.matmul(out, lhsT=lhsT, rhs=rhs, start=start, stop=stop), reads, writes, inc)

    def tr(self, out, in_, ident, reads, writes, inc=True):
        self.P.op("pe", lambda e: e.transpose(out=out, in_=in_, identity=ident), reads, writes, inc)

    def act(self, out, in_, func, reads, writes, bias=None, scale=None, accum_out=None, eng="act"):
        kw = {}
        if bias is not None:
            kw["bias"] = bias
        if scale is not None:
            kw["scale"] = scale
        if accum_out is not None:
            kw["accum_out"] = accum_out
        self.P.op(eng, lambda e: e.activation(out=out, in_=in_, func=func, **kw), reads, writes)

    def cp(self, eng, out, in_, reads, writes):
        if eng == "act":
            self.P.op("act", lambda e: e.copy(out=out, in_=in_), reads, writes)
        else:
            self.P.op(eng, lambda e: e.tensor_copy(out=out, in_=in_), reads, writes)

    def ts(self, out, in0, s1, s2, op0, op1, reads, writes, eng="dve"):
        if op1 is None:
            self.P.op(eng, lambda e: e.tensor_scalar(out=out, in0=in0, scalar1=s1, scalar2=None, op0=op0), reads, writes)
        else:
            self.P.op(eng, lambda e: e.tensor_scalar(out=out, in0=in0, scalar1=s1, scalar2=s2, op0=op0, op1=op1), reads, writes)

    def stt(self, out, in0, scalar, in1, op0, op1, reads, writes, eng="dve"):
        self.P.op(eng, lambda e: e.scalar_tensor_tensor(out=out, in0=in0, scalar=scalar, in1=in1, op0=op0, op1=op1), reads, writes)

    def tt(self, out, in0, in1, op, reads, writes, eng="dve"):
        self.P.op(eng, lambda e: e.tensor_tensor(out=out, in0=in0, in1=in1, op=op), reads, writes)

    def recip(self, out, in_, reads, writes):
        self.P.op("dve", lambda e: e.reciprocal(out=out, in_=in_), reads, writes)

    def memset(self, ap, val, writes, eng="dve"):
        self.P.op(eng, lambda e: e.memset(ap, val), (), writes)

    def ld(self, out, in_, buf, eng="sp", reads=()):
        self.P.dma(eng, lambda e: e.dma_start(out=out, in_=in_), buf, reads=reads, writes=[buf])

    def st(self, out, in_, buf, dbuf, eng="sp"):
        self.P.dma(eng, lambda e: e.dma_start(out=out, in_=in_), buf, reads=[buf], writes=[dbuf])

    def ldw(self, dst3, src, kc, buf, eng="pool"):
        s3 = src.rearrange("(kc p) n -> p kc n", p=128)
        for k in range(kc):
            o = dst3[:, k, :]
            i = s3[:, k, :]
            self.P.dma(eng, lambda e, o=o, i=i: e.dma_start(out=o, in_=i), buf, writes=[buf])

    def rstd(self, ss, inv_n, tmp, out, rb, wb):
        self.act(tmp, ss, AF.Ln, list(rb) + [self.b_eps], wb, bias=self.eps_ap, scale=inv_n)
        self.act(out, tmp, AF.Exp, wb, wb, scale=-0.5)


def build_program(dbg=False, stop=None, NT=128, dummy=None, NE=32):
    nc = bass.Bass("TRN2", target_bir_lowering=False)
    k = K(nc, dbg)
    P, A = k.P, k.A

    def din(name, shape, dt=F32):
        return nc.dram_tensor(name, list(shape), dt, kind="ExternalInput").ap()

    x_shift = din("x_shift", [NT * 128, 1024])
    x_own = din("x_own", [16 * 128, 1024])
    p_own = din("p_own", [16 * 128, 256])
    g_mix = din("g_mix", [1, 1024]); g_ffn = din("g_ffn", [1, 1024]); g_ple = din("g_ple", [1, 1024]); g_fin = din("g_fin", [1, 1024])
    g_ml = din("g_ml", [1, 512])
    w_kv = [din("w_kv%d" % h, [1024, 512]) for h in range(2)]
    w_q = [din("w_q%d" % h, [1024, 256]) for h in range(2)]
    w_ms = din("w_ms", [1024, 1032])
    w_mo = din("w_mo", [1024, 1024])
    w_g = din("w_g", [1024, 2048])
    b_gate = din("b_gate", [1, 2048])
    conv_wb = din("conv_wb", [128, 40])
    ifb = din("ifb", [1, 8])
    w_ua = din("w_ua", [512, 1024]); w_um = din("w_um", [512, 1024]); w_out = din("w_out", [1024, 1024])
    w_r = din("w_r", [1024, 36]); b_r = din("b_r", [1, 36])
    c_f32 = din("c_f32", [128, 3 * 128])
    c_bf = din("c_bf", [128, 4 * 128], BF16)
    c_e = din("c_e", [64, 64 * 128], BF16)
    c_core = din("c_core", [128, 130])
    c_gmask = din("c_gmask", [1, 16 * 64])
    out_own = nc.dram_tensor("out_own", [16 * 128, 1024], F32, kind="ExternalOutput").ap()
    okind = "ExternalOutput" if dbg else "Internal"
    s_xnT = nc.dram_tensor("s_xnT", [(NT + 16) * 128, 1024], BF16).ap()
    s_ya = nc.dram_tensor("s_ya", [16 * 128, 512], BF16, kind=okind).ap()
    s_ym = nc.dram_tensor("s_ym", [16 * 128, 512], BF16, kind=okind).ap()
    d_xnT = [Buf("dxnT%d" % i) for i in range(NT + 16)]
    d_ya = [Buf("dya%d" % i) for i in range(16)]
    d_ym = [Buf("dym%d" % i) for i in range(16)]
    d_out = Buf("dout")

    cf = A.alloc(384); b_cf = Buf("cf")
    cb = A.alloc(512, BF16); b_cb = Buf("cb")
    cc = A.alloc(130); b_cc = Buf("cc")
    epsb = A.alloc(1); b_eps = Buf("eps")
    k.ld(cf, c_f32, b_cf); k.ld(cb, c_bf, b_cb); k.ld(cc, c_core, b_cc)
    k.memset(epsb, EPS, [b_eps])
    k.eps_ap = epsb[:, 0:1]; k.b_eps = b_eps
    NUT, NONES, ID32 = cf[:, 0:128], cf[:, 128:256], cf[:, 256:384]
    IDB, UTB, CM = cb[:, 0:128], cb[:, 128:256], [cb[:, 256:384], cb[:, 384:512]]
    VALID, PAR, NPAR = cc[:, 0:128], cc[:, 128:129], cc[:, 129:130]
    junk = A.alloc(1024, BF16); b_junk = Buf("junk")
    st4 = [A.alloc(4) for _ in range(2)]; b_st4 = [Buf("st%d" % i) for i in range(2)]
    base_mark = A.mark()

    def bc_load(dst, src_row, buf):
        k.ld(dst, src_row.partition_broadcast(128), buf)

    gb = A.alloc(1024); b_gb = Buf("gb")
    bc_load(gb, g_mix, b_gb)
    xt = [A.alloc(1024) for _ in range(2)]; b_xt = [Buf("xt%d" % i) for i in range(2)]
    xn = [A.alloc(1024, BF16) for _ in range(2)]; b_xn = [Buf("xn%d" % i) for i in range(2)]
    xT_s = [A.alloc(1024, BF16) for _ in range(2)]; b_xTs = [Buf("xTs%d" % i) for i in range(2)]

    def norm_tile(src_ap, sl, gbt, b_gbt, dst_bf, b_dst):
        s = st4[sl]; bs = b_st4[sl]
        k.act(junk, src_ap[0], AF.Square, [src_ap[1]], [b_junk, bs], accum_out=s[:, 0:1])
        k.rstd(s[:, 0:1], 1.0 / 1024, s[:, 1:2], s[:, 2:3], [bs], [bs])
        k.stt(dst_bf, src_ap[0], s[:, 2:3], gbt, ALU.mult, ALU.mult, [src_ap[1], bs, b_gbt], [b_dst])

    def transpose_to(dst_bf3, b_dst, src_bf, b_src, nchunk, grp="misc", banks=(5, 6, 7), eng="act"):
        pt, bp = k.ps(grp, banks)
        ptb = pt[:, :].bitcast(BF16)
        for c in range(nchunk):
            k.tr(ptb[:, c * 128:(c + 1) * 128], src_bf[:, c * 128:(c + 1) * 128], IDB, [b_src, b_cb], [bp], inc=(c == nchunk - 1))
        k.cp(eng, dst_bf3, ptb[:, 0:nchunk * 128], [bp], [b_dst])

    for T in range(NT + 16):
        sl = T % 2
        src = x_shift[T * 128:(T + 1) * 128, :] if T < NT else x_own[(T - NT) * 128:(T - NT + 1) * 128, :]
        k.ld(xt[sl], src, b_xt[sl])
        norm_tile((xt[sl], b_xt[sl]), sl, gb, b_gb, xn[sl], b_xn[sl])
        transpose_to(xT_s[sl], b_xTs[sl], xn[sl], b_xn[sl], 8)
        k.st(s_xnT[T * 128:(T + 1) * 128, :], xT_s[sl], b_xTs[sl], d_xnT[T], eng="act")
    P.barrier()
    A.release(base_mark)

    wms = A.alloc(8 * 1032, BF16); b_wms = Buf("wms"); wms3 = v3(wms, 8)
    wmo = A.alloc(8 * 1024, BF16); b_wmo = Buf("wmo"); wmo3 = v3(wmo, 8)
    k.ldw(wms3, w_ms, 8, b_wms); k.ldw(wmo3, w_mo, 8, b_wmo)
    cwb = A.alloc(40); b_cwb = Buf("cwb"); k.ld(cwb, conv_wb, b_cwb)
    ifbb = A.alloc(8); b_ifbb = Buf("ifbb"); bc_load(ifbb, ifb, b_ifbb)
    gml = A.alloc(512); b_gml = Buf("gml"); bc_load(gml, g_ml, b_gml)
    xT = [A.alloc(1024, BF16) for _ in range(2)]; b_xT = [Buf("xT%d" % i) for i in range(2)]
    C32 = A.alloc(4 * 129); b_C32 = [Buf("C32_%d" % h) for h in range(4)]; C32v = v3(C32, 4)
    Cb = A.alloc(4 * 130, BF16); b_Cb = [Buf("Cb_%d" % h) for h in range(4)]; Cbv = v3(Cb, 4)
    k.memset(C32, 0.0, b_C32); k.memset(Cb, 0.0, b_Cb)
    kpre = [A.alloc(132) for _ in range(4)]; b_kpre = [Buf("kpre%d" % h) for h in range(4)]
    qpre = [A.alloc(132) for _ in range(4)]; b_qpre = [Buf("qpre%d" % h) for h in range(4)]
    for h in range(4):
        k.memset(kpre[h], 0.0, [b_kpre[h]]); k.memset(qpre[h], 0.0, [b_qpre[h]])
    NSL = 4
    def mk(n, cols, dt=F32, pre=""):
        return [A.alloc(cols, dt) for _ in range(n)], [Buf(pre + str(i)) for i in range(n)]
    lif, b_lif = mk(2, 8, pre="lif"); gt, b_gt = mk(2, 24, pre="gt")
    ck, b_ck = mk(NSL, 128, pre="ck"); ex, b_ex = mk(NSL, 128, pre="ex")
    kTb, b_kTb = mk(NSL, 128, BF16, "kTb"); qTb, b_qTb = mk(NSL, 128, BF16, "qTb")
    ktok, b_ktok = mk(NSL, 128, BF16, "ktok"); vp, b_vp = mk(NSL, 130, BF16, "vp")
    tmpc, b_tmpc = mk(NSL, 129, pre="tmpc"); Sm, b_Sm = mk(NSL, 128, BF16, "Sm")
    hc, b_hc = mk(NSL, 128, pre="hc"); sm8, b_sm8 = mk(NSL, 8, pre="sm8")
    sig2, b_sig2 = mk(2, 512, pre="sig")
    ympos = [A.alloc(512, BF16) for _ in range(2)]; b_ympos = [Buf("ympos%d" % i) for i in range(2)]
    ymd = A.alloc(512); b_ymd = Buf("ymd")
    ymo = [A.alloc(512, BF16) for _ in range(2)]; b_ymo = [Buf("ymo%d" % i) for i in range(2)]

    def conv_silu(pre, b_pre, ps_src, b_ps, hq, out_bf, b_out, sl):
        k.cp("act", pre[:, 3:131], ps_src, [b_ps], [b_pre])
        c = ck[sl]; bcx = b_ck[sl]; e_ = ex[sl]; be = b_ex[sl]
        w0 = cwb[:, hq * 4:hq * 4 + 1]
        k.ts(c, pre[:, 0:128], w0, cwb[:, 32 + hq:33 + hq], ALU.mult, ALU.add, [b_pre, b_cwb], [bcx])
        for tap in range(1, 4):
            k.stt(c, pre[:, tap:tap + 128], cwb[:, hq * 4 + tap:hq * 4 + tap + 1], c, ALU.mult, ALU.add, [b_pre, b_cwb, bcx], [bcx])
        k.cp("pool", pre[:, 0:3], pre[:, 128:131], [b_pre], [b_pre])
        k.act(out_bf, c, AF.Silu, [bcx], [b_out])

    def pm_prologue(T):
        sl = T % 2
        x3 = v3(xT[sl], 8); bx = b_xT[sl]
        if T + 1 < NT:
            k.ld(xT[(T + 1) % 2], s_xnT[(T + 1) * 128:(T + 2) * 128, :], b_xT[(T + 1) % 2], reads=[d_xnT[T + 1]])
        pos = T % 8
        outp = pos >= 6
        pv, bpv = k.ps("pmv", (0, 1))
        for kc in range(8):
            k.mm(pv[:, 0:512], x3[:, kc, :], wms3[:, kc, 512:1024], kc == 0, kc == 7, [bx, b_wms], [bpv], inc=(kc == 7))
        pif, bpif = k.ps("pmisc", (5, 6, 7))
        for kc in range(8):
            k.mm(pif[:, 0:8], x3[:, kc, :], wms3[:, kc, 1024:1032], kc == 0, kc == 7, [bx, b_wms], [bpif], inc=(kc == 7))
        L = lif[sl]; bL = b_lif[sl]; G = gt[sl]; bG = b_gt[sl]
        k.tt(L, pif[:, 0:8], ifbb, ALU.add, [bpif, b_ifbb], [bL])
        k.act(G[:, 0:4], L[:, 4:8], AF.Exp, [bL], [bG], scale=-1.0)
        k.act(G[:, 0:4], G[:, 0:4], AF.Ln, [bG], [bG], bias=1.0)
        pb, bpb = k.ps("pmisc", (5, 6, 7))
        k.mm(pb[:, 0:4], NUT, G[:, 0:4], True, True, [b_cf, bG], [bpb], inc=False)
        k.mm(pb[:, 4:8], NONES, G[:, 0:4], True, True, [b_cf, bG], [bpb])
        k.act(G[:, 8:16], pb[:, 0:8], AF.Exp, [bpb], [bG])
        k.tt(G[:, 16:20], L[:, 0:4], pb[:, 0:4], ALU.subtract, [bL, bpb], [bG])
        k.act(G[:, 4:8], G[:, 16:20], AF.Exp, [bG], [bG])
        k.ts(G[:, 4:8], G[:, 4:8], VALID[:, T:T + 1], 128 ** -0.5, ALU.mult, ALU.mult, [bG, b_cc], [bG])
        if not outp:
            k.tt(G[:, 20:24], G[:, 4:8], G[:, 12:16], ALU.mult, [bG], [bG])
        sig = b_sig = None
        if outp:
            sig = sig2[pos - 6]; b_sig = b_sig2[pos - 6]
            po, bpo = k.ps("pmk", (2, 3, 4))
            for kc in range(8):
                k.mm(po[:, 0:512], x3[:, kc, :], wmo3[:, kc, 512:1024], kc == 0, kc == 7, [bx, b_wmo], [bpo], inc=(kc == 7))
            k.act(sig, po[:, 0:512], AF.Exp, [bpo], [b_sig], scale=-1.0)
            k.ts(sig, sig, 1.0, None, ALU.add, None, [b_sig], [b_sig])
            k.recip(sig, sig, [b_sig], [b_sig])
        return (T, x3, bx, pos, outp, pv, bpv, G, bG, sig, b_sig)

    def pm_stage_a(ctx, h, hs):
        T, x3, bx, pos, outp, pv, bpv, G, bG, sig, b_sig = ctx
        pk, bpk = k.ps("pmk", (2, 3, 4))
        for kc in range(8):
            k.mm(pk[:, 0:128], wms3[:, kc, 128 * h:128 * h + 128], x3[:, kc, :], kc == 0, kc == 7, [bx, b_wms], [bpk], inc=(kc == 7))
        conv_silu(kpre[h], b_kpre[h], pk[:, 0:128], bpk, 4 + h, kTb[hs], b_kTb[hs], hs)
        if pos >= 5:
            pq, bpq = k.ps("pmk", (2, 3, 4))
            for kc in range(8):
                k.mm(pq[:, 0:128], wmo3[:, kc, 128 * h:128 * h + 128], x3[:, kc, :], kc == 0, kc == 7, [bx, b_wmo], [bpq], inc=(kc == 7))
            if outp:
                conv_silu(qpre[h], b_qpre[h], pq[:, 0:128], bpq, h, qTb[hs], b_qTb[hs], hs)
            else:
                k.cp("act", qpre[h][:, 3:131], pq[:, 0:128], [bpq], [b_qpre[h]])
                k.cp("pool", qpre[h][:, 0:3], qpre[h][:, 128:131], [b_qpre[h]], [b_qpre[h]])
        V = vp[hs]; bV = b_vp[hs]
        gc = (4 + h) if outp else (20 + h)
        k.ts(V[:, 0:128], pv[:, 128 * h:128 * h + 128], G[:, gc:gc + 1], None, ALU.mult, None, [bpv, bG], [bV])
        k.cp("pool", V[:, 128:129], G[:, gc:gc + 1], [bG], [bV])

    def pm_stage_b(ctx, h, hs):
        T, x3, bx, pos, outp, pv, bpv, G, bG, sig, b_sig = ctx
        V = vp[hs]; bV = b_vp[hs]
        ptk, bptk = k.ps("pmisc", (5, 6, 7))
        ptkb = ptk[:, :].bitcast(BF16)
        k.tr(ptkb[:, 0:128], kTb[hs], IDB, [b_kTb[hs], b_cb], [bptk])
        k.cp("act", ktok[hs], ptkb[:, 0:128], [bptk], [b_ktok[hs]])
        if outp:
            pss, bpss = k.ps("pmk", (2, 3, 4))
            k.mm(pss[:, 0:128], kTb[hs], qTb[hs], True, True, [b_kTb[hs], b_qTb[hs]], [bpss])
            k.tt(Sm[hs], pss[:, 0:128], UTB, ALU.mult, [bpss, b_cb], [b_Sm[hs]])
            pn, bpn = k.ps("pmk", (2, 3, 4))
            k.mm(pn[:, 0:129], Sm[hs], V[:, 0:129], True, False, [b_Sm[hs], bV], [bpn], inc=False)
            k.mm(pn[:, 0:129], qTb[hs], Cbv[:, h, 0:129], False, True, [b_qTb[hs], b_Cb[h]], [bpn])
            s8 = sm8[hs]; bs8 = b_sm8[hs]
            et = G[:, 8 + h:9 + h]
            k.tt(s8[:, 0:1], pn[:, 128:129], et, ALU.mult, [bpn, bG], [bs8])
            k.stt(s8[:, 1:2], s8[:, 0:1], -1.0, s8[:, 0:1], ALU.mult, ALU.max, [bs8], [bs8])
            k.ts(s8[:, 1:2], s8[:, 1:2], 1.0, None, ALU.max, None, [bs8], [bs8])
            k.recip(s8[:, 2:3], s8[:, 1:2], [bs8], [bs8])
            k.tt(s8[:, 3:4], s8[:, 2:3], et, ALU.mult, [bs8, bG], [bs8])
            H = hc[hs]; bH = b_hc[hs]
            k.stt(H, pn[:, 0:128], s8[:, 3:4], sig[:, 128 * h:128 * h + 128], ALU.mult, ALU.mult, [bpn, bs8, b_sig], [bH])
            k.act(ex[hs], H, AF.Square, [bH], [b_ex[hs], bs8], accum_out=s8[:, 4:5])
            k.rstd(s8[:, 4:5], 1.0 / 128, s8[:, 5:6], s8[:, 6:7], [bs8], [bs8])
            k.stt(ympos[pos - 6][:, 128 * h:128 * h + 128], H, s8[:, 6:7], gml[:, 128 * h:128 * h + 128], ALU.mult, ALU.mult,
                  [bH, bs8, b_gml], [b_ympos[pos - 6]])
        pc, bpc = k.ps("pmk", (2, 3, 4))
        k.mm(pc[:, 0:129], ktok[hs], V[:, 0:129], True, True, [b_ktok[hs], bV], [bpc])
        if outp:
            tc_, btc = tmpc[hs], b_tmpc[hs]
            k.tt(tc_, pc[:, 0:129], C32v[:, h, :], ALU.add, [bpc, b_C32[h]], [btc])
            k.ts(C32v[:, h, :], tc_, G[:, 12 + h:13 + h], None, ALU.mult, None, [btc, bG], [b_C32[h]])
            if pos == 6:
                k.ts(Cbv[:, h, 0:129], tc_, G[:, 12 + h:13 + h], None, ALU.mult, None, [btc, bG], [b_Cb[h]])
        else:
            k.stt(C32v[:, h, :], C32v[:, h, :], G[:, 12 + h:13 + h], pc[:, 0:129], ALU.mult, ALU.add, [b_C32[h], bG, bpc], [b_C32[h]])
            if pos == 5:
                k.cp("act", Cbv[:, h, 0:129], C32v[:, h, :], [b_C32[h]], [b_Cb[h]])
        if h == 3 and pos == 7:
            j = T // 8
            k.tt(ymd, ympos[1], ympos[0], ALU.subtract, b_ympos, [b_ymd])
            yo = ymo[j % 2]; byo = b_ymo[j % 2]
            k.stt(yo, ymd, PAR, ympos[0], ALU.mult, ALU.add, [b_ymd, b_cc, b_ympos[0]], [byo])
            k.st(s_ym[j * 128:(j + 1) * 128, :], yo, byo, d_ym[j])

    k.ld(xT[0], s_xnT[0:128, :], b_xT[0], reads=[d_xnT[0]])
    items = [(T, h) for T in range(NT) for h in range(4)]
    ctxs = {}

    def pm_emit_a(i):
        T, h = items[i]
        if h == 0:
            ctxs[T] = pm_prologue(T)
            ctxs.pop(T - 2, None)
        pm_stage_a(ctxs[T], h, i % NSL)

    pm_emit_a(0)
    for i in range(len(items)):
        if i + 1 < len(items):
            pm_emit_a(i + 1)
        T, h = items[i]
        pm_stage_b(ctxs[T], h, i % NSL)
    P.barrier()
    A.release(base_mark)
    if stop == "PM":
        P.emit(); print("instructions:", P.n_inst, {e: P.cnt[e] for e in ENGS}); return nc

    KT = A.alloc(2 * 16384, BF16); KT3 = v3(KT, 2)
    b_KT = [Buf("KT%d" % t) for t in range(NT)]
    V1 = A.alloc(NT * 260, BF16); V14 = V1.rearrange("p (t h d) -> p t h d", t=NT, h=4)
    b_V1 = [Buf("V1_%d" % t) for t in range(NT)]
    Eb = A.alloc(64 * 128, BF16); b_Eb = Buf("Eb"); Eb3 = v3(Eb, 64)
    k.ld(Eb[0:64, :], c_e, b_Eb)
    gmask = A.alloc(1024); b_gmask = Buf("gmask"); bc_load(gmask, c_gmask, b_gmask); gmask3 = v3(gmask, 16)
    wkv = A.alloc(8 * 512, BF16); b_wkv = Buf("wkv"); wkv3 = v3(wkv, 8)
    wq = A.alloc(8 * 256, BF16); b_wq = Buf("wq"); wq3 = v3(wq, 8)
    xT = [A.alloc(1024, BF16) for _ in range(2)]; b_xT = [Buf("axT%d" % i) for i in range(2)]
    xTo = A.alloc(1024, BF16); b_xTo = Buf("xTo")
    ksum = A.alloc(2 * NT); b_ksum = Buf("ksum"); ksum3 = v3(ksum, 2)
    kmT = A.alloc(2 * 64); b_kmT = Buf("kmT"); kmT3 = v3(kmT, 2)
    QTs = A.alloc(512, BF16); b_QTs = Buf("QTs"); QTs4 = QTs.rearrange("p (a s q) -> p a s q", a=2, s=2)
    QT32 = A.alloc(512); b_QT32 = Buf("QT32"); QT324 = QT32.rearrange("p (a s q) -> p a s q", a=2, s=2)
    k.memset(QTs, 0.0, [b_QTs]); k.memset(QT32, 0.0, [b_QT32])
    gm_, b_gm = mk(4, 64, pre="gm"); t8, b_t8 = mk(4, 16, pre="t8")
    b01, b_b01 = mk(4, 64, BF16, "b01"); BT, b_BT = mk(4, 128, BF16, "BT")
    PT, b_PT = mk(3, 512, BF16, "PT")
    yat = [A.alloc(512, BF16) for _ in range(2)]; b_yat = [Buf("yat%d" % i) for i in range(2)]
    r1, b_r1 = mk(2, 2, pre="r1")
    hcount = [0]; gcount = [0]

    for hp in range(2):
        if hp == 1:
            P.barrier()
        k.ldw(wkv3, w_kv[hp], 8, b_wkv); k.ldw(wq3, w_q[hp], 8, b_wq)
        k.memset(kmT, 0.0, [b_kmT]); k.memset(ksum, 0.0, [b_ksum])
        if hp == 0:
            for t0 in range(0, NT, 16):
                k.memset(V1[:, t0 * 260:(t0 + 16) * 260], 1.0, b_V1[t0:t0 + 16])
        k.ld(xT[0], s_xnT[0:128, :], b_xT[0], reads=[d_xnT[0]])
        for T in range(NT):
            sl = T % 2
            x3 = v3(xT[sl], 8); bx = b_xT[sl]
            if T + 1 < NT:
                k.ld(xT[(T + 1) % 2], s_xnT[(T + 1) * 128:(T + 2) * 128, :], b_xT[(T + 1) % 2], reads=[d_xnT[T + 1]])
            for p in range(2):
                pk, bpk = k.ps("paproj", (5, 6, 7))
                for kc in range(8):
                    k.mm(pk[:, 0:128], wkv3[:, kc, 128 * p:128 * p + 128], x3[:, kc, :], kc == 0, kc == 7, [bx, b_wkv], [bpk], inc=(kc == 7))
                k.act(KT3[:, p, T * 128:(T + 1) * 128], pk[:, 0:128], AF.Identity, [bpk], [b_KT[T], b_ksum], accum_out=ksum3[:, p, T:T + 1])
            pv, bpv = k.ps("paproj", (5, 6, 7))
            for kc in range(8):
                k.mm(pv[:, 0:256], x3[:, kc, :], wkv3[:, kc, 256:512], kc == 0, kc == 7, [bx, b_wkv], [bpv], inc=(kc == 7))
            k.cp("dve", V14[:, T, :, 0:64], pv[:, 0:256].rearrange("p (h d) -> p h d", h=4), [bpv], [b_V1[T]])
            if T % 2 == 1:
                n = T // 2
                k.tt(kmT3[:, :, n:n + 1], ksum3[:, :, T - 1:T], ksum3[:, :, T:T + 1], ALU.add, [b_ksum], [b_kmT])
            if T % 8 != 7:
                continue
            j = T // 8
            k.ld(xTo, s_xnT[(NT + j) * 128:(NT + j + 1) * 128, :], b_xTo, reads=[d_xnT[NT + j]])
            xo3 = v3(xTo, 8)
            for p in range(2):
                pq, bpq = k.ps("paproj", (5, 6, 7))
                for kc in range(8):
                    k.mm(pq[:, 0:128], wq3[:, kc, 128 * p:128 * p + 128], xo3[:, kc, :], kc == 0, kc == 7, [b_xTo, b_wq], [bpq], inc=(kc == 7))
                for s_ in range(2):
                    k.ts(QTs4[64 * s_:64 * s_ + 64, p, s_, :], pq[64 * s_:64 * s_ + 64, 0:128], 0.125, None, ALU.mult, None, [bpq], [b_QTs])
                    k.cp("dve", QT324[64 * s_:64 * s_ + 64, p, s_, :], pq[64 * s_:64 * s_ + 64, 0:128], [bpq], [b_QT32])
            ya_t = yat[j % 2]; b_ya_t = b_yat[j % 2]
            nkt = 8 * j + 8
            def prologue_a(hl):
                p, s = hl // 2, hl % 2
                pg, bpg = k.ps("paproj", (5, 6, 7))
                k.mm(pg[:, 0:64], QT324[:, p, s, :], kmT3[:, p, :], True, True, [b_QT32, b_kmT], [bpg])
                g_ = gm_[hl]; bg_ = b_gm[hl]; t_ = t8[hl]; bt_ = b_t8[hl]
                k.tt(g_, pg[:, 0:64], gmask3[:, j, :], ALU.add, [bpg, b_gmask], [bg_])
                P.op("dve", lambda e, o=t_[:, 0:8], i=g_: e.max(out=o, in_=i), [bg_], [bt_])
                k.ts(t_[:, 8:9], t_[:, 2:3], -1e29, None, ALU.max, None, [bt_], [bt_])
                k.ts(b01[hl], g_, t_[:, 8:9], 1.0, ALU.is_ge, ALU.subtract, [bg_, bt_], [b_b01[hl]])

            def prologue_b(hl):
                pbt, bpbt = k.ps("paproj", (5, 6, 7))
                pbtb = pbt[:, :].bitcast(BF16)
                k.tr(pbtb[0:64, 0:128], b01[hl], IDB, [b_b01[hl], b_cb], [bpbt])
                k.cp("dve", BT[hl][0:64, :], pbtb[0:64, 0:128], [bpbt], [b_BT[hl]])

            def qk_group(hl, g0):
                p, s = hl // 2, hl % 2
                pS, bpS = k.ps("pas", (2, 3, 4))
                for i in range(4):
                    kt = g0 + i
                    col = pS[:, i * 128:(i + 1) * 128]
                    k.mm(col, KT3[:, p, kt * 128:(kt + 1) * 128], QTs4[:, p, s, :], True, False, [b_KT[kt], b_QTs], [bpS], inc=False)
                    if kt < nkt - 2:
                        k.mm(col, Eb3[0:64, kt // 2, :], BT[hl][0:64, :], False, True, [b_Eb, b_BT[hl]], [bpS], inc=(i == 3))
                    else:
                        k.mm(col, IDB, CM[kt - (nkt - 2)], False, True, [b_cb], [bpS], inc=(i == 3))
                return pS, bpS

            def pv_group(hl, g0, pS, bpS, po, bpo):
                pt_ = PT[gcount[0] % 3]; bpt_ = b_PT[gcount[0] % 3]; gcount[0] += 1
                k.act(pt_, pS[:, 0:512], AF.Exp, [bpS], [bpt_])
                for i in range(4):
                    kt = g0 + i
                    k.mm(po[:, 0:65], pt_[:, i * 128:(i + 1) * 128], V14[:, kt, hl, :], kt == 0, kt == nkt - 1, [bpt_, b_V1[kt]], [bpo],
                         inc=(i == 3))

            groups = list(range(0, nkt, 4))
            prologue_a(0); prologue_b(0)
            for hl in range(4):
                po, bpo = k.ps("pao", (0, 1))
                cur = qk_group(hl, groups[0])
                for gi, g0 in enumerate(groups):
                    nxt = qk_group(hl, groups[gi + 1]) if gi + 1 < len(groups) else None
                    if hl < 3 and gi == 0:
                        prologue_a(hl + 1)
                    if hl < 3 and gi == min(1, len(groups) - 1):
                        prologue_b(hl + 1)
                    pv_group(hl, g0, cur[0], cur[1], po, bpo)
                    cur = nxt
                r_ = r1[hl % 2]; br_ = b_r1[hl % 2]
                k.recip(r_[:, 0:1], po[:, 64:65], [bpo], [br_])
                k.ts(ya_t[:, 64 * hl:64 * hl + 64], po[:, 0:64], r_[:, 0:1], None, ALU.mult, None, [bpo, br_], [b_ya_t])
            k.st(s_ya[j * 128:(j + 1) * 128, 256 * hp:256 * hp + 256], ya_t[:, 0:256], b_ya_t, d_ya[j])
    P.barrier()
    A.release(base_mark)
    if stop == "PA":
        P.emit(); print("instructions:", P.n_inst, {e: P.cnt[e] for e in ENGS}); return nc

    e_wg = din("e_wg", [NE * 1024, 512]); e_wu = din("e_wu", [NE * 1024, 512]); e_wd = din("e_wd", [NE * 512, 1024])
    w_pg = din("w_pg", [1024, 1024]); w_pp = din("w_pp", [256, 1024])
    h1 = A.alloc(16 * 1024); h13 = v3(h1, 16); b_h1 = [Buf("h1_%d" % j) for j in range(16)]
    hnT = A.alloc(8 * 2048, BF16); hnT3 = v3(hnT, 8); b_hnT = [Buf("hnT%d" % j) for j in range(16)]
    cw = A.alloc(16 * 32); cw3 = v3(cw, 16); b_cw = [Buf("cw%d" % j) for j in range(16)]
    NJ = NT // 8
    if NJ < 16:
        k.memset(hnT, 0.0, b_hnT); k.memset(cw, 0.0, b_cw); k.memset(h1, 0.0, b_h1)
    pb_mark = A.mark()
    wg_ = A.alloc(8 * 2048, BF16); b_wg = Buf("wg"); wg3 = v3(wg_, 8)
    wua = A.alloc(4 * 1024, BF16); b_wua = Buf("wua"); wua3 = v3(wua, 4)
    wum = A.alloc(4 * 1024, BF16); b_wum = Buf("wum"); wum3 = v3(wum, 4)
    wo = A.alloc(8 * 1024, BF16); b_wo = Buf("wo"); wo3 = v3(wo, 8)
    wr = A.alloc(8 * 36); b_wr = Buf("wr"); wr3 = v3(wr, 8)
    k.ldw(wg3, w_g, 8, b_wg); k.ldw(wua3, w_ua, 4, b_wua); k.ldw(wum3, w_um, 4, b_wum); k.ldw(wo3, w_out, 8, b_wo)
    for kc in range(8):
        k.ld(wr3[:, kc, :], w_r[kc * 128:(kc + 1) * 128, :], b_wr)
    bgb = A.alloc(2048, BF16); b_bgb = Buf("bgb")
    br32 = A.alloc(36); b_br32 = Buf("br32"); bc_load(br32, b_r, b_br32)
    ones = A.alloc(128); b_ones = Buf("ones"); k.memset(ones, 1.0, [b_ones])
    onesb = A.alloc(128, BF16); b_onesb = Buf("onesb"); k.memset(onesb, 1.0, [b_onesb])
    gfb = A.alloc(1024); b_gfb = Buf("gfb"); bc_load(gfb, g_ffn, b_gfb)
    xTo = A.alloc(1024, BF16); b_xTo = Buf("bxTo")
    yab = A.alloc(512, BF16); b_yab = Buf("yab"); ymb = A.alloc(512, BF16); b_ymb = Buf("ymb")
    yaT = A.alloc(512, BF16); b_yaT = Buf("yaT"); ymT = A.alloc(512, BF16); b_ymT = Buf("ymT")
    yaT3 = v3(yaT, 4); ymT3 = v3(ymT, 4)
    sga = A.alloc(512); b_sga = Buf("sga"); sgm = A.alloc(512); b_sgm = Buf("sgm")
    mrg = A.alloc(1024, BF16); b_mrg = Buf("mrg"); mrgT = A.alloc(1024, BF16); b_mrgT = Buf("mrgT"); mrgT3 = v3(mrgT, 8)
    hn32 = A.alloc(1024); b_hn32 = Buf("hn32")
    hnhi = A.alloc(1024, BF16); b_hnhi = Buf("hnhi"); hnlo = A.alloc(1024, BF16); b_hnlo = Buf("hnlo")
    hiT = A.alloc(1024, BF16); b_hiT = Buf("hiT"); hiT3 = v3(hiT, 8)
    loT = A.alloc(1024, BF16); b_loT = Buf("loT"); loT3 = v3(loT, 8)
    wrh = A.alloc(8 * 36, BF16); b_wrh = Buf("wrh"); wrh3 = v3(wrh, 8)
    wrl = A.alloc(8 * 36, BF16); b_wrl = Buf("wrl"); wrl3 = v3(wrl, 8)
    k.cp("dve", wrh, wr, [b_wr], [b_wrh])
    k.tt(wrl, wr, wrh, ALU.subtract, [b_wr, b_wrh], [b_wrl])
    Lr = A.alloc(40); b_Lr = Buf("Lr"); rs = A.alloc(16); b_rs = Buf("rs"); elm = A.alloc(32); b_elm = Buf("elm")
    c1 = A.alloc(32); b_c1 = Buf("c1")
    for q4 in range(4):
        k.ld(sga, b_gate[:, 512 * q4:512 * q4 + 512].partition_broadcast(128), b_sga)
        k.cp("dve", bgb[:, 512 * q4:512 * q4 + 512], sga, [b_sga], [b_bgb])

    def sigmoid_from(dst, b_dst, ps_ap, b_ps):
        k.act(dst, ps_ap, AF.Exp, [b_ps], [b_dst], scale=-1.0)
        k.ts(dst, dst, 1.0, None, ALU.add, None, [b_dst], [b_dst])
        k.recip(dst, dst, [b_dst], [b_dst])

    for j in range(NJ):
        k.ld(h13[:, j, :], x_own[j * 128:(j + 1) * 128, :], b_h1[j])
        k.ld(xTo, s_xnT[(NT + j) * 128:(NT + j + 1) * 128, :], b_xTo, reads=[d_xnT[NT + j]])
        k.ld(yab, s_ya[j * 128:(j + 1) * 128, :], b_yab, reads=[d_ya[j]])
        k.ld(ymb, s_ym[j * 128:(j + 1) * 128, :], b_ymb, reads=[d_ym[j]])
        xo3 = v3(xTo, 8)
        transpose_to(yaT, b_yaT, yab, b_yab, 4, grp="pbm", banks=(6, 7), eng="dve")
        transpose_to(ymT, b_ymT, ymb, b_ymb, 4, grp="pbm", banks=(6, 7), eng="dve")
        for b in range(2):
            cs = slice(512 * b, 512 * b + 512)
            pga, bpga = k.ps("pbg", (0, 1, 2))
            for kc in range(8):
                k.mm(pga[:, 0:512], xo3[:, kc, :], wg3[:, kc, 512 * b:512 * b + 512], kc == 0, kc == 7, [b_xTo, b_wg], [bpga], inc=(kc == 7))
            k.tt(sga, pga[:, 0:512], bgb[:, 512 * b:512 * b + 512], ALU.add, [bpga, b_bgb], [b_sga])
            sigmoid_from(sga, b_sga, sga, b_sga)
            pua, bpua = k.ps("pbu", (3, 4, 5))
            for fc in range(4):
                k.mm(pua[:, 0:512], yaT3[:, fc, :], wua3[:, fc, cs], fc == 0, fc == 3, [b_yaT, b_wua], [bpua], inc=(fc == 3))
            k.tt(sga, sga, pua[:, 0:512], ALU.mult, [b_sga, bpua], [b_sga])
            pgm, bpgm = k.ps("pbg", (0, 1, 2))
            for kc in range(8):
                k.mm(pgm[:, 0:512], xo3[:, kc, :], wg3[:, kc, 1024 + 512 * b:1024 + 512 * b + 512], kc == 0, kc == 7, [b_xTo, b_wg], [bpgm], inc=(kc == 7))
            k.tt(sgm, pgm[:, 0:512], bgb[:, 1024 + 512 * b:1024 + 512 * b + 512], ALU.add, [bpgm, b_bgb], [b_sgm])
            sigmoid_from(sgm, b_sgm, sgm, b_sgm)
            pum, bpum = k.ps("pbu", (3, 4, 5))
            for fc in range(4):
                k.mm(pum[:, 0:512], ymT3[:, fc, :], wum3[:, fc, cs], fc == 0, fc == 3, [b_ymT, b_wum], [bpum], inc=(fc == 3))
            k.tt(sgm, sgm, pum[:, 0:512], ALU.mult, [b_sgm, bpum], [b_sgm])
            k.tt(mrg[:, cs], sga, sgm, ALU.add, [b_sga, b_sgm], [b_mrg])
        transpose_to(mrgT, b_mrgT, mrg, b_mrg, 8, grp="pbm", banks=(6, 7), eng="act")
        for b in range(2):
            ph, bph = k.ps("pbg", (0, 1, 2))
            for kc in range(8):
                k.mm(ph[:, 0:512], mrgT3[:, kc, :], wo3[:, kc, 512 * b:512 * b + 512], kc == 0, kc == 7, [b_mrgT, b_wo], [bph], inc=(kc == 7))
            k.tt(h13[:, j, 512 * b:512 * b + 512], h13[:, j, 512 * b:512 * b + 512], ph[:, 0:512], ALU.add, [b_h1[j], bph], [b_h1[j]])
        if stop == "PB1":
            continue
        s = st4[0]; bs = b_st4[0]
        k.act(junk, h13[:, j, :], AF.Square, [b_h1[j]], [b_junk, bs], accum_out=s[:, 0:1])
        k.rstd(s[:, 0:1], 1.0 / 1024, s[:, 1:2], s[:, 2:3], [bs], [bs])
        k.stt(hn32, h13[:, j, :], s[:, 2:3], gfb, ALU.mult, ALU.mult, [b_h1[j], bs, b_gfb], [b_hn32])
        k.cp("dve", hnhi, hn32, [b_hn32], [b_hnhi])
        k.tt(hnlo, hn32, hnhi, ALU.subtract, [b_hn32, b_hnhi], [b_hnlo])
        transpose_to(hiT, b_hiT, hnhi, b_hnhi, 8, grp="pbm", banks=(6, 7), eng="act")
        transpose_to(loT, b_loT, hnlo, b_hnlo, 8, grp="pbm", banks=(6, 7), eng="act")
        k.cp("dve", hnT3[:, :, j * 128:(j + 1) * 128], hiT3, [b_hiT], [b_hnT[j]])
        pr, bpr = k.ps("pbu", (3, 4, 5))
        n_ = 0
        for (aT, baT, w_, bw_) in ((hiT3, b_hiT, wrh3, b_wrh), (hiT3, b_hiT, wrl3, b_wrl), (loT3, b_loT, wrh3, b_wrh)):
            for kc in range(8):
                k.mm(pr[:, 0:36], aT[:, kc, :], w_[:, kc, :], n_ == 0, n_ == 23, [baT, bw_], [bpr], inc=(n_ == 23))
                n_ += 1
        if stop == "PB2":
            continue
        k.tt(Lr[:, 0:36], pr[:, 0:36], br32, ALU.add, [bpr, b_br32], [b_Lr])
        P.op("dve", lambda e, o=rs[:, 0:1], i=Lr[:, 0:4]: e.tensor_reduce(out=o, in_=i, axis=AX.X, op=ALU.max), [b_Lr], [b_rs])
        k.ts(rs[:, 1:2], rs[:, 0:1], -1.0, None, ALU.mult, None, [b_rs], [b_rs])
        k.act(Lr[:, 36:40], Lr[:, 0:4], AF.Exp, [b_Lr, b_rs], [b_Lr, b_rs], bias=rs[:, 1:2], accum_out=rs[:, 2:3])
        k.recip(rs[:, 3:4], rs[:, 2:3], [b_rs], [b_rs])
        k.ts(Lr[:, 36:40], Lr[:, 0:4], rs[:, 0:1], 1.0, ALU.is_equal, ALU.subtract, [b_Lr, b_rs], [b_Lr])
        k.ts(Lr[:, 36:40], Lr[:, 36:40], 1e30, None, ALU.mult, None, [b_Lr], [b_Lr])
        for g in range(4):
            k.ts(elm[:, 8 * g:8 * g + 8], Lr[:, 4 + 8 * g:12 + 8 * g], Lr[:, 36 + g:37 + g], None, ALU.add, None, [b_Lr], [b_elm])
        P.op("dve", lambda e, o=rs[:, 8:16], i=elm: e.max(out=o, in_=i), [b_elm], [b_rs])
        k.tt(rs[:, 4:5], rs[:, 9:10], rs[:, 8:9], ALU.subtract, [b_rs], [b_rs])
        k.act(rs[:, 4:5], rs[:, 4:5], AF.Exp, [b_rs], [b_rs])
        k.ts(rs[:, 4:5], rs[:, 4:5], 1.0, None, ALU.add, None, [b_rs], [b_rs])
        k.recip(rs[:, 5:6], rs[:, 4:5], [b_rs], [b_rs])
        k.tt(rs[:, 6:7], rs[:, 5:6], rs[:, 3:4], ALU.mult, [b_rs], [b_rs])
        k.tt(rs[:, 7:8], rs[:, 3:4], rs[:, 6:7], ALU.subtract, [b_rs], [b_rs])
        k.ts(c1, elm, rs[:, 8:9], rs[:, 6:7], ALU.is_equal, ALU.mult, [b_elm, b_rs], [b_c1])
        k.ts(cw3[:, j, :], elm, rs[:, 9:10], rs[:, 7:8], ALU.is_equal, ALU.mult, [b_elm, b_rs], [b_cw[j]])
        k.tt(cw3[:, j, :], cw3[:, j, :], c1, ALU.add, [b_cw[j], b_c1], [b_cw[j]])
    P.barrier()
    A.release(pb_mark)
    if stop in ("PB", "PB1", "PB2"):
        P.emit(); return nc

    ewg = [A.alloc(8 * 512, BF16) for _ in range(2)]; ewu = [A.alloc(8 * 512, BF16) for _ in range(2)]
    ewd = [A.alloc(4 * 1024, BF16) for _ in range(2)]
    b_ew = [[Buf("ewg%d" % i), Buf("ewu%d" % i), Buf("ewd%d" % i)] for i in range(2)]
    hidT = A.alloc(4 * 2048, BF16); hidT3 = v3(hidT, 4); b_hid = [Buf("hid%d" % t) for t in range(4)]
    sgt = [A.alloc(512, BF16) for _ in range(2)]; b_sgt = [Buf("sgt%d" % i) for i in range(2)]

    def load_expert(e):
        sl = e % 2
        k.ldw(v3(ewg[sl], 8), e_wg[e * 1024:(e + 1) * 1024, :], 8, b_ew[sl][0])
        k.ldw(v3(ewu[sl], 8), e_wu[e * 1024:(e + 1) * 1024, :], 8, b_ew[sl][1])
        k.ldw(v3(ewd[sl], 4), e_wd[e * 512:(e + 1) * 512, :], 4, b_ew[sl][2])

    load_expert(0)
    scnt = [0]
    for e in range(NE):
        sl = e % 2
        if e + 1 < NE:
            load_expert(e + 1)
        g3, u3, d3 = v3(ewg[sl], 8), v3(ewu[sl], 8), v3(ewd[sl], 4)
        bwg, bwu, bwd = b_ew[sl]
        for tg in range((NJ + 3) // 4):
            rb = [b_hnT[4 * tg + i] for i in range(4)]
            for fc in range(4):
                pG, bpG = k.ps("peg", (0, 1, 2, 3))
                for kc in range(8):
                    k.mm(pG[:, 0:512], g3[:, kc, fc * 128:(fc + 1) * 128], hnT3[:, kc, tg * 512:(tg + 1) * 512], kc == 0, kc == 7, rb + [bwg], [bpG],
                         inc=(kc == 7))
                pU, bpU = k.ps("peg", (0, 1, 2, 3))
                for kc in range(8):
                    k.mm(pU[:, 0:512], u3[:, kc, fc * 128:(fc + 1) * 128], hnT3[:, kc, tg * 512:(tg + 1) * 512], kc == 0, kc == 7, rb + [bwu], [bpU],
                         inc=(kc == 7))
                sg = sgt[scnt[0] % 2]; bsg = b_sgt[scnt[0] % 2]; scnt[0] += 1
                k.act(sg, pG[:, 0:512], AF.Silu, [bpG], [bsg])
                k.tt(hidT3[:, fc, tg * 512:(tg + 1) * 512], sg, pU[:, 0:512], ALU.mult, [bsg, bpU], [b_hid[tg]])
        for jt in range(NJ):
            for b in range(2):
                py, bpy = k.ps("pey", (4, 5, 6, 7))
                for fc in range(4):
                    k.mm(py[:, 0:512], hidT3[:, fc, jt * 128:(jt + 1) * 128], d3[:, fc, 512 * b:512 * b + 512], fc == 0, fc == 3, [b_hid[jt // 4], bwd], [bpy],
                         inc=(fc == 3))
                hsl = h13[:, jt, 512 * b:512 * b + 512]
                k.stt(hsl, py[:, 0:512], cw3[:, jt, e:e + 1], hsl, ALU.mult, ALU.add, [bpy, b_cw[jt], b_h1[jt]], [b_h1[jt]])
    P.barrier()
    A.release(pb_mark)
    if stop == "PE":
        P.emit(); return nc

    wpg = A.alloc(8 * 1024, BF16); b_wpg = Buf("wpg"); wpg3 = v3(wpg, 8)
    wpp = A.alloc(2 * 1024, BF16); b_wpp = Buf("wpp"); wpp3 = v3(wpp, 2)
    k.ldw(wpg3, w_pg, 8, b_wpg); k.ldw(wpp3, w_pp, 2, b_wpp)
    gpb = A.alloc(1024); b_gpb = Buf("gpb"); bc_load(gpb, g_ple, b_gpb)
    gfin = A.alloc(1024); b_gfin = Buf("gfin"); bc_load(gfin, g_fin, b_gfin)
    pn = [A.alloc(1024, BF16) for _ in range(2)]; b_pn = [Buf("pn%d" % i) for i in range(2)]
    pnT = [A.alloc(1024, BF16) for _ in range(2)]; b_pnT = [Buf("pnT%d" % i) for i in range(2)]
    p32 = [A.alloc(256) for _ in range(2)]; b_p32 = [Buf("p32%d" % i) for i in range(2)]
    pbf = [A.alloc(256, BF16) for _ in range(2)]; b_pbf = [Buf("pbf%d" % i) for i in range(2)]
    pT = [A.alloc(256, BF16) for _ in range(2)]; b_pT = [Buf("pT%d" % i) for i in range(2)]
    sgp = [A.alloc(512) for _ in range(2)]; b_sgp = [Buf("sgp%d" % i) for i in range(2)]
    ot = [A.alloc(1024) for _ in range(2)]; b_ot = [Buf("ot%d" % i) for i in range(2)]
    cnt = [0]
    for j in range(NJ):
        sl = j % 2
        k.ld(p32[sl], p_own[j * 128:(j + 1) * 128, :], b_p32[sl])
        k.cp("dve", pbf[sl], p32[sl], [b_p32[sl]], [b_pbf[sl]])
        transpose_to(pT[sl], b_pT[sl], pbf[sl], b_pbf[sl], 2, grp="pfm", banks=(6, 7), eng="act")
        norm_tile((h13[:, j, :], b_h1[j]), sl, gpb, b_gpb, pn[sl], b_pn[sl])
        transpose_to(pnT[sl], b_pnT[sl], pn[sl], b_pn[sl], 8, grp="pfm", banks=(6, 7), eng="act")
        n3 = v3(pnT[sl], 8); t3 = v3(pT[sl], 2)
        for b in range(2):
            pgt, bpgt = k.ps("pfg", (0, 1, 2))
            for kc in range(8):
                k.mm(pgt[:, 0:512], n3[:, kc, :], wpg3[:, kc, 512 * b:512 * b + 512], kc == 0, kc == 7, [b_pnT[sl], b_wpg], [bpgt], inc=(kc == 7))
            sg = sgp[cnt[0] % 2]; bsg = b_sgp[cnt[0] % 2]; cnt[0] += 1
            sigmoid_from(sg, bsg, pgt[:, 0:512], bpgt)
            ppp, bppp = k.ps("pfp", (3, 4, 5))
            for c in range(2):
                k.mm(ppp[:, 0:512], t3[:, c, :], wpp3[:, c, 512 * b:512 * b + 512], c == 0, c == 1, [b_pT[sl], b_wpp], [bppp], inc=(c == 1))
            k.tt(sg, sg, ppp[:, 0:512], ALU.mult, [bsg, bppp], [bsg])
            hsl = h13[:, j, 512 * b:512 * b + 512]
            k.tt(hsl, hsl, sg, ALU.add, [b_h1[j], bsg], [b_h1[j]])
        s = st4[sl]; bs = b_st4[sl]
        k.act(junk, h13[:, j, :], AF.Square, [b_h1[j]], [b_junk, bs], accum_out=s[:, 0:1])
        k.rstd(s[:, 0:1], 1.0 / 1024, s[:, 1:2], s[:, 2:3], [bs], [bs])
        k.stt(ot[sl], h13[:, j, :], s[:, 2:3], gfin, ALU.mult, ALU.mult, [b_h1[j], bs, b_gfin], [b_ot[sl]])
        k.st(out_own[j * 128:(j + 1) * 128, :], ot[sl], b_ot[sl], d_out)
    if dummy is not None:
        dz = A.alloc(8); b_dz = Buf("dz")
        for i in range(dummy[1]):
            if dummy[0] == "pe":
                k.mm(k.psb[0][0][:, 0:2], IDB, IDB[:, 0:2], True, True, [b_cb], [k.psb[0][1]], inc=(i % 64 == 63))
            else:
                k.memset(dz, float(i % 7), [b_dz], eng=dummy[0])
    P.barrier()
    P.emit()
    print("per-engine stream lengths (instr + waits):", {e: len(P.ops[e]) + sum(len(w) for w, _, _ in P.ops[e]) for e in ENGS})
    print("instructions:", P.n_inst, "sems:", len(P.semkeys), "sbuf peak words:", A.peak,
          "counts:", {e: P.cnt[e] for e in ENGS})
    return nc


def _consts():
    bf = ml_dtypes.bfloat16
    s = np.arange(128)[:, None]; t = np.arange(128)[None, :]
    ut = (s <= t).astype(np.float32)
    c_f32 = np.concatenate([-ut, -np.ones((128, 128), np.float32), np.eye(128, dtype=np.float32)], axis=1)
    c_e = np.zeros((64, 64, 128), np.float32)
    for n in range(64):
        c_e[n, n, :] = NEGB
    return c_f32, ut, c_e.reshape(64, 64 * 128).astype(bf)


def make_in_maps(inp):
    bf = ml_dtypes.bfloat16
    f = lambda a: np.ascontiguousarray(np.asarray(a, dtype=np.float32))
    x = f(inp["x"])[0]; p = f(inp["p"])[0, 0]
    w_in = f(inp["w_in"])[0]
    o = np.cumsum([0, 512, 512, 512, 512, 512, 512, 512, 4, 4, 1024, 1024])
    aq, ak, av, mq, mk_, mv, mo, mi, mf, ga, gm = [w_in[:, o[i]:o[i + 1]] for i in range(11)]
    shared = {
        "g_mix": f(inp["mix_norm_g"]), "g_ffn": f(inp["ffn_norm_g"]), "g_ple": f(inp["ple_norm_g"]),
        "g_fin": f(inp["final_norm_g"]).reshape(1, 1024), "g_ml": f(inp["mlstm_norm_g"]),
        "w_kv0": f(np.concatenate([ak[:, 0:256], av[:, 0:256]], 1)), "w_kv1": f(np.concatenate([ak[:, 256:512], av[:, 256:512]], 1)),
        "w_q0": f(aq[:, 0:256]), "w_q1": f(aq[:, 256:512]),
        "w_ms": f(np.concatenate([mk_, mv, mi, mf], 1)), "w_mo": f(np.concatenate([mq, mo], 1)),
        "w_g": f(np.concatenate([ga, gm], 1)), "b_gate": f(inp["b_gate"]),
        "ifb": f(inp["mlstm_if_b"]),
        "w_ua": f(inp["w_up_attn"])[0], "w_um": f(inp["w_up_mlstm"])[0], "w_out": f(inp["w_out"])[0],
        "w_r": f(np.concatenate([f(inp["router_group_w"])[0], f(inp["router_expert_w"])[0]], 1)),
        "b_r": f(np.concatenate([f(inp["router_group_b"]), f(inp["router_expert_b"])], 1)),
        "e_wg": f(inp["expert_w_gate"])[0].reshape(32 * 1024, 512), "e_wu": f(inp["expert_w_up"])[0].reshape(32 * 1024, 512),
        "e_wd": f(inp["expert_w_down"])[0].reshape(32 * 512, 1024),
        "w_pg": f(inp["w_ple_gate"])[0], "w_pp": f(inp["w_ple_proj"])[0],
    }
    cw_ = f(inp["conv_w"])[0]; cb_ = f(inp["conv_b"])[0]
    conv_wb = np.zeros((128, 40), np.float32)
    for hq in range(8):
        ch = np.arange(128) + 128 * hq
        conv_wb[:, hq * 4:hq * 4 + 4] = cw_[:, ch].T
        conv_wb[:, 32 + hq] = cb_[ch]
    shared["conv_wb"] = conv_wb
    c_f32, ut, c_e = _consts()
    shared["c_f32"] = c_f32; shared["c_e"] = c_e
    x_t = x.reshape(128, 128, 1024); p_t = p.reshape(128, 128, 256)
    kk = np.arange(128)[:, None]; qq = np.arange(128)[None, :]
    tri = np.where(kk <= qq, 0.0, -NEGB).astype(np.float32)
    maps = []
    for c in range(NCORES):
        par = c % 2; pad = 2 * ((7 - c) // 2)
        xs = np.zeros((128, 128, 1024), np.float32)
        xs[pad:] = x_t[:128 - pad]
        own = [8 * j + c for j in range(16)]
        cm0 = tri if par == 0 else np.zeros((128, 128), np.float32)
        cm1 = np.full((128, 128), -NEGB, np.float32) if par == 0 else tri
        c_bf = np.concatenate([np.eye(128, dtype=np.float32), ut, cm0, cm1], 1).astype(bf)
        c_core = np.zeros((128, 130), np.float32)
        c_core[:, 0:128] = (np.arange(128)[None, :] >= pad).astype(np.float32)
        c_core[:, 128] = par; c_core[:, 129] = 1 - par
        gmask = np.full((16, 64), -1e30, np.float32)
        for j in range(16):
            gmask[j, pad // 2:4 * j + 3] = 0.0
        m = dict(shared)
        m.update({"x_shift": xs.reshape(128 * 128, 1024), "x_own": np.ascontiguousarray(x_t[own]).reshape(2048, 1024),
                  "p_own": np.ascontiguousarray(p_t[own]).reshape(2048, 256), "c_bf": c_bf, "c_core": c_core,
                  "c_gmask": gmask.reshape(1, 1024)})
        maps.append(m)
    return maps


_NC_CACHE = {}


def kernel(**inputs):
    if "nc" not in _NC_CACHE:
        _NC_CACHE["nc"] = build_program(False)
    nc = _NC_CACHE["nc"]
    maps = make_in_maps(inputs)
    res = run_bass_kernel_spmd(nc, maps, core_ids=list(range(NCORES)))
    out = np.zeros((128, 128, 1024), np.float32)
    for c in range(NCORES):
        o = np.asarray(res.results[c]["out_own"], dtype=np.float32).reshape(16, 128, 1024)
        for j in range(16):
            out[8 * j + c] = o[j]
    return out.reshape(1, 16384, 1024)
```

```python
import numpy as np
import ml_dtypes
from contextlib import ExitStack
import concourse.bass as bass
import concourse.mybir as mybir
from concourse.bass_utils import run_bass_kernel_spmd

F32 = mybir.dt.float32
BF16 = mybir.dt.bfloat16
ALU = mybir.AluOpType
AF = mybir.ActivationFunctionType
AX = mybir.AxisListType
ENGS = ("pe", "act", "dve", "pool", "sp")
NCORES = 8
EPS = 1e-6
NEGB = 30000.0


class Buf:
    __slots__ = ("name", "w", "r", "dsem")

    def __init__(self, name):
        self.name = name
        self.w = None
        self.r = []
        self.dsem = None


class Prog:
    def __init__(self, nc):
        self.nc = nc
        self.ops = {e: [] for e in ENGS}
        self.cnt = {e: 0 for e in ENGS}
        self.seen = {e: {} for e in ENGS}
        self.dma_cnt = {}
        self.semkeys = []
        self.semset = set()
        self.n_inst = 0

    LIM = 4000

    def _tok(self, eng, val):
        ep = (val - 1) // self.LIM
        key = "%s#%d" % (eng, ep)
        if key not in self.semset:
            self.semset.add(key)
            self.semkeys.append(key)
        return (key, val - ep * self.LIM)

    def _need(self, eng, deps):
        best = {}
        for k, v in deps:
            if eng == "pe" and k.startswith("pe#"):
                continue
            if v <= self.seen[eng].get(k, 0):
                continue
            if v > best.get(k, 0):
                best[k] = v
        for k, v in best.items():
            self.seen[eng][k] = v
        return list(best.items())

    def _deps(self, reads, writes):
        deps = []
        for b in reads:
            if b.w is not None:
                deps.append(b.w)
        for b in writes:
            if b.w is not None:
                deps.append(b.w)
            deps.extend(b.r)
        return deps

    def op(self, eng, fn, reads=(), writes=(), inc=True):
        waits = self._need(eng, self._deps(reads, writes))
        val = self.cnt[eng] + 1
        if inc:
            self.cnt[eng] = val
        tok = self._tok(eng, val)
        self.ops[eng].append((waits, fn, (tok[0], 1) if inc else None))
        for b in reads:
            b.r.append(tok)
            if len(b.r) > 24:
                b.r = self._compact(b.r)
        for b in writes:
            b.w = tok
            b.r = []
        self.n_inst += 1

    @staticmethod
    def _compact(r):
        best = {}
        for k, v in r:
            if v > best.get(k, 0):
                best[k] = v
        return list(best.items())

    def dma(self, eng, fn, anchor, reads=(), writes=(), amt=16):
        if anchor.dsem is None:
            anchor.dsem = "d%d" % len(self.semkeys)
            self.semkeys.append(anchor.dsem)
            self.dma_cnt[anchor.dsem] = 0
        k = anchor.dsem
        waits = self._need(eng, self._deps(reads, writes))
        self.dma_cnt[k] += amt
        tok = (k, self.dma_cnt[k])
        self.ops[eng].append((waits, fn, (k, amt)))
        for b in reads:
            b.r.append(tok)
        for b in writes:
            b.w = tok
            b.r = []
        self.n_inst += 1

    def barrier(self):
        deps = [self._tok(e, self.cnt[e]) for e in ENGS if self.cnt[e] > 0]
        deps += [(k, v) for k, v in self.dma_cnt.items() if v > 0]
        for e in ENGS:
            waits = self._need(e, deps)
            if waits:
                self.ops[e].append((waits, None, None))

    def emit(self):
        nc = self.nc
        with ExitStack() as st:
            sems = {}
            for k in self.semkeys:
                sems[k] = st.enter_context(nc.semaphore("s_" + k.replace("#", "_")))
            block = st.enter_context(nc.Block())

            def run(eobj, name):
                for waits, fn, inc in self.ops[name]:
                    for k, v in waits:
                        eobj.wait_ge(sems[k], v)
                    if fn is None:
                        continue
                    ins = fn(eobj)
                    if inc is not None:
                        ins.then_inc(sems[inc[0]], inc[1])

            @block.tensor
            def _(e):
                run(e, "pe")

            @block.scalar
            def _(e):
                run(e, "act")

            @block.vector
            def _(e):
                run(e, "dve")

            @block.gpsimd
            def _(e):
                run(e, "pool")

            @block.sync
            def _(e):
                run(e, "sp")


class Arena:
    def __init__(self, nc, words):
        self.t = nc.alloc_sbuf_tensor("arena", [128, words], F32)
        self.words = words
        self.top = 0
        self.peak = 0

    def mark(self):
        return self.top

    def release(self, m):
        self.top = m

    def alloc(self, cols, dtype=F32):
        w = cols if dtype == F32 else (cols + 1) // 2
        w = (w + 7) // 8 * 8
        a = self.top
        self.top += w
        self.peak = max(self.peak, self.top)
        assert self.top <= self.words, "SBUF arena overflow %d > %d" % (self.top, self.words)
        ap = self.t[:, a:a + w]
        if dtype != F32:
            ap = ap.bitcast(dtype)
        return ap[:, 0:cols]


def v3(ap, a):
    return ap.rearrange("p (a b) -> p a b", a=a)


class K:
    def __init__(self, nc, dbg=False):
        self.nc = nc
        self.P = Prog(nc)
        self.A = Arena(nc, 52600)
        self.dbg = dbg
        self.psb = []
        for i in range(8):
            t = nc.alloc_psum_tensor("psb%d" % i, [128, 512], F32)
            self.psb.append((t, Buf("ps%d" % i)))
        self.rr = {}

    def ps(self, group, banks):
        i = self.rr.get(group, 0)
        self.rr[group] = i + 1
        t, b = self.psb[banks[i % len(banks)]]
        return t, b

    def mm(self, out, lhsT, rhs, start, stop, reads, writes, inc=True):
        self.P.op("pe", lambda e: e.matmul(out, lhsT=lhsT, rhs=rhs, start=start, stop=stop), reads, writes, inc)

    def tr(self, out, in_, ident, reads, writes, inc=True):
        self.P.op("pe", lambda e: e.transpose(out=out, in_=in_, identity=ident), reads, writes, inc)

    def act(self, out, in_, func, reads, writes, bias=None, scale=None, accum_out=None, eng="act"):
        kw = {}
        if bias is not None:
            kw["bias"] = bias
        if scale is not None:
            kw["scale"] = scale
        if accum_out is not None:
            kw["accum_out"] = accum_out
        self.P.op(eng, lambda e: e.activation(out=out, in_=in_, func=func, **kw), reads, writes)

    def cp(self, eng, out, in_, reads, writes):
        if eng == "act":
            self.P.op("act", lambda e: e.copy(out=out, in_=in_), reads, writes)
        else:
            self.P.op(eng, lambda e: e.tensor_copy(out=out, in_=in_), reads, writes)

    def ts(self, out, in0, s1, s2, op0, op1, reads, writes, eng="dve"):
        if op1 is None:
            self.P.op(eng, lambda e: e.tensor_scalar(out=out, in0=in0, scalar1=s1, scalar2=None, op0=op0), reads, writes)
        else:
            self.P.op(eng, lambda e: e.tensor_scalar(out=out, in0=in0, scalar1=s1, scalar2=s2, op0=op0, op1=op1), reads, writes)

    def stt(self, out, in0, scalar, in1, op0, op1, reads, writes, eng="dve"):
        self.P.op(eng, lambda e: e.scalar_tensor_tensor(out=out, in0=in0, scalar=scalar, in1=in1, op0=op0, op1=op1), reads, writes)

    def tt(self, out, in0, in1, op, reads, writes, eng="dve"):
        self.P.op(eng, lambda e: e.tensor_tensor(out=out, in0=in0, in1=in1, op=op), reads, writes)

    def recip(self, out, in_, reads, writes):
        self.P.op("dve", lambda e: e.reciprocal(out=out, in_=in_), reads, writes)

    def memset(self, ap, val, writes, eng="dve"):
        self.P.op(eng, lambda e: e.memset(ap, val), (), writes)

    def ld(self, out, in_, buf, eng="sp", reads=()):
        self.P.dma(eng, lambda e: e.dma_start(out=out, in_=in_), buf, reads=reads, writes=[buf])

    def st(self, out, in_, buf, dbuf, eng="sp"):
        self.P.dma(eng, lambda e: e.dma_start(out=out, in_=in_), buf, reads=[buf], writes=[dbuf])

    def ldw(self, dst3, src, kc, buf, eng="pool"):
        s3 = src.rearrange("(kc p) n -> p kc n", p=128)
        for k in range(kc):
            o = dst3[:, k, :]
            i = s3[:, k, :]
            self.P.dma(eng, lambda e, o=o, i=i: e.dma_start(out=o, in_=i), buf, writes=[buf])

    def rstd(self, ss, inv_n, tmp, out, rb, wb):
        self.act(tmp, ss, AF.Ln, list(rb) + [self.b_eps], wb, bias=self.eps_ap, scale=inv_n)
        self.act(out, tmp, AF.Exp, wb, wb, scale=-0.5)


def build_program(dbg=False, stop=None, NT=128, dummy=None, NE=32):
    nc = bass.Bass("TRN2", target_bir_lowering=False)
    k = K(nc, dbg)
    P, A = k.P, k.A

    def din(name, shape, dt=F32):
        return nc.dram_tensor(name, list(shape), dt, kind="ExternalInput").ap()

    x_shift = din("x_shift", [NT * 128, 1024])
    x_own = din("x_own", [16 * 128, 1024])
    p_own = din("p_own", [16 * 128, 256])
    g_mix = din("g_mix", [1, 1024]); g_ffn = din("g_ffn", [1, 1024]); g_ple = din("g_ple", [1, 1024]); g_fin = din("g_fin", [1, 1024])
    g_ml = din("g_ml", [1, 512])
    w_kv = [din("w_kv%d" % h, [1024, 512]) for h in range(2)]
    w_q = [din("w_q%d" % h, [1024, 256]) for h in range(2)]
    w_ms = din("w_ms", [1024, 1032])
    w_mo = din("w_mo", [1024, 1024])
    w_g = din("w_g", [1024, 2048])
    b_gate = din("b_gate", [1, 2048])
    conv_wb = din("conv_wb", [128, 40])
    ifb = din("ifb", [1, 8])
    w_ua = din("w_ua", [512, 1024]); w_um = din("w_um", [512, 1024]); w_out = din("w_out", [1024, 1024])
    w_r = din("w_r", [1024, 36]); b_r = din("b_r", [1, 36])
    c_f32 = din("c_f32", [128, 3 * 128])
    c_bf = din("c_bf", [128, 4 * 128], BF16)
    c_e = din("c_e", [64, 64 * 128], BF16)
    c_core = din("c_core", [128, 130])
    c_gmask = din("c_gmask", [1, 16 * 64])
    out_own = nc.dram_tensor("out_own", [16 * 128, 1024], F32, kind="ExternalOutput").ap()
    okind = "ExternalOutput" if dbg else "Internal"
    s_xnT = nc.dram_tensor("s_xnT", [(NT + 16) * 128, 1024], BF16).ap()
    s_ya = nc.dram_tensor("s_ya", [16 * 128, 512], BF16, kind=okind).ap()
    s_ym = nc.dram_tensor("s_ym", [16 * 128, 512], BF16, kind=okind).ap()
    d_xnT = [Buf("dxnT%d" % i) for i in range(NT + 16)]
    d_ya = [Buf("dya%d" % i) for i in range(16)]
    d_ym = [Buf("dym%d" % i) for i in range(16)]
    d_out = Buf("dout")

    cf = A.alloc(384); b_cf = Buf("cf")
    cb = A.alloc(512, BF16); b_cb = Buf("cb")
    cc = A.alloc(130); b_cc = Buf("cc")
    epsb = A.alloc(1); b_eps = Buf("eps")
    k.ld(cf, c_f32, b_cf); k.ld(cb, c_bf, b_cb); k.ld(cc, c_core, b_cc)
    k.memset(epsb, EPS, [b_eps])
    k.eps_ap = epsb[:, 0:1]; k.b_eps = b_eps
    NUT, NONES, ID32 = cf[:, 0:128], cf[:, 128:256], cf[:, 256:384]
    IDB, UTB, CM = cb[:, 0:128], cb[:, 128:256], [cb[:, 256:384], cb[:, 384:512]]
    VALID, PAR, NPAR = cc[:, 0:128], cc[:, 128:129], cc[:, 129:130]
    junk = A.alloc(1024, BF16); b_junk = Buf("junk")
    st4 = [A.alloc(4) for _ in range(2)]; b_st4 = [Buf("st%d" % i) for i in range(2)]
    base_mark = A.mark()

    def bc_load(dst, src_row, buf):
        k.ld(dst, src_row.partition_broadcast(128), buf)

    gb = A.alloc(1024); b_gb = Buf("gb")
    bc_load(gb, g_mix, b_gb)
    xt = [A.alloc(1024) for _ in range(2)]; b_xt = [Buf("xt%d" % i) for i in range(2)]
    xn = [A.alloc(1024, BF16) for _ in range(2)]; b_xn = [Buf("xn%d" % i) for i in range(2)]
    xT_s = [A.alloc(1024, BF16) for _ in range(2)]; b_xTs = [Buf("xTs%d" % i) for i in range(2)]

    def norm_tile(src_ap, sl, gbt, b_gbt, dst_bf, b_dst):
        s = st4[sl]; bs = b_st4[sl]
        k.act(junk, src_ap[0], AF.Square, [src_ap[1]], [b_junk, bs], accum_out=s[:, 0:1])
        k.rstd(s[:, 0:1], 1.0 / 1024, s[:, 1:2], s[:, 2:3], [bs], [bs])
        k.stt(dst_bf, src_ap[0], s[:, 2:3], gbt, ALU.mult, ALU.mult, [src_ap[1], bs, b_gbt], [b_dst])

    def transpose_to(dst_bf3, b_dst, src_bf, b_src, nchunk, grp="misc", banks=(5, 6, 7), eng="act"):
        pt, bp = k.ps(grp, banks)
        ptb = pt[:, :].bitcast(BF16)
        for c in range(nchunk):
            k.tr(ptb[:, c * 128:(c + 1) * 128], src_bf[:, c * 128:(c + 1) * 128], IDB, [b_src, b_cb], [bp], inc=(c == nchunk - 1))
        k.cp(eng, dst_bf3, ptb[:, 0:nchunk * 128], [bp], [b_dst])

    for T in range(NT + 16):
        sl = T % 2
        src = x_shift[T * 128:(T + 1) * 128, :] if T < NT else x_own[(T - NT) * 128:(T - NT + 1) * 128, :]
        k.ld(xt[sl], src, b_xt[sl])
        norm_tile((xt[sl], b_xt[sl]), sl, gb, b_gb, xn[sl], b_xn[sl])
        transpose_to(xT_s[sl], b_xTs[sl], xn[sl], b_xn[sl], 8)
        k.st(s_xnT[T * 128:(T + 1) * 128, :], xT_s[sl], b_xTs[sl], d_xnT[T], eng="act")
    P.barrier()
    A.release(base_mark)

    wms = A.alloc(8 * 1032, BF16); b_wms = Buf("wms"); wms3 = v3(wms, 8)
    wmo = A.alloc(8 * 1024, BF16); b_wmo = Buf("wmo"); wmo3 = v3(wmo, 8)
    k.ldw(wms3, w_ms, 8, b_wms); k.ldw(wmo3, w_mo, 8, b_wmo)
    cwb = A.alloc(40); b_cwb = Buf("cwb"); k.ld(cwb, conv_wb, b_cwb)
    ifbb = A.alloc(8); b_ifbb = Buf("ifbb"); bc_load(ifbb, ifb, b_ifbb)
    gml = A.alloc(512); b_gml = Buf("gml"); bc_load(gml, g_ml, b_gml)
    xT = [A.alloc(1024, BF16) for _ in range(2)]; b_xT = [Buf("xT%d" % i) for i in range(2)]
    C32 = A.alloc(4 * 129); b_C32 = [Buf("C32_%d" % h) for h in range(4)]; C32v = v3(C32, 4)
    Cb = A.alloc(4 * 130, BF16); b_Cb = [Buf("Cb_%d" % h) for h in range(4)]; Cbv = v3(Cb, 4)
    k.memset(C32, 0.0, b_C32); k.memset(Cb, 0.0, b_Cb)
    kpre = [A.alloc(132) for _ in range(4)]; b_kpre = [Buf("kpre%d" % h) for h in range(4)]
    qpre = [A.alloc(132) for _ in range(4)]; b_qpre = [Buf("qpre%d" % h) for h in range(4)]
    for h in range(4):
        k.memset(kpre[h], 0.0, [b_kpre[h]]); k.memset(qpre[h], 0.0, [b_qpre[h]])
    NSL = 4
    def mk(n, cols, dt=F32, pre=""):
        return [A.alloc(cols, dt) for _ in range(n)], [Buf(pre + str(i)) for i in range(n)]
    lif, b_lif = mk(2, 8, pre="lif"); gt, b_gt = mk(2, 24, pre="gt")
    ck, b_ck = mk(NSL, 128, pre="ck"); ex, b_ex = mk(NSL, 128, pre="ex")
    kTb, b_kTb = mk(NSL, 128, BF16, "kTb"); qTb, b_qTb = mk(NSL, 128, BF16, "qTb")
    ktok, b_ktok = mk(NSL, 128, BF16, "ktok"); vp, b_vp = mk(NSL, 130, BF16, "vp")
    tmpc, b_tmpc = mk(NSL, 129, pre="tmpc"); Sm, b_Sm = mk(NSL, 128, BF16, "Sm")
    hc, b_hc = mk(NSL, 128, pre="hc"); sm8, b_sm8 = mk(NSL, 8, pre="sm8")
    sig2, b_sig2 = mk(2, 512, pre="sig")
    ympos = [A.alloc(512, BF16) for _ in range(2)]; b_ympos = [Buf("ympos%d" % i) for i in range(2)]
    ymd = A.alloc(512); b_ymd = Buf("ymd")
    ymo = [A.alloc(512, BF16) for _ in range(2)]; b_ymo = [Buf("ymo%d" % i) for i in range(2)]

    ckq, b_ckq = mk(NSL, 128, pre="ckq")
    pcs = [(k.psb[6 + i][0][:, 0:129], Buf("pcs%d" % i)) for i in range(2)]

    def conv_silu(pre, b_pre, hq, out_bf, b_out, c, bcx):
        w0 = cwb[:, hq * 4:hq * 4 + 1]
        k.ts(c, pre[:, 0:128], w0, cwb[:, 32 + hq:33 + hq], ALU.mult, ALU.add, [b_pre, b_cwb], [bcx])
        for tap in range(1, 4):
            k.stt(c, pre[:, tap:tap + 128], cwb[:, hq * 4 + tap:hq * 4 + tap + 1], c, ALU.mult, ALU.add, [b_pre, b_cwb, bcx], [bcx])
        k.cp("pool", pre[:, 0:3], pre[:, 128:131], [b_pre], [b_pre])
        k.act(out_bf, c, AF.Silu, [bcx], [b_out])

    def pm_prologue(T):
        sl = T % 2
        x3 = v3(xT[sl], 8); bx = b_xT[sl]
        if T + 1 < NT:
            k.ld(xT[(T + 1) % 2], s_xnT[(T + 1) * 128:(T + 2) * 128, :], b_xT[(T + 1) % 2], reads=[d_xnT[T + 1]])
        pos = T % 8
        outp = pos >= 6
        pv, bpv = k.ps("pmv", (0, 1))
        for kc in range(8):
            k.mm(pv[:, 0:512], x3[:, kc, :], wms3[:, kc, 512:1024], kc == 0, kc == 7, [bx, b_wms], [bpv], inc=(kc == 7))
        pif, bpif = k.ps("pmisc", (5,))
        for kc in range(8):
            k.mm(pif[:, 0:8], x3[:, kc, :], wms3[:, kc, 1024:1032], kc == 0, kc == 7, [bx, b_wms], [bpif], inc=(kc == 7))
        L = lif[sl]; bL = b_lif[sl]; G = gt[sl]; bG = b_gt[sl]
        k.tt(L, pif[:, 0:8], ifbb, ALU.add, [bpif, b_ifbb], [bL])
        k.act(G[:, 0:4], L[:, 4:8], AF.Exp, [bL], [bG], scale=-1.0)
        k.act(G[:, 0:4], G[:, 0:4], AF.Ln, [bG], [bG], bias=1.0)
        pb, bpb = k.ps("pmisc", (5,))
        k.mm(pb[:, 0:4], NUT, G[:, 0:4], True, True, [b_cf, bG], [bpb], inc=False)
        k.mm(pb[:, 4:8], NONES, G[:, 0:4], True, True, [b_cf, bG], [bpb])
        k.act(G[:, 8:16], pb[:, 0:8], AF.Exp, [bpb], [bG])
        k.tt(G[:, 16:20], L[:, 0:4], pb[:, 0:4], ALU.subtract, [bL, bpb], [bG])
        k.act(G[:, 4:8], G[:, 16:20], AF.Exp, [bG], [bG])
        k.ts(G[:, 4:8], G[:, 4:8], VALID[:, T:T + 1], 128 ** -0.5, ALU.mult, ALU.mult, [bG, b_cc], [bG])
        if not outp:
            k.tt(G[:, 20:24], G[:, 4:8], G[:, 12:16], ALU.mult, [bG], [bG])
        sig = b_sig = None
        if outp:
            sig = sig2[pos - 6]; b_sig = b_sig2[pos - 6]
            po, bpo = k.ps("pmk", (2, 3, 4))
            for kc in range(8):
                k.mm(po[:, 0:512], x3[:, kc, :], wmo3[:, kc, 512:1024], kc == 0, kc == 7, [bx, b_wmo], [bpo], inc=(kc == 7))
            k.act(sig, po[:, 0:512], AF.Exp, [bpo], [b_sig], scale=-1.0)
            k.ts(sig, sig, 1.0, None, ALU.add, None, [b_sig], [b_sig])
            k.recip(sig, sig, [b_sig], [b_sig])
        return (T, x3, bx, pos, outp, pv, bpv, G, bG, sig, b_sig)

    def pm_a1(ctx, h):
        T, x3, bx, pos, outp, pv, bpv, G, bG, sig, b_sig = ctx
        pk, bpk = k.ps("pmk", (2, 3, 4))
        for kc in range(8):
            k.mm(pk[:, 0:128], wms3[:, kc, 128 * h:128 * h + 128], x3[:, kc, :], kc == 0, kc == 7, [bx, b_wms], [bpk], inc=(kc == 7))
        k.cp("act", kpre[h][:, 3:131], pk[:, 0:128], [bpk], [b_kpre[h]])
        if pos >= 5:
            pq, bpq = k.ps("pmk", (2, 3, 4))
            for kc in range(8):
                k.mm(pq[:, 0:128], wmo3[:, kc, 128 * h:128 * h + 128], x3[:, kc, :], kc == 0, kc == 7, [bx, b_wmo], [bpq], inc=(kc == 7))
            k.cp("act", qpre[h][:, 3:131], pq[:, 0:128], [bpq], [b_qpre[h]])

    def pm_a2(ctx, h, hs):
        T, x3, bx, pos, outp, pv, bpv, G, bG, sig, b_sig = ctx
        conv_silu(kpre[h], b_kpre[h], 4 + h, kTb[hs], b_kTb[hs], ck[hs], b_ck[hs])
        if outp:
            conv_silu(qpre[h], b_qpre[h], h, qTb[hs], b_qTb[hs], ckq[hs], b_ckq[hs])
        elif pos == 5:
            k.cp("pool", qpre[h][:, 0:3], qpre[h][:, 128:131], [b_qpre[h]], [b_qpre[h]])
        V = vp[hs]; bV = b_vp[hs]
        gc = (4 + h) if outp else (20 + h)
        k.ts(V[:, 0:128], pv[:, 128 * h:128 * h + 128], G[:, gc:gc + 1], None, ALU.mult, None, [bpv, bG], [bV])
        k.cp("pool", V[:, 128:129], G[:, gc:gc + 1], [bG], [bV])

    def pm_bpe(ctx, h, hs, pcslot):
        T, x3, bx, pos, outp, pv, bpv, G, bG, sig, b_sig = ctx
        V = vp[hs]; bV = b_vp[hs]
        ptk, bptk = k.ps("pmisc", (5,))
        ptkb = ptk[:, :].bitcast(BF16)
        k.tr(ptkb[:, 0:128], kTb[hs], IDB, [b_kTb[hs], b_cb], [bptk])
        k.cp("act", ktok[hs], ptkb[:, 0:128], [bptk], [b_ktok[hs]])
        if outp:
            pss, bpss = k.ps("pmk", (2, 3, 4))
            k.mm(pss[:, 0:128], kTb[hs], qTb[hs], True, True, [b_kTb[hs], b_qTb[hs]], [bpss])
            k.tt(Sm[hs], pss[:, 0:128], UTB, ALU.mult, [bpss, b_cb], [b_Sm[hs]])
            pn, bpn = k.ps("pmk", (2, 3, 4))
            k.mm(pn[:, 0:129], Sm[hs], V[:, 0:129], True, False, [b_Sm[hs], bV], [bpn], inc=False)
            k.mm(pn[:, 0:129], qTb[hs], Cbv[:, h, 0:129], False, True, [b_qTb[hs], b_Cb[h]], [bpn])
            s8 = sm8[hs]; bs8 = b_sm8[hs]
            et = G[:, 8 + h:9 + h]
            k.tt(s8[:, 0:1], pn[:, 128:129], et, ALU.mult, [bpn, bG], [bs8])
            k.stt(s8[:, 1:2], s8[:, 0:1], -1.0, s8[:, 0:1], ALU.mult, ALU.max, [bs8], [bs8])
            k.ts(s8[:, 1:2], s8[:, 1:2], 1.0, None, ALU.max, None, [bs8], [bs8])
            k.recip(s8[:, 2:3], s8[:, 1:2], [bs8], [bs8])
            k.tt(s8[:, 3:4], s8[:, 2:3], et, ALU.mult, [bs8, bG], [bs8])
            H = hc[hs]; bH = b_hc[hs]
            k.stt(H, pn[:, 0:128], s8[:, 3:4], sig[:, 128 * h:128 * h + 128], ALU.mult, ALU.mult, [bpn, bs8, b_sig], [bH])
            k.act(ex[hs], H, AF.Square, [bH], [b_ex[hs], bs8], accum_out=s8[:, 4:5])
            k.rstd(s8[:, 4:5], 1.0 / 128, s8[:, 5:6], s8[:, 6:7], [bs8], [bs8])
            k.stt(ympos[pos - 6][:, 128 * h:128 * h + 128], H, s8[:, 6:7], gml[:, 128 * h:128 * h + 128], ALU.mult, ALU.mult,
                  [bH, bs8, b_gml], [b_ympos[pos - 6]])
        pc, bpc = pcslot
        k.mm(pc[:, 0:129], ktok[hs], V[:, 0:129], True, True, [b_ktok[hs], bV], [bpc])
        if h == 3 and pos == 7:
            j = T // 8
            k.tt(ymd, ympos[1], ympos[0], ALU.subtract, b_ympos, [b_ymd])
            yo = ymo[j % 2]; byo = b_ymo[j % 2]
            k.stt(yo, ymd, PAR, ympos[0], ALU.mult, ALU.add, [b_ymd, b_cc, b_ympos[0]], [byo])
            k.st(s_ym[j * 128:(j + 1) * 128, :], yo, byo, d_ym[j])

    def pm_bdve(ctx, h, hs, pcslot):
        T, x3, bx, pos, outp, pv, bpv, G, bG, sig, b_sig = ctx
        pc, bpc = pcslot
        if outp:
            tc_, btc = tmpc[hs], b_tmpc[hs]
            k.tt(tc_, pc[:, 0:129], C32v[:, h, :], ALU.add, [bpc, b_C32[h]], [btc])
            k.ts(C32v[:, h, :], tc_, G[:, 12 + h:13 + h], None, ALU.mult, None, [btc, bG], [b_C32[h]])
            if pos == 6:
                k.ts(Cbv[:, h, 0:129], tc_, G[:, 12 + h:13 + h], None, ALU.mult, None, [btc, bG], [b_Cb[h]])
        else:
            k.stt(C32v[:, h, :], C32v[:, h, :], G[:, 12 + h:13 + h], pc[:, 0:129], ALU.mult, ALU.add, [b_C32[h], bG, bpc], [b_C32[h]])
            if pos == 5:
                k.cp("act", Cbv[:, h, 0:129], C32v[:, h, :], [b_C32[h]], [b_Cb[h]])

    k.ld(xT[0], s_xnT[0:128, :], b_xT[0], reads=[d_xnT[0]])
    items = [(T, h) for T in range(NT) for h in range(4)]
    NI = len(items)
    ctxs = {}
    for s in range(-3, NI):
        i = s + 3
        if 0 <= i < NI:
            T, h = items[i]
            if h == 0:
                ctxs[T] = pm_prologue(T)
            pm_a1(ctxs[T], h)
        i = s + 1
        if 0 <= i < NI:
            T, h = items[i]
            pm_bpe(ctxs[T], h, i % NSL, pcs[i % 2])
        i = s + 2
        if 0 <= i < NI:
            T, h = items[i]
            pm_a2(ctxs[T], h, i % NSL)
        i = s
        if 0 <= i < NI:
            T, h = items[i]
            pm_bdve(ctxs[T], h, i % NSL, pcs[i % 2])
    P.barrier()
    A.release(base_mark)
    if stop == "PM":
        P.emit(); print("instructions:", P.n_inst, {e: P.cnt[e] for e in ENGS}); return nc

    KT = A.alloc(2 * 16384, BF16); KT3 = v3(KT, 2)
    b_KT = [Buf("KT%d" % t) for t in range(NT)]
    V1 = A.alloc(NT * 260, BF16); V14 = V1.rearrange("p (t h d) -> p t h d", t=NT, h=4)
    b_V1 = [Buf("V1_%d" % t) for t in range(NT)]
    Eb = A.alloc(64 * 128, BF16); b_Eb = Buf("Eb"); Eb3 = v3(Eb, 64)
    k.ld(Eb[0:64, :], c_e, b_Eb)
    gmask = A.alloc(1024); b_gmask = Buf("gmask"); bc_load(gmask, c_gmask, b_gmask); gmask3 = v3(gmask, 16)
    wkv = A.alloc(8 * 512, BF16); b_wkv = Buf("wkv"); wkv3 = v3(wkv, 8)
    wq = A.alloc(8 * 256, BF16); b_wq = Buf("wq"); wq3 = v3(wq, 8)
    xT = [A.alloc(1024, BF16) for _ in range(2)]; b_xT = [Buf("axT%d" % i) for i in range(2)]
    xTo = A.alloc(1024, BF16); b_xTo = Buf("xTo")
    ksum = A.alloc(2 * NT); b_ksum = Buf("ksum"); ksum3 = v3(ksum, 2)
    kmT = A.alloc(2 * 64); b_kmT = Buf("kmT"); kmT3 = v3(kmT, 2)
    QTs = A.alloc(512, BF16); b_QTs = Buf("QTs"); QTs4 = QTs.rearrange("p (a s q) -> p a s q", a=2, s=2)
    QT32 = A.alloc(512); b_QT32 = Buf("QT32"); QT324 = QT32.rearrange("p (a s q) -> p a s q", a=2, s=2)
    k.memset(QTs, 0.0, [b_QTs]); k.memset(QT32, 0.0, [b_QT32])
    gm_, b_gm = mk(4, 64, pre="gm"); t8, b_t8 = mk(4, 16, pre="t8")
    b01, b_b01 = mk(4, 64, BF16, "b01"); BT, b_BT = mk(4, 128, BF16, "BT")
    PT, b_PT = mk(3, 512, BF16, "PT")
    yat = [A.alloc(512, BF16) for _ in range(2)]; b_yat = [Buf("yat%d" % i) for i in range(2)]
    r1, b_r1 = mk(2, 2, pre="r1")
    hcount = [0]; gcount = [0]

    for hp in range(2):
        if hp == 1:
            P.barrier()
        k.ldw(wkv3, w_kv[hp], 8, b_wkv); k.ldw(wq3, w_q[hp], 8, b_wq)
        k.memset(kmT, 0.0, [b_kmT]); k.memset(ksum, 0.0, [b_ksum])
        if hp == 0:
            for t0 in range(0, NT, 16):
                k.memset(V1[:, t0 * 260:(t0 + 16) * 260], 1.0, b_V1[t0:t0 + 16])
        k.ld(xT[0], s_xnT[0:128, :], b_xT[0], reads=[d_xnT[0]])
        for T in range(NT):
            sl = T % 2
            x3 = v3(xT[sl], 8); bx = b_xT[sl]
            if T + 1 < NT:
                k.ld(xT[(T + 1) % 2], s_xnT[(T + 1) * 128:(T + 2) * 128, :], b_xT[(T + 1) % 2], reads=[d_xnT[T + 1]])
            for p in range(2):
                pk, bpk = k.ps("paproj", (5, 6, 7))
                for kc in range(8):
                    k.mm(pk[:, 0:128], wkv3[:, kc, 128 * p:128 * p + 128], x3[:, kc, :], kc == 0, kc == 7, [bx, b_wkv], [bpk], inc=(kc == 7))
                k.act(KT3[:, p, T * 128:(T + 1) * 128], pk[:, 0:128], AF.Identity, [bpk], [b_KT[T], b_ksum], accum_out=ksum3[:, p, T:T + 1])
            pv, bpv = k.ps("paproj", (5, 6, 7))
            for kc in range(8):
                k.mm(pv[:, 0:256], x3[:, kc, :], wkv3[:, kc, 256:512], kc == 0, kc == 7, [bx, b_wkv], [bpv], inc=(kc == 7))
            k.cp("dve", V14[:, T, :, 0:64], pv[:, 0:256].rearrange("p (h d) -> p h d", h=4), [bpv], [b_V1[T]])
            if T % 2 == 1:
                n = T // 2
                k.tt(kmT3[:, :, n:n + 1], ksum3[:, :, T - 1:T], ksum3[:, :, T:T + 1], ALU.add, [b_ksum], [b_kmT])
            if T % 8 != 7:
                continue
            j = T // 8
            k.ld(xTo, s_xnT[(NT + j) * 128:(NT + j + 1) * 128, :], b_xTo, reads=[d_xnT[NT + j]])
            xo3 = v3(xTo, 8)
            for p in range(2):
                pq, bpq = k.ps("paproj", (5, 6, 7))
                for kc in range(8):
                    k.mm(pq[:, 0:128], wq3[:, kc, 128 * p:128 * p + 128], xo3[:, kc, :], kc == 0, kc == 7, [b_xTo, b_wq], [bpq], inc=(kc == 7))
                for s_ in range(2):
                    k.ts(QTs4[64 * s_:64 * s_ + 64, p, s_, :], pq[64 * s_:64 * s_ + 64, 0:128], 0.125, None, ALU.mult, None, [bpq], [b_QTs])
                    k.cp("dve", QT324[64 * s_:64 * s_ + 64, p, s_, :], pq[64 * s_:64 * s_ + 64, 0:128], [bpq], [b_QT32])
            ya_t = yat[j % 2]; b_ya_t = b_yat[j % 2]
            nkt = 8 * j + 8
            def prologue_a(hl):
                p, s = hl // 2, hl % 2
                pg, bpg = k.ps("paproj", (5, 6, 7))
                k.mm(pg[:, 0:64], QT324[:, p, s, :], kmT3[:, p, :], True, True, [b_QT32, b_kmT], [bpg])
                g_ = gm_[hl]; bg_ = b_gm[hl]; t_ = t8[hl]; bt_ = b_t8[hl]
                k.tt(g_, pg[:, 0:64], gmask3[:, j, :], ALU.add, [bpg, b_gmask], [bg_])
                P.op("dve", lambda e, o=t_[:, 0:8], i=g_: e.max(out=o, in_=i), [bg_], [bt_])
                k.ts(t_[:, 8:9], t_[:, 2:3], -1e29, None, ALU.max, None, [bt_], [bt_])
                k.ts(b01[hl], g_, t_[:, 8:9], 1.0, ALU.is_ge, ALU.subtract, [bg_, bt_], [b_b01[hl]])

            def prologue_b(hl):
                pbt, bpbt = k.ps("paproj", (5, 6, 7))
                pbtb = pbt[:, :].bitcast(BF16)
                k.tr(pbtb[0:64, 0:128], b01[hl], IDB, [b_b01[hl], b_cb], [bpbt])
                k.cp("dve", BT[hl][0:64, :], pbtb[0:64, 0:128], [bpbt], [b_BT[hl]])

            def qk_group(hl, g0):
                p, s = hl // 2, hl % 2
                pS, bpS = k.ps("pas", (2, 3, 4))
                for i in range(4):
                    kt = g0 + i
                    col = pS[:, i * 128:(i + 1) * 128]
                    k.mm(col, KT3[:, p, kt * 128:(kt + 1) * 128], QTs4[:, p, s, :], True, False, [b_KT[kt], b_QTs], [bpS], inc=False)
                    if kt < nkt - 2:
                        k.mm(col, Eb3[0:64, kt // 2, :], BT[hl][0:64, :], False, True, [b_Eb, b_BT[hl]], [bpS], inc=(i == 3))
                    else:
                        k.mm(col, IDB, CM[kt - (nkt - 2)], False, True, [b_cb], [bpS], inc=(i == 3))
                return pS, bpS

            def pv_group(hl, g0, pS, bpS, po, bpo):
                pt_ = PT[gcount[0] % 3]; bpt_ = b_PT[gcount[0] % 3]; gcount[0] += 1
                k.act(pt_, pS[:, 0:512], AF.Exp, [bpS], [bpt_])
                for i in range(4):
                    kt = g0 + i
                    k.mm(po[:, 0:65], pt_[:, i * 128:(i + 1) * 128], V14[:, kt, hl, :], kt == 0, kt == nkt - 1, [bpt_, b_V1[kt]], [bpo],
                         inc=(i == 3))

            groups = list(range(0, nkt, 4))
            prologue_a(0); prologue_b(0)
            for hl in range(4):
                po, bpo = k.ps("pao", (0, 1))
                cur = qk_group(hl, groups[0])
                for gi, g0 in enumerate(groups):
                    nxt = qk_group(hl, groups[gi + 1]) if gi + 1 < len(groups) else None
                    if hl < 3 and gi == 0:
                        prologue_a(hl + 1)
                    if hl < 3 and gi == min(1, len(groups) - 1):
                        prologue_b(hl + 1)
                    pv_group(hl, g0, cur[0], cur[1], po, bpo)
                    cur = nxt
                r_ = r1[hl % 2]; br_ = b_r1[hl % 2]
                k.recip(r_[:, 0:1], po[:, 64:65], [bpo], [br_])
                k.ts(ya_t[:, 64 * hl:64 * hl + 64], po[:, 0:64], r_[:, 0:1], None, ALU.mult, None, [bpo, br_], [b_ya_t])
            k.st(s_ya[j * 128:(j + 1) * 128, 256 * hp:256 * hp + 256], ya_t[:, 0:256], b_ya_t, d_ya[j])
    P.barrier()
    A.release(base_mark)
    if stop == "PA":
        P.emit(); print("instructions:", P.n_inst, {e: P.cnt[e] for e in ENGS}); return nc

    e_wg = din("e_wg", [NE * 1024, 512]); e_wu = din("e_wu", [NE * 1024, 512]); e_wd = din("e_wd", [NE * 512, 1024])
    w_pg = din("w_pg", [1024, 1024]); w_pp = din("w_pp", [256, 1024])
    h1 = A.alloc(16 * 1024); h13 = v3(h1, 16); b_h1 = [Buf("h1_%d" % j) for j in range(16)]
    hnT = A.alloc(8 * 2048, BF16); hnT3 = v3(hnT, 8); b_hnT = [Buf("hnT%d" % j) for j in range(16)]
    cw = A.alloc(16 * 32); cw3 = v3(cw, 16); b_cw = [Buf("cw%d" % j) for j in range(16)]
    NJ = NT // 8
    if NJ < 16:
        k.memset(hnT, 0.0, b_hnT); k.memset(cw, 0.0, b_cw); k.memset(h1, 0.0, b_h1)
    pb_mark = A.mark()
    wg_ = A.alloc(8 * 2048, BF16); b_wg = Buf("wg"); wg3 = v3(wg_, 8)
    wua = A.alloc(4 * 1024, BF16); b_wua = Buf("wua"); wua3 = v3(wua, 4)
    wum = A.alloc(4 * 1024, BF16); b_wum = Buf("wum"); wum3 = v3(wum, 4)
    wo = A.alloc(8 * 1024, BF16); b_wo = Buf("wo"); wo3 = v3(wo, 8)
    wr = A.alloc(8 * 36); b_wr = Buf("wr"); wr3 = v3(wr, 8)
    k.ldw(wg3, w_g, 8, b_wg); k.ldw(wua3, w_ua, 4, b_wua); k.ldw(wum3, w_um, 4, b_wum); k.ldw(wo3, w_out, 8, b_wo)
    for kc in range(8):
        k.ld(wr3[:, kc, :], w_r[kc * 128:(kc + 1) * 128, :], b_wr)
    bgb = A.alloc(2048, BF16); b_bgb = Buf("bgb")
    br32 = A.alloc(36); b_br32 = Buf("br32"); bc_load(br32, b_r, b_br32)
    ones = A.alloc(128); b_ones = Buf("ones"); k.memset(ones, 1.0, [b_ones])
    onesb = A.alloc(128, BF16); b_onesb = Buf("onesb"); k.memset(onesb, 1.0, [b_onesb])
    gfb = A.alloc(1024); b_gfb = Buf("gfb"); bc_load(gfb, g_ffn, b_gfb)
    xTo = A.alloc(1024, BF16); b_xTo = Buf("bxTo")
    yab = A.alloc(512, BF16); b_yab = Buf("yab"); ymb = A.alloc(512, BF16); b_ymb = Buf("ymb")
    yaT = A.alloc(512, BF16); b_yaT = Buf("yaT"); ymT = A.alloc(512, BF16); b_ymT = Buf("ymT")
    yaT3 = v3(yaT, 4); ymT3 = v3(ymT, 4)
    sga = A.alloc(512); b_sga = Buf("sga"); sgm = A.alloc(512); b_sgm = Buf("sgm")
    mrg = A.alloc(1024, BF16); b_mrg = Buf("mrg"); mrgT = A.alloc(1024, BF16); b_mrgT = Buf("mrgT"); mrgT3 = v3(mrgT, 8)
    hn32 = A.alloc(1024); b_hn32 = Buf("hn32")
    hnhi = A.alloc(1024, BF16); b_hnhi = Buf("hnhi"); hnlo = A.alloc(1024, BF16); b_hnlo = Buf("hnlo")
    hiT = A.alloc(1024, BF16); b_hiT = Buf("hiT"); hiT3 = v3(hiT, 8)
    loT = A.alloc(1024, BF16); b_loT = Buf("loT"); loT3 = v3(loT, 8)
    wrh = A.alloc(8 * 36, BF16); b_wrh = Buf("wrh"); wrh3 = v3(wrh, 8)
    wrl = A.alloc(8 * 36, BF16); b_wrl = Buf("wrl"); wrl3 = v3(wrl, 8)
    k.cp("dve", wrh, wr, [b_wr], [b_wrh])
    k.tt(wrl, wr, wrh, ALU.subtract, [b_wr, b_wrh], [b_wrl])
    Lr = A.alloc(40); b_Lr = Buf("Lr"); rs = A.alloc(16); b_rs = Buf("rs"); elm = A.alloc(32); b_elm = Buf("elm")
    c1 = A.alloc(32); b_c1 = Buf("c1")
    for q4 in range(4):
        k.ld(sga, b_gate[:, 512 * q4:512 * q4 + 512].partition_broadcast(128), b_sga)
        k.cp("dve", bgb[:, 512 * q4:512 * q4 + 512], sga, [b_sga], [b_bgb])

    def sigmoid_from(dst, b_dst, ps_ap, b_ps):
        k.act(dst, ps_ap, AF.Exp, [b_ps], [b_dst], scale=-1.0)
        k.ts(dst, dst, 1.0, None, ALU.add, None, [b_dst], [b_dst])
        k.recip(dst, dst, [b_dst], [b_dst])

    for j in range(NJ):
        k.ld(h13[:, j, :], x_own[j * 128:(j + 1) * 128, :], b_h1[j])
        k.ld(xTo, s_xnT[(NT + j) * 128:(NT + j + 1) * 128, :], b_xTo, reads=[d_xnT[NT + j]])
        k.ld(yab, s_ya[j * 128:(j + 1) * 128, :], b_yab, reads=[d_ya[j]])
        k.ld(ymb, s_ym[j * 128:(j + 1) * 128, :], b_ymb, reads=[d_ym[j]])
        xo3 = v3(xTo, 8)
        transpose_to(yaT, b_yaT, yab, b_yab, 4, grp="pbm", banks=(6, 7), eng="dve")
        transpose_to(ymT, b_ymT, ymb, b_ymb, 4, grp="pbm", banks=(6, 7), eng="dve")
        for b in range(2):
            cs = slice(512 * b, 512 * b + 512)
            pga, bpga = k.ps("pbg", (0, 1, 2))
            for kc in range(8):
                k.mm(pga[:, 0:512], xo3[:, kc, :], wg3[:, kc, 512 * b:512 * b + 512], kc == 0, kc == 7, [b_xTo, b_wg], [bpga], inc=(kc == 7))
            k.tt(sga, pga[:, 0:512], bgb[:, 512 * b:512 * b + 512], ALU.add, [bpga, b_bgb], [b_sga])
            sigmoid_from(sga, b_sga, sga, b_sga)
            pua, bpua = k.ps("pbu", (3, 4, 5))
            for fc in range(4):
                k.mm(pua[:, 0:512], yaT3[:, fc, :], wua3[:, fc, cs], fc == 0, fc == 3, [b_yaT, b_wua], [bpua], inc=(fc == 3))
            k.tt(sga, sga, pua[:, 0:512], ALU.mult, [b_sga, bpua], [b_sga])
            pgm, bpgm = k.ps("pbg", (0, 1, 2))
            for kc in range(8):
                k.mm(pgm[:, 0:512], xo3[:, kc, :], wg3[:, kc, 1024 + 512 * b:1024 + 512 * b + 512], kc == 0, kc == 7, [b_xTo, b_wg], [bpgm], inc=(kc == 7))
            k.tt(sgm, pgm[:, 0:512], bgb[:, 1024 + 512 * b:1024 + 512 * b + 512], ALU.add, [bpgm, b_bgb], [b_sgm])
            sigmoid_from(sgm, b_sgm, sgm, b_sgm)
            pum, bpum = k.ps("pbu", (3, 4, 5))
            for fc in range(4):
                k.mm(pum[:, 0:512], ymT3[:, fc, :], wum3[:, fc, cs], fc == 0, fc == 3, [b_ymT, b_wum], [bpum], inc=(fc == 3))
            k.tt(sgm, sgm, pum[:, 0:512], ALU.mult, [b_sgm, bpum], [b_sgm])
            k.tt(mrg[:, cs], sga, sgm, ALU.add, [b_sga, b_sgm], [b_mrg])
        transpose_to(mrgT, b_mrgT, mrg, b_mrg, 8, grp="pbm", banks=(6, 7), eng="act")
        for b in range(2):
            ph, bph = k.ps("pbg", (0, 1, 2))
            for kc in range(8):
                k.mm(ph[:, 0:512], mrgT3[:, kc, :], wo3[:, kc, 512 * b:512 * b + 512], kc == 0, kc == 7, [b_mrgT, b_wo], [bph], inc=(kc == 7))
            k.tt(h13[:, j, 512 * b:512 * b + 512], h13[:, j, 512 * b:512 * b + 512], ph[:, 0:512], ALU.add, [b_h1[j], bph], [b_h1[j]])
        if stop == "PB1":
            continue
        s = st4[0]; bs = b_st4[0]
        k.act(junk, h13[:, j, :], AF.Square, [b_h1[j]], [b_junk, bs], accum_out=s[:, 0:1])
        k.rstd(s[:, 0:1], 1.0 / 1024, s[:, 1:2], s[:, 2:3], [bs], [bs])
        k.stt(hn32, h13[:, j, :], s[:, 2:3], gfb, ALU.mult, ALU.mult, [b_h1[j], bs, b_gfb], [b_hn32])
        k.cp("dve", hnhi, hn32, [b_hn32], [b_hnhi])
        k.tt(hnlo, hn32, hnhi, ALU.subtract, [b_hn32, b_hnhi], [b_hnlo])
        transpose_to(hiT, b_hiT, hnhi, b_hnhi, 8, grp="pbm", banks=(6, 7), eng="act")
        transpose_to(loT, b_loT, hnlo, b_hnlo, 8, grp="pbm", banks=(6, 7), eng="act")
        k.cp("dve", hnT3[:, :, j * 128:(j + 1) * 128], hiT3, [b_hiT], [b_hnT[j]])
        pr, bpr = k.ps("pbu", (3, 4, 5))
        n_ = 0
        for (aT, baT, w_, bw_) in ((hiT3, b_hiT, wrh3, b_wrh), (hiT3, b_hiT, wrl3, b_wrl), (loT3, b_loT, wrh3, b_wrh)):
            for kc in range(8):
                k.mm(pr[:, 0:36], aT[:, kc, :], w_[:, kc, :], n_ == 0, n_ == 23, [baT, bw_], [bpr], inc=(n_ == 23))
                n_ += 1
        if stop == "PB2":
            continue
        k.tt(Lr[:, 0:36], pr[:, 0:36], br32, ALU.add, [bpr, b_br32], [b_Lr])
        P.op("dve", lambda e, o=rs[:, 0:1], i=Lr[:, 0:4]: e.tensor_reduce(out=o, in_=i, axis=AX.X, op=ALU.max), [b_Lr], [b_rs])
        k.ts(rs[:, 1:2], rs[:, 0:1], -1.0, None, ALU.mult, None, [b_rs], [b_rs])
        k.act(Lr[:, 36:40], Lr[:, 0:4], AF.Exp, [b_Lr, b_rs], [b_Lr, b_rs], bias=rs[:, 1:2], accum_out=rs[:, 2:3])
        k.recip(rs[:, 3:4], rs[:, 2:3], [b_rs], [b_rs])
        k.ts(Lr[:, 36:40], Lr[:, 0:4], rs[:, 0:1], 1.0, ALU.is_equal, ALU.subtract, [b_Lr, b_rs], [b_Lr])
        k.ts(Lr[:, 36:40], Lr[:, 36:40], 1e30, None, ALU.mult, None, [b_Lr], [b_Lr])
        for g in range(4):
            k.ts(elm[:, 8 * g:8 * g + 8], Lr[:, 4 + 8 * g:12 + 8 * g], Lr[:, 36 + g:37 + g], None, ALU.add, None, [b_Lr], [b_elm])
        P.op("dve", lambda e, o=rs[:, 8:16], i=elm: e.max(out=o, in_=i), [b_elm], [b_rs])
        k.tt(rs[:, 4:5], rs[:, 9:10], rs[:, 8:9], ALU.subtract, [b_rs], [b_rs])
        k.act(rs[:, 4:5], rs[:, 4:5], AF.Exp, [b_rs], [b_rs])
        k.ts(rs[:, 4:5], rs[:, 4:5], 1.0, None, ALU.add, None, [b_rs], [b_rs])
        k.recip(rs[:, 5:6], rs[:, 4:5], [b_rs], [b_rs])
        k.tt(rs[:, 6:7], rs[:, 5:6], rs[:, 3:4], ALU.mult, [b_rs], [b_rs])
        k.tt(rs[:, 7:8], rs[:, 3:4], rs[:, 6:7], ALU.subtract, [b_rs], [b_rs])
        k.ts(c1, elm, rs[:, 8:9], rs[:, 6:7], ALU.is_equal, ALU.mult, [b_elm, b_rs], [b_c1])
        k.ts(cw3[:, j, :], elm, rs[:, 9:10], rs[:, 7:8], ALU.is_equal, ALU.mult, [b_elm, b_rs], [b_cw[j]])
        k.tt(cw3[:, j, :], cw3[:, j, :], c1, ALU.add, [b_cw[j], b_c1], [b_cw[j]])
    P.barrier()
    A.release(pb_mark)
    if stop in ("PB", "PB1", "PB2"):
        P.emit(); return nc

    ewg = [A.alloc(8 * 512, BF16) for _ in range(2)]; ewu = [A.alloc(8 * 512, BF16) for _ in range(2)]
    ewd = [A.alloc(4 * 1024, BF16) for _ in range(2)]
    b_ew = [[Buf("ewg%d" % i), Buf("ewu%d" % i), Buf("ewd%d" % i)] for i in range(2)]
    hidT = A.alloc(4 * 2048, BF16); hidT3 = v3(hidT, 4); b_hid = [Buf("hid%d" % t) for t in range(4)]
    sgt = [A.alloc(512, BF16) for _ in range(2)]; b_sgt = [Buf("sgt%d" % i) for i in range(2)]

    def load_expert(e):
        sl = e % 2
        k.ldw(v3(ewg[sl], 8), e_wg[e * 1024:(e + 1) * 1024, :], 8, b_ew[sl][0])
        k.ldw(v3(ewu[sl], 8), e_wu[e * 1024:(e + 1) * 1024, :], 8, b_ew[sl][1])
        k.ldw(v3(ewd[sl], 4), e_wd[e * 512:(e + 1) * 512, :], 4, b_ew[sl][2])

    load_expert(0)
    scnt = [0]
    for e in range(NE):
        sl = e % 2
        if e + 1 < NE:
            load_expert(e + 1)
        g3, u3, d3 = v3(ewg[sl], 8), v3(ewu[sl], 8), v3(ewd[sl], 4)
        bwg, bwu, bwd = b_ew[sl]
        for tg in range((NJ + 3) // 4):
            rb = [b_hnT[4 * tg + i] for i in range(4)]
            for fc in range(4):
                pG, bpG = k.ps("peg", (0, 1, 2, 3))
                for kc in range(8):
                    k.mm(pG[:, 0:512], g3[:, kc, fc * 128:(fc + 1) * 128], hnT3[:, kc, tg * 512:(tg + 1) * 512], kc == 0, kc == 7, rb + [bwg], [bpG],
                         inc=(kc == 7))
                pU, bpU = k.ps("peg", (0, 1, 2, 3))
                for kc in range(8):
                    k.mm(pU[:, 0:512], u3[:, kc, fc * 128:(fc + 1) * 128], hnT3[:, kc, tg * 512:(tg + 1) * 512], kc == 0, kc == 7, rb + [bwu], [bpU],
                         inc=(kc == 7))
                sg = sgt[scnt[0] % 2]; bsg = b_sgt[scnt[0] % 2]; scnt[0] += 1
                k.act(sg, pG[:, 0:512], AF.Silu, [bpG], [bsg])
                k.tt(hidT3[:, fc, tg * 512:(tg + 1) * 512], sg, pU[:, 0:512], ALU.mult, [bsg, bpU], [b_hid[tg]])
        for jt in range(NJ):
            for b in range(2):
                py, bpy = k.ps("pey", (4, 5, 6, 7))
                for fc in range(4):
                    k.mm(py[:, 0:512], hidT3[:, fc, jt * 128:(jt + 1) * 128], d3[:, fc, 512 * b:512 * b + 512], fc == 0, fc == 3, [b_hid[jt // 4], bwd], [bpy],
                         inc=(fc == 3))
                hsl = h13[:, jt, 512 * b:512 * b + 512]
                k.stt(hsl, py[:, 0:512], cw3[:, jt, e:e + 1], hsl, ALU.mult, ALU.add, [bpy, b_cw[jt], b_h1[jt]], [b_h1[jt]])
    P.barrier()
    A.release(pb_mark)
    if stop == "PE":
        P.emit(); return nc

    wpg = A.alloc(8 * 1024, BF16); b_wpg = Buf("wpg"); wpg3 = v3(wpg, 8)
    wpp = A.alloc(2 * 1024, BF16); b_wpp = Buf("wpp"); wpp3 = v3(wpp, 2)
    k.ldw(wpg3, w_pg, 8, b_wpg); k.ldw(wpp3, w_pp, 2, b_wpp)
    gpb = A.alloc(1024); b_gpb = Buf("gpb"); bc_load(gpb, g_ple, b_gpb)
    gfin = A.alloc(1024); b_gfin = Buf("gfin"); bc_load(gfin, g_fin, b_gfin)
    pn = [A.alloc(1024, BF16) for _ in range(2)]; b_pn = [Buf("pn%d" % i) for i in range(2)]
    pnT = [A.alloc(1024, BF16) for _ in range(2)]; b_pnT = [Buf("pnT%d" % i) for i in range(2)]
    p32 = [A.alloc(256) for _ in range(2)]; b_p32 = [Buf("p32%d" % i) for i in range(2)]
    pbf = [A.alloc(256, BF16) for _ in range(2)]; b_pbf = [Buf("pbf%d" % i) for i in range(2)]
    pT = [A.alloc(256, BF16) for _ in range(2)]; b_pT = [Buf("pT%d" % i) for i in range(2)]
    sgp = [A.alloc(512) for _ in range(2)]; b_sgp = [Buf("sgp%d" % i) for i in range(2)]
    ot = [A.alloc(1024) for _ in range(2)]; b_ot = [Buf("ot%d" % i) for i in range(2)]
    cnt = [0]
    for j in range(NJ):
        sl = j % 2
        k.ld(p32[sl], p_own[j * 128:(j + 1) * 128, :], b_p32[sl])
        k.cp("dve", pbf[sl], p32[sl], [b_p32[sl]], [b_pbf[sl]])
        transpose_to(pT[sl], b_pT[sl], pbf[sl], b_pbf[sl], 2, grp="pfm", banks=(6, 7), eng="act")
        norm_tile((h13[:, j, :], b_h1[j]), sl, gpb, b_gpb, pn[sl], b_pn[sl])
        transpose_to(pnT[sl], b_pnT[sl], pn[sl], b_pn[sl], 8, grp="pfm", banks=(6, 7), eng="act")
        n3 = v3(pnT[sl], 8); t3 = v3(pT[sl], 2)
        for b in range(2):
            pgt, bpgt = k.ps("pfg", (0, 1, 2))
            for kc in range(8):
                k.mm(pgt[:, 0:512], n3[:, kc, :], wpg3[:, kc, 512 * b:512 * b + 512], kc == 0, kc == 7, [b_pnT[sl], b_wpg], [bpgt], inc=(kc == 7))
            sg = sgp[cnt[0] % 2]; bsg = b_sgp[cnt[0] % 2]; cnt[0] += 1
            sigmoid_from(sg, bsg, pgt[:, 0:512], bpgt)
            ppp, bppp = k.ps("pfp", (3, 4, 5))
            for c in range(2):
                k.mm(ppp[:, 0:512], t3[:, c, :], wpp3[:, c, 512 * b:512 * b + 512], c == 0, c == 1, [b_pT[sl], b_wpp], [bppp], inc=(c == 1))
            k.tt(sg, sg, ppp[:, 0:512], ALU.mult, [bsg, bppp], [bsg])
            hsl = h13[:, j, 512 * b:512 * b + 512]
            k.tt(hsl, hsl, sg, ALU.add, [b_h1[j], bsg], [b_h1[j]])
        s = st4[sl]; bs = b_st4[sl]
        k.act(junk, h13[:, j, :], AF.Square, [b_h1[j]], [b_junk, bs], accum_out=s[:, 0:1])
        k.rstd(s[:, 0:1], 1.0 / 1024, s[:, 1:2], s[:, 2:3], [bs], [bs])
        k.stt(ot[sl], h13[:, j, :], s[:, 2:3], gfin, ALU.mult, ALU.mult, [b_h1[j], bs, b_gfin], [b_ot[sl]])
        k.st(out_own[j * 128:(j + 1) * 128, :], ot[sl], b_ot[sl], d_out)
    if dummy is not None:
        dz = A.alloc(8); b_dz = Buf("dz")
        for i in range(dummy[1]):
            if dummy[0] == "pe":
                k.mm(k.psb[0][0][:, 0:2], IDB, IDB[:, 0:2], True, True, [b_cb], [k.psb[0][1]], inc=(i % 64 == 63))
            else:
                k.memset(dz, float(i % 7), [b_dz], eng=dummy[0])
    P.barrier()
    P.emit()
    print("per-engine stream lengths (instr + waits):", {e: len(P.ops[e]) + sum(len(w) for w, _, _ in P.ops[e]) for e in ENGS})
    print("instructions:", P.n_inst, "sems:", len(P.semkeys), "sbuf peak words:", A.peak,
          "counts:", {e: P.cnt[e] for e in ENGS})
    return nc


def _consts():
    bf = ml_dtypes.bfloat16
    s = np.arange(128)[:, None]; t = np.arange(128)[None, :]
    ut = (s <= t).astype(np.float32)
    c_f32 = np.concatenate([-ut, -np.ones((128, 128), np.float32), np.eye(128, dtype=np.float32)], axis=1)
    c_e = np.zeros((64, 64, 128), np.float32)
    for n in range(64):
        c_e[n, n, :] = NEGB
    return c_f32, ut, c_e.reshape(64, 64 * 128).astype(bf)


def make_in_maps(inp):
    bf = ml_dtypes.bfloat16
    f = lambda a: np.ascontiguousarray(np.asarray(a, dtype=np.float32))
    x = f(inp["x"])[0]; p = f(inp["p"])[0, 0]
    w_in = f(inp["w_in"])[0]
    o = np.cumsum([0, 512, 512, 512, 512, 512, 512, 512, 4, 4, 1024, 1024])
    aq, ak, av, mq, mk_, mv, mo, mi, mf, ga, gm = [w_in[:, o[i]:o[i + 1]] for i in range(11)]
    shared = {
        "g_mix": f(inp["mix_norm_g"]), "g_ffn": f(inp["ffn_norm_g"]), "g_ple": f(inp["ple_norm_g"]),
        "g_fin": f(inp["final_norm_g"]).reshape(1, 1024), "g_ml": f(inp["mlstm_norm_g"]),
        "w_kv0": f(np.concatenate([ak[:, 0:256], av[:, 0:256]], 1)), "w_kv1": f(np.concatenate([ak[:, 256:512], av[:, 256:512]], 1)),
        "w_q0": f(aq[:, 0:256]), "w_q1": f(aq[:, 256:512]),
        "w_ms": f(np.concatenate([mk_, mv, mi, mf], 1)), "w_mo": f(np.concatenate([mq, mo], 1)),
        "w_g": f(np.concatenate([ga, gm], 1)), "b_gate": f(inp["b_gate"]),
        "ifb": f(inp["mlstm_if_b"]),
        "w_ua": f(inp["w_up_attn"])[0], "w_um": f(inp["w_up_mlstm"])[0], "w_out": f(inp["w_out"])[0],
        "w_r": f(np.concatenate([f(inp["router_group_w"])[0], f(inp["router_expert_w"])[0]], 1)),
        "b_r": f(np.concatenate([f(inp["router_group_b"]), f(inp["router_expert_b"])], 1)),
        "e_wg": f(inp["expert_w_gate"])[0].reshape(32 * 1024, 512), "e_wu": f(inp["expert_w_up"])[0].reshape(32 * 1024, 512),
        "e_wd": f(inp["expert_w_down"])[0].reshape(32 * 512, 1024),
        "w_pg": f(inp["w_ple_gate"])[0], "w_pp": f(inp["w_ple_proj"])[0],
    }
    cw_ = f(inp["conv_w"])[0]; cb_ = f(inp["conv_b"])[0]
    conv_wb = np.zeros((128, 40), np.float32)
    for hq in range(8):
        ch = np.arange(128) + 128 * hq
        conv_wb[:, hq * 4:hq * 4 + 4] = cw_[:, ch].T
        conv_wb[:, 32 + hq] = cb_[ch]
    shared["conv_wb"] = conv_wb
    c_f32, ut, c_e = _consts()
    shared["c_f32"] = c_f32; shared["c_e"] = c_e
    x_t = x.reshape(128, 128, 1024); p_t = p.reshape(128, 128, 256)
    kk = np.arange(128)[:, None]; qq = np.arange(128)[None, :]
    tri = np.where(kk <= qq, 0.0, -NEGB).astype(np.float32)
    maps = []
    for c in range(NCORES):
        par = c % 2; pad = 2 * ((7 - c) // 2)
        xs = np.zeros((128, 128, 1024), np.float32)
        xs[pad:] = x_t[:128 - pad]
        own = [8 * j + c for j in range(16)]
        cm0 = tri if par == 0 else np.zeros((128, 128), np.float32)
        cm1 = np.full((128, 128), -NEGB, np.float32) if par == 0 else tri
        c_bf = np.concatenate([np.eye(128, dtype=np.float32), ut, cm0, cm1], 1).astype(bf)
        c_core = np.zeros((128, 130), np.float32)
        c_core[:, 0:128] = (np.arange(128)[None, :] >= pad).astype(np.float32)
        c_core[:, 128] = par; c_core[:, 129] = 1 - par
        gmask = np.full((16, 64), -1e30, np.float32)
        for j in range(16):
            gmask[j, pad // 2:4 * j + 3] = 0.0
        m = dict(shared)
        m.update({"x_shift": xs.reshape(128 * 128, 1024), "x_own": np.ascontiguousarray(x_t[own]).reshape(2048, 1024),
                  "p_own": np.ascontiguousarray(p_t[own]).reshape(2048, 256), "c_bf": c_bf, "c_core": c_core,
                  "c_gmask": gmask.reshape(1, 1024)})
        maps.append(m)
    return maps


_NC_CACHE = {}


def kernel(**inputs):
    if "nc" not in _NC_CACHE:
        _NC_CACHE["nc"] = build_program(False)
    nc = _NC_CACHE["nc"]
    maps = make_in_maps(inputs)
    res = run_bass_kernel_spmd(nc, maps, core_ids=list(range(NCORES)))
    out = np.zeros((128, 128, 1024), np.float32)
    for c in range(NCORES):
        o = np.asarray(res.results[c]["out_own"], dtype=np.float32).reshape(16, 128, 1024)
        for j in range(16):
            out[8 * j + c] = o[j]
    return out.reshape(1, 16384, 1024)
```

```python
import numpy as np
import ml_dtypes
from contextlib import ExitStack
import concourse.bass as bass
import concourse.mybir as mybir
from concourse.bass_utils import run_bass_kernel_spmd

F32 = mybir.dt.float32
BF16 = mybir.dt.bfloat16
ALU = mybir.AluOpType
AF = mybir.ActivationFunctionType
AX = mybir.AxisListType
ENGS = ("pe", "act", "dve", "pool", "sp")
NCORES = 8
EPS = 1e-6
NEGB = 30000.0


class Buf:
    __slots__ = ("name", "w", "r", "dsem")

    def __init__(self, name):
        self.name = name
        self.w = None
        self.r = []
        self.dsem = None


class Prog:
    def __init__(self, nc):
        self.nc = nc
        self.ops = {e: [] for e in ENGS}
        self.cnt = {e: 0 for e in ENGS}
        self.seen = {e: {} for e in ENGS}
        self.dma_cnt = {}
        self.semkeys = []
        self.semset = set()
        self.n_inst = 0

    LIM = 4000

    def _tok(self, eng, val):
        ep = (val - 1) // self.LIM
        key = "%s#%d" % (eng, ep)
        if key not in self.semset:
            self.semset.add(key)
            self.semkeys.append(key)
        return (key, val - ep * self.LIM)

    def _need(self, eng, deps):
        best = {}
        for k, v in deps:
            if eng == "pe" and k.startswith("pe#"):
                continue
            if v <= self.seen[eng].get(k, 0):
                continue
            if v > best.get(k, 0):
                best[k] = v
        for k, v in best.items():
            self.seen[eng][k] = v
        return list(best.items())

    def _deps(self, reads, writes):
        deps = []
        for b in reads:
            if b.w is not None:
                deps.append(b.w)
        for b in writes:
            if b.w is not None:
                deps.append(b.w)
            deps.extend(b.r)
        return deps

    def op(self, eng, fn, reads=(), writes=(), inc=True):
        waits = self._need(eng, self._deps(reads, writes))
        val = self.cnt[eng] + 1
        if inc:
            self.cnt[eng] = val
        tok = self._tok(eng, val)
        self.ops[eng].append((waits, fn, (tok[0], 1) if inc else None))
        for b in reads:
            b.r.append(tok)
            if len(b.r) > 24:
                b.r = self._compact(b.r)
        for b in writes:
            b.w = tok
            b.r = []
        self.n_inst += 1

    @staticmethod
    def _compact(r):
        best = {}
        for k, v in r:
            if v > best.get(k, 0):
                best[k] = v
        return list(best.items())

    def dma(self, eng, fn, anchor, reads=(), writes=(), amt=16):
        if anchor.dsem is None:
            anchor.dsem = "d%d" % len(self.semkeys)
            self.semkeys.append(anchor.dsem)
            self.dma_cnt[anchor.dsem] = 0
        k = anchor.dsem
        waits = self._need(eng, self._deps(reads, writes))
        self.dma_cnt[k] += amt
        tok = (k, self.dma_cnt[k])
        self.ops[eng].append((waits, fn, (k, amt)))
        for b in reads:
            b.r.append(tok)
        for b in writes:
            b.w = tok
            b.r = []
        self.n_inst += 1

    def barrier(self):
        deps = [self._tok(e, self.cnt[e]) for e in ENGS if self.cnt[e] > 0]
        deps += [(k, v) for k, v in self.dma_cnt.items() if v > 0]
        for e in ENGS:
            waits = self._need(e, deps)
            if waits:
                self.ops[e].append((waits, None, None))

    def emit(self):
        nc = self.nc
        with ExitStack() as st:
            sems = {}
            for k in self.semkeys:
                sems[k] = st.enter_context(nc.semaphore("s_" + k.replace("#", "_")))
            block = st.enter_context(nc.Block())

            def run(eobj, name):
                for waits, fn, inc in self.ops[name]:
                    for k, v in waits:
                        eobj.wait_ge(sems[k], v)
                    if fn is None:
                        continue
                    ins = fn(eobj)
                    if inc is not None:
                        ins.then_inc(sems[inc[0]], inc[1])

            @block.tensor
            def _(e):
                run(e, "pe")

            @block.scalar
            def _(e):
                run(e, "act")

            @block.vector
            def _(e):
                run(e, "dve")

            @block.gpsimd
            def _(e):
                run(e, "pool")

            @block.sync
            def _(e):
                run(e, "sp")


class Arena:
    def __init__(self, nc, words):
        self.t = nc.alloc_sbuf_tensor("arena", [128, words], F32)
        self.words = words
        self.top = 0
        self.peak = 0

    def mark(self):
        return self.top

    def release(self, m):
        self.top = m

    def alloc(self, cols, dtype=F32):
        w = cols if dtype == F32 else (cols + 1) // 2
        w = (w + 7) // 8 * 8
        a = self.top
        self.top += w
        self.peak = max(self.peak, self.top)
        assert self.top <= self.words, "SBUF arena overflow %d > %d" % (self.top, self.words)
        ap = self.t[:, a:a + w]
        if dtype != F32:
            ap = ap.bitcast(dtype)
        return ap[:, 0:cols]


def v3(ap, a):
    return ap.rearrange("p (a b) -> p a b", a=a)


class K:
    def __init__(self, nc, dbg=False):
        self.nc = nc
        self.P = Prog(nc)
        self.A = Arena(nc, 52600)
        self.dbg = dbg
        self.psb = []
        for i in range(8):
            t = nc.alloc_psum_tensor("psb%d" % i, [128, 512], F32)
            self.psb.append((t, Buf("ps%d" % i)))
        self.rr = {}

    def ps(self, group, banks):
        i = self.rr.get(group, 0)
        self.rr[group] = i + 1
        t, b = self.psb[banks[i % len(banks)]]
        return t, b

    def mm(self, out, lhsT, rhs, start, stop, reads, writes, inc=True):
        self.P.op("pe", lambda e: e.matmul(out, lhsT=lhsT, rhs=rhs, start=start, stop=stop), reads, writes, inc)

    def tr(self, out, in_, ident, reads, writes, inc=True):
        self.P.op("pe", lambda e: e.transpose(out=out, in_=in_, identity=ident), reads, writes, inc)

    def act(self, out, in_, func, reads, writes, bias=None, scale=None, accum_out=None, eng="act"):
        kw = {}
        if bias is not None:
            kw["bias"] = bias
        if scale is not None:
            kw["scale"] = scale
        if accum_out is not None:
            kw["accum_out"] = accum_out
        self.P.op(eng, lambda e: e.activation(out=out, in_=in_, func=func, **kw), reads, writes)

    def cp(self, eng, out, in_, reads, writes):
        if eng == "act":
            self.P.op("act", lambda e: e.copy(out=out, in_=in_), reads, writes)
        else:
            self.P.op(eng, lambda e: e.tensor_copy(out=out, in_=in_), reads, writes)

    def ts(self, out, in0, s1, s2, op0, op1, reads, writes, eng="dve"):
        if op1 is None:
            self.P.op(eng, lambda e: e.tensor_scalar(out=out, in0=in0, scalar1=s1, scalar2=None, op0=op0), reads, writes)
        else:
            self.P.op(eng, lambda e: e.tensor_scalar(out=out, in0=in0, scalar1=s1, scalar2=s2, op0=op0, op1=op1), reads, writes)

    def stt(self, out, in0, scalar, in1, op0, op1, reads, writes, eng="dve"):
        self.P.op(eng, lambda e: e.scalar_tensor_tensor(out=out, in0=in0, scalar=scalar, in1=in1, op0=op0, op1=op1), reads, writes)

    def tt(self, out, in0, in1, op, reads, writes, eng="dve"):
        self.P.op(eng, lambda e: e.tensor_tensor(out=out, in0=in0, in1=in1, op=op), reads, writes)

    def recip(self, out, in_, reads, writes):
        self.P.op("dve", lambda e: e.reciprocal(out=out, in_=in_), reads, writes)

    def memset(self, ap, val, writes, eng="dve"):
        self.P.op(eng, lambda e: e.memset(ap, val), (), writes)

    def ld(self, out, in_, buf, eng="sp", reads=()):
        self.P.dma(eng, lambda e: e.dma_start(out=out, in_=in_), buf, reads=reads, writes=[buf])

    def st(self, out, in_, buf, dbuf, eng="sp"):
        self.P.dma(eng, lambda e: e.dma_start(out=out, in_=in_), buf, reads=[buf], writes=[dbuf])

    def ldw(self, dst3, src, kc, buf, eng="pool"):
        s3 = src.rearrange("(kc p) n -> p kc n", p=128)
        for k in range(kc):
            o = dst3[:, k, :]
            i = s3[:, k, :]
            self.P.dma(eng, lambda e, o=o, i=i: e.dma_start(out=o, in_=i), buf, writes=[buf])

    def rstd(self, ss, inv_n, tmp, out, rb, wb):
        self.act(tmp, ss, AF.Ln, list(rb) + [self.b_eps], wb, bias=self.eps_ap, scale=inv_n)
        self.act(out, tmp, AF.Exp, wb, wb, scale=-0.5)


def build_program(dbg=False, stop=None, NT=128, dummy=None, NE=32):
    nc = bass.Bass("TRN2", target_bir_lowering=False)
    k = K(nc, dbg)
    P, A = k.P, k.A

    def din(name, shape, dt=F32):
        return nc.dram_tensor(name, list(shape), dt, kind="ExternalInput").ap()

    x_shift = din("x_shift", [NT * 128, 1024])
    x_own = din("x_own", [16 * 128, 1024])
    p_own = din("p_own", [16 * 128, 256])
    g_mix = din("g_mix", [1, 1024]); g_ffn = din("g_ffn", [1, 1024]); g_ple = din("g_ple", [1, 1024]); g_fin = din("g_fin", [1, 1024])
    g_ml = din("g_ml", [1, 512])
    w_kv = [din("w_kv%d" % h, [1024, 512]) for h in range(2)]
    w_q = [din("w_q%d" % h, [1024, 256]) for h in range(2)]
    w_ms = din("w_ms", [1024, 1032])
    w_mo = din("w_mo", [1024, 1024])
    w_g = din("w_g", [1024, 2048])
    b_gate = din("b_gate", [1, 2048])
    conv_wb = din("conv_wb", [128, 40])
    ifb = din("ifb", [1, 8])
    w_ua = din("w_ua", [512, 1024]); w_um = din("w_um", [512, 1024]); w_out = din("w_out", [1024, 1024])
    w_r = din("w_r", [1024, 36]); b_r = din("b_r", [1, 36])
    c_f32 = din("c_f32", [128, 3 * 128])
    c_bf = din("c_bf", [128, 4 * 128], BF16)
    c_e = din("c_e", [64, 64 * 128], BF16)
    c_core = din("c_core", [128, 130])
    c_gmask = din("c_gmask", [1, 16 * 64])
    out_own = nc.dram_tensor("out_own", [16 * 128, 1024], F32, kind="ExternalOutput").ap()
    okind = "ExternalOutput" if dbg else "Internal"
    s_xnT = nc.dram_tensor("s_xnT", [(NT + 16) * 128, 1024], BF16).ap()
    s_ya = nc.dram_tensor("s_ya", [16 * 128, 512], BF16, kind=okind).ap()
    s_ym = nc.dram_tensor("s_ym", [16 * 128, 512], BF16, kind=okind).ap()
    d_xnT = [Buf("dxnT%d" % i) for i in range(NT + 16)]
    d_ya = [Buf("dya%d" % i) for i in range(16)]
    d_ym = [Buf("dym%d" % i) for i in range(16)]
    d_out = Buf("dout")

    cf = A.alloc(384); b_cf = Buf("cf")
    cb = A.alloc(512, BF16); b_cb = Buf("cb")
    cc = A.alloc(130); b_cc = Buf("cc")
    epsb = A.alloc(1); b_eps = Buf("eps")
    k.ld(cf, c_f32, b_cf); k.ld(cb, c_bf, b_cb); k.ld(cc, c_core, b_cc)
    k.memset(epsb, EPS, [b_eps])
    k.eps_ap = epsb[:, 0:1]; k.b_eps = b_eps
    NUT, NONES, ID32 = cf[:, 0:128], cf[:, 128:256], cf[:, 256:384]
    IDB, UTB, CM = cb[:, 0:128], cb[:, 128:256], [cb[:, 256:384], cb[:, 384:512]]
    VALID, PAR, NPAR = cc[:, 0:128], cc[:, 128:129], cc[:, 129:130]
    junk = A.alloc(1024, BF16); b_junk = Buf("junk")
    st4 = [A.alloc(4) for _ in range(2)]; b_st4 = [Buf("st%d" % i) for i in range(2)]
    base_mark = A.mark()

    def bc_load(dst, src_row, buf):
        k.ld(dst, src_row.partition_broadcast(128), buf)

    gb = A.alloc(1024); b_gb = Buf("gb")
    bc_load(gb, g_mix, b_gb)
    xt = [A.alloc(1024) for _ in range(2)]; b_xt = [Buf("xt%d" % i) for i in range(2)]
    xn = [A.alloc(1024, BF16) for _ in range(2)]; b_xn = [Buf("xn%d" % i) for i in range(2)]
    xT_s = [A.alloc(1024, BF16) for _ in range(2)]; b_xTs = [Buf("xTs%d" % i) for i in range(2)]

    def norm_tile(src_ap, sl, gbt, b_gbt, dst_bf, b_dst):
        s = st4[sl]; bs = b_st4[sl]
        k.act(junk, src_ap[0], AF.Square, [src_ap[1]], [b_junk, bs], accum_out=s[:, 0:1])
        k.rstd(s[:, 0:1], 1.0 / 1024, s[:, 1:2], s[:, 2:3], [bs], [bs])
        k.stt(dst_bf, src_ap[0], s[:, 2:3], gbt, ALU.mult, ALU.mult, [src_ap[1], bs, b_gbt], [b_dst])

    def transpose_to(dst_bf3, b_dst, src_bf, b_src, nchunk, grp="misc", banks=(5, 6, 7), eng="act"):
        pt, bp = k.ps(grp, banks)
        ptb = pt[:, :].bitcast(BF16)
        for c in range(nchunk):
            k.tr(ptb[:, c * 128:(c + 1) * 128], src_bf[:, c * 128:(c + 1) * 128], IDB, [b_src, b_cb], [bp], inc=(c == nchunk - 1))
        k.cp(eng, dst_bf3, ptb[:, 0:nchunk * 128], [bp], [b_dst])

    for T in range(NT + 16):
        sl = T % 2
        src = x_shift[T * 128:(T + 1) * 128, :] if T < NT else x_own[(T - NT) * 128:(T - NT + 1) * 128, :]
        k.ld(xt[sl], src, b_xt[sl])
        norm_tile((xt[sl], b_xt[sl]), sl, gb, b_gb, xn[sl], b_xn[sl])
        transpose_to(xT_s[sl], b_xTs[sl], xn[sl], b_xn[sl], 8)
        k.st(s_xnT[T * 128:(T + 1) * 128, :], xT_s[sl], b_xTs[sl], d_xnT[T], eng="act")
    P.barrier()
    A.release(base_mark)

    wms = A.alloc(8 * 1032, BF16); b_wms = Buf("wms"); wms3 = v3(wms, 8)
    wmo = A.alloc(8 * 1024, BF16); b_wmo = Buf("wmo"); wmo3 = v3(wmo, 8)
    k.ldw(wms3, w_ms, 8, b_wms); k.ldw(wmo3, w_mo, 8, b_wmo)
    cwb = A.alloc(40); b_cwb = Buf("cwb"); k.ld(cwb, conv_wb, b_cwb)
    ifbb = A.alloc(8); b_ifbb = Buf("ifbb"); bc_load(ifbb, ifb, b_ifbb)
    gml = A.alloc(512); b_gml = Buf("gml"); bc_load(gml, g_ml, b_gml)
    xT = [A.alloc(1024, BF16) for _ in range(2)]; b_xT = [Buf("xT%d" % i) for i in range(2)]
    C32 = A.alloc(4 * 129); b_C32 = [Buf("C32_%d" % h) for h in range(4)]; C32v = v3(C32, 4)
    Cb = A.alloc(4 * 130, BF16); b_Cb = [Buf("Cb_%d" % h) for h in range(4)]; Cbv = v3(Cb, 4)
    k.memset(C32, 0.0, b_C32); k.memset(Cb, 0.0, b_Cb)
    kpre = [A.alloc(132) for _ in range(4)]; b_kpre = [Buf("kpre%d" % h) for h in range(4)]
    qpre = [A.alloc(132) for _ in range(4)]; b_qpre = [Buf("qpre%d" % h) for h in range(4)]
    for h in range(4):
        k.memset(kpre[h], 0.0, [b_kpre[h]]); k.memset(qpre[h], 0.0, [b_qpre[h]])
    NSL = 4
    def mk(n, cols, dt=F32, pre=""):
        return [A.alloc(cols, dt) for _ in range(n)], [Buf(pre + str(i)) for i in range(n)]
    lif, b_lif = mk(2, 8, pre="lif"); gt, b_gt = mk(2, 24, pre="gt")
    ck, b_ck = mk(NSL, 128, pre="ck"); ex, b_ex = mk(NSL, 128, pre="ex")
    kTb, b_kTb = mk(NSL, 128, BF16, "kTb"); qTb, b_qTb = mk(NSL, 128, BF16, "qTb")
    ktok, b_ktok = mk(NSL, 128, BF16, "ktok"); vp, b_vp = mk(NSL, 130, BF16, "vp")
    tmpc, b_tmpc = mk(NSL, 129, pre="tmpc"); Sm, b_Sm = mk(NSL, 128, BF16, "Sm")
    hc, b_hc = mk(NSL, 128, pre="hc"); sm8, b_sm8 = mk(NSL, 8, pre="sm8")
    sig2, b_sig2 = mk(2, 512, pre="sig")
    ympos = [A.alloc(512, BF16) for _ in range(2)]; b_ympos = [Buf("ympos%d" % i) for i in range(2)]
    ymd = A.alloc(512); b_ymd = Buf("ymd")
    ymo = [A.alloc(512, BF16) for _ in range(2)]; b_ymo = [Buf("ymo%d" % i) for i in range(2)]

    ckq, b_ckq = mk(NSL, 128, pre="ckq")
    pcs = [(k.psb[6 + i][0][:, 0:129], Buf("pcs%d" % i)) for i in range(2)]

    def conv_silu(pre, b_pre, hq, out_bf, b_out, c, bcx):
        w0 = cwb[:, hq * 4:hq * 4 + 1]
        k.ts(c, pre[:, 0:128], w0, cwb[:, 32 + hq:33 + hq], ALU.mult, ALU.add, [b_pre, b_cwb], [bcx])
        for tap in range(1, 4):
            k.stt(c, pre[:, tap:tap + 128], cwb[:, hq * 4 + tap:hq * 4 + tap + 1], c, ALU.mult, ALU.add, [b_pre, b_cwb, bcx], [bcx])
        k.cp("pool", pre[:, 0:3], pre[:, 128:131], [b_pre], [b_pre])
        k.act(out_bf, c, AF.Silu, [bcx], [b_out])

    def pm_prologue(T):
        sl = T % 2
        x3 = v3(xT[sl], 8); bx = b_xT[sl]
        if T + 1 < NT:
            k.ld(xT[(T + 1) % 2], s_xnT[(T + 1) * 128:(T + 2) * 128, :], b_xT[(T + 1) % 2], reads=[d_xnT[T + 1]])
        pos = T % 8
        outp = pos >= 6
        pv, bpv = k.ps("pmv", (0, 1))
        for kc in range(8):
            k.mm(pv[:, 0:512], x3[:, kc, :], wms3[:, kc, 512:1024], kc == 0, kc == 7, [bx, b_wms], [bpv], inc=(kc == 7))
        pif, bpif = k.ps("pmisc", (5,))
        for kc in range(8):
            k.mm(pif[:, 0:8], x3[:, kc, :], wms3[:, kc, 1024:1032], kc == 0, kc == 7, [bx, b_wms], [bpif], inc=(kc == 7))
        L = lif[sl]; bL = b_lif[sl]; G = gt[sl]; bG = b_gt[sl]
        k.tt(L, pif[:, 0:8], ifbb, ALU.add, [bpif, b_ifbb], [bL])
        k.act(G[:, 0:4], L[:, 4:8], AF.Exp, [bL], [bG], scale=-1.0)
        k.act(G[:, 0:4], G[:, 0:4], AF.Ln, [bG], [bG], bias=1.0)
        pb, bpb = k.ps("pmisc", (5,))
        k.mm(pb[:, 0:4], NUT, G[:, 0:4], True, True, [b_cf, bG], [bpb], inc=False)
        k.mm(pb[:, 4:8], NONES, G[:, 0:4], True, True, [b_cf, bG], [bpb])
        k.act(G[:, 8:16], pb[:, 0:8], AF.Exp, [bpb], [bG])
        k.tt(G[:, 16:20], L[:, 0:4], pb[:, 0:4], ALU.subtract, [bL, bpb], [bG])
        k.act(G[:, 4:8], G[:, 16:20], AF.Exp, [bG], [bG])
        k.ts(G[:, 4:8], G[:, 4:8], VALID[:, T:T + 1], 128 ** -0.5, ALU.mult, ALU.mult, [bG, b_cc], [bG])
        if not outp:
            k.tt(G[:, 20:24], G[:, 4:8], G[:, 12:16], ALU.mult, [bG], [bG])
        sig = b_sig = None
        if outp:
            sig = sig2[pos - 6]; b_sig = b_sig2[pos - 6]
            po, bpo = k.ps("pmk", (2, 3, 4))
            for kc in range(8):
                k.mm(po[:, 0:512], x3[:, kc, :], wmo3[:, kc, 512:1024], kc == 0, kc == 7, [bx, b_wmo], [bpo], inc=(kc == 7))
            k.act(sig, po[:, 0:512], AF.Exp, [bpo], [b_sig], scale=-1.0)
            k.ts(sig, sig, 1.0, None, ALU.add, None, [b_sig], [b_sig])
            k.recip(sig, sig, [b_sig], [b_sig])
        return (T, x3, bx, pos, outp, pv, bpv, G, bG, sig, b_sig)

    def pm_a1(ctx, h):
        T, x3, bx, pos, outp, pv, bpv, G, bG, sig, b_sig = ctx
        pk, bpk = k.ps("pmk", (2, 3, 4))
        for kc in range(8):
            k.mm(pk[:, 0:128], wms3[:, kc, 128 * h:128 * h + 128], x3[:, kc, :], kc == 0, kc == 7, [bx, b_wms], [bpk], inc=(kc == 7))
        k.cp("act", kpre[h][:, 3:131], pk[:, 0:128], [bpk], [b_kpre[h]])
        if pos >= 5:
            pq, bpq = k.ps("pmk", (2, 3, 4))
            for kc in range(8):
                k.mm(pq[:, 0:128], wmo3[:, kc, 128 * h:128 * h + 128], x3[:, kc, :], kc == 0, kc == 7, [bx, b_wmo], [bpq], inc=(kc == 7))
            k.cp("act", qpre[h][:, 3:131], pq[:, 0:128], [bpq], [b_qpre[h]])

    def pm_a2(ctx, h, hs):
        T, x3, bx, pos, outp, pv, bpv, G, bG, sig, b_sig = ctx
        conv_silu(kpre[h], b_kpre[h], 4 + h, kTb[hs], b_kTb[hs], ck[hs], b_ck[hs])
        if outp:
            conv_silu(qpre[h], b_qpre[h], h, qTb[hs], b_qTb[hs], ckq[hs], b_ckq[hs])
        elif pos == 5:
            k.cp("pool", qpre[h][:, 0:3], qpre[h][:, 128:131], [b_qpre[h]], [b_qpre[h]])
        V = vp[hs]; bV = b_vp[hs]
        gc = (4 + h) if outp else (20 + h)
        k.ts(V[:, 0:128], pv[:, 128 * h:128 * h + 128], G[:, gc:gc + 1], None, ALU.mult, None, [bpv, bG], [bV])
        k.cp("pool", V[:, 128:129], G[:, gc:gc + 1], [bG], [bV])

    def pm_bpe(ctx, h, hs, pcslot):
        T, x3, bx, pos, outp, pv, bpv, G, bG, sig, b_sig = ctx
        V = vp[hs]; bV = b_vp[hs]
        ptk, bptk = k.ps("pmisc", (5,))
        ptkb = ptk[:, :].bitcast(BF16)
        k.tr(ptkb[:, 0:128], kTb[hs], IDB, [b_kTb[hs], b_cb], [bptk])
        k.cp("act", ktok[hs], ptkb[:, 0:128], [bptk], [b_ktok[hs]])
        if outp:
            pss, bpss = k.ps("pmk", (2, 3, 4))
            k.mm(pss[:, 0:128], kTb[hs], qTb[hs], True, True, [b_kTb[hs], b_qTb[hs]], [bpss])
            k.tt(Sm[hs], pss[:, 0:128], UTB, ALU.mult, [bpss, b_cb], [b_Sm[hs]])
            pn, bpn = k.ps("pmk", (2, 3, 4))
            k.mm(pn[:, 0:129], Sm[hs], V[:, 0:129], True, False, [b_Sm[hs], bV], [bpn], inc=False)
            k.mm(pn[:, 0:129], qTb[hs], Cbv[:, h, 0:129], False, True, [b_qTb[hs], b_Cb[h]], [bpn])
            s8 = sm8[hs]; bs8 = b_sm8[hs]
            et = G[:, 8 + h:9 + h]
            k.tt(s8[:, 0:1], pn[:, 128:129], et, ALU.mult, [bpn, bG], [bs8])
            k.stt(s8[:, 1:2], s8[:, 0:1], -1.0, s8[:, 0:1], ALU.mult, ALU.max, [bs8], [bs8])
            k.ts(s8[:, 1:2], s8[:, 1:2], 1.0, None, ALU.max, None, [bs8], [bs8])
            k.recip(s8[:, 2:3], s8[:, 1:2], [bs8], [bs8])
            k.tt(s8[:, 3:4], s8[:, 2:3], et, ALU.mult, [bs8, bG], [bs8])
            H = hc[hs]; bH = b_hc[hs]
            k.stt(H, pn[:, 0:128], s8[:, 3:4], sig[:, 128 * h:128 * h + 128], ALU.mult, ALU.mult, [bpn, bs8, b_sig], [bH])
            k.act(ex[hs], H, AF.Square, [bH], [b_ex[hs], bs8], accum_out=s8[:, 4:5])
            k.rstd(s8[:, 4:5], 1.0 / 128, s8[:, 5:6], s8[:, 6:7], [bs8], [bs8])
            k.stt(ympos[pos - 6][:, 128 * h:128 * h + 128], H, s8[:, 6:7], gml[:, 128 * h:128 * h + 128], ALU.mult, ALU.mult,
                  [bH, bs8, b_gml], [b_ympos[pos - 6]])
        pc, bpc = pcslot
        k.mm(pc[:, 0:129], ktok[hs], V[:, 0:129], True, True, [b_ktok[hs], bV], [bpc])
        if h == 3 and pos == 7:
            j = T // 8
            k.tt(ymd, ympos[1], ympos[0], ALU.subtract, b_ympos, [b_ymd])
            yo = ymo[j % 2]; byo = b_ymo[j % 2]
            k.stt(yo, ymd, PAR, ympos[0], ALU.mult, ALU.add, [b_ymd, b_cc, b_ympos[0]], [byo])
            k.st(s_ym[j * 128:(j + 1) * 128, :], yo, byo, d_ym[j])

    def pm_bdve(ctx, h, hs, pcslot):
        T, x3, bx, pos, outp, pv, bpv, G, bG, sig, b_sig = ctx
        pc, bpc = pcslot
        if outp:
            tc_, btc = tmpc[hs], b_tmpc[hs]
            k.tt(tc_, pc[:, 0:129], C32v[:, h, :], ALU.add, [bpc, b_C32[h]], [btc])
            k.ts(C32v[:, h, :], tc_, G[:, 12 + h:13 + h], None, ALU.mult, None, [btc, bG], [b_C32[h]])
            if pos == 6:
                k.ts(Cbv[:, h, 0:129], tc_, G[:, 12 + h:13 + h], None, ALU.mult, None, [btc, bG], [b_Cb[h]])
        else:
            k.stt(C32v[:, h, :], C32v[:, h, :], G[:, 12 + h:13 + h], pc[:, 0:129], ALU.mult, ALU.add, [b_C32[h], bG, bpc], [b_C32[h]])
            if pos == 5:
                k.cp("act", Cbv[:, h, 0:129], C32v[:, h, :], [b_C32[h]], [b_Cb[h]])

    k.ld(xT[0], s_xnT[0:128, :], b_xT[0], reads=[d_xnT[0]])
    items = [(T, h) for T in range(NT) for h in range(4)]
    NI = len(items)
    ctxs = {}
    for s in range(-3, NI):
        i = s + 3
        if 0 <= i < NI:
            T, h = items[i]
            if h == 0:
                ctxs[T] = pm_prologue(T)
            pm_a1(ctxs[T], h)
        i = s + 1
        if 0 <= i < NI:
            T, h = items[i]
            pm_bpe(ctxs[T], h, i % NSL, pcs[i % 2])
        i = s + 2
        if 0 <= i < NI:
            T, h = items[i]
            pm_a2(ctxs[T], h, i % NSL)
        i = s
        if 0 <= i < NI:
            T, h = items[i]
            pm_bdve(ctxs[T], h, i % NSL, pcs[i % 2])
    P.barrier()
    A.release(base_mark)
    if stop == "PM":
        P.emit(); print("instructions:", P.n_inst, {e: P.cnt[e] for e in ENGS}); return nc

    KT = A.alloc(2 * 16384, BF16); KT3 = v3(KT, 2)
    b_KT = [Buf("KT%d" % t) for t in range(NT)]
    V1 = A.alloc(NT * 260, BF16); V14 = V1.rearrange("p (t h d) -> p t h d", t=NT, h=4)
    b_V1 = [Buf("V1_%d" % t) for t in range(NT)]
    Eb = A.alloc(64 * 128, BF16); b_Eb = Buf("Eb"); Eb3 = v3(Eb, 64)
    k.memset(Eb, 0.0, [b_Eb])
    k.ld(Eb[0:64, :], c_e, b_Eb)
    gmask = A.alloc(1024); b_gmask = Buf("gmask"); bc_load(gmask, c_gmask, b_gmask); gmask3 = v3(gmask, 16)
    wkv = A.alloc(8 * 512, BF16); b_wkv = Buf("wkv"); wkv3 = v3(wkv, 8)
    wq = A.alloc(8 * 256, BF16); b_wq = Buf("wq"); wq3 = v3(wq, 8)
    xT = [A.alloc(1024, BF16) for _ in range(2)]; b_xT = [Buf("axT%d" % i) for i in range(2)]
    xTo = A.alloc(1024, BF16); b_xTo = Buf("xTo")
    ksum = A.alloc(2 * NT); b_ksum = Buf("ksum"); ksum3 = v3(ksum, 2)
    kmT = A.alloc(2 * 64); b_kmT = Buf("kmT"); kmT3 = v3(kmT, 2)
    QTs = A.alloc(512, BF16); b_QTs = Buf("QTs"); QTs4 = QTs.rearrange("p (a s q) -> p a s q", a=2, s=2)
    QT32 = A.alloc(512); b_QT32 = Buf("QT32"); QT324 = QT32.rearrange("p (a s q) -> p a s q", a=2, s=2)
    k.memset(QTs, 0.0, [b_QTs]); k.memset(QT32, 0.0, [b_QT32])
    gm_, b_gm = mk(4, 64, pre="gm"); t8, b_t8 = mk(4, 16, pre="t8")
    b01, b_b01 = mk(4, 64, BF16, "b01"); BT, b_BT = mk(4, 128, BF16, "BT")
    for i_ in range(4):
        k.memset(BT[i_], 0.0, [b_BT[i_]])
    PT, b_PT = mk(3, 512, BF16, "PT")
    yat = [A.alloc(512, BF16) for _ in range(2)]; b_yat = [Buf("yat%d" % i) for i in range(2)]
    r1, b_r1 = mk(2, 2, pre="r1")
    hcount = [0]; gcount = [0]

    for hp in range(2):
        if hp == 1:
            P.barrier()
        k.ldw(wkv3, w_kv[hp], 8, b_wkv); k.ldw(wq3, w_q[hp], 8, b_wq)
        k.memset(kmT, 0.0, [b_kmT]); k.memset(ksum, 0.0, [b_ksum])
        if hp == 0:
            for t0 in range(0, NT, 16):
                k.memset(V1[:, t0 * 260:(t0 + 16) * 260], 1.0, b_V1[t0:t0 + 16])
        k.ld(xT[0], s_xnT[0:128, :], b_xT[0], reads=[d_xnT[0]])
        for T in range(NT):
            sl = T % 2
            x3 = v3(xT[sl], 8); bx = b_xT[sl]
            if T + 1 < NT:
                k.ld(xT[(T + 1) % 2], s_xnT[(T + 1) * 128:(T + 2) * 128, :], b_xT[(T + 1) % 2], reads=[d_xnT[T + 1]])
            for p in range(2):
                pk, bpk = k.ps("paproj", (5, 6, 7))
                for kc in range(8):
                    k.mm(pk[:, 0:128], wkv3[:, kc, 128 * p:128 * p + 128], x3[:, kc, :], kc == 0, kc == 7, [bx, b_wkv], [bpk], inc=(kc == 7))
                k.act(KT3[:, p, T * 128:(T + 1) * 128], pk[:, 0:128], AF.Identity, [bpk], [b_KT[T], b_ksum], accum_out=ksum3[:, p, T:T + 1])
            pv, bpv = k.ps("paproj", (5, 6, 7))
            for kc in range(8):
                k.mm(pv[:, 0:256], x3[:, kc, :], wkv3[:, kc, 256:512], kc == 0, kc == 7, [bx, b_wkv], [bpv], inc=(kc == 7))
            k.cp("dve", V14[:, T, :, 0:64], pv[:, 0:256].rearrange("p (h d) -> p h d", h=4), [bpv], [b_V1[T]])
            if T % 2 == 1:
                n = T // 2
                k.tt(kmT3[:, :, n:n + 1], ksum3[:, :, T - 1:T], ksum3[:, :, T:T + 1], ALU.add, [b_ksum], [b_kmT])
            if T % 8 != 7:
                continue
            j = T // 8
            k.ld(xTo, s_xnT[(NT + j) * 128:(NT + j + 1) * 128, :], b_xTo, reads=[d_xnT[NT + j]])
            xo3 = v3(xTo, 8)
            for p in range(2):
                pq, bpq = k.ps("paproj", (5, 6, 7))
                for kc in range(8):
                    k.mm(pq[:, 0:128], wq3[:, kc, 128 * p:128 * p + 128], xo3[:, kc, :], kc == 0, kc == 7, [b_xTo, b_wq], [bpq], inc=(kc == 7))
                for s_ in range(2):
                    k.ts(QTs4[64 * s_:64 * s_ + 64, p, s_, :], pq[64 * s_:64 * s_ + 64, 0:128], 0.125, None, ALU.mult, None, [bpq], [b_QTs])
                    k.cp("dve", QT324[64 * s_:64 * s_ + 64, p, s_, :], pq[64 * s_:64 * s_ + 64, 0:128], [bpq], [b_QT32])
            ya_t = yat[j % 2]; b_ya_t = b_yat[j % 2]
            nkt = 8 * j + 8
            def prologue_a(hl):
                p, s = hl // 2, hl % 2
                pg, bpg = k.ps("paproj", (5, 6, 7))
                k.mm(pg[:, 0:64], QT324[:, p, s, :], kmT3[:, p, :], True, True, [b_QT32, b_kmT], [bpg])
                g_ = gm_[hl]; bg_ = b_gm[hl]; t_ = t8[hl]; bt_ = b_t8[hl]
                k.tt(g_, pg[:, 0:64], gmask3[:, j, :], ALU.add, [bpg, b_gmask], [bg_])
                P.op("dve", lambda e, o=t_[:, 0:8], i=g_: e.max(out=o, in_=i), [bg_], [bt_])
                k.ts(t_[:, 8:9], t_[:, 2:3], -1e29, None, ALU.max, None, [bt_], [bt_])
                k.ts(b01[hl], g_, t_[:, 8:9], 1.0, ALU.is_ge, ALU.subtract, [bg_, bt_], [b_b01[hl]])

            def prologue_b(hl):
                pbt, bpbt = k.ps("paproj", (5, 6, 7))
                pbtb = pbt[:, :].bitcast(BF16)
                k.tr(pbtb[0:64, 0:128], b01[hl], IDB, [b_b01[hl], b_cb], [bpbt])
                k.cp("dve", BT[hl][0:64, :], pbtb[0:64, 0:128], [bpbt], [b_BT[hl]])

            def qk_group(hl, g0):
                p, s = hl // 2, hl % 2
                pS, bpS = k.ps("pas", (2, 3, 4))
                for i in range(4):
                    kt = g0 + i
                    col = pS[:, i * 128:(i + 1) * 128]
                    k.mm(col, KT3[:, p, kt * 128:(kt + 1) * 128], QTs4[:, p, s, :], True, False, [b_KT[kt], b_QTs], [bpS], inc=False)
                    if kt < nkt - 2:
                        k.mm(col, Eb3[:, kt // 2, :], BT[hl][:, :], False, True, [b_Eb, b_BT[hl]], [bpS], inc=(i == 3))
                    else:
                        k.mm(col, IDB, CM[kt - (nkt - 2)], False, True, [b_cb], [bpS], inc=(i == 3))
                return pS, bpS

            def pv_group(hl, g0, pS, bpS, po, bpo):
                pt_ = PT[gcount[0] % 3]; bpt_ = b_PT[gcount[0] % 3]; gcount[0] += 1
                k.act(pt_, pS[:, 0:512], AF.Exp, [bpS], [bpt_])
                for i in range(4):
                    kt = g0 + i
                    k.mm(po[:, 0:65], pt_[:, i * 128:(i + 1) * 128], V14[:, kt, hl, :], kt == 0, kt == nkt - 1, [bpt_, b_V1[kt]], [bpo],
                         inc=(i == 3))

            groups = list(range(0, nkt, 4))
            prologue_a(0); prologue_b(0)
            for hl in range(4):
                po, bpo = k.ps("pao", (0, 1))
                cur = qk_group(hl, groups[0])
                for gi, g0 in enumerate(groups):
                    nxt = qk_group(hl, groups[gi + 1]) if gi + 1 < len(groups) else None
                    if hl < 3 and gi == 0:
                        prologue_a(hl + 1)
                    if hl < 3 and gi == min(1, len(groups) - 1):
                        prologue_b(hl + 1)
                    pv_group(hl, g0, cur[0], cur[1], po, bpo)
                    cur = nxt
                r_ = r1[hl % 2]; br_ = b_r1[hl % 2]
                k.recip(r_[:, 0:1], po[:, 64:65], [bpo], [br_])
                k.ts(ya_t[:, 64 * hl:64 * hl + 64], po[:, 0:64], r_[:, 0:1], None, ALU.mult, None, [bpo, br_], [b_ya_t])
            k.st(s_ya[j * 128:(j + 1) * 128, 256 * hp:256 * hp + 256], ya_t[:, 0:256], b_ya_t, d_ya[j])
    P.barrier()
    A.release(base_mark)
    if stop == "PA":
        P.emit(); print("instructions:", P.n_inst, {e: P.cnt[e] for e in ENGS}); return nc

    e_wg = din("e_wg", [NE * 1024, 512]); e_wu = din("e_wu", [NE * 1024, 512]); e_wd = din("e_wd", [NE * 512, 1024])
    w_pg = din("w_pg", [1024, 1024]); w_pp = din("w_pp", [256, 1024])
    h1 = A.alloc(16 * 1024); h13 = v3(h1, 16); b_h1 = [Buf("h1_%d" % j) for j in range(16)]
    hnT = A.alloc(8 * 2048, BF16); hnT3 = v3(hnT, 8); b_hnT = [Buf("hnT%d" % j) for j in range(16)]
    cw = A.alloc(16 * 32); cw3 = v3(cw, 16); b_cw = [Buf("cw%d" % j) for j in range(16)]
    NJ = NT // 8
    if NJ < 16:
        k.memset(hnT, 0.0, b_hnT); k.memset(cw, 0.0, b_cw); k.memset(h1, 0.0, b_h1)
    pb_mark = A.mark()
    wg_ = A.alloc(8 * 2048, BF16); b_wg = Buf("wg"); wg3 = v3(wg_, 8)
    wua = A.alloc(4 * 1024, BF16); b_wua = Buf("wua"); wua3 = v3(wua, 4)
    wum = A.alloc(4 * 1024, BF16); b_wum = Buf("wum"); wum3 = v3(wum, 4)
    wo = A.alloc(8 * 1024, BF16); b_wo = Buf("wo"); wo3 = v3(wo, 8)
    wr = A.alloc(8 * 36); b_wr = Buf("wr"); wr3 = v3(wr, 8)
    k.ldw(wg3, w_g, 8, b_wg); k.ldw(wua3, w_ua, 4, b_wua); k.ldw(wum3, w_um, 4, b_wum); k.ldw(wo3, w_out, 8, b_wo)
    for kc in range(8):
        k.ld(wr3[:, kc, :], w_r[kc * 128:(kc + 1) * 128, :], b_wr)
    bgb = A.alloc(2048, BF16); b_bgb = Buf("bgb")
    br32 = A.alloc(36); b_br32 = Buf("br32"); bc_load(br32, b_r, b_br32)
    ones = A.alloc(128); b_ones = Buf("ones"); k.memset(ones, 1.0, [b_ones])
    onesb = A.alloc(128, BF16); b_onesb = Buf("onesb"); k.memset(onesb, 1.0, [b_onesb])
    gfb = A.alloc(1024); b_gfb = Buf("gfb"); bc_load(gfb, g_ffn, b_gfb)
    xTo = A.alloc(1024, BF16); b_xTo = Buf("bxTo")
    yab = A.alloc(512, BF16); b_yab = Buf("yab"); ymb = A.alloc(512, BF16); b_ymb = Buf("ymb")
    yaT = A.alloc(512, BF16); b_yaT = Buf("yaT"); ymT = A.alloc(512, BF16); b_ymT = Buf("ymT")
    yaT3 = v3(yaT, 4); ymT3 = v3(ymT, 4)
    sga = A.alloc(512); b_sga = Buf("sga"); sgm = A.alloc(512); b_sgm = Buf("sgm")
    mrg = A.alloc(1024, BF16); b_mrg = Buf("mrg"); mrgT = A.alloc(1024, BF16); b_mrgT = Buf("mrgT"); mrgT3 = v3(mrgT, 8)
    hn32 = A.alloc(1024); b_hn32 = Buf("hn32")
    hnhi = A.alloc(1024, BF16); b_hnhi = Buf("hnhi"); hnlo = A.alloc(1024, BF16); b_hnlo = Buf("hnlo")
    hiT = A.alloc(1024, BF16); b_hiT = Buf("hiT"); hiT3 = v3(hiT, 8)
    loT = A.alloc(1024, BF16); b_loT = Buf("loT"); loT3 = v3(loT, 8)
    wrh = A.alloc(8 * 36, BF16); b_wrh = Buf("wrh"); wrh3 = v3(wrh, 8)
    wrl = A.alloc(8 * 36, BF16); b_wrl = Buf("wrl"); wrl3 = v3(wrl, 8)
    k.cp("dve", wrh, wr, [b_wr], [b_wrh])
    k.tt(wrl, wr, wrh, ALU.subtract, [b_wr, b_wrh], [b_wrl])
    Lr = A.alloc(40); b_Lr = Buf("Lr"); rs = A.alloc(16); b_rs = Buf("rs"); elm = A.alloc(32); b_elm = Buf("elm")
    c1 = A.alloc(32); b_c1 = Buf("c1")
    for q4 in range(4):
        k.ld(sga, b_gate[:, 512 * q4:512 * q4 + 512].partition_broadcast(128), b_sga)
        k.cp("dve", bgb[:, 512 * q4:512 * q4 + 512], sga, [b_sga], [b_bgb])

    def sigmoid_from(dst, b_dst, ps_ap, b_ps):
        k.act(dst, ps_ap, AF.Exp, [b_ps], [b_dst], scale=-1.0)
        k.ts(dst, dst, 1.0, None, ALU.add, None, [b_dst], [b_dst])
        k.recip(dst, dst, [b_dst], [b_dst])

    for j in range(NJ):
        k.ld(h13[:, j, :], x_own[j * 128:(j + 1) * 128, :], b_h1[j])
        k.ld(xTo, s_xnT[(NT + j) * 128:(NT + j + 1) * 128, :], b_xTo, reads=[d_xnT[NT + j]])
        k.ld(yab, s_ya[j * 128:(j + 1) * 128, :], b_yab, reads=[d_ya[j]])
        k.ld(ymb, s_ym[j * 128:(j + 1) * 128, :], b_ymb, reads=[d_ym[j]])
        xo3 = v3(xTo, 8)
        transpose_to(yaT, b_yaT, yab, b_yab, 4, grp="pbm", banks=(6, 7), eng="dve")
        transpose_to(ymT, b_ymT, ymb, b_ymb, 4, grp="pbm", banks=(6, 7), eng="dve")
        for b in range(2):
            cs = slice(512 * b, 512 * b + 512)
            pga, bpga = k.ps("pbg", (0, 1, 2))
            for kc in range(8):
                k.mm(pga[:, 0:512], xo3[:, kc, :], wg3[:, kc, 512 * b:512 * b + 512], kc == 0, kc == 7, [b_xTo, b_wg], [bpga], inc=(kc == 7))
            k.tt(sga, pga[:, 0:512], bgb[:, 512 * b:512 * b + 512], ALU.add, [bpga, b_bgb], [b_sga])
            sigmoid_from(sga, b_sga, sga, b_sga)
            pua, bpua = k.ps("pbu", (3, 4, 5))
            for fc in range(4):
                k.mm(pua[:, 0:512], yaT3[:, fc, :], wua3[:, fc, cs], fc == 0, fc == 3, [b_yaT, b_wua], [bpua], inc=(fc == 3))
            k.tt(sga, sga, pua[:, 0:512], ALU.mult, [b_sga, bpua], [b_sga])
            pgm, bpgm = k.ps("pbg", (0, 1, 2))
            for kc in range(8):
                k.mm(pgm[:, 0:512], xo3[:, kc, :], wg3[:, kc, 1024 + 512 * b:1024 + 512 * b + 512], kc == 0, kc == 7, [b_xTo, b_wg], [bpgm], inc=(kc == 7))
            k.tt(sgm, pgm[:, 0:512], bgb[:, 1024 + 512 * b:1024 + 512 * b + 512], ALU.add, [bpgm, b_bgb], [b_sgm])
            sigmoid_from(sgm, b_sgm, sgm, b_sgm)
            pum, bpum = k.ps("pbu", (3, 4, 5))
            for fc in range(4):
                k.mm(pum[:, 0:512], ymT3[:, fc, :], wum3[:, fc, cs], fc == 0, fc == 3, [b_ymT, b_wum], [bpum], inc=(fc == 3))
            k.tt(sgm, sgm, pum[:, 0:512], ALU.mult, [b_sgm, bpum], [b_sgm])
            k.tt(mrg[:, cs], sga, sgm, ALU.add, [b_sga, b_sgm], [b_mrg])
        transpose_to(mrgT, b_mrgT, mrg, b_mrg, 8, grp="pbm", banks=(6, 7), eng="act")
        for b in range(2):
            ph, bph = k.ps("pbg", (0, 1, 2))
            for kc in range(8):
                k.mm(ph[:, 0:512], mrgT3[:, kc, :], wo3[:, kc, 512 * b:512 * b + 512], kc == 0, kc == 7, [b_mrgT, b_wo], [bph], inc=(kc == 7))
            k.tt(h13[:, j, 512 * b:512 * b + 512], h13[:, j, 512 * b:512 * b + 512], ph[:, 0:512], ALU.add, [b_h1[j], bph], [b_h1[j]])
        if stop == "PB1":
            continue
        s = st4[0]; bs = b_st4[0]
        k.act(junk, h13[:, j, :], AF.Square, [b_h1[j]], [b_junk, bs], accum_out=s[:, 0:1])
        k.rstd(s[:, 0:1], 1.0 / 1024, s[:, 1:2], s[:, 2:3], [bs], [bs])
        k.stt(hn32, h13[:, j, :], s[:, 2:3], gfb, ALU.mult, ALU.mult, [b_h1[j], bs, b_gfb], [b_hn32])
        k.cp("dve", hnhi, hn32, [b_hn32], [b_hnhi])
        k.tt(hnlo, hn32, hnhi, ALU.subtract, [b_hn32, b_hnhi], [b_hnlo])
        transpose_to(hiT, b_hiT, hnhi, b_hnhi, 8, grp="pbm", banks=(6, 7), eng="act")
        transpose_to(loT, b_loT, hnlo, b_hnlo, 8, grp="pbm", banks=(6, 7), eng="act")
        k.cp("dve", hnT3[:, :, j * 128:(j + 1) * 128], hiT3, [b_hiT], [b_hnT[j]])
        pr, bpr = k.ps("pbu", (3, 4, 5))
        n_ = 0
        for (aT, baT, w_, bw_) in ((hiT3, b_hiT, wrh3, b_wrh), (hiT3, b_hiT, wrl3, b_wrl), (loT3, b_loT, wrh3, b_wrh)):
            for kc in range(8):
                k.mm(pr[:, 0:36], aT[:, kc, :], w_[:, kc, :], n_ == 0, n_ == 23, [baT, bw_], [bpr], inc=(n_ == 23))
                n_ += 1
        if stop == "PB2":
            continue
        k.tt(Lr[:, 0:36], pr[:, 0:36], br32, ALU.add, [bpr, b_br32], [b_Lr])
        P.op("dve", lambda e, o=rs[:, 0:1], i=Lr[:, 0:4]: e.tensor_reduce(out=o, in_=i, axis=AX.X, op=ALU.max), [b_Lr], [b_rs])
        k.ts(rs[:, 1:2], rs[:, 0:1], -1.0, None, ALU.mult, None, [b_rs], [b_rs])
        k.act(Lr[:, 36:40], Lr[:, 0:4], AF.Exp, [b_Lr, b_rs], [b_Lr, b_rs], bias=rs[:, 1:2], accum_out=rs[:, 2:3])
        k.recip(rs[:, 3:4], rs[:, 2:3], [b_rs], [b_rs])
        k.ts(Lr[:, 36:40], Lr[:, 0:4], rs[:, 0:1], 1.0, ALU.is_equal, ALU.subtract, [b_Lr, b_rs], [b_Lr])
        k.ts(Lr[:, 36:40], Lr[:, 36:40], 1e30, None, ALU.mult, None, [b_Lr], [b_Lr])
        for g in range(4):
            k.ts(elm[:, 8 * g:8 * g + 8], Lr[:, 4 + 8 * g:12 + 8 * g], Lr[:, 36 + g:37 + g], None, ALU.add, None, [b_Lr], [b_elm])
        P.op("dve", lambda e, o=rs[:, 8:16], i=elm: e.max(out=o, in_=i), [b_elm], [b_rs])
        k.tt(rs[:, 4:5], rs[:, 9:10], rs[:, 8:9], ALU.subtract, [b_rs], [b_rs])
        k.act(rs[:, 4:5], rs[:, 4:5], AF.Exp, [b_rs], [b_rs])
        k.ts(rs[:, 4:5], rs[:, 4:5], 1.0, None, ALU.add, None, [b_rs], [b_rs])
        k.recip(rs[:, 5:6], rs[:, 4:5], [b_rs], [b_rs])
        k.tt(rs[:, 6:7], rs[:, 5:6], rs[:, 3:4], ALU.mult, [b_rs], [b_rs])
        k.tt(rs[:, 7:8], rs[:, 3:4], rs[:, 6:7], ALU.subtract, [b_rs], [b_rs])
        k.ts(c1, elm, rs[:, 8:9], rs[:, 6:7], ALU.is_equal, ALU.mult, [b_elm, b_rs], [b_c1])
        k.ts(cw3[:, j, :], elm, rs[:, 9:10], rs[:, 7:8], ALU.is_equal, ALU.mult, [b_elm, b_rs], [b_cw[j]])
        k.tt(cw3[:, j, :], cw3[:, j, :], c1, ALU.add, [b_cw[j], b_c1], [b_cw[j]])
    P.barrier()
    A.release(pb_mark)
    if stop in ("PB", "PB1", "PB2"):
        P.emit(); return nc

    ewg = [A.alloc(8 * 512, BF16) for _ in range(2)]; ewu = [A.alloc(8 * 512, BF16) for _ in range(2)]
    ewd = [A.alloc(4 * 1024, BF16) for _ in range(2)]
    b_ew = [[Buf("ewg%d" % i), Buf("ewu%d" % i), Buf("ewd%d" % i)] for i in range(2)]
    hidT = A.alloc(4 * 2048, BF16); hidT3 = v3(hidT, 4); b_hid = [Buf("hid%d" % t) for t in range(4)]
    sgt = [A.alloc(512, BF16) for _ in range(2)]; b_sgt = [Buf("sgt%d" % i) for i in range(2)]

    def load_expert(e):
        sl = e % 2
        k.ldw(v3(ewg[sl], 8), e_wg[e * 1024:(e + 1) * 1024, :], 8, b_ew[sl][0])
        k.ldw(v3(ewu[sl], 8), e_wu[e * 1024:(e + 1) * 1024, :], 8, b_ew[sl][1])
        k.ldw(v3(ewd[sl], 4), e_wd[e * 512:(e + 1) * 512, :], 4, b_ew[sl][2])

    load_expert(0)
    scnt = [0]
    for e in range(NE):
        sl = e % 2
        if e + 1 < NE:
            load_expert(e + 1)
        g3, u3, d3 = v3(ewg[sl], 8), v3(ewu[sl], 8), v3(ewd[sl], 4)
        bwg, bwu, bwd = b_ew[sl]
        for tg in range((NJ + 3) // 4):
            rb = [b_hnT[4 * tg + i] for i in range(4)]
            for fc in range(4):
                pG, bpG = k.ps("peg", (0, 1, 2, 3))
                for kc in range(8):
                    k.mm(pG[:, 0:512], g3[:, kc, fc * 128:(fc + 1) * 128], hnT3[:, kc, tg * 512:(tg + 1) * 512], kc == 0, kc == 7, rb + [bwg], [bpG],
                         inc=(kc == 7))
                pU, bpU = k.ps("peg", (0, 1, 2, 3))
                for kc in range(8):
                    k.mm(pU[:, 0:512], u3[:, kc, fc * 128:(fc + 1) * 128], hnT3[:, kc, tg * 512:(tg + 1) * 512], kc == 0, kc == 7, rb + [bwu], [bpU],
                         inc=(kc == 7))
                sg = sgt[scnt[0] % 2]; bsg = b_sgt[scnt[0] % 2]; scnt[0] += 1
                k.act(sg, pG[:, 0:512], AF.Silu, [bpG], [bsg])
                k.tt(hidT3[:, fc, tg * 512:(tg + 1) * 512], sg, pU[:, 0:512], ALU.mult, [bsg, bpU], [b_hid[tg]])
        for jt in range(NJ):
            for b in range(2):
                py, bpy = k.ps("pey", (4, 5, 6, 7))
                for fc in range(4):
                    k.mm(py[:, 0:512], hidT3[:, fc, jt * 128:(jt + 1) * 128], d3[:, fc, 512 * b:512 * b + 512], fc == 0, fc == 3, [b_hid[jt // 4], bwd], [bpy],
                         inc=(fc == 3))
                hsl = h13[:, jt, 512 * b:512 * b + 512]
                k.stt(hsl, py[:, 0:512], cw3[:, jt, e:e + 1], hsl, ALU.mult, ALU.add, [bpy, b_cw[jt], b_h1[jt]], [b_h1[jt]])
    P.barrier()
    A.release(pb_mark)
    if stop == "PE":
        P.emit(); return nc

    wpg = A.alloc(8 * 1024, BF16); b_wpg = Buf("wpg"); wpg3 = v3(wpg, 8)
    wpp = A.alloc(2 * 1024, BF16); b_wpp = Buf("wpp"); wpp3 = v3(wpp, 2)
    k.ldw(wpg3, w_pg, 8, b_wpg); k.ldw(wpp3, w_pp, 2, b_wpp)
    gpb = A.alloc(1024); b_gpb = Buf("gpb"); bc_load(gpb, g_ple, b_gpb)
    gfin = A.alloc(1024); b_gfin = Buf("gfin"); bc_load(gfin, g_fin, b_gfin)
    pn = [A.alloc(1024, BF16) for _ in range(2)]; b_pn = [Buf("pn%d" % i) for i in range(2)]
    pnT = [A.alloc(1024, BF16) for _ in range(2)]; b_pnT = [Buf("pnT%d" % i) for i in range(2)]
    p32 = [A.alloc(256) for _ in range(2)]; b_p32 = [Buf("p32%d" % i) for i in range(2)]
    pbf = [A.alloc(256, BF16) for _ in range(2)]; b_pbf = [Buf("pbf%d" % i) for i in range(2)]
    pT = [A.alloc(256, BF16) for _ in range(2)]; b_pT = [Buf("pT%d" % i) for i in range(2)]
    sgp = [A.alloc(512) for _ in range(2)]; b_sgp = [Buf("sgp%d" % i) for i in range(2)]
    ot = [A.alloc(1024) for _ in range(2)]; b_ot = [Buf("ot%d" % i) for i in range(2)]
    cnt = [0]
    for j in range(NJ):
        sl = j % 2
        k.ld(p32[sl], p_own[j * 128:(j + 1) * 128, :], b_p32[sl])
        k.cp("dve", pbf[sl], p32[sl], [b_p32[sl]], [b_pbf[sl]])
        transpose_to(pT[sl], b_pT[sl], pbf[sl], b_pbf[sl], 2, grp="pfm", banks=(6, 7), eng="act")
        norm_tile((h13[:, j, :], b_h1[j]), sl, gpb, b_gpb, pn[sl], b_pn[sl])
        transpose_to(pnT[sl], b_pnT[sl], pn[sl], b_pn[sl], 8, grp="pfm", banks=(6, 7), eng="act")
        n3 = v3(pnT[sl], 8); t3 = v3(pT[sl], 2)
        for b in range(2):
            pgt, bpgt = k.ps("pfg", (0, 1, 2))
            for kc in range(8):
                k.mm(pgt[:, 0:512], n3[:, kc, :], wpg3[:, kc, 512 * b:512 * b + 512], kc == 0, kc == 7, [b_pnT[sl], b_wpg], [bpgt], inc=(kc == 7))
            sg = sgp[cnt[0] % 2]; bsg = b_sgp[cnt[0] % 2]; cnt[0] += 1
            sigmoid_from(sg, bsg, pgt[:, 0:512], bpgt)
            ppp, bppp = k.ps("pfp", (3, 4, 5))
            for c in range(2):
                k.mm(ppp[:, 0:512], t3[:, c, :], wpp3[:, c, 512 * b:512 * b + 512], c == 0, c == 1, [b_pT[sl], b_wpp], [bppp], inc=(c == 1))
            k.tt(sg, sg, ppp[:, 0:512], ALU.mult, [bsg, bppp], [bsg])
            hsl = h13[:, j, 512 * b:512 * b + 512]
            k.tt(hsl, hsl, sg, ALU.add, [b_h1[j], bsg], [b_h1[j]])
        s = st4[sl]; bs = b_st4[sl]
        k.act(junk, h13[:, j, :], AF.Square, [b_h1[j]], [b_junk, bs], accum_out=s[:, 0:1])
        k.rstd(s[:, 0:1], 1.0 / 1024, s[:, 1:2], s[:, 2:3], [bs], [bs])
        k.stt(ot[sl], h13[:, j, :], s[:, 2:3], gfin, ALU.mult, ALU.mult, [b_h1[j], bs, b_gfin], [b_ot[sl]])
        k.st(out_own[j * 128:(j + 1) * 128, :], ot[sl], b_ot[sl], d_out)
    if dummy is not None:
        dz = A.alloc(8); b_dz = Buf("dz")
        for i in range(dummy[1]):
            if dummy[0] == "pe":
                k.mm(k.psb[0][0][:, 0:2], IDB, IDB[:, 0:2], True, True, [b_cb], [k.psb[0][1]], inc=(i % 64 == 63))
            else:
                k.memset(dz, float(i % 7), [b_dz], eng=dummy[0])
    P.barrier()
    P.emit()
    print("per-engine stream lengths (instr + waits):", {e: len(P.ops[e]) + sum(len(w) for w, _, _ in P.ops[e]) for e in ENGS})
    print("instructions:", P.n_inst, "sems:", len(P.semkeys), "sbuf peak words:", A.peak,
          "counts:", {e: P.cnt[e] for e in ENGS})
    return nc


def _consts():
    bf = ml_dtypes.bfloat16
    s = np.arange(128)[:, None]; t = np.arange(128)[None, :]
    ut = (s <= t).astype(np.float32)
    c_f32 = np.concatenate([-ut, -np.ones((128, 128), np.float32), np.eye(128, dtype=np.float32)], axis=1)
    c_e = np.zeros((64, 64, 128), np.float32)
    for n in range(64):
        c_e[n, n, :] = NEGB
    return c_f32, ut, c_e.reshape(64, 64 * 128).astype(bf)


def make_in_maps(inp):
    bf = ml_dtypes.bfloat16
    f = lambda a: np.ascontiguousarray(np.asarray(a, dtype=np.float32))
    x = f(inp["x"])[0]; p = f(inp["p"])[0, 0]
    w_in = f(inp["w_in"])[0]
    o = np.cumsum([0, 512, 512, 512, 512, 512, 512, 512, 4, 4, 1024, 1024])
    aq, ak, av, mq, mk_, mv, mo, mi, mf, ga, gm = [w_in[:, o[i]:o[i + 1]] for i in range(11)]
    shared = {
        "g_mix": f(inp["mix_norm_g"]), "g_ffn": f(inp["ffn_norm_g"]), "g_ple": f(inp["ple_norm_g"]),
        "g_fin": f(inp["final_norm_g"]).reshape(1, 1024), "g_ml": f(inp["mlstm_norm_g"]),
        "w_kv0": f(np.concatenate([ak[:, 0:256], av[:, 0:256]], 1)), "w_kv1": f(np.concatenate([ak[:, 256:512], av[:, 256:512]], 1)),
        "w_q0": f(aq[:, 0:256]), "w_q1": f(aq[:, 256:512]),
        "w_ms": f(np.concatenate([mk_, mv, mi, mf], 1)), "w_mo": f(np.concatenate([mq, mo], 1)),
        "w_g": f(np.concatenate([ga, gm], 1)), "b_gate": f(inp["b_gate"]),
        "ifb": f(inp["mlstm_if_b"]),
        "w_ua": f(inp["w_up_attn"])[0], "w_um": f(inp["w_up_mlstm"])[0], "w_out": f(inp["w_out"])[0],
        "w_r": f(np.concatenate([f(inp["router_group_w"])[0], f(inp["router_expert_w"])[0]], 1)),
        "b_r": f(np.concatenate([f(inp["router_group_b"]), f(inp["router_expert_b"])], 1)),
        "e_wg": f(inp["expert_w_gate"])[0].reshape(32 * 1024, 512), "e_wu": f(inp["expert_w_up"])[0].reshape(32 * 1024, 512),
        "e_wd": f(inp["expert_w_down"])[0].reshape(32 * 512, 1024),
        "w_pg": f(inp["w_ple_gate"])[0], "w_pp": f(inp["w_ple_proj"])[0],
    }
    cw_ = f(inp["conv_w"])[0]; cb_ = f(inp["conv_b"])[0]
    conv_wb = np.zeros((128, 40), np.float32)
    for hq in range(8):
        ch = np.arange(128) + 128 * hq
        conv_wb[:, hq * 4:hq * 4 + 4] = cw_[:, ch].T
        conv_wb[:, 32 + hq] = cb_[ch]
    shared["conv_wb"] = conv_wb
    c_f32, ut, c_e = _consts()
    shared["c_f32"] = c_f32; shared["c_e"] = c_e
    x_t = x.reshape(128, 128, 1024); p_t = p.reshape(128, 128, 256)
    kk = np.arange(128)[:, None]; qq = np.arange(128)[None, :]
    tri = np.where(kk <= qq, 0.0, -NEGB).astype(np.float32)
    maps = []
    for c in range(NCORES):
        par = c % 2; pad = 2 * ((7 - c) // 2)
        xs = np.zeros((128, 128, 1024), np.float32)
        xs[pad:] = x_t[:128 - pad]
        own = [8 * j + c for j in range(16)]
        cm0 = tri if par == 0 else np.zeros((128, 128), np.float32)
        cm1 = np.full((128, 128), -NEGB, np.float32) if par == 0 else tri
        c_bf = np.concatenate([np.eye(128, dtype=np.float32), ut, cm0, cm1], 1).astype(bf)
        c_core = np.zeros((128, 130), np.float32)
        c_core[:, 0:128] = (np.arange(128)[None, :] >= pad).astype(np.float32)
        c_core[:, 128] = par; c_core[:, 129] = 1 - par
        gmask = np.full((16, 64), -1e30, np.float32)
        for j in range(16):
            gmask[j, pad // 2:4 * j + 3] = 0.0
        m = dict(shared)
        m.update({"x_shift": xs.reshape(128 * 128, 1024), "x_own": np.ascontiguousarray(x_t[own]).reshape(2048, 1024),
                  "p_own": np.ascontiguousarray(p_t[own]).reshape(2048, 256), "c_bf": c_bf, "c_core": c_core,
                  "c_gmask": gmask.reshape(1, 1024)})
        maps.append(m)
    return maps


_NC_CACHE = {}


def kernel(**inputs):
    if "nc" not in _NC_CACHE:
        _NC_CACHE["nc"] = build_program(False)
    nc = _NC_CACHE["nc"]
    maps = make_in_maps(inputs)
    res = run_bass_kernel_spmd(nc, maps, core_ids=list(range(NCORES)))
    out = np.zeros((128, 128, 1024), np.float32)
    for c in range(NCORES):
        o = np.asarray(res.results[c]["out_own"], dtype=np.float32).reshape(16, 128, 1024)
        for j in range(16):
            out[8 * j + c] = o[j]
    return out.reshape(1, 16384, 1024)
```

```python
import numpy as np
import ml_dtypes
from contextlib import ExitStack
import concourse.bass as bass
import concourse.mybir as mybir
from concourse.bass_utils import run_bass_kernel_spmd

F32 = mybir.dt.float32
BF16 = mybir.dt.bfloat16
ALU = mybir.AluOpType
AF = mybir.ActivationFunctionType
AX = mybir.AxisListType
ENGS = ("pe", "act", "dve", "pool", "sp")
NCORES = 8
EPS = 1e-6
NEGB = 30000.0


class Buf:
    __slots__ = ("name", "w", "r", "dsem")

    def __init__(self, name):
        self.name = name
        self.w = None
        self.r = []
        self.dsem = None


class Prog:
    def __init__(self, nc):
        self.nc = nc
        self.ops = {e: [] for e in ENGS}
        self.cnt = {e: 0 for e in ENGS}
        self.seen = {e: {} for e in ENGS}
        self.dma_cnt = {}
        self.semkeys = []
        self.semset = set()
        self.n_inst = 0

    LIM = 4000

    def _tok(self, eng, val):
        ep = (val - 1) // self.LIM
        key = "%s#%d" % (eng, ep)
        if key not in self.semset:
            self.semset.add(key)
            self.semkeys.append(key)
        return (key, val - ep * self.LIM)

    def _need(self, eng, deps):
        best = {}
        for k, v in deps:
            if eng == "pe" and k.startswith("pe#"):
                continue
            if v <= self.seen[eng].get(k, 0):
                continue
            if v > best.get(k, 0):
                best[k] = v
        for k, v in best.items():
            self.seen[eng][k] = v
        return list(best.items())

    def _deps(self, reads, writes):
        deps = []
        for b in reads:
            if b.w is not None:
                deps.append(b.w)
        for b in writes:
            if b.w is not None:
                deps.append(b.w)
            deps.extend(b.r)
        return deps

    def op(self, eng, fn, reads=(), writes=(), inc=True):
        waits = self._need(eng, self._deps(reads, writes))
        val = self.cnt[eng] + 1
        if inc:
            self.cnt[eng] = val
        tok = self._tok(eng, val)
        self.ops[eng].append((waits, fn, (tok[0], 1) if inc else None))
        for b in reads:
            b.r.append(tok)
            if len(b.r) > 24:
                b.r = self._compact(b.r)
        for b in writes:
            b.w = tok
            b.r = []
        self.n_inst += 1

    @staticmethod
    def _compact(r):
        best = {}
        for k, v in r:
            if v > best.get(k, 0):
                best[k] = v
        return list(best.items())

    def dma(self, eng, fn, anchor, reads=(), writes=(), amt=16):
        if anchor.dsem is None:
            anchor.dsem = "d%d" % len(self.semkeys)
            self.semkeys.append(anchor.dsem)
            self.dma_cnt[anchor.dsem] = 0
        k = anchor.dsem
        waits = self._need(eng, self._deps(reads, writes))
        self.dma_cnt[k] += amt
        tok = (k, self.dma_cnt[k])
        self.ops[eng].append((waits, fn, (k, amt)))
        for b in reads:
            b.r.append(tok)
        for b in writes:
            b.w = tok
            b.r = []
        self.n_inst += 1

    def barrier(self):
        deps = [self._tok(e, self.cnt[e]) for e in ENGS if self.cnt[e] > 0]
        deps += [(k, v) for k, v in self.dma_cnt.items() if v > 0]
        for e in ENGS:
            waits = self._need(e, deps)
            if waits:
                self.ops[e].append((waits, None, None))

    def emit(self):
        nc = self.nc
        with ExitStack() as st:
            sems = {}
            for k in self.semkeys:
                sems[k] = st.enter_context(nc.semaphore("s_" + k.replace("#", "_")))
            block = st.enter_context(nc.Block())

            def run(eobj, name):
                for waits, fn, inc in self.ops[name]:
                    for k, v in waits:
                        eobj.wait_ge(sems[k], v)
                    if fn is None:
                        continue
                    ins = fn(eobj)
                    if inc is not None:
                        ins.then_inc(sems[inc[0]], inc[1])

            @block.tensor
            def _(e):
                run(e, "pe")

            @block.scalar
            def _(e):
                run(e, "act")

            @block.vector
            def _(e):
                run(e, "dve")

            @block.gpsimd
            def _(e):
                run(e, "pool")

            @block.sync
            def _(e):
                run(e, "sp")


class Arena:
    def __init__(self, nc, words):
        self.t = nc.alloc_sbuf_tensor("arena", [128, words], F32)
        self.words = words
        self.top = 0
        self.peak = 0

    def mark(self):
        return self.top

    def release(self, m):
        self.top = m

    def alloc(self, cols, dtype=F32):
        w = cols if dtype == F32 else (cols + 1) // 2
        w = (w + 7) // 8 * 8
        a = self.top
        self.top += w
        self.peak = max(self.peak, self.top)
        assert self.top <= self.words, "SBUF arena overflow %d > %d" % (self.top, self.words)
        ap = self.t[:, a:a + w]
        if dtype != F32:
            ap = ap.bitcast(dtype)
        return ap[:, 0:cols]


def v3(ap, a):
    return ap.rearrange("p (a b) -> p a b", a=a)


class K:
    def __init__(self, nc, dbg=False):
        self.nc = nc
        self.P = Prog(nc)
        self.A = Arena(nc, 52600)
        self.dbg = dbg
        self.psb = []
        for i in range(8):
            t = nc.alloc_psum_tensor("psb%d" % i, [128, 512], F32)
            self.psb.append((t, Buf("ps%d" % i)))
        self.rr = {}

    def ps(self, group, banks):
        i = self.rr.get(group, 0)
        self.rr[group] = i + 1
        t, b = self.psb[banks[i % len(banks)]]
        return t, b

    def mm(self, out, lhsT, rhs, start, stop, reads, writes, inc=True):
        self.P.op("pe", lambda e: e.matmul(out, lhsT=lhsT, rhs=rhs, start=start, stop=stop), reads, writes, inc)

    def tr(self, out, in_, ident, reads, writes, inc=True):
        self.P.op("pe", lambda e: e.transpose(out=out, in_=in_, identity=ident), reads, writes, inc)

    def act(self, out, in_, func, reads, writes, bias=None, scale=None, accum_out=None, eng="act"):
        kw = {}
        if bias is not None:
            kw["bias"] = bias
        if scale is not None:
            kw["scale"] = scale
        if accum_out is not None:
            kw["accum_out"] = accum_out
        self.P.op(eng, lambda e: e.activation(out=out, in_=in_, func=func, **kw), reads, writes)

    def cp(self, eng, out, in_, reads, writes):
        if eng == "act":
            self.P.op("act", lambda e: e.copy(out=out, in_=in_), reads, writes)
        else:
            self.P.op(eng, lambda e: e.tensor_copy(out=out, in_=in_), reads, writes)

    def ts(self, out, in0, s1, s2, op0, op1, reads, writes, eng="dve"):
        if op1 is None:
            self.P.op(eng, lambda e: e.tensor_scalar(out=out, in0=in0, scalar1=s1, scalar2=None, op0=op0), reads, writes)
        else:
            self.P.op(eng, lambda e: e.tensor_scalar(out=out, in0=in0, scalar1=s1, scalar2=s2, op0=op0, op1=op1), reads, writes)

    def stt(self, out, in0, scalar, in1, op0, op1, reads, writes, eng="dve"):
        self.P.op(eng, lambda e: e.scalar_tensor_tensor(out=out, in0=in0, scalar=scalar, in1=in1, op0=op0, op1=op1), reads, writes)

    def tt(self, out, in0, in1, op, reads, writes, eng="dve"):
        self.P.op(eng, lambda e: e.tensor_tensor(out=out, in0=in0, in1=in1, op=op), reads, writes)

    def recip(self, out, in_, reads, writes):
        self.P.op("dve", lambda e: e.reciprocal(out=out, in_=in_), reads, writes)

    def memset(self, ap, val, writes, eng="dve"):
        self.P.op(eng, lambda e: e.memset(ap, val), (), writes)

    def ld(self, out, in_, buf, eng="sp", reads=()):
        self.P.dma(eng, lambda e: e.dma_start(out=out, in_=in_), buf, reads=reads, writes=[buf])

    def st(self, out, in_, buf, dbuf, eng="sp"):
        self.P.dma(eng, lambda e: e.dma_start(out=out, in_=in_), buf, reads=[buf], writes=[dbuf])

    def ldw(self, dst3, src, kc, buf, eng="pool"):
        s3 = src.rearrange("(kc p) n -> p kc n", p=128)
        for k in range(kc):
            o = dst3[:, k, :]
            i = s3[:, k, :]
            self.P.dma(eng, lambda e, o=o, i=i: e.dma_start(out=o, in_=i), buf, writes=[buf])

    def rstd(self, ss, inv_n, tmp, out, rb, wb):
        self.act(tmp, ss, AF.Ln, list(rb) + [self.b_eps], wb, bias=self.eps_ap, scale=inv_n)
        self.act(out, tmp, AF.Exp, wb, wb, scale=-0.5)


def build_program(dbg=False, stop=None, NT=128, dummy=None, NE=32):
    nc = bass.Bass("TRN2", target_bir_lowering=False)
    k = K(nc, dbg)
    P, A = k.P, k.A

    def din(name, shape, dt=F32):
        return nc.dram_tensor(name, list(shape), dt, kind="ExternalInput").ap()

    x_shift = din("x_shift", [NT * 128, 1024])
    x_own = din("x_own", [16 * 128, 1024])
    p_own = din("p_own", [16 * 128, 256])
    g_mix = din("g_mix", [1, 1024]); g_ffn = din("g_ffn", [1, 1024]); g_ple = din("g_ple", [1, 1024]); g_fin = din("g_fin", [1, 1024])
    g_ml = din("g_ml", [1, 512])
    w_kv = [din("w_kv%d" % h, [1024, 512]) for h in range(2)]
    w_q = [din("w_q%d" % h, [1024, 256]) for h in range(2)]
    w_ms = din("w_ms", [1024, 1032])
    w_mo = din("w_mo", [1024, 1024])
    w_g = din("w_g", [1024, 2048])
    b_gate = din("b_gate", [1, 2048])
    conv_wb = din("conv_wb", [128, 40])
    ifb = din("ifb", [1, 8])
    w_ua = din("w_ua", [512, 1024]); w_um = din("w_um", [512, 1024]); w_out = din("w_out", [1024, 1024])
    w_r = din("w_r", [1024, 36]); b_r = din("b_r", [1, 36])
    c_f32 = din("c_f32", [128, 3 * 128])
    c_bf = din("c_bf", [128, 4 * 128], BF16)
    c_e = din("c_e", [64, 64 * 128], BF16)
    c_core = din("c_core", [128, 130])
    c_gmask = din("c_gmask", [1, 16 * 64])
    out_own = nc.dram_tensor("out_own", [16 * 128, 1024], F32, kind="ExternalOutput").ap()
    okind = "ExternalOutput" if dbg else "Internal"
    s_xnT = nc.dram_tensor("s_xnT", [(NT + 16) * 128, 1024], BF16).ap()
    s_ya = nc.dram_tensor("s_ya", [16 * 128, 512], BF16, kind=okind).ap()
    s_ym = nc.dram_tensor("s_ym", [16 * 128, 512], BF16, kind=okind).ap()
    d_xnT = [Buf("dxnT%d" % i) for i in range(NT + 16)]
    d_ya = [Buf("dya%d" % i) for i in range(16)]
    d_ym = [Buf("dym%d" % i) for i in range(16)]
    d_out = Buf("dout")

    cf = A.alloc(384); b_cf = Buf("cf")
    cb = A.alloc(512, BF16); b_cb = Buf("cb")
    cc = A.alloc(130); b_cc = Buf("cc")
    epsb = A.alloc(1); b_eps = Buf("eps")
    k.ld(cf, c_f32, b_cf); k.ld(cb, c_bf, b_cb); k.ld(cc, c_core, b_cc)
    k.memset(epsb, EPS, [b_eps])
    k.eps_ap = epsb[:, 0:1]; k.b_eps = b_eps
    NUT, NONES, ID32 = cf[:, 0:128], cf[:, 128:256], cf[:, 256:384]
    IDB, UTB, CM = cb[:, 0:128], cb[:, 128:256], [cb[:, 256:384], cb[:, 384:512]]
    VALID, PAR, NPAR = cc[:, 0:128], cc[:, 128:129], cc[:, 129:130]
    junk = A.alloc(1024, BF16); b_junk = Buf("junk")
    st4 = [A.alloc(4) for _ in range(2)]; b_st4 = [Buf("st%d" % i) for i in range(2)]
    base_mark = A.mark()

    def bc_load(dst, src_row, buf):
        k.ld(dst, src_row.partition_broadcast(128), buf)

    gb = A.alloc(1024); b_gb = Buf("gb")
    bc_load(gb, g_mix, b_gb)
    xt = [A.alloc(1024) for _ in range(2)]; b_xt = [Buf("xt%d" % i) for i in range(2)]
    xn = [A.alloc(1024, BF16) for _ in range(2)]; b_xn = [Buf("xn%d" % i) for i in range(2)]
    xT_s = [A.alloc(1024, BF16) for _ in range(2)]; b_xTs = [Buf("xTs%d" % i) for i in range(2)]

    def norm_tile(src_ap, sl, gbt, b_gbt, dst_bf, b_dst):
        s = st4[sl]; bs = b_st4[sl]
        k.act(junk, src_ap[0], AF.Square, [src_ap[1]], [b_junk, bs], accum_out=s[:, 0:1])
        k.rstd(s[:, 0:1], 1.0 / 1024, s[:, 1:2], s[:, 2:3], [bs], [bs])
        k.stt(dst_bf, src_ap[0], s[:, 2:3], gbt, ALU.mult, ALU.mult, [src_ap[1], bs, b_gbt], [b_dst])

    def transpose_to(dst_bf3, b_dst, src_bf, b_src, nchunk, grp="misc", banks=(5, 6, 7), eng="act"):
        pt, bp = k.ps(grp, banks)
        ptb = pt[:, :].bitcast(BF16)
        for c in range(nchunk):
            k.tr(ptb[:, c * 128:(c + 1) * 128], src_bf[:, c * 128:(c + 1) * 128], IDB, [b_src, b_cb], [bp], inc=(c == nchunk - 1))
        k.cp(eng, dst_bf3, ptb[:, 0:nchunk * 128], [bp], [b_dst])

    for T in range(NT + 16):
        sl = T % 2
        src = x_shift[T * 128:(T + 1) * 128, :] if T < NT else x_own[(T - NT) * 128:(T - NT + 1) * 128, :]
        k.ld(xt[sl], src, b_xt[sl])
        norm_tile((xt[sl], b_xt[sl]), sl, gb, b_gb, xn[sl], b_xn[sl])
        transpose_to(xT_s[sl], b_xTs[sl], xn[sl], b_xn[sl], 8)
        k.st(s_xnT[T * 128:(T + 1) * 128, :], xT_s[sl], b_xTs[sl], d_xnT[T], eng="act")
    P.barrier()
    A.release(base_mark)

    wms = A.alloc(8 * 1032, BF16); b_wms = Buf("wms"); wms3 = v3(wms, 8)
    wmo = A.alloc(8 * 1024, BF16); b_wmo = Buf("wmo"); wmo3 = v3(wmo, 8)
    k.ldw(wms3, w_ms, 8, b_wms); k.ldw(wmo3, w_mo, 8, b_wmo)
    cwb = A.alloc(40); b_cwb = Buf("cwb"); k.ld(cwb, conv_wb, b_cwb)
    ifbb = A.alloc(8); b_ifbb = Buf("ifbb"); bc_load(ifbb, ifb, b_ifbb)
    gml = A.alloc(512); b_gml = Buf("gml"); bc_load(gml, g_ml, b_gml)
    xT = [A.alloc(1024, BF16) for _ in range(2)]; b_xT = [Buf("xT%d" % i) for i in range(2)]
    C32 = A.alloc(4 * 129); b_C32 = [Buf("C32_%d" % h) for h in range(4)]; C32v = v3(C32, 4)
    Cb = A.alloc(4 * 130, BF16); b_Cb = [Buf("Cb_%d" % h) for h in range(4)]; Cbv = v3(Cb, 4)
    k.memset(C32, 0.0, b_C32); k.memset(Cb, 0.0, b_Cb)
    kpre = [A.alloc(132) for _ in range(4)]; b_kpre = [Buf("kpre%d" % h) for h in range(4)]
    qpre = [A.alloc(132) for _ in range(4)]; b_qpre = [Buf("qpre%d" % h) for h in range(4)]
    for h in range(4):
        k.memset(kpre[h], 0.0, [b_kpre[h]]); k.memset(qpre[h], 0.0, [b_qpre[h]])
    NSL = 4
    def mk(n, cols, dt=F32, pre=""):
        return [A.alloc(cols, dt) for _ in range(n)], [Buf(pre + str(i)) for i in range(n)]
    lif, b_lif = mk(2, 8, pre="lif"); gt, b_gt = mk(2, 24, pre="gt")
    ck, b_ck = mk(NSL, 128, pre="ck"); ex, b_ex = mk(NSL, 128, pre="ex")
    kTb, b_kTb = mk(NSL, 128, BF16, "kTb"); qTb, b_qTb = mk(NSL, 128, BF16, "qTb")
    ktok, b_ktok = mk(NSL, 128, BF16, "ktok"); vp, b_vp = mk(NSL, 130, BF16, "vp")
    tmpc, b_tmpc = mk(NSL, 129, pre="tmpc"); Sm, b_Sm = mk(NSL, 128, BF16, "Sm")
    hc, b_hc = mk(NSL, 128, pre="hc"); sm8, b_sm8 = mk(NSL, 8, pre="sm8")
    sig2, b_sig2 = mk(2, 512, pre="sig")
    ympos = [A.alloc(512, BF16) for _ in range(2)]; b_ympos = [Buf("ympos%d" % i) for i in range(2)]
    ymd = A.alloc(512); b_ymd = Buf("ymd")
    ymo = [A.alloc(512, BF16) for _ in range(2)]; b_ymo = [Buf("ymo%d" % i) for i in range(2)]

    ckq, b_ckq = mk(NSL, 128, pre="ckq")
    pcs = [(k.psb[6 + i][0][:, 0:129], Buf("pcs%d" % i)) for i in range(2)]

    def conv_silu(pre, b_pre, hq, out_bf, b_out, c, bcx):
        w0 = cwb[:, hq * 4:hq * 4 + 1]
        k.ts(c, pre[:, 0:128], w0, cwb[:, 32 + hq:33 + hq], ALU.mult, ALU.add, [b_pre, b_cwb], [bcx])
        for tap in range(1, 4):
            k.stt(c, pre[:, tap:tap + 128], cwb[:, hq * 4 + tap:hq * 4 + tap + 1], c, ALU.mult, ALU.add, [b_pre, b_cwb, bcx], [bcx])
        k.cp("pool", pre[:, 0:3], pre[:, 128:131], [b_pre], [b_pre])
        k.act(out_bf, c, AF.Silu, [bcx], [b_out])

    def pm_prologue(T):
        sl = T % 2
        x3 = v3(xT[sl], 8); bx = b_xT[sl]
        if T + 1 < NT:
            k.ld(xT[(T + 1) % 2], s_xnT[(T + 1) * 128:(T + 2) * 128, :], b_xT[(T + 1) % 2], reads=[d_xnT[T + 1]])
        pos = T % 8
        outp = pos >= 6
        pv, bpv = k.ps("pmv", (0, 1))
        for kc in range(8):
            k.mm(pv[:, 0:512], x3[:, kc, :], wms3[:, kc, 512:1024], kc == 0, kc == 7, [bx, b_wms], [bpv], inc=(kc == 7))
        pif, bpif = k.ps("pmisc", (5,))
        for kc in range(8):
            k.mm(pif[:, 0:8], x3[:, kc, :], wms3[:, kc, 1024:1032], kc == 0, kc == 7, [bx, b_wms], [bpif], inc=(kc == 7))
        L = lif[sl]; bL = b_lif[sl]; G = gt[sl]; bG = b_gt[sl]
        k.tt(L, pif[:, 0:8], ifbb, ALU.add, [bpif, b_ifbb], [bL])
        k.act(G[:, 0:4], L[:, 4:8], AF.Exp, [bL], [bG], scale=-1.0)
        k.act(G[:, 0:4], G[:, 0:4], AF.Ln, [bG], [bG], bias=1.0)
        pb, bpb = k.ps("pmisc", (5,))
        k.mm(pb[:, 0:4], NUT, G[:, 0:4], True, True, [b_cf, bG], [bpb], inc=False)
        k.mm(pb[:, 4:8], NONES, G[:, 0:4], True, True, [b_cf, bG], [bpb])
        k.act(G[:, 8:16], pb[:, 0:8], AF.Exp, [bpb], [bG])
        k.tt(G[:, 16:20], L[:, 0:4], pb[:, 0:4], ALU.subtract, [bL, bpb], [bG])
        k.act(G[:, 4:8], G[:, 16:20], AF.Exp, [bG], [bG])
        k.ts(G[:, 4:8], G[:, 4:8], VALID[:, T:T + 1], 128 ** -0.5, ALU.mult, ALU.mult, [bG, b_cc], [bG])
        if not outp:
            k.tt(G[:, 20:24], G[:, 4:8], G[:, 12:16], ALU.mult, [bG], [bG])
        sig = b_sig = None
        if outp:
            sig = sig2[pos - 6]; b_sig = b_sig2[pos - 6]
            po, bpo = k.ps("pmk", (2, 3, 4))
            for kc in range(8):
                k.mm(po[:, 0:512], x3[:, kc, :], wmo3[:, kc, 512:1024], kc == 0, kc == 7, [bx, b_wmo], [bpo], inc=(kc == 7))
            k.act(sig, po[:, 0:512], AF.Sigmoid, [bpo], [b_sig])
        return (T, x3, bx, pos, outp, pv, bpv, G, bG, sig, b_sig)

    def pm_a1(ctx, h):
        T, x3, bx, pos, outp, pv, bpv, G, bG, sig, b_sig = ctx
        pk, bpk = k.ps("pmk", (2, 3, 4))
        for kc in range(8):
            k.mm(pk[:, 0:128], wms3[:, kc, 128 * h:128 * h + 128], x3[:, kc, :], kc == 0, kc == 7, [bx, b_wms], [bpk], inc=(kc == 7))
        k.cp("act", kpre[h][:, 3:131], pk[:, 0:128], [bpk], [b_kpre[h]])
        if pos >= 5:
            pq, bpq = k.ps("pmk", (2, 3, 4))
            for kc in range(8):
                k.mm(pq[:, 0:128], wmo3[:, kc, 128 * h:128 * h + 128], x3[:, kc, :], kc == 0, kc == 7, [bx, b_wmo], [bpq], inc=(kc == 7))
            k.cp("act", qpre[h][:, 3:131], pq[:, 0:128], [bpq], [b_qpre[h]])

    def pm_a2(ctx, h, hs):
        T, x3, bx, pos, outp, pv, bpv, G, bG, sig, b_sig = ctx
        conv_silu(kpre[h], b_kpre[h], 4 + h, kTb[hs], b_kTb[hs], ck[hs], b_ck[hs])
        if outp:
            conv_silu(qpre[h], b_qpre[h], h, qTb[hs], b_qTb[hs], ckq[hs], b_ckq[hs])
        elif pos == 5:
            k.cp("pool", qpre[h][:, 0:3], qpre[h][:, 128:131], [b_qpre[h]], [b_qpre[h]])
        V = vp[hs]; bV = b_vp[hs]
        gc = (4 + h) if outp else (20 + h)
        k.ts(V[:, 0:128], pv[:, 128 * h:128 * h + 128], G[:, gc:gc + 1], None, ALU.mult, None, [bpv, bG], [bV])
        k.cp("pool", V[:, 128:129], G[:, gc:gc + 1], [bG], [bV])

    def pm_bpe(ctx, h, hs, pcslot):
        T, x3, bx, pos, outp, pv, bpv, G, bG, sig, b_sig = ctx
        V = vp[hs]; bV = b_vp[hs]
        ptk, bptk = k.ps("pmisc", (5,))
        ptkb = ptk[:, :].bitcast(BF16)
        k.tr(ptkb[:, 0:128], kTb[hs], IDB, [b_kTb[hs], b_cb], [bptk])
        k.cp("act", ktok[hs], ptkb[:, 0:128], [bptk], [b_ktok[hs]])
        if outp:
            pss, bpss = k.ps("pmk", (2, 3, 4))
            k.mm(pss[:, 0:128], kTb[hs], qTb[hs], True, True, [b_kTb[hs], b_qTb[hs]], [bpss])
            k.tt(Sm[hs], pss[:, 0:128], UTB, ALU.mult, [bpss, b_cb], [b_Sm[hs]])
            pn, bpn = k.ps("pmk", (2, 3, 4))
            k.mm(pn[:, 0:129], Sm[hs], V[:, 0:129], True, False, [b_Sm[hs], bV], [bpn], inc=False)
            k.mm(pn[:, 0:129], qTb[hs], Cbv[:, h, 0:129], False, True, [b_qTb[hs], b_Cb[h]], [bpn])
            s8 = sm8[hs]; bs8 = b_sm8[hs]
            et = G[:, 8 + h:9 + h]
            k.tt(s8[:, 0:1], pn[:, 128:129], et, ALU.mult, [bpn, bG], [bs8])
            k.stt(s8[:, 1:2], s8[:, 0:1], -1.0, s8[:, 0:1], ALU.mult, ALU.max, [bs8], [bs8])
            k.ts(s8[:, 1:2], s8[:, 1:2], 1.0, None, ALU.max, None, [bs8], [bs8])
            k.recip(s8[:, 2:3], s8[:, 1:2], [bs8], [bs8])
            k.tt(s8[:, 3:4], s8[:, 2:3], et, ALU.mult, [bs8, bG], [bs8])
            H = hc[hs]; bH = b_hc[hs]
            k.stt(H, pn[:, 0:128], s8[:, 3:4], sig[:, 128 * h:128 * h + 128], ALU.mult, ALU.mult, [bpn, bs8, b_sig], [bH])
            k.act(ex[hs], H, AF.Square, [bH], [b_ex[hs], bs8], accum_out=s8[:, 4:5])
            k.rstd(s8[:, 4:5], 1.0 / 128, s8[:, 5:6], s8[:, 6:7], [bs8], [bs8])
            k.stt(ympos[pos - 6][:, 128 * h:128 * h + 128], H, s8[:, 6:7], gml[:, 128 * h:128 * h + 128], ALU.mult, ALU.mult,
                  [bH, bs8, b_gml], [b_ympos[pos - 6]])
        pc, bpc = pcslot
        k.mm(pc[:, 0:129], ktok[hs], V[:, 0:129], True, True, [b_ktok[hs], bV], [bpc])
        if h == 3 and pos == 7:
            j = T // 8
            k.tt(ymd, ympos[1], ympos[0], ALU.subtract, b_ympos, [b_ymd])
            yo = ymo[j % 2]; byo = b_ymo[j % 2]
            k.stt(yo, ymd, PAR, ympos[0], ALU.mult, ALU.add, [b_ymd, b_cc, b_ympos[0]], [byo])
            k.st(s_ym[j * 128:(j + 1) * 128, :], yo, byo, d_ym[j])

    def pm_bdve(ctx, h, hs, pcslot):
        T, x3, bx, pos, outp, pv, bpv, G, bG, sig, b_sig = ctx
        pc, bpc = pcslot
        if outp:
            tc_, btc = tmpc[hs], b_tmpc[hs]
            k.tt(tc_, pc[:, 0:129], C32v[:, h, :], ALU.add, [bpc, b_C32[h]], [btc])
            k.ts(C32v[:, h, :], tc_, G[:, 12 + h:13 + h], None, ALU.mult, None, [btc, bG], [b_C32[h]])
            if pos == 6:
                k.ts(Cbv[:, h, 0:129], tc_, G[:, 12 + h:13 + h], None, ALU.mult, None, [btc, bG], [b_Cb[h]])
        else:
            k.stt(C32v[:, h, :], C32v[:, h, :], G[:, 12 + h:13 + h], pc[:, 0:129], ALU.mult, ALU.add, [b_C32[h], bG, bpc], [b_C32[h]])
            if pos == 5:
                k.cp("act", Cbv[:, h, 0:129], C32v[:, h, :], [b_C32[h]], [b_Cb[h]])

    k.ld(xT[0], s_xnT[0:128, :], b_xT[0], reads=[d_xnT[0]])
    items = [(T, h) for T in range(NT) for h in range(4)]
    NI = len(items)
    ctxs = {}
    for s in range(-3, NI):
        i = s + 3
        if 0 <= i < NI:
            T, h = items[i]
            if h == 0:
                ctxs[T] = pm_prologue(T)
            pm_a1(ctxs[T], h)
        i = s + 1
        if 0 <= i < NI:
            T, h = items[i]
            pm_bpe(ctxs[T], h, i % NSL, pcs[i % 2])
        i = s + 2
        if 0 <= i < NI:
            T, h = items[i]
            pm_a2(ctxs[T], h, i % NSL)
        i = s
        if 0 <= i < NI:
            T, h = items[i]
            pm_bdve(ctxs[T], h, i % NSL, pcs[i % 2])
    P.barrier()
    A.release(base_mark)
    if stop == "PM":
        P.emit(); print("instructions:", P.n_inst, {e: P.cnt[e] for e in ENGS}); return nc

    KT = A.alloc(2 * 16384, BF16); KT3 = v3(KT, 2)
    b_KT = [Buf("KT%d" % t) for t in range(NT)]
    V1 = A.alloc(NT * 260, BF16); V14 = V1.rearrange("p (t h d) -> p t h d", t=NT, h=4)
    b_V1 = [Buf("V1_%d" % t) for t in range(NT)]
    Eb = A.alloc(64 * 128, BF16); b_Eb = Buf("Eb"); Eb3 = v3(Eb, 64)
    k.memset(Eb, 0.0, [b_Eb])
    k.ld(Eb[0:64, :], c_e, b_Eb)
    gmask = A.alloc(1024); b_gmask = Buf("gmask"); bc_load(gmask, c_gmask, b_gmask); gmask3 = v3(gmask, 16)
    wkv = A.alloc(8 * 512, BF16); b_wkv = Buf("wkv"); wkv3 = v3(wkv, 8)
    wq = A.alloc(8 * 256, BF16); b_wq = Buf("wq"); wq3 = v3(wq, 8)
    xT = [A.alloc(1024, BF16) for _ in range(2)]; b_xT = [Buf("axT%d" % i) for i in range(2)]
    xTo = A.alloc(1024, BF16); b_xTo = Buf("xTo")
    ksum = A.alloc(2 * NT); b_ksum = Buf("ksum"); ksum3 = v3(ksum, 2)
    kmT = A.alloc(2 * 64); b_kmT = Buf("kmT"); kmT3 = v3(kmT, 2)
    QTs = A.alloc(512, BF16); b_QTs = Buf("QTs"); QTs4 = QTs.rearrange("p (a s q) -> p a s q", a=2, s=2)
    QT32 = A.alloc(512); b_QT32 = Buf("QT32"); QT324 = QT32.rearrange("p (a s q) -> p a s q", a=2, s=2)
    k.memset(QTs, 0.0, [b_QTs]); k.memset(QT32, 0.0, [b_QT32])
    gm_, b_gm = mk(4, 64, pre="gm"); t8, b_t8 = mk(4, 16, pre="t8")
    b01, b_b01 = mk(4, 64, BF16, "b01"); BT, b_BT = mk(4, 128, BF16, "BT")
    for i_ in range(4):
        k.memset(BT[i_], 0.0, [b_BT[i_]])
    PT, b_PT = mk(3, 512, BF16, "PT")
    yat = [A.alloc(512, BF16) for _ in range(2)]; b_yat = [Buf("yat%d" % i) for i in range(2)]
    r1, b_r1 = mk(2, 2, pre="r1")
    hcount = [0]; gcount = [0]

    for hp in range(2):
        if hp == 1:
            P.barrier()
        k.ldw(wkv3, w_kv[hp], 8, b_wkv); k.ldw(wq3, w_q[hp], 8, b_wq)
        k.memset(kmT, 0.0, [b_kmT]); k.memset(ksum, 0.0, [b_ksum])
        if hp == 0:
            for t0 in range(0, NT, 16):
                k.memset(V1[:, t0 * 260:(t0 + 16) * 260], 1.0, b_V1[t0:t0 + 16])
        k.ld(xT[0], s_xnT[0:128, :], b_xT[0], reads=[d_xnT[0]])
        for T in range(NT):
            sl = T % 2
            x3 = v3(xT[sl], 8); bx = b_xT[sl]
            if T + 1 < NT:
                k.ld(xT[(T + 1) % 2], s_xnT[(T + 1) * 128:(T + 2) * 128, :], b_xT[(T + 1) % 2], reads=[d_xnT[T + 1]])
            for p in range(2):
                pk, bpk = k.ps("paproj", (5, 6, 7))
                for kc in range(8):
                    k.mm(pk[:, 0:128], wkv3[:, kc, 128 * p:128 * p + 128], x3[:, kc, :], kc == 0, kc == 7, [bx, b_wkv], [bpk], inc=(kc == 7))
                k.act(KT3[:, p, T * 128:(T + 1) * 128], pk[:, 0:128], AF.Identity, [bpk], [b_KT[T], b_ksum], accum_out=ksum3[:, p, T:T + 1])
            pv, bpv = k.ps("paproj", (5, 6, 7))
            for kc in range(8):
                k.mm(pv[:, 0:256], x3[:, kc, :], wkv3[:, kc, 256:512], kc == 0, kc == 7, [bx, b_wkv], [bpv], inc=(kc == 7))
            k.cp("dve", V14[:, T, :, 0:64], pv[:, 0:256].rearrange("p (h d) -> p h d", h=4), [bpv], [b_V1[T]])
            if T % 2 == 1:
                n = T // 2
                k.tt(kmT3[:, :, n:n + 1], ksum3[:, :, T - 1:T], ksum3[:, :, T:T + 1], ALU.add, [b_ksum], [b_kmT])
            if T % 8 != 7:
                continue
            j = T // 8
            k.ld(xTo, s_xnT[(NT + j) * 128:(NT + j + 1) * 128, :], b_xTo, reads=[d_xnT[NT + j]])
            xo3 = v3(xTo, 8)
            for p in range(2):
                pq, bpq = k.ps("paproj", (5, 6, 7))
                for kc in range(8):
                    k.mm(pq[:, 0:128], wq3[:, kc, 128 * p:128 * p + 128], xo3[:, kc, :], kc == 0, kc == 7, [b_xTo, b_wq], [bpq], inc=(kc == 7))
                for s_ in range(2):
                    k.ts(QTs4[64 * s_:64 * s_ + 64, p, s_, :], pq[64 * s_:64 * s_ + 64, 0:128], 0.125, None, ALU.mult, None, [bpq], [b_QTs])
                    k.cp("dve", QT324[64 * s_:64 * s_ + 64, p, s_, :], pq[64 * s_:64 * s_ + 64, 0:128], [bpq], [b_QT32])
            ya_t = yat[j % 2]; b_ya_t = b_yat[j % 2]
            nkt = 8 * j + 8
            def prologue_a(hl):
                p, s = hl // 2, hl % 2
                pg, bpg = k.ps("paproj", (5, 6, 7))
                k.mm(pg[:, 0:64], QT324[:, p, s, :], kmT3[:, p, :], True, True, [b_QT32, b_kmT], [bpg])
                g_ = gm_[hl]; bg_ = b_gm[hl]; t_ = t8[hl]; bt_ = b_t8[hl]
                k.tt(g_, pg[:, 0:64], gmask3[:, j, :], ALU.add, [bpg, b_gmask], [bg_])
                P.op("dve", lambda e, o=t_[:, 0:8], i=g_: e.max(out=o, in_=i), [bg_], [bt_])
                k.ts(t_[:, 8:9], t_[:, 2:3], -1e29, None, ALU.max, None, [bt_], [bt_])
                k.ts(b01[hl], g_, t_[:, 8:9], 1.0, ALU.is_ge, ALU.subtract, [bg_, bt_], [b_b01[hl]])

            def prologue_b(hl):
                pbt, bpbt = k.ps("paproj", (5, 6, 7))
                pbtb = pbt[:, :].bitcast(BF16)
                k.tr(pbtb[0:64, 0:128], b01[hl], IDB, [b_b01[hl], b_cb], [bpbt])
                k.cp("dve", BT[hl][0:64, :], pbtb[0:64, 0:128], [bpbt], [b_BT[hl]])

            def qk_group(hl, g0):
                p, s = hl // 2, hl % 2
                pS, bpS = k.ps("pas", (2, 3, 4))
                for i in range(4):
                    kt = g0 + i
                    col = pS[:, i * 128:(i + 1) * 128]
                    k.mm(col, KT3[:, p, kt * 128:(kt + 1) * 128], QTs4[:, p, s, :], True, False, [b_KT[kt], b_QTs], [bpS], inc=False)
                    if kt < nkt - 2:
                        k.mm(col, Eb3[:, kt // 2, :], BT[hl][:, :], False, True, [b_Eb, b_BT[hl]], [bpS], inc=(i == 3))
                    else:
                        k.mm(col, IDB, CM[kt - (nkt - 2)], False, True, [b_cb], [bpS], inc=(i == 3))
                return pS, bpS

            def pv_group(hl, g0, pS, bpS, po, bpo):
                pt_ = PT[gcount[0] % 3]; bpt_ = b_PT[gcount[0] % 3]; gcount[0] += 1
                k.act(pt_, pS[:, 0:512], AF.Exp, [bpS], [bpt_])
                for i in range(4):
                    kt = g0 + i
                    k.mm(po[:, 0:65], pt_[:, i * 128:(i + 1) * 128], V14[:, kt, hl, :], kt == 0, kt == nkt - 1, [bpt_, b_V1[kt]], [bpo],
                         inc=(i == 3))

            groups = list(range(0, nkt, 4))
            prologue_a(0); prologue_b(0)
            for hl in range(4):
                po, bpo = k.ps("pao", (0, 1))
                cur = qk_group(hl, groups[0])
                for gi, g0 in enumerate(groups):
                    nxt = qk_group(hl, groups[gi + 1]) if gi + 1 < len(groups) else None
                    if hl < 3 and gi == 0:
                        prologue_a(hl + 1)
                    if hl < 3 and gi == min(1, len(groups) - 1):
                        prologue_b(hl + 1)
                    pv_group(hl, g0, cur[0], cur[1], po, bpo)
                    cur = nxt
                r_ = r1[hl % 2]; br_ = b_r1[hl % 2]
                k.recip(r_[:, 0:1], po[:, 64:65], [bpo], [br_])
                k.ts(ya_t[:, 64 * hl:64 * hl + 64], po[:, 0:64], r_[:, 0:1], None, ALU.mult, None, [bpo, br_], [b_ya_t])
            k.st(s_ya[j * 128:(j + 1) * 128, 256 * hp:256 * hp + 256], ya_t[:, 0:256], b_ya_t, d_ya[j])
    P.barrier()
    A.release(base_mark)
    if stop == "PA":
        P.emit(); print("instructions:", P.n_inst, {e: P.cnt[e] for e in ENGS}); return nc

    e_wg = din("e_wg", [NE * 1024, 512]); e_wu = din("e_wu", [NE * 1024, 512]); e_wd = din("e_wd", [NE * 512, 1024])
    w_pg = din("w_pg", [1024, 1024]); w_pp = din("w_pp", [256, 1024])
    h1 = A.alloc(16 * 1024); h13 = v3(h1, 16); b_h1 = [Buf("h1_%d" % j) for j in range(16)]
    hnT = A.alloc(8 * 2048, BF16); hnT3 = v3(hnT, 8); b_hnT = [Buf("hnT%d" % j) for j in range(16)]
    cw = A.alloc(16 * 32); cw3 = v3(cw, 16); b_cw = [Buf("cw%d" % j) for j in range(16)]
    NJ = NT // 8
    if NJ < 16:
        k.memset(hnT, 0.0, b_hnT); k.memset(cw, 0.0, b_cw); k.memset(h1, 0.0, b_h1)
    pb_mark = A.mark()
    wg_ = A.alloc(8 * 2048, BF16); b_wg = Buf("wg"); wg3 = v3(wg_, 8)
    wua = A.alloc(4 * 1024, BF16); b_wua = Buf("wua"); wua3 = v3(wua, 4)
    wum = A.alloc(4 * 1024, BF16); b_wum = Buf("wum"); wum3 = v3(wum, 4)
    wo = A.alloc(8 * 1024, BF16); b_wo = Buf("wo"); wo3 = v3(wo, 8)
    wr = A.alloc(8 * 36); b_wr = Buf("wr"); wr3 = v3(wr, 8)
    k.ldw(wg3, w_g, 8, b_wg); k.ldw(wua3, w_ua, 4, b_wua); k.ldw(wum3, w_um, 4, b_wum); k.ldw(wo3, w_out, 8, b_wo)
    for kc in range(8):
        k.ld(wr3[:, kc, :], w_r[kc * 128:(kc + 1) * 128, :], b_wr)
    bgb = A.alloc(2048, BF16); b_bgb = Buf("bgb")
    br32 = A.alloc(36); b_br32 = Buf("br32"); bc_load(br32, b_r, b_br32)
    ones = A.alloc(128); b_ones = Buf("ones"); k.memset(ones, 1.0, [b_ones])
    onesb = A.alloc(128, BF16); b_onesb = Buf("onesb"); k.memset(onesb, 1.0, [b_onesb])
    gfb = A.alloc(1024); b_gfb = Buf("gfb"); bc_load(gfb, g_ffn, b_gfb)
    xTo = A.alloc(1024, BF16); b_xTo = Buf("bxTo")
    yab = A.alloc(512, BF16); b_yab = Buf("yab"); ymb = A.alloc(512, BF16); b_ymb = Buf("ymb")
    yaT = A.alloc(512, BF16); b_yaT = Buf("yaT"); ymT = A.alloc(512, BF16); b_ymT = Buf("ymT")
    yaT3 = v3(yaT, 4); ymT3 = v3(ymT, 4)
    sga = A.alloc(512); b_sga = Buf("sga"); sgm = A.alloc(512); b_sgm = Buf("sgm")
    mrg = A.alloc(1024, BF16); b_mrg = Buf("mrg"); mrgT = A.alloc(1024, BF16); b_mrgT = Buf("mrgT"); mrgT3 = v3(mrgT, 8)
    hn32 = A.alloc(1024); b_hn32 = Buf("hn32")
    hnhi = A.alloc(1024, BF16); b_hnhi = Buf("hnhi"); hnlo = A.alloc(1024, BF16); b_hnlo = Buf("hnlo")
    hiT = A.alloc(1024, BF16); b_hiT = Buf("hiT"); hiT3 = v3(hiT, 8)
    loT = A.alloc(1024, BF16); b_loT = Buf("loT"); loT3 = v3(loT, 8)
    wrh = A.alloc(8 * 36, BF16); b_wrh = Buf("wrh"); wrh3 = v3(wrh, 8)
    wrl = A.alloc(8 * 36, BF16); b_wrl = Buf("wrl"); wrl3 = v3(wrl, 8)
    k.cp("dve", wrh, wr, [b_wr], [b_wrh])
    k.tt(wrl, wr, wrh, ALU.subtract, [b_wr, b_wrh], [b_wrl])
    Lr = A.alloc(40); b_Lr = Buf("Lr"); rs = A.alloc(16); b_rs = Buf("rs"); elm = A.alloc(32); b_elm = Buf("elm")
    c1 = A.alloc(32); b_c1 = Buf("c1")
    for q4 in range(4):
        k.ld(sga, b_gate[:, 512 * q4:512 * q4 + 512].partition_broadcast(128), b_sga)
        k.cp("dve", bgb[:, 512 * q4:512 * q4 + 512], sga, [b_sga], [b_bgb])

    def sigmoid_from(dst, b_dst, ps_ap, b_ps):
        k.act(dst, ps_ap, AF.Sigmoid, [b_ps], [b_dst])

    for j in range(NJ):
        k.ld(h13[:, j, :], x_own[j * 128:(j + 1) * 128, :], b_h1[j])
        k.ld(xTo, s_xnT[(NT + j) * 128:(NT + j + 1) * 128, :], b_xTo, reads=[d_xnT[NT + j]])
        k.ld(yab, s_ya[j * 128:(j + 1) * 128, :], b_yab, reads=[d_ya[j]])
        k.ld(ymb, s_ym[j * 128:(j + 1) * 128, :], b_ymb, reads=[d_ym[j]])
        xo3 = v3(xTo, 8)
        transpose_to(yaT, b_yaT, yab, b_yab, 4, grp="pbm", banks=(6, 7), eng="dve")
        transpose_to(ymT, b_ymT, ymb, b_ymb, 4, grp="pbm", banks=(6, 7), eng="dve")
        for b in range(2):
            cs = slice(512 * b, 512 * b + 512)
            pga, bpga = k.ps("pbg", (0, 1, 2))
            for kc in range(8):
                k.mm(pga[:, 0:512], xo3[:, kc, :], wg3[:, kc, 512 * b:512 * b + 512], kc == 0, kc == 7, [b_xTo, b_wg], [bpga], inc=(kc == 7))
            k.tt(sga, pga[:, 0:512], bgb[:, 512 * b:512 * b + 512], ALU.add, [bpga, b_bgb], [b_sga])
            sigmoid_from(sga, b_sga, sga, b_sga)
            pua, bpua = k.ps("pbu", (3, 4, 5))
            for fc in range(4):
                k.mm(pua[:, 0:512], yaT3[:, fc, :], wua3[:, fc, cs], fc == 0, fc == 3, [b_yaT, b_wua], [bpua], inc=(fc == 3))
            k.tt(sga, sga, pua[:, 0:512], ALU.mult, [b_sga, bpua], [b_sga])
            pgm, bpgm = k.ps("pbg", (0, 1, 2))
            for kc in range(8):
                k.mm(pgm[:, 0:512], xo3[:, kc, :], wg3[:, kc, 1024 + 512 * b:1024 + 512 * b + 512], kc == 0, kc == 7, [b_xTo, b_wg], [bpgm], inc=(kc == 7))
            k.tt(sgm, pgm[:, 0:512], bgb[:, 1024 + 512 * b:1024 + 512 * b + 512], ALU.add, [bpgm, b_bgb], [b_sgm])
            sigmoid_from(sgm, b_sgm, sgm, b_sgm)
            pum, bpum = k.ps("pbu", (3, 4, 5))
            for fc in range(4):
                k.mm(pum[:, 0:512], ymT3[:, fc, :], wum3[:, fc, cs], fc == 0, fc == 3, [b_ymT, b_wum], [bpum], inc=(fc == 3))
            k.tt(sgm, sgm, pum[:, 0:512], ALU.mult, [b_sgm, bpum], [b_sgm])
            k.tt(mrg[:, cs], sga, sgm, ALU.add, [b_sga, b_sgm], [b_mrg])
        transpose_to(mrgT, b_mrgT, mrg, b_mrg, 8, grp="pbm", banks=(6, 7), eng="act")
        for b in range(2):
            ph, bph = k.ps("pbg", (0, 1, 2))
            for kc in range(8):
                k.mm(ph[:, 0:512], mrgT3[:, kc, :], wo3[:, kc, 512 * b:512 * b + 512], kc == 0, kc == 7, [b_mrgT, b_wo], [bph], inc=(kc == 7))
            k.tt(h13[:, j, 512 * b:512 * b + 512], h13[:, j, 512 * b:512 * b + 512], ph[:, 0:512], ALU.add, [b_h1[j], bph], [b_h1[j]])
        if stop == "PB1":
            continue
        s = st4[0]; bs = b_st4[0]
        k.act(junk, h13[:, j, :], AF.Square, [b_h1[j]], [b_junk, bs], accum_out=s[:, 0:1])
        k.rstd(s[:, 0:1], 1.0 / 1024, s[:, 1:2], s[:, 2:3], [bs], [bs])
        k.stt(hn32, h13[:, j, :], s[:, 2:3], gfb, ALU.mult, ALU.mult, [b_h1[j], bs, b_gfb], [b_hn32])
        k.cp("dve", hnhi, hn32, [b_hn32], [b_hnhi])
        k.tt(hnlo, hn32, hnhi, ALU.subtract, [b_hn32, b_hnhi], [b_hnlo])
        transpose_to(hiT, b_hiT, hnhi, b_hnhi, 8, grp="pbm", banks=(6, 7), eng="act")
        transpose_to(loT, b_loT, hnlo, b_hnlo, 8, grp="pbm", banks=(6, 7), eng="act")
        k.cp("dve", hnT3[:, :, j * 128:(j + 1) * 128], hiT3, [b_hiT], [b_hnT[j]])
        pr, bpr = k.ps("pbu", (3, 4, 5))
        n_ = 0
        for (aT, baT, w_, bw_) in ((hiT3, b_hiT, wrh3, b_wrh), (hiT3, b_hiT, wrl3, b_wrl), (loT3, b_loT, wrh3, b_wrh)):
            for kc in range(8):
                k.mm(pr[:, 0:36], aT[:, kc, :], w_[:, kc, :], n_ == 0, n_ == 23, [baT, bw_], [bpr], inc=(n_ == 23))
                n_ += 1
        if stop == "PB2":
            continue
        k.tt(Lr[:, 0:36], pr[:, 0:36], br32, ALU.add, [bpr, b_br32], [b_Lr])
        P.op("dve", lambda e, o=rs[:, 0:1], i=Lr[:, 0:4]: e.tensor_reduce(out=o, in_=i, axis=AX.X, op=ALU.max), [b_Lr], [b_rs])
        k.ts(rs[:, 1:2], rs[:, 0:1], -1.0, None, ALU.mult, None, [b_rs], [b_rs])
        k.act(Lr[:, 36:40], Lr[:, 0:4], AF.Exp, [b_Lr, b_rs], [b_Lr, b_rs], bias=rs[:, 1:2], accum_out=rs[:, 2:3])
        k.recip(rs[:, 3:4], rs[:, 2:3], [b_rs], [b_rs])
        k.ts(Lr[:, 36:40], Lr[:, 0:4], rs[:, 0:1], 1.0, ALU.is_equal, ALU.subtract, [b_Lr, b_rs], [b_Lr])
        k.ts(Lr[:, 36:40], Lr[:, 36:40], 1e30, None, ALU.mult, None, [b_Lr], [b_Lr])
        for g in range(4):
            k.ts(elm[:, 8 * g:8 * g + 8], Lr[:, 4 + 8 * g:12 + 8 * g], Lr[:, 36 + g:37 + g], None, ALU.add, None, [b_Lr], [b_elm])
        P.op("dve", lambda e, o=rs[:, 8:16], i=elm: e.max(out=o, in_=i), [b_elm], [b_rs])
        k.tt(rs[:, 4:5], rs[:, 9:10], rs[:, 8:9], ALU.subtract, [b_rs], [b_rs])
        k.act(rs[:, 4:5], rs[:, 4:5], AF.Exp, [b_rs], [b_rs])
        k.ts(rs[:, 4:5], rs[:, 4:5], 1.0, None, ALU.add, None, [b_rs], [b_rs])
        k.recip(rs[:, 5:6], rs[:, 4:5], [b_rs], [b_rs])
        k.tt(rs[:, 6:7], rs[:, 5:6], rs[:, 3:4], ALU.mult, [b_rs], [b_rs])
        k.tt(rs[:, 7:8], rs[:, 3:4], rs[:, 6:7], ALU.subtract, [b_rs], [b_rs])
        k.ts(c1, elm, rs[:, 8:9], rs[:, 6:7], ALU.is_equal, ALU.mult, [b_elm, b_rs], [b_c1])
        k.ts(cw3[:, j, :], elm, rs[:, 9:10], rs[:, 7:8], ALU.is_equal, ALU.mult, [b_elm, b_rs], [b_cw[j]])
        k.tt(cw3[:, j, :], cw3[:, j, :], c1, ALU.add, [b_cw[j], b_c1], [b_cw[j]])
    P.barrier()
    A.release(pb_mark)
    if stop in ("PB", "PB1", "PB2"):
        P.emit(); return nc

    ewg = [A.alloc(8 * 512, BF16) for _ in range(2)]; ewu = [A.alloc(8 * 512, BF16) for _ in range(2)]
    ewd = [A.alloc(4 * 1024, BF16) for _ in range(2)]
    b_ew = [[Buf("ewg%d" % i), Buf("ewu%d" % i), Buf("ewd%d" % i)] for i in range(2)]
    hidT = A.alloc(4 * 2048, BF16); hidT3 = v3(hidT, 4); b_hid = [Buf("hid%d" % t) for t in range(4)]
    sgt = [A.alloc(512, BF16) for _ in range(2)]; b_sgt = [Buf("sgt%d" % i) for i in range(2)]

    def load_expert(e):
        sl = e % 2
        k.ldw(v3(ewg[sl], 8), e_wg[e * 1024:(e + 1) * 1024, :], 8, b_ew[sl][0])
        k.ldw(v3(ewu[sl], 8), e_wu[e * 1024:(e + 1) * 1024, :], 8, b_ew[sl][1])
        k.ldw(v3(ewd[sl], 4), e_wd[e * 512:(e + 1) * 512, :], 4, b_ew[sl][2])

    load_expert(0)
    scnt = [0]
    for e in range(NE):
        sl = e % 2
        if e + 1 < NE:
            load_expert(e + 1)
        g3, u3, d3 = v3(ewg[sl], 8), v3(ewu[sl], 8), v3(ewd[sl], 4)
        bwg, bwu, bwd = b_ew[sl]
        for tg in range((NJ + 3) // 4):
            rb = [b_hnT[4 * tg + i] for i in range(4)]
            for fc in range(4):
                pG, bpG = k.ps("peg", (0, 1, 2, 3))
                for kc in range(8):
                    k.mm(pG[:, 0:512], g3[:, kc, fc * 128:(fc + 1) * 128], hnT3[:, kc, tg * 512:(tg + 1) * 512], kc == 0, kc == 7, rb + [bwg], [bpG],
                         inc=(kc == 7))
                pU, bpU = k.ps("peg", (0, 1, 2, 3))
                for kc in range(8):
                    k.mm(pU[:, 0:512], u3[:, kc, fc * 128:(fc + 1) * 128], hnT3[:, kc, tg * 512:(tg + 1) * 512], kc == 0, kc == 7, rb + [bwu], [bpU],
                         inc=(kc == 7))
                sg = sgt[scnt[0] % 2]; bsg = b_sgt[scnt[0] % 2]; scnt[0] += 1
                k.act(sg, pG[:, 0:512], AF.Silu, [bpG], [bsg])
                k.tt(hidT3[:, fc, tg * 512:(tg + 1) * 512], sg, pU[:, 0:512], ALU.mult, [bsg, bpU], [b_hid[tg]])
        for jt in range(NJ):
            for b in range(2):
                py, bpy = k.ps("pey", (4, 5, 6, 7))
                for fc in range(4):
                    k.mm(py[:, 0:512], hidT3[:, fc, jt * 128:(jt + 1) * 128], d3[:, fc, 512 * b:512 * b + 512], fc == 0, fc == 3, [b_hid[jt // 4], bwd], [bpy],
                         inc=(fc == 3))
                hsl = h13[:, jt, 512 * b:512 * b + 512]
                k.stt(hsl, py[:, 0:512], cw3[:, jt, e:e + 1], hsl, ALU.mult, ALU.add, [bpy, b_cw[jt], b_h1[jt]], [b_h1[jt]])
    P.barrier()
    A.release(pb_mark)
    if stop == "PE":
        P.emit(); return nc

    wpg = A.alloc(8 * 1024, BF16); b_wpg = Buf("wpg"); wpg3 = v3(wpg, 8)
    wpp = A.alloc(2 * 1024, BF16); b_wpp = Buf("wpp"); wpp3 = v3(wpp, 2)
    k.ldw(wpg3, w_pg, 8, b_wpg); k.ldw(wpp3, w_pp, 2, b_wpp)
    gpb = A.alloc(1024); b_gpb = Buf("gpb"); bc_load(gpb, g_ple, b_gpb)
    gfin = A.alloc(1024); b_gfin = Buf("gfin"); bc_load(gfin, g_fin, b_gfin)
    pn = [A.alloc(1024, BF16) for _ in range(2)]; b_pn = [Buf("pn%d" % i) for i in range(2)]
    pnT = [A.alloc(1024, BF16) for _ in range(2)]; b_pnT = [Buf("pnT%d" % i) for i in range(2)]
    p32 = [A.alloc(256) for _ in range(2)]; b_p32 = [Buf("p32%d" % i) for i in range(2)]
    pbf = [A.alloc(256, BF16) for _ in range(2)]; b_pbf = [Buf("pbf%d" % i) for i in range(2)]
    pT = [A.alloc(256, BF16) for _ in range(2)]; b_pT = [Buf("pT%d" % i) for i in range(2)]
    sgp = [A.alloc(512) for _ in range(2)]; b_sgp = [Buf("sgp%d" % i) for i in range(2)]
    ot = [A.alloc(1024) for _ in range(2)]; b_ot = [Buf("ot%d" % i) for i in range(2)]
    cnt = [0]
    for j in range(NJ):
        sl = j % 2
        k.ld(p32[sl], p_own[j * 128:(j + 1) * 128, :], b_p32[sl])
        k.cp("dve", pbf[sl], p32[sl], [b_p32[sl]], [b_pbf[sl]])
        transpose_to(pT[sl], b_pT[sl], pbf[sl], b_pbf[sl], 2, grp="pfm", banks=(6, 7), eng="act")
        norm_tile((h13[:, j, :], b_h1[j]), sl, gpb, b_gpb, pn[sl], b_pn[sl])
        transpose_to(pnT[sl], b_pnT[sl], pn[sl], b_pn[sl], 8, grp="pfm", banks=(6, 7), eng="act")
        n3 = v3(pnT[sl], 8); t3 = v3(pT[sl], 2)
        for b in range(2):
            pgt, bpgt = k.ps("pfg", (0, 1, 2))
            for kc in range(8):
                k.mm(pgt[:, 0:512], n3[:, kc, :], wpg3[:, kc, 512 * b:512 * b + 512], kc == 0, kc == 7, [b_pnT[sl], b_wpg], [bpgt], inc=(kc == 7))
            sg = sgp[cnt[0] % 2]; bsg = b_sgp[cnt[0] % 2]; cnt[0] += 1
            sigmoid_from(sg, bsg, pgt[:, 0:512], bpgt)
            ppp, bppp = k.ps("pfp", (3, 4, 5))
            for c in range(2):
                k.mm(ppp[:, 0:512], t3[:, c, :], wpp3[:, c, 512 * b:512 * b + 512], c == 0, c == 1, [b_pT[sl], b_wpp], [bppp], inc=(c == 1))
            k.tt(sg, sg, ppp[:, 0:512], ALU.mult, [bsg, bppp], [bsg])
            hsl = h13[:, j, 512 * b:512 * b + 512]
            k.tt(hsl, hsl, sg, ALU.add, [b_h1[j], bsg], [b_h1[j]])
        s = st4[sl]; bs = b_st4[sl]
        k.act(junk, h13[:, j, :], AF.Square, [b_h1[j]], [b_junk, bs], accum_out=s[:, 0:1])
        k.rstd(s[:, 0:1], 1.0 / 1024, s[:, 1:2], s[:, 2:3], [bs], [bs])
        k.stt(ot[sl], h13[:, j, :], s[:, 2:3], gfin, ALU.mult, ALU.mult, [b_h1[j], bs, b_gfin], [b_ot[sl]])
        k.st(out_own[j * 128:(j + 1) * 128, :], ot[sl], b_ot[sl], d_out)
    if dummy is not None:
        dz = A.alloc(8); b_dz = Buf("dz")
        for i in range(dummy[1]):
            if dummy[0] == "pe":
                k.mm(k.psb[0][0][:, 0:2], IDB, IDB[:, 0:2], True, True, [b_cb], [k.psb[0][1]], inc=(i % 64 == 63))
            else:
                k.memset(dz, float(i % 7), [b_dz], eng=dummy[0])
    P.barrier()
    P.emit()
    print("per-engine stream lengths (instr + waits):", {e: len(P.ops[e]) + sum(len(w) for w, _, _ in P.ops[e]) for e in ENGS})
    print("instructions:", P.n_inst, "sems:", len(P.semkeys), "sbuf peak words:", A.peak,
          "counts:", {e: P.cnt[e] for e in ENGS})
    return nc


def _consts():
    bf = ml_dtypes.bfloat16
    s = np.arange(128)[:, None]; t = np.arange(128)[None, :]
    ut = (s <= t).astype(np.float32)
    c_f32 = np.concatenate([-ut, -np.ones((128, 128), np.float32), np.eye(128, dtype=np.float32)], axis=1)
    c_e = np.zeros((64, 64, 128), np.float32)
    for n in range(64):
        c_e[n, n, :] = NEGB
    return c_f32, ut, c_e.reshape(64, 64 * 128).astype(bf)


def make_in_maps(inp):
    bf = ml_dtypes.bfloat16
    f = lambda a: np.ascontiguousarray(np.asarray(a, dtype=np.float32))
    x = f(inp["x"])[0]; p = f(inp["p"])[0, 0]
    w_in = f(inp["w_in"])[0]
    o = np.cumsum([0, 512, 512, 512, 512, 512, 512, 512, 4, 4, 1024, 1024])
    aq, ak, av, mq, mk_, mv, mo, mi, mf, ga, gm = [w_in[:, o[i]:o[i + 1]] for i in range(11)]
    shared = {
        "g_mix": f(inp["mix_norm_g"]), "g_ffn": f(inp["ffn_norm_g"]), "g_ple": f(inp["ple_norm_g"]),
        "g_fin": f(inp["final_norm_g"]).reshape(1, 1024), "g_ml": f(inp["mlstm_norm_g"]),
        "w_kv0": f(np.concatenate([ak[:, 0:256], av[:, 0:256]], 1)), "w_kv1": f(np.concatenate([ak[:, 256:512], av[:, 256:512]], 1)),
        "w_q0": f(aq[:, 0:256]), "w_q1": f(aq[:, 256:512]),
        "w_ms": f(np.concatenate([mk_, mv, mi, mf], 1)), "w_mo": f(np.concatenate([mq, mo], 1)),
        "w_g": f(np.concatenate([ga, gm], 1)), "b_gate": f(inp["b_gate"]),
        "ifb": f(inp["mlstm_if_b"]),
        "w_ua": f(inp["w_up_attn"])[0], "w_um": f(inp["w_up_mlstm"])[0], "w_out": f(inp["w_out"])[0],
        "w_r": f(np.concatenate([f(inp["router_group_w"])[0], f(inp["router_expert_w"])[0]], 1)),
        "b_r": f(np.concatenate([f(inp["router_group_b"]), f(inp["router_expert_b"])], 1)),
        "e_wg": f(inp["expert_w_gate"])[0].reshape(32 * 1024, 512), "e_wu": f(inp["expert_w_up"])[0].reshape(32 * 1024, 512),
        "e_wd": f(inp["expert_w_down"])[0].reshape(32 * 512, 1024),
        "w_pg": f(inp["w_ple_gate"])[0], "w_pp": f(inp["w_ple_proj"])[0],
    }
    cw_ = f(inp["conv_w"])[0]; cb_ = f(inp["conv_b"])[0]
    conv_wb = np.zeros((128, 40), np.float32)
    for hq in range(8):
        ch = np.arange(128) + 128 * hq
        conv_wb[:, hq * 4:hq * 4 + 4] = cw_[:, ch].T
        conv_wb[:, 32 + hq] = cb_[ch]
    shared["conv_wb"] = conv_wb
    c_f32, ut, c_e = _consts()
    shared["c_f32"] = c_f32; shared["c_e"] = c_e
    x_t = x.reshape(128, 128, 1024); p_t = p.reshape(128, 128, 256)
    kk = np.arange(128)[:, None]; qq = np.arange(128)[None, :]
    tri = np.where(kk <= qq, 0.0, -NEGB).astype(np.float32)
    maps = []
    for c in range(NCORES):
        par = c % 2; pad = 2 * ((7 - c) // 2)
        xs = np.zeros((128, 128, 1024), np.float32)
        xs[pad:] = x_t[:128 - pad]
        own = [8 * j + c for j in range(16)]
        cm0 = tri if par == 0 else np.zeros((128, 128), np.float32)
        cm1 = np.full((128, 128), -NEGB, np.float32) if par == 0 else tri
        c_bf = np.concatenate([np.eye(128, dtype=np.float32), ut, cm0, cm1], 1).astype(bf)
        c_core = np.zeros((128, 130), np.float32)
        c_core[:, 0:128] = (np.arange(128)[None, :] >= pad).astype(np.float32)
        c_core[:, 128] = par; c_core[:, 129] = 1 - par
        gmask = np.full((16, 64), -1e30, np.float32)
        for j in range(16):
            gmask[j, pad // 2:4 * j + 3] = 0.0
        m = dict(shared)
        m.update({"x_shift": xs.reshape(128 * 128, 1024), "x_own": np.ascontiguousarray(x_t[own]).reshape(2048, 1024),
                  "p_own": np.ascontiguousarray(p_t[own]).reshape(2048, 256), "c_bf": c_bf, "c_core": c_core,
                  "c_gmask": gmask.reshape(1, 1024)})
        maps.append(m)
    return maps


_NC_CACHE = {}


def kernel(**inputs):
    if "nc" not in _NC_CACHE:
        _NC_CACHE["nc"] = build_program(False)
    nc = _NC_CACHE["nc"]
    maps = make_in_maps(inputs)
    res = run_bass_kernel_spmd(nc, maps, core_ids=list(range(NCORES)))
    out = np.zeros((128, 128, 1024), np.float32)
    for c in range(NCORES):
        o = np.asarray(res.results[c]["out_own"], dtype=np.float32).reshape(16, 128, 1024)
        for j in range(16):
            out[8 * j + c] = o[j]
    return out.reshape(1, 16384, 1024)
```
